# Optimizing a Trainium2 kernel written in Bass

```python
import jax
import jax.numpy as jnp
from jax import lax
import numpy as np

D_MODEL = 1024
BATCH = 16
SEQ = 2048
DEPTH = 2

GRID_W = 64
CTX_LEN = 256
HEAD_DIM = 64
ROPE_BASE = 10000.0
EPS = 1e-6
NEG_INF = -1e30

ATT_HEADS = 8
ATT_KV_HEADS = 2
ATT_WIDTH = ATT_HEADS * HEAD_DIM
KV_WIDTH = ATT_KV_HEADS * HEAD_DIM
WINDOW = 128
BLOCK = 128

RET_HEADS = 4
RET_WIDTH = RET_HEADS * HEAD_DIM
RET_CHUNK = 128

CONV_WIDTH = D_MODEL - ATT_WIDTH - RET_WIDTH
CONV_KERNEL = 31
CONV_PAD = CONV_KERNEL // 2

MIX_WIDTH = ATT_WIDTH + RET_WIDTH + CONV_WIDTH
PROJ_SIZES = (ATT_WIDTH, KV_WIDTH, KV_WIDTH, RET_WIDTH, RET_WIDTH, RET_WIDTH, RET_WIDTH, 2 * CONV_WIDTH)
IN_COLS = ATT_WIDTH + 2 * KV_WIDTH + 4 * RET_WIDTH + 2 * CONV_WIDTH

N_EXPERTS = 16
CAPACITY_FACTOR = 2
D_FF = 2816

kernel_name = "hybrid_dit_window_gqa_retention_conformer_ecmoe"


def rms_norm(x, g):
    xf = x.astype(jnp.float32)
    y = xf * lax.rsqrt(jnp.mean(xf * xf, axis=-1, keepdims=True) + EPS)
    return (y * g.astype(jnp.float32)).astype(x.dtype)


def modulate(h, shift, scale):
    return h * (1 + scale) + shift


def heads(t, n):
    return t.reshape(t.shape[:-1] + (n, HEAD_DIM))


def split_proj(u):
    cuts = [int(s) for s in np.cumsum(PROJ_SIZES)[:-1]]
    return jnp.split(u, cuts, axis=-1)


def rotary(x, pos):
    half = x.shape[-1] // 2
    inv = ROPE_BASE ** (-jnp.arange(half, dtype=jnp.float32) / half)
    ang = pos[:, None] * inv[None, :]
    cos = jnp.cos(ang)[:, None, :]
    sin = jnp.sin(ang)[:, None, :]
    xf = x.astype(jnp.float32)
    x1, x2 = xf[..., :half], xf[..., half:]
    return jnp.concatenate([x1 * cos - x2 * sin, x1 * sin + x2 * cos], axis=-1).astype(x.dtype)


def axial_rotary(x, rows, cols):
    half = x.shape[-1] // 2
    return jnp.concatenate([rotary(x[..., :half], rows), rotary(x[..., half:], cols)], axis=-1)


def window_attention(q, k, v, k_ctx, v_ctx, sink):
    B, S, H, d = q.shape
    G = H // ATT_KV_HEADS
    nb = S // BLOCK
    W3 = 3 * BLOCK
    C = k_ctx.shape[1]
    scale = d ** -0.5
    qb = q.reshape(B, nb, BLOCK, ATT_KV_HEADS, G, d)
    pad = ((0, 0), (BLOCK, BLOCK), (0, 0), (0, 0))
    kp = jnp.pad(k, pad).reshape(B, nb + 2, BLOCK, ATT_KV_HEADS, d)
    vp = jnp.pad(v, pad).reshape(B, nb + 2, BLOCK, ATT_KV_HEADS, d)
    kw = jnp.concatenate([kp[:, :-2], kp[:, 1:-1], kp[:, 2:]], axis=2)
    vw = jnp.concatenate([vp[:, :-2], vp[:, 1:-1], vp[:, 2:]], axis=2)
    s_loc = jnp.einsum('bnqkgd,bnwkd->bnkgqw', qb, kw).astype(jnp.float32) * scale
    s_ctx = jnp.einsum('bnqkgd,bckd->bnkgqc', qb, k_ctx).astype(jnp.float32) * scale
    a = jnp.arange(BLOCK)[:, None]
    w = jnp.arange(W3)[None, :]
    kpos = jnp.arange(nb)[:, None, None] * BLOCK - BLOCK + w[None]
    valid = (jnp.abs(w - BLOCK - a)[None] <= WINDOW) & (kpos >= 0) & (kpos < S)
    s_loc = jnp.where(valid[None, :, None, None], s_loc, NEG_INF)
    sink_b = jnp.broadcast_to(sink.astype(jnp.float32).reshape(1, 1, ATT_KV_HEADS, G, 1, 1),
                              s_loc.shape[:-1] + (1,))
    p = jax.nn.softmax(jnp.concatenate([s_loc, s_ctx, sink_b], axis=-1), axis=-1).astype(v.dtype)
    out = (jnp.einsum('bnkgqw,bnwkd->bnqkgd', p[..., :W3], vw)
           + jnp.einsum('bnkgqc,bckd->bnqkgd', p[..., W3:W3 + C], v_ctx))
    return out.reshape(B, S, H * d)


def context_attention(q, k, v, sink):
    B, C, H, d = q.shape
    G = H // ATT_KV_HEADS
    qg = q.reshape(B, C, ATT_KV_HEADS, G, d)
    s = jnp.einsum('bqkgd,bckd->bkgqc', qg, k).astype(jnp.float32) * (d ** -0.5)
    sink_b = jnp.broadcast_to(sink.astype(jnp.float32).reshape(1, ATT_KV_HEADS, G, 1, 1), s.shape[:-1] + (1,))
    p = jax.nn.softmax(jnp.concatenate([s, sink_b], axis=-1), axis=-1)[..., :C].astype(v.dtype)
    return jnp.einsum('bkgqc,bckd->bqkgd', p, v).reshape(B, C, H * d)


def retention_dir(q, k, v, log_gamma, s0, inclusive):
    B, L, H, d = q.shape
    T = RET_CHUNK
    n = L // T
    qc = q.reshape(B, n, T, H, d)
    kc = k.reshape(B, n, T, H, d)
    vc = v.reshape(B, n, T, H, d)
    i = jnp.arange(T, dtype=jnp.float32)
    diff = i[:, None] - i[None, :]
    mask = (diff >= 0) if inclusive else (diff > 0)
    dec = jnp.where(mask[None], jnp.exp(log_gamma[:, None, None] * jnp.maximum(diff, 0.0)[None]), 0.0)
    s = jnp.einsum('bnihd,bnjhd->bnhij', qc, kc) * dec[None, None]
    o_intra = jnp.einsum('bnhij,bnjhd->bnihd', s, vc)
    k_dec = jnp.exp((T - 1 - i)[:, None] * log_gamma[None, :])
    kv = jnp.einsum('bnjhd,jh,bnjhe->bnhde', kc, k_dec, vc)
    chunk_decay = jnp.exp(log_gamma * T)[None, :, None, None]

    def step(state, kv_n):
        return chunk_decay * state + kv_n, state

    s_final, s_prev = lax.scan(step, s0, jnp.moveaxis(kv, 1, 0))
    s_prev = jnp.moveaxis(s_prev, 0, 1)
    q_dec = jnp.exp((i + 1.0)[:, None] * log_gamma[None, :])
    o_inter = jnp.einsum('bnihd,ih,bnhde->bnihe', qc, q_dec, s_prev)
    return (o_intra + o_inter).reshape(B, L, H, d), s_final


def retention_bidir(q_c, k_c, v_c, q_l, k_l, v_l, log_g_f, log_g_b):
    f32 = jnp.float32
    q_c, k_c, v_c, q_l, k_l, v_l = (t.astype(f32) for t in (q_c, k_c, v_c, q_l, k_l, v_l))
    B, _, H, d = q_l.shape
    zeros = jnp.zeros((B, H, d, d), f32)
    rev = lambda t: t[:, ::-1]
    o_cf, s_cf = retention_dir(q_c, k_c, v_c, log_g_f, zeros, True)
    o_cb, s_cb = retention_dir(rev(q_c), rev(k_c), rev(v_c), log_g_b, zeros, False)
    o_lf, _ = retention_dir(q_l, k_l, v_l, log_g_f, s_cf, True)
    o_lb, _ = retention_dir(rev(q_l), rev(k_l), rev(v_l), log_g_b, s_cb, False)
    return o_cf + rev(o_cb), o_lf + rev(o_lb)


def retention_out(o, gate, g):
    B, L, H, d = o.shape
    mu = jnp.mean(o, axis=-1, keepdims=True)
    var = jnp.mean(jnp.square(o - mu), axis=-1, keepdims=True)
    y = ((o - mu) * lax.rsqrt(var + EPS)).reshape(B, L, H * d) * g.astype(jnp.float32)
    return y.astype(gate.dtype) * jax.nn.silu(gate)


def conformer_conv(u, w_dw, b_dw, g, b):
    val, gt = jnp.split(u, 2, axis=-1)
    h = val * jax.nn.sigmoid(gt)
    h = lax.conv_general_dilated(h, w_dw[:, None, :].astype(h.dtype), window_strides=(1,),
                                 padding=((CONV_PAD, CONV_PAD),),
                                 dimension_numbers=('NWC', 'WIO', 'NWC'),
                                 feature_group_count=CONV_WIDTH) + b_dw
    hf = h.astype(jnp.float32)
    mu = jnp.mean(hf, axis=-1, keepdims=True)
    var = jnp.mean(jnp.square(hf - mu), axis=-1, keepdims=True)
    y = (hf - mu) * lax.rsqrt(var + EPS) * g.astype(jnp.float32) + b.astype(jnp.float32)
    return jax.nn.silu(y).astype(u.dtype)


def expert_choice_ffn(h, w_router, w_gate, w_up, w_down):
    B, L, D = h.shape
    cap = CAPACITY_FACTOR * L // N_EXPERTS
    aff = jax.nn.softmax(jnp.einsum('bld,de->ble', h, w_router).astype(jnp.float32), axis=-1)
    g, idx = lax.top_k(jnp.swapaxes(aff, 1, 2), cap)
    xe = jax.vmap(lambda hb, ib: hb[ib])(h, idx)
    a = jnp.einsum('becd,edf->becf', xe, w_gate)
    u = jnp.einsum('becd,edf->becf', xe, w_up)
    y = jnp.einsum('becf,efd->becd', jax.nn.silu(a) * u, w_down) * g[..., None].astype(h.dtype)
    return jax.vmap(lambda ib, yb: jnp.zeros((L, D), yb.dtype).at[ib.reshape(-1)].add(yb.reshape(-1, D)))(idx, y)


def setup_inputs(seed: int = 0) -> dict:
    key = jax.random.key(seed)
    ks = jax.random.split(key, 24)
    f32 = jnp.float32
    nrm = lambda k, shape, s: jax.random.normal(k, shape, f32) * s
    D = D_MODEL
    gamma0 = 1.0 - 2.0 ** (-5.0 - jnp.arange(RET_HEADS, dtype=f32))
    return {
        "x": nrm(ks[0], (BATCH, SEQ, D), 1.0),
        "c": nrm(ks[1], (BATCH, D), 1.0),
        "ctx": nrm(ks[2], (BATCH, CTX_LEN, D), 1.0),
        "c_ctx": nrm(ks[3], (D,), 1.0),
        "w_mod": nrm(ks[4], (DEPTH, D, 6 * D), 0.5 * D ** -0.5),
        "b_mod": nrm(ks[5], (DEPTH, 6 * D), 0.02),
        "g_mix": 1.0 + nrm(ks[6], (DEPTH, D), 0.02),
        "g_ffn": 1.0 + nrm(ks[7], (DEPTH, D), 0.02),
        "w_in": nrm(ks[8], (DEPTH, D, IN_COLS), D ** -0.5),
        "q_norm_g": 1.0 + nrm(ks[9], (DEPTH, HEAD_DIM), 0.02),
        "k_norm_g": 1.0 + nrm(ks[10], (DEPTH, HEAD_DIM), 0.02),
        "att_sink": nrm(ks[11], (DEPTH, ATT_HEADS), 0.5),
        "ret_decay_logit": jnp.log(gamma0 / (1.0 - gamma0)) + nrm(ks[12], (DEPTH, 2, RET_HEADS), 0.1),
        "ret_norm_g": 1.0 + nrm(ks[13], (DEPTH, RET_WIDTH), 0.02),
        "conv_w": nrm(ks[14], (DEPTH, CONV_KERNEL, CONV_WIDTH), CONV_KERNEL ** -0.5),
        "conv_b": nrm(ks[15], (DEPTH, CONV_WIDTH), 0.02),
        "conv_norm_g": 1.0 + nrm(ks[16], (DEPTH, CONV_WIDTH), 0.02),
        "conv_norm_b": nrm(ks[17], (DEPTH, CONV_WIDTH), 0.02),
        "w_out": nrm(ks[18], (DEPTH, MIX_WIDTH, D), MIX_WIDTH ** -0.5),
        "w_router": nrm(ks[19], (DEPTH, D, N_EXPERTS), D ** -0.5),
        "w_gate": nrm(ks[20], (DEPTH, N_EXPERTS, D, D_FF), D ** -0.5),
        "w_up": nrm(ks[21], (DEPTH, N_EXPERTS, D, D_FF), D ** -0.5),
        "w_down": nrm(ks[22], (DEPTH, N_EXPERTS, D_FF, D), D_FF ** -0.5),
    }


def reference(x, c, ctx, c_ctx, w_mod, b_mod, g_mix, g_ffn, w_in, q_norm_g, k_norm_g, att_sink,
              ret_decay_logit, ret_norm_g, conv_w, conv_b, conv_norm_g, conv_norm_b, w_out,
              w_router, w_gate, w_up, w_down):
    f32 = jnp.float32
    B, S, D = x.shape
    C = ctx.shape[1]
    n_rows = S // GRID_W
    rows = jnp.repeat(jnp.arange(n_rows), GRID_W).astype(f32)
    cols = jnp.tile(jnp.arange(GRID_W), n_rows).astype(f32)
    pos_ctx = jnp.arange(C, dtype=f32)
    pos_lat = C + jnp.arange(S, dtype=f32)
    xc = ctx
    cs = jax.nn.silu(c)
    ccs = jax.nn.silu(c_ctx)
    for l in range(DEPTH):
        last = l == DEPTH - 1
        mod_l = (cs @ w_mod[l] + b_mod[l])[:, None, :]
        mod_c = (ccs @ w_mod[l] + b_mod[l])[None, None, :]
        sh_a, sc_a, gt_a, sh_f, sc_f, gt_f = jnp.split(mod_l, 6, axis=-1)
        csh_a, csc_a, cgt_a, csh_f, csc_f, cgt_f = jnp.split(mod_c, 6, axis=-1)

        h_l = modulate(rms_norm(x, g_mix[l]), sh_a, sc_a)
        h_c = modulate(rms_norm(xc, g_mix[l]), csh_a, csc_a)
        aq_l, ak_l, av_l, rq_l, rk_l, rv_l, rg_l, cv_l = split_proj(h_l @ w_in[l])
        aq_c, ak_c, av_c, rq_c, rk_c, rv_c, rg_c, cv_c = split_proj(h_c @ w_in[l])

        k_att_c = rms_norm(heads(ak_c, ATT_KV_HEADS), k_norm_g[l])
        v_att_c = heads(av_c, ATT_KV_HEADS)
        q_att_l = axial_rotary(rms_norm(heads(aq_l, ATT_HEADS), q_norm_g[l]), rows, cols)
        k_att_l = axial_rotary(rms_norm(heads(ak_l, ATT_KV_HEADS), k_norm_g[l]), rows, cols)
        att_l = window_attention(q_att_l, k_att_l, heads(av_l, ATT_KV_HEADS), k_att_c, v_att_c, att_sink[l])

        log_g = jax.nn.log_sigmoid(ret_decay_logit[l].astype(f32))
        kscale = HEAD_DIM ** -0.5
        o_c, o_l = retention_bidir(
            rotary(heads(rq_c, RET_HEADS), pos_ctx), rotary(heads(rk_c, RET_HEADS), pos_ctx) * kscale,
            heads(rv_c, RET_HEADS),
            rotary(heads(rq_l, RET_HEADS), pos_lat), rotary(heads(rk_l, RET_HEADS), pos_lat) * kscale,
            heads(rv_l, RET_HEADS), log_g[0], log_g[1])
        ret_l = retention_out(o_l, rg_l, ret_norm_g[l])

        conv_l = conformer_conv(cv_l, conv_w[l], conv_b[l], conv_norm_g[l], conv_norm_b[l])

        mix_l = jnp.concatenate([att_l, ret_l, conv_l], axis=-1) @ w_out[l]
        x_mid = x + gt_a * mix_l

        if not last:
            q_att_c = rms_norm(heads(aq_c, ATT_HEADS), q_norm_g[l])
            att_c = context_attention(q_att_c, k_att_c, v_att_c, att_sink[l])
            ret_c = retention_out(o_c, rg_c, ret_norm_g[l])
            conv_c = conformer_conv(cv_c, conv_w[l], conv_b[l], conv_norm_g[l], conv_norm_b[l])
            xc = xc + cgt_a * (jnp.concatenate([att_c, ret_c, conv_c], axis=-1) @ w_out[l])
            hf_c = modulate(rms_norm(xc, g_ffn[l]), csh_f, csc_f)
            xc = xc + cgt_f * expert_choice_ffn(hf_c, w_router[l], w_gate[l], w_up[l], w_down[l])

        hf_l = modulate(rms_norm(x_mid, g_ffn[l]), sh_f, sc_f)
        x = x_mid + gt_f * expert_choice_ffn(hf_l, w_router[l], w_gate[l], w_up[l], w_down[l])
    return x
```

```python
import os
import numpy as np
import concourse.bass as bass
import concourse.mybir as mybir
from concourse.bass_utils import run_bass_kernel_spmd
from contextlib import ExitStack, contextmanager

F32 = mybir.dt.float32
F32R = mybir.dt.float32r
BF16 = mybir.dt.bfloat16
U32 = mybir.dt.uint32
I32 = mybir.dt.int32
AF = mybir.ActivationFunctionType
ALU = mybir.AluOpType
AX = mybir.AxisListType

NCORES = 8
NS = 2
D = 1024
S = 2048
C = 256
NT = (S + C) // 128
L = 2
E = 16
DFF = 2816
NFC = DFF // 128
CAPL = 256
CAPC = 32
EPS = 1e-6
UC = 1792
NLAT = NS * S
NROW = NS * (S + C)


class KB:
    ENGS = ("pe", "dve", "act", "pool", "sp")

    def __init__(self, nc, ndma=24):
        self.nc = nc
        self.st = ExitStack()
        self.q = {e: [] for e in self.ENGS}
        self.cnt = {e: 0 for e in self.ENGS}
        self.sem = {e: self.st.enter_context(nc.semaphore("prog_" + e)) for e in self.ENGS}
        self.waited = {(c, p): 0 for c in self.ENGS for p in self.ENGS}
        self.waited_dma = {}
        self.lastw = {}
        self.readers = {}
        self.dma_sems = []
        self.ndma = {"sp": 0, "act": 0, "pool": 0}
        self.pool_of = {"sp": (0, 10), "act": (10, 8), "pool": (18, 14)}
        for i in range(32):
            s = self.st.enter_context(nc.semaphore("dma%d" % i))
            self.dma_sems.append([s, 0, None])
        self.cur = self.st

    def sb(self, name, shape, dt):
        self.uid = getattr(self, "uid", 0) + 1
        return self.cur.enter_context(self.nc.sbuf_tensor("%s_s%d" % (name, self.uid), shape, dt))

    def ps(self, name, shape, dt):
        self.uid = getattr(self, "uid", 0) + 1
        return self.cur.enter_context(self.nc.psum_tensor("%s_p%d" % (name, self.uid), shape, dt))

    @contextmanager
    def phase(self):
        prev = self.cur
        with ExitStack() as es:
            self.cur = es
            yield
            self.barrier()
        self.cur = prev

    def barrier(self):
        toks = [(p, self.cnt[p]) for p in self.ENGS if self.cnt[p] > 0]
        dtoks = [d[2] for d in self.dma_sems if d[2] is not None]
        for e in self.ENGS:
            for t in toks:
                if t[0] != e:
                    self._wait(e, t)
            for t in dtoks:
                self._wait(e, t)
        self.lastw = {}
        self.readers = {}

    def _key(self, x):
        if isinstance(x, (str, tuple)):
            return x
        return x.tensor.name if hasattr(x, "tensor") else x.name

    def _wait(self, eng, tok):
        if tok is None:
            return
        if tok[0] == "dma":
            _, i, val = tok
            if self.waited_dma.get((eng, i), 0) >= val:
                return
            self.waited_dma[(eng, i)] = val
            sem = self.dma_sems[i][0]
            self.q[eng].append(lambda e, sem=sem, val=val: e.wait_ge(sem, val))
            return
        p, c = tok
        if self.waited[(eng, p)] >= c:
            return
        self.waited[(eng, p)] = c
        s = self.sem[p]
        self.q[eng].append(lambda e, s=s, c=c: e.wait_ge(s, c))

    def _deps(self, eng, rk, wk):
        for k in rk:
            self._wait(eng, self.lastw.get(k))
        for k in wk:
            self._wait(eng, self.lastw.get(k))
            for t in self.readers.get(k, ()):
                self._wait(eng, t)

    def _record(self, tok, rk, wk):
        for k in wk:
            self.lastw[k] = tok
            self.readers[k] = []
        for k in rk:
            self.readers.setdefault(k, []).append(tok)

    def op(self, eng, fn, reads=(), writes=()):
        rk = [self._key(r) for r in reads]
        wk = [self._key(w) for w in writes]
        self._deps(eng, rk, wk)
        self.cnt[eng] += 1
        tok = (eng, self.cnt[eng])
        s = self.sem[eng]
        self.q[eng].append(lambda e, fn=fn, s=s: fn(e).then_inc(s, 1))
        self._record(tok, rk, wk)
        return tok

    def V(self, fn, r=(), w=()):
        return self.op("dve", fn, r, w)

    def A(self, fn, r=(), w=()):
        return self.op("act", fn, r, w)

    def G(self, fn, r=(), w=()):
        return self.op("pool", fn, r, w)

    def T(self, fn, r=(), w=()):
        return self.op("pe", fn, r, w)

    def dma(self, eng, fn, reads=(), writes=()):
        rk = [self._key(r) for r in reads]
        wk = [self._key(w) for w in writes]
        self._deps(eng, rk, wk)
        base, n = self.pool_of[eng]
        i = base + self.ndma[eng] % n
        self.ndma[eng] += 1
        ent = self.dma_sems[i]
        if ent[2] is not None:
            self._wait(eng, ent[2])
        ent[1] += 16
        tok = ("dma", i, ent[1])
        ent[2] = tok
        sem = ent[0]
        self.q[eng].append(lambda e, fn=fn, sem=sem: fn(e).then_inc(sem, 16))
        self._record(tok, rk, wk)
        return tok

    def finish(self):
        nc = self.nc
        self.barrier()
        q = self.q
        with nc.Block() as block:
            @block.tensor
            def _(e):
                for f in q["pe"]:
                    f(e)

            @block.vector
            def _(e):
                for f in q["dve"]:
                    f(e)

            @block.scalar
            def _(e):
                for f in q["act"]:
                    f(e)

            @block.gpsimd
            def _(e):
                for f in q["pool"]:
                    f(e)

            @block.sync
            def _(e):
                for f in q["sp"]:
                    f(e)
        self.st.close()


def host_consts():
    f32 = np.float32
    half = 16
    inv = (10000.0 ** (-np.arange(half, dtype=f32) / half)).astype(f32)
    t = np.arange(S)
    rows = (t // 64).astype(f32)
    cols = (t % 64).astype(f32)
    ar = (rows[:, None] * inv[None, :]).astype(f32)
    ac = (cols[:, None] * inv[None, :]).astype(f32)
    attC = np.concatenate([np.cos(ar), np.cos(ar), np.cos(ac), np.cos(ac)], axis=1).astype(f32)
    attS = np.concatenate([-np.sin(ar), np.sin(ar), -np.sin(ac), np.sin(ac)], axis=1).astype(f32)
    half = 32
    inv = (10000.0 ** (-np.arange(half, dtype=f32) / half)).astype(f32)
    pos = np.arange(S + C).astype(f32)
    a = (pos[:, None] * inv[None, :]).astype(f32)
    retC = np.concatenate([np.cos(a), np.cos(a)], axis=1).astype(f32)
    retS = np.concatenate([-np.sin(a), np.sin(a)], axis=1).astype(f32)
    j = np.arange(128)[:, None]
    i = np.arange(128)[None, :]
    masks = np.stack([(j <= i), (j >= i), (j > i)], axis=0).astype(f32)
    ident = np.eye(128, dtype=f32)
    p = np.arange(128, dtype=f32)
    dcoef = np.stack([p + 1, -(p + 1), -p, p], axis=1).astype(f32)
    return {"attC": attC, "attS": attS, "retC": retC, "retS": retS, "masks": masks,
            "ident": ident, "dcoef": dcoef}


WNAMES = ["w_mod", "b_mod", "g_mix", "g_ffn", "w_in", "q_norm_g", "k_norm_g", "att_sink",
          "ret_decay_logit", "ret_norm_g", "conv_w", "conv_b", "conv_norm_g", "conv_norm_b",
          "w_out", "w_router", "w_gate", "w_up", "w_down"]
WSHAPES = {
    "w_mod": [L, D, 6 * D], "b_mod": [L, 6 * D], "g_mix": [L, D], "g_ffn": [L, D], "w_in": [L, D, 2304],
    "q_norm_g": [L, 64], "k_norm_g": [L, 64], "att_sink": [L, 8], "ret_decay_logit": [L, 8],
    "ret_norm_g": [L, 256], "conv_w": [L, 31, 256], "conv_b": [L, 256], "conv_norm_g": [L, 256],
    "conv_norm_b": [L, 256], "w_out": [L, D, D], "w_router": [L, D, E], "w_gate": [L, E, D, DFF],
    "w_up": [L, E, D, DFF], "w_down": [L, E, DFF, D],
}
CSHAPES = {"attC": [S, 64], "attS": [S, 64], "retC": [S + C, 64], "retS": [S + C, 64],
           "masks": [3, 128, 128], "ident": [128, 128], "dcoef": [128, 4]}

STOP = os.environ.get("MK_STOP", "")
DEBUG = os.environ.get("MK_DEBUG", "") == "1"


def build_program():
    nc = bass.Bass("TRN2", target_bir_lowering=False)
    A = {}
    A["x"] = nc.dram_tensor("x", [NS * S, D], F32, kind="ExternalInput").ap()
    A["c"] = nc.dram_tensor("c", [NS, D], F32, kind="ExternalInput").ap()
    A["ctx"] = nc.dram_tensor("ctx", [NS * C, D], F32, kind="ExternalInput").ap()
    A["c_ctx"] = nc.dram_tensor("c_ctx", [1, D], F32, kind="ExternalInput").ap()
    for n in WNAMES:
        A[n] = nc.dram_tensor(n, WSHAPES[n], F32, kind="ExternalInput").ap()
    for n, shp in CSHAPES.items():
        A[n] = nc.dram_tensor(n, shp, F32, kind="ExternalInput").ap()
    A["y"] = nc.dram_tensor("y", [NS * S, D], F32, kind="ExternalOutput").ap()
    dk = "ExternalOutput" if DEBUG else "Internal"
    A["mod_d"] = nc.dram_tensor("mod_d", [L, 4, 6 * D], F32, kind=dk).ap()
    A["u_d"] = nc.dram_tensor("u_d", [NS, S + C, UC], F32, kind=dk).ap()
    A["cv_d"] = nc.dram_tensor("cv_d", [NS, 512, S + C], F32, kind=dk).ap()
    A["mixin_d"] = nc.dram_tensor("mixin_d", [NS, S + C, 768], F32, kind=dk).ap()
    A["convT_d"] = nc.dram_tensor("convT_d", [NS, 256, S + C], F32, kind=dk).ap()
    A["res_d"] = nc.dram_tensor("res_d", [NROW, D], F32, kind=dk).ap()
    A["hf_d"] = nc.dram_tensor("hf_d", [NROW, D], F32, kind=dk).ap()
    A["aff_d"] = nc.dram_tensor("aff_d", [48, S], F32, kind=dk).ap()
    A["affc_d"] = nc.dram_tensor("affc_d", [48, C], F32, kind=dk).ap()

    k = KB(nc)
    P = {}
    P["ident"] = k.sb("ident", [128, 128], F32)
    P["identb"] = k.sb("identb", [128, 128], BF16)
    P["masks"] = k.sb("masks", [128, 3, 128], F32)
    P["modT"] = [k.sb("modT%d" % l, [128, 48, 4], F32) for l in range(L)]
    P["gT"] = k.sb("gT", [128, 8, 4], F32)
    P["gsA"] = [k.sb("gsA%d" % l, [128, 8, 4], F32) for l in range(L)]
    k.dma("sp", lambda e: e.dma_start(out=P["ident"][:], in_=A["ident"]), writes=[P["ident"]])
    k.dma("sp", lambda e: e.dma_start(out=P["masks"][:], in_=A["masks"].rearrange("m j i -> j m i")),
          writes=[P["masks"]])
    k.V(lambda e: e.tensor_copy(out=P["identb"][:], in_=P["ident"][:]), [P["ident"]], [P["identb"]])

    phase_mod(k, A, P)
    if STOP == "mod":
        k.finish()
        return nc
    for l in range(L):
        last = l == L - 1
        src_lat = A["x"] if l == 0 else A["res_d"][0:NLAT]
        src_ctx = A["ctx"] if l == 0 else A["res_d"][NLAT:NROW]
        dst_lat = A["y"] if last else A["res_d"][0:NLAT]
        phase_proj(k, A, P, l, src_lat, src_ctx)
        if STOP == "proj%d" % l:
            break
        phase_att(k, A, P, l, last)
        if STOP == "att%d" % l:
            break
        phase_ret(k, A, P, l, last)
        if STOP == "ret%d" % l:
            break
        phase_conv(k, A, P, l, last)
        if STOP == "conv%d" % l:
            break
        phase_out(k, A, P, l, last, src_lat, src_ctx, dst_lat)
        if STOP == "out%d" % l:
            break
        phase_moe(k, A, P, l, last, dst_lat)
        if STOP == "moe%d" % l:
            break
    k.finish()
    return nc


def phase_mod(k, A, P):
    with k.phase():
        cc = k.sb("cc", [4, D], F32)
        cs = k.sb("cs", [4, D], F32)
        csT = k.sb("csT", [128, 8, 4], F32R)
        gg = k.sb("gg", [4, D], F32)
        pt = k.ps("pt", [128, 8, 4], F32)
        pm = [k.ps("pm%d" % i, [4, 512], F32) for i in range(2)]
        ptm = k.ps("ptm", [128, 48, 4], F32)
        wm = [k.sb("wm%d" % i, [128, 8, 512], F32R) for i in range(2)]
        bm = k.sb("bm", [4, 6 * D], F32)
        modrow = k.sb("modrow", [4, 6 * D], F32)
        ident = P["ident"]
        k.V(lambda e: e.memset(cc[:], 0.0), [], [cc])
        k.dma("sp", lambda e: e.dma_start(out=cc[0:NS, :], in_=A["c"]), writes=[cc])
        k.dma("sp", lambda e: e.dma_start(out=cc[NS:NS + 1, :], in_=A["c_ctx"]), writes=[cc])
        k.A(lambda e: e.activation(out=cs[:], in_=cc[:], func=AF.Silu), [cc], [cs])
        for kc in range(8):
            k.T(lambda e, kc=kc: e.transpose(pt[:, kc, :], cs[0:4, kc * 128:(kc + 1) * 128], ident[0:4, 0:4]),
                [cs, ident], [pt])
        k.V(lambda e: e.tensor_copy(out=csT[:], in_=pt[:]), [pt], [csT])
        k.dma("sp", lambda e: e.dma_start(out=gg[0:2, :], in_=A["g_mix"]), writes=[gg])
        k.dma("sp", lambda e: e.dma_start(out=gg[2:4, :], in_=A["g_ffn"]), writes=[gg])
        for kc in range(8):
            k.T(lambda e, kc=kc: e.transpose(pt[:, kc, :], gg[0:4, kc * 128:(kc + 1) * 128], ident[0:4, 0:4]),
                [gg, ident], [pt])
        k.V(lambda e: e.tensor_copy(out=P["gT"][:], in_=pt[:]), [pt], [P["gT"]])
        for l in range(L):
            k.dma("sp", lambda e, l=l: e.dma_start(out=bm[:], in_=A["b_mod"][l, :].partition_broadcast(4)),
                  writes=[bm])
            wv = A["w_mod"][l].rearrange("(kc p) f -> p kc f", p=128)
            for j in range(12):
                w = wm[j % 2]
                k.dma("pool", lambda e, w=w, j=j, wv=wv: e.dma_start(out=w[:], in_=wv[:, :, j * 512:(j + 1) * 512]),
                      writes=[w])
                p = pm[j % 2]
                for kc in range(8):
                    k.T(lambda e, p=p, w=w, kc=kc: e.matmul(p[:], lhsT=csT[:, kc, :], rhs=w[:, kc, :],
                                                          start=(kc == 0), stop=(kc == 7)), [csT, w], [p])
                k.V(lambda e, p=p, j=j: e.tensor_tensor(out=modrow[:, j * 512:(j + 1) * 512], in0=p[:],
                                                        in1=bm[:, j * 512:(j + 1) * 512], op=ALU.add),
                    [p, bm], [modrow])
            k.dma("sp", lambda e, l=l: e.dma_start(out=A["mod_d"][l], in_=modrow[:]), reads=[modrow],
                  writes=["mod_d"])
            for j in range(48):
                k.T(lambda e, j=j: e.transpose(ptm[:, j, :], modrow[0:4, j * 128:(j + 1) * 128], ident[0:4, 0:4]),
                    [modrow, ident], [ptm])
            mT = P["modT"][l]
            k.V(lambda e, mT=mT: e.tensor_copy(out=mT[:], in_=ptm[:]), [ptm], [mT])
            gs = P["gsA"][l]
            k.V(lambda e, gs=gs, mT=mT: e.tensor_scalar(out=gs[:], in0=mT[:, 8:16, :], scalar1=1.0, scalar2=None,
                                                        op0=ALU.add), [mT], [gs])
            k.V(lambda e, gs=gs, l=l: e.tensor_tensor(out=gs[:], in0=gs[:],
                                                      in1=P["gT"][:, :, l:l + 1].to_broadcast([128, 8, 4]),
                                                      op=ALU.mult), [gs, P["gT"]], [gs])


def rms_rstd(k, xt, junk, ss, rstd, n):
    k.A(lambda e: e.activation(out=junk, in_=xt, func=AF.Square, accum_out=ss[:, 0:1]), [xt], [junk, ss])
    k.V(lambda e: e.tensor_scalar(out=ss[:, 0:1], in0=ss[:, 0:1], scalar1=1.0 / n, scalar2=EPS, op0=ALU.mult,
                                  op1=ALU.add), [ss], [ss])
    k.A(lambda e: e.sqrt(out=ss[:, 0:1], in_=ss[:, 0:1]), [ss], [ss])
    k.V(lambda e: e.reciprocal(out=rstd[:, 0:1], in_=ss[:, 0:1]), [ss], [rstd])


def phase_proj(k, A, P, l, src_lat, src_ctx):
    with k.phase():
        win = k.sb("win", [128, 8, 2304], F32R)
        wv = A["w_in"][l].rearrange("(kc p) f -> p kc f", p=128)
        for j in range(0, 2304, 384):
            k.dma("pool", lambda e, j=j: e.dma_start(out=win[:, :, j:j + 384], in_=wv[:, :, j:j + 384]),
                  writes=[win])
        xts = [k.sb("xt%d" % i, [128, D], F32) for i in range(2)]
        xn = [k.sb("xn%d" % i, [128, D], F32) for i in range(2)]
        junk = k.sb("junk", [128, D], F32)
        ss = [k.sb("ss%d" % i, [128, 1], F32) for i in range(2)]
        rstd = [k.sb("rstd%d" % i, [128, 1], F32) for i in range(2)]
        hT = [k.sb("hT%d" % i, [128, 8, 512], F32R) for i in range(2)]
        ptr = [k.ps("ptr%d" % i, [128, D], F32) for i in range(2)]
        pu = [k.ps("pu%d" % i, [128, 512], F32) for i in range(2)]
        pc = [k.ps("pc%d" % i, [128, 512], F32) for i in range(2)]
        usb = [k.sb("usb%d" % i, [128, UC], F32) for i in range(2)]
        cvs = [k.sb("cvs%d" % i, [128, 4, 512], F32) for i in range(2)]
        ident = P["ident"]
        gs = P["gsA"][l]
        mT = P["modT"][l]
        gi = 0
        ti = 0
        for s in range(NS):
            groups = [(0, 2, src_ctx[s * C:(s + 1) * C], NS)]
            for g in range(4):
                groups.append((C + g * 512, 4, src_lat[s * S + g * 512: s * S + (g + 1) * 512], s))
            for (tok0, ntl, src, r) in groups:
                h = hT[gi % 2]
                ntok = ntl * 128
                for t in range(ntl):
                    xt = xts[ti % 2]
                    xnn = xn[ti % 2]
                    sst = ss[ti % 2]
                    rs = rstd[ti % 2]
                    pt = ptr[ti % 2]
                    k.dma("sp", lambda e, xt=xt, src=src, t=t: e.dma_start(out=xt[:], in_=src[t * 128:(t + 1) * 128, :]),
                          writes=[xt])
                    rms_rstd(k, xt[:], junk[:], sst, rs, D)
                    k.V(lambda e, xnn=xnn, xt=xt, rs=rs: e.tensor_scalar(out=xnn[:], in0=xt[:], scalar1=rs[:, 0:1],
                                                                       scalar2=None, op0=ALU.mult), [xt, rs], [xnn])
                    for kc in range(8):
                        k.T(lambda e, pt=pt, xnn=xnn, kc=kc: e.transpose(pt[:, kc * 128:(kc + 1) * 128],
                                                                       xnn[:, kc * 128:(kc + 1) * 128], ident[:]),
                            [xnn, ident], [pt])
                    for kc in range(8):
                        eng = k.A if kc % 2 == 0 else k.V
                        if kc % 2 == 0:
                            k.A(lambda e, h=h, pt=pt, kc=kc, t=t, r=r: e.activation(
                                out=h[:, kc, t * 128:(t + 1) * 128], in_=pt[:, kc * 128:(kc + 1) * 128],
                                func=AF.Identity, scale=gs[:, kc, r:r + 1], bias=mT[:, kc, r:r + 1]),
                                [pt, gs, mT], [h])
                        else:
                            k.V(lambda e, h=h, pt=pt, kc=kc, t=t, r=r: e.tensor_scalar(
                                out=h[:, kc, t * 128:(t + 1) * 128], in0=pt[:, kc * 128:(kc + 1) * 128],
                                scalar1=gs[:, kc, r:r + 1], scalar2=mT[:, kc, r:r + 1], op0=ALU.mult, op1=ALU.add),
                                [pt, gs, mT], [h])
                    ti += 1
                for t in range(ntl):
                    ub = usb[t % 2]
                    for ci, (c0, w) in enumerate([(0, 512), (512, 512), (1024, 512), (1536, 256)]):
                        p = pu[ci % 2]
                        for kc in range(8):
                            k.T(lambda e, p=p, h=h, kc=kc, t=t, c0=c0, w=w: e.matmul(
                                p[:, 0:w], lhsT=h[:, kc, t * 128:(t + 1) * 128], rhs=win[:, kc, c0:c0 + w],
                                start=(kc == 0), stop=(kc == 7)), [h, win], [p])
                        if ci % 2 == 0:
                            k.A(lambda e, ub=ub, p=p, c0=c0, w=w: e.copy(out=ub[:, c0:c0 + w], in_=p[:, 0:w]), [p], [ub])
                        else:
                            k.V(lambda e, ub=ub, p=p, c0=c0, w=w: e.tensor_copy(out=ub[:, c0:c0 + w], in_=p[:, 0:w]),
                                [p], [ub])
                    k.dma("sp", lambda e, ub=ub, s=s, tok0=tok0, t=t: e.dma_start(
                        out=A["u_d"][s, tok0 + t * 128: tok0 + (t + 1) * 128, :], in_=ub[:]), reads=[ub], writes=["u_d"])
                cv = cvs[gi % 2]
                for j in range(4):
                    p = pc[j % 2]
                    for kc in range(8):
                        k.T(lambda e, p=p, h=h, kc=kc, j=j, ntok=ntok: e.matmul(
                            p[:, 0:ntok], lhsT=win[:, kc, UC + j * 128: UC + (j + 1) * 128], rhs=h[:, kc, 0:ntok],
                            start=(kc == 0), stop=(kc == 7)), [h, win], [p])
                    if j % 2 == 0:
                        k.A(lambda e, cv=cv, p=p, j=j, ntok=ntok: e.copy(out=cv[:, j, 0:ntok], in_=p[:, 0:ntok]), [p], [cv])
                    else:
                        k.V(lambda e, cv=cv, p=p, j=j, ntok=ntok: e.tensor_copy(out=cv[:, j, 0:ntok], in_=p[:, 0:ntok]),
                            [p], [cv])
                k.dma("act", lambda e, cv=cv, s=s, tok0=tok0, ntok=ntok: e.dma_start(
                    out=A["cv_d"][s, :, tok0:tok0 + ntok].rearrange("(j p) t -> p j t", p=128), in_=cv[:, :, 0:ntok]),
                    reads=[cv], writes=["cv_d"])
                gi += 1


def phase_att(k, A, P, l, last):
    with k.phase():
        ident = P["ident"]
        identb = P["identb"]
        attC = k.sb("attC", [128, 16, 64], F32)
        attS = k.sb("attS", [128, 16, 64], F32)
        k.dma("sp", lambda e: e.dma_start(out=attC[:], in_=A["attC"].rearrange("(t p) d -> p t d", p=128)), writes=[attC])
        k.dma("sp", lambda e: e.dma_start(out=attS[:], in_=A["attS"].rearrange("(t p) d -> p t d", p=128)), writes=[attS])
        gqk = k.sb("gqk", [128, 10, 64], F32)
        k.dma("sp", lambda e: e.dma_start(out=gqk[:, 0, :], in_=A["q_norm_g"][l, :].partition_broadcast(128)), writes=[gqk])
        k.dma("sp", lambda e: e.dma_start(out=gqk[:, 8, :], in_=A["k_norm_g"][l, :].partition_broadcast(128)), writes=[gqk])
        k.V(lambda e: e.tensor_scalar(out=gqk[:, 0, :], in0=gqk[:, 0, :], scalar1=0.125, scalar2=None, op0=ALU.mult),
            [gqk], [gqk])
        for h in range(1, 8):
            k.V(lambda e, h=h: e.tensor_copy(out=gqk[:, h, :], in_=gqk[:, 0, :]), [gqk], [gqk])
        k.V(lambda e: e.tensor_copy(out=gqk[:, 9, :], in_=gqk[:, 8, :]), [gqk], [gqk])
        sink = k.sb("sink", [128, 8], F32)
        k.dma("sp", lambda e: e.dma_start(out=sink[:], in_=A["att_sink"][l, :].partition_broadcast(128)), writes=[sink])
        k.A(lambda e: e.activation(out=sink[:], in_=sink[:], func=AF.Exp), [sink], [sink])
        maskb = k.sb("maskb", [128, 3, 128], BF16)
        k.V(lambda e: e.tensor_copy(out=maskb[:], in_=P["masks"][:]), [P["masks"]], [maskb])

        qT = k.sb("qT", [64, 8, NT * 128], BF16)
        kT = k.sb("kT", [64, 2, NT * 128], BF16)
        vx = k.sb("vx", [128, NT, 2, 65], BF16)
        k.V(lambda e: e.memset(vx[:], 1.0), [], [vx])
        uq = [k.sb("uq%d" % i, [128, 768], F32) for i in range(2)]
        sq = k.sb("sq", [128, 640], F32)
        ssq = k.sb("ssq", [128, 10], F32)
        qn = k.sb("qn", [128, 640], F32)
        t1 = k.sb("t1", [128, 640], F32)
        t2 = k.sb("t2", [128, 640], F32)
        qb = [k.sb("qb%d" % i, [128, 640], BF16) for i in range(2)]
        pq = [k.ps("pq%d" % i, [64, 8, 128], BF16) for i in range(2)]
        pkk = k.ps("pkk", [64, 2, 128], BF16)
        psc = [k.ps("psc%d" % i, [128, 512], F32) for i in range(2)]
        po = [k.ps("po%d" % i, [128, 4, 65], F32) for i in range(2)]
        Pm = [k.sb("Pm%d" % i, [128, 5, 512], BF16) for i in range(2)]
        den = k.sb("den", [128, 8], F32)
        ao = [k.sb("ao%d" % i, [128, 512], F32) for i in range(2)]

        for s in range(NS):
            for t in range(NT):
                u = uq[t % 2]
                k.dma("sp", lambda e, u=u, s=s, t=t: e.dma_start(out=u[:], in_=A["u_d"][s, t * 128:(t + 1) * 128, 0:768]),
                      reads=["u_d"], writes=[u])
                k.G(lambda e, u=u: e.tensor_tensor(out=sq[:], in0=u[:, 0:640], in1=u[:, 0:640], op=ALU.mult), [u], [sq])
                k.V(lambda e: e.tensor_reduce(out=ssq[:], in_=sq[:].rearrange("p (h d) -> p h d", d=64), axis=AX.X,
                                              op=ALU.add), [sq], [ssq])
                k.V(lambda e: e.tensor_scalar(out=ssq[:], in0=ssq[:], scalar1=1.0 / 64, scalar2=EPS, op0=ALU.mult,
                                              op1=ALU.add), [ssq], [ssq])
                k.A(lambda e: e.sqrt(out=ssq[:], in_=ssq[:]), [ssq], [ssq])
                k.V(lambda e: e.reciprocal(out=ssq[:], in_=ssq[:]), [ssq], [ssq])
                k.V(lambda e, u=u: e.tensor_tensor(out=qn[:].rearrange("p (h d) -> p h d", d=64),
                                                   in0=u[:, 0:640].rearrange("p (h d) -> p h d", d=64),
                                                   in1=ssq[:].unsqueeze(2).to_broadcast([128, 10, 64]), op=ALU.mult),
                    [u, ssq], [qn])
                k.G(lambda e: e.tensor_tensor(out=qn[:], in0=qn[:], in1=gqk[:].rearrange("p h d -> p (h d)"), op=ALU.mult),
                    [qn, gqk], [qn])
                q_b = qb[t % 2]
                if t >= 2:
                    tl = t - 2
                    qv = qn[:].rearrange("p (h a x d) -> p h a x d", h=10, a=2, x=2, d=16)
                    t1v = t1[:].rearrange("p (h a x d) -> p h a x d", h=10, a=2, x=2, d=16)
                    t2v = t2[:].rearrange("p (h a x d) -> p h a x d", h=10, a=2, x=2, d=16)
                    Cv = attC[:, tl, :].rearrange("p (a x d) -> p a x d", a=2, x=2, d=16)
                    Sv = attS[:, tl, :].rearrange("p (a x d) -> p a x d", a=2, x=2, d=16)
                    for a in range(2):
                        for x in range(2):
                            k.V(lambda e, a=a, x=x, qv=qv, t1v=t1v, Cv=Cv: e.tensor_tensor(
                                out=t1v[:, :, a, x, :], in0=qv[:, :, a, x, :],
                                in1=Cv[:, a, x, :].unsqueeze(1).to_broadcast([128, 10, 16]), op=ALU.mult),
                                [qn, attC], [t1])
                            k.G(lambda e, a=a, x=x, qv=qv, t2v=t2v, Sv=Sv: e.tensor_tensor(
                                out=t2v[:, :, a, x, :], in0=qv[:, :, a, 1 - x, :],
                                in1=Sv[:, a, x, :].unsqueeze(1).to_broadcast([128, 10, 16]), op=ALU.mult),
                                [qn, attS], [t2])
                    k.V(lambda e, q_b=q_b: e.tensor_tensor(out=q_b[:], in0=t1[:], in1=t2[:], op=ALU.add), [t1, t2], [q_b])
                else:
                    k.V(lambda e, q_b=q_b: e.tensor_copy(out=q_b[:], in_=qn[:]), [qn], [q_b])
                k.G(lambda e, u=u, t=t: e.tensor_copy(out=vx[:, t, :, 0:64],
                                                      in_=u[:, 640:768].rearrange("p (h d) -> p h d", d=64)),
                    [u], [vx])
                p = pq[t % 2]
                for h in range(8):
                    k.T(lambda e, p=p, q_b=q_b, h=h: e.transpose(p[:, h, :], q_b[:, h * 64:(h + 1) * 64], identb[:]),
                        [q_b, identb], [p])
                for h in range(2):
                    k.T(lambda e, q_b=q_b, h=h: e.transpose(pkk[:, h, :], q_b[:, (8 + h) * 64:(9 + h) * 64], identb[:]),
                        [q_b, identb], [pkk])
                k.A(lambda e, p=p, t=t: e.copy(out=qT[:, :, t * 128:(t + 1) * 128], in_=p[:]), [p], [qT])
                k.V(lambda e, t=t: e.tensor_copy(out=kT[:, :, t * 128:(t + 1) * 128], in_=pkk[:]), [pkk], [kT])
            qblocks = list(range(2, NT)) + ([] if last else [0, 1])
            bi = 0
            for t in qblocks:
                if t >= 2:
                    kbl = []
                    if t - 1 >= 2:
                        kbl.append((t - 1, 1))
                    kbl.append((t, None))
                    if t + 1 < NT:
                        kbl.append((t + 1, 0))
                    kbl += [(0, None), (1, None)]
                else:
                    kbl = [(0, None), (1, None)]
                a_o = ao[bi % 2]
                for kh in range(2):
                    Pt = Pm[(bi * 2 + kh) % 2]
                    for bj, (kb_, mk) in enumerate(kbl):
                        ps = psc[bj % 2]
                        k.T(lambda e, ps=ps, kb_=kb_, kh=kh, t=t: e.matmul(
                            ps[:], lhsT=kT[:, kh, kb_ * 128:(kb_ + 1) * 128],
                            rhs=qT[:, kh * 4:(kh + 1) * 4, t * 128:(t + 1) * 128], start=True, stop=True),
                            [kT, qT], [ps])
                        k.A(lambda e, ps=ps, Pt=Pt, bj=bj: e.activation(out=Pt[:, bj, :], in_=ps[:], func=AF.Exp),
                            [ps], [Pt])
                        if mk is not None:
                            k.G(lambda e, Pt=Pt, bj=bj, mk=mk: e.tensor_tensor(
                                out=Pt[:, bj, :].rearrange("p (g q) -> p g q", g=4),
                                in0=Pt[:, bj, :].rearrange("p (g q) -> p g q", g=4),
                                in1=maskb[:, mk, :].unsqueeze(1).to_broadcast([128, 4, 128]), op=ALU.mult),
                                [Pt, maskb], [Pt])
                    pso = po[kh]
                    nb = len(kbl)
                    for g in range(4):
                        for bj, (kb_, mk) in enumerate(kbl):
                            k.T(lambda e, pso=pso, Pt=Pt, g=g, bj=bj, kb_=kb_, kh=kh, nb=nb: e.matmul(
                                pso[:, g, :], lhsT=Pt[:, bj, g * 128:(g + 1) * 128], rhs=vx[:, kb_, kh, :],
                                start=(bj == 0), stop=(bj == nb - 1)), [Pt, vx], [pso])
                    k.V(lambda e, pso=pso, kh=kh: e.tensor_tensor(out=den[:, kh * 4:(kh + 1) * 4], in0=pso[:, :, 64],
                                                                 in1=sink[:, kh * 4:(kh + 1) * 4], op=ALU.add),
                        [pso, sink], [den])
                    k.V(lambda e, kh=kh: e.reciprocal(out=den[:, kh * 4:(kh + 1) * 4], in_=den[:, kh * 4:(kh + 1) * 4]),
                        [den], [den])
                    k.V(lambda e, pso=pso, kh=kh, a_o=a_o: e.tensor_tensor(
                        out=a_o[:, kh * 256:(kh + 1) * 256].rearrange("p (g d) -> p g d", g=4),
                        in0=pso[:, :, 0:64], in1=den[:, kh * 4:(kh + 1) * 4].unsqueeze(2).to_broadcast([128, 4, 64]),
                        op=ALU.mult), [pso, den], [a_o])
                k.dma("act", lambda e, a_o=a_o, s=s, t=t: e.dma_start(
                    out=A["mixin_d"][s, t * 128:(t + 1) * 128, 0:512], in_=a_o[:]), reads=[a_o], writes=["mixin_d"])
                bi += 1


def phase_ret(k, A, P, l, last):
    with k.phase():
        identb = P["identb"]
        retC = k.sb("retC", [128, NT, 64], F32)
        retS = k.sb("retS", [128, NT, 64], F32)
        k.dma("sp", lambda e: e.dma_start(out=retC[:], in_=A["retC"].rearrange("(t p) d -> p t d", p=128)), writes=[retC])
        k.dma("sp", lambda e: e.dma_start(out=retS[:], in_=A["retS"].rearrange("(t p) d -> p t d", p=128)), writes=[retS])
        lg = k.sb("lg", [128, 8], F32)
        k.dma("sp", lambda e: e.dma_start(out=lg[:], in_=A["ret_decay_logit"][l, :].partition_broadcast(128)), writes=[lg])
        k.A(lambda e: e.activation(out=lg[:], in_=lg[:], func=AF.Exp, scale=-1.0), [lg], [lg])
        k.A(lambda e: e.activation(out=lg[:], in_=lg[:], func=AF.Ln, bias=1.0), [lg], [lg])
        k.V(lambda e: e.tensor_scalar(out=lg[:], in0=lg[:], scalar1=-1.0, scalar2=None, op0=ALU.mult), [lg], [lg])
        dco = k.sb("dco", [128, 4], F32)
        k.dma("sp", lambda e: e.dma_start(out=dco[:], in_=A["dcoef"]), writes=[dco])
        DT = k.sb("DT", [128, 2, 2, 4], F32)
        for d_ in range(2):
            for qk in range(2):
                k.A(lambda e, d_=d_, qk=qk: e.activation(out=DT[:, d_, qk, :], in_=lg[:, d_ * 4:(d_ + 1) * 4], func=AF.Exp,
                                                         scale=dco[:, d_ * 2 + qk: d_ * 2 + qk + 1]), [lg, dco], [DT])
            k.V(lambda e, d_=d_: e.tensor_scalar(out=DT[:, d_, 1, :], in0=DT[:, d_, 1, :], scalar1=0.125, scalar2=None,
                                                 op0=ALU.mult), [DT], [DT])
        g128 = k.sb("g128", [128, 2, 2], F32)
        for d_ in range(2):
            for pr in range(2):
                for hh in range(2):
                    h = 2 * pr + hh
                    k.A(lambda e, d_=d_, pr=pr, hh=hh, h=h: e.activation(
                        out=g128[hh * 64:(hh + 1) * 64, d_, pr:pr + 1], in_=lg[hh * 64:(hh + 1) * 64, d_ * 4 + h: d_ * 4 + h + 1],
                        func=AF.Exp, scale=128.0), [lg], [g128])
        gn = k.sb("gn", [128, 256], F32)
        k.dma("sp", lambda e: e.dma_start(out=gn[:], in_=A["ret_norm_g"][l, :].partition_broadcast(128)), writes=[gn])
        maskb = k.sb("maskb", [128, 3, 128], BF16)
        k.V(lambda e: e.tensor_copy(out=maskb[:], in_=P["masks"][:]), [P["masks"]], [maskb])

        RT = k.sb("RT", [128, 2, 2, 2, NT * 128], BF16)
        KD = k.sb("KD", [128, NT, 2, 256], BF16)
        Vb = k.sb("Vb", [128, NT, 256], BF16)
        SG = k.sb("SG", [128, NT, 256], F32)
        OA = k.sb("OA", [128, NT, 256], F32)
        ur = [k.sb("ur%d" % i, [128, 1024], F32) for i in range(2)]
        t1 = k.sb("t1", [128, 512], F32)
        t2 = k.sb("t2", [128, 512], F32)
        rot = k.sb("rot", [128, 512], F32)
        vr = [k.sb("vr%d" % i, [128, 2, 2, 256], BF16) for i in range(2)]
        ptr = [k.ps("ptr%d" % i, [128, 8, 128], BF16) for i in range(2)]
        pst = [k.ps("pst%d" % i, [128, 4, 128], F32) for i in range(2)]
        pso = [k.ps("pso%d" % i, [128, 4, 64], F32) for i in range(2)]
        pkv = [k.ps("pkv%d" % i, [128, 2, 128], F32) for i in range(2)]
        STm = [k.sb("STm%d" % i, [128, 4, 128], BF16) for i in range(2)]
        Sst = [k.sb("Sst%d" % i, [128, 2, 128], F32) for i in range(2)]
        Sb = [k.sb("Sb%d" % i, [128, 2, 128], BF16) for i in range(2)]
        tmp = k.sb("tmp", [128, 2, 128], F32)
        mean = k.sb("mean", [128, 4], F32)
        cen = k.sb("cen", [128, 256], F32)
        sq = k.sb("sq", [128, 256], F32)
        var = k.sb("var", [128, 4], F32)
        ro = [k.sb("ro%d" % i, [128, 256], F32) for i in range(2)]

        for s in range(NS):
            for t in range(NT):
                u = ur[t % 2]
                k.dma("sp", lambda e, u=u, s=s, t=t: e.dma_start(out=u[:], in_=A["u_d"][s, t * 128:(t + 1) * 128, 768:1792]),
                      reads=["u_d"], writes=[u])
                qk_v = u[:, 0:512].rearrange("p (h x d) -> p h x d", h=8, x=2, d=32)
                t1v = t1[:].rearrange("p (h x d) -> p h x d", h=8, x=2, d=32)
                t2v = t2[:].rearrange("p (h x d) -> p h x d", h=8, x=2, d=32)
                Cv = retC[:, t, :].rearrange("p (x d) -> p x d", x=2, d=32)
                Sv = retS[:, t, :].rearrange("p (x d) -> p x d", x=2, d=32)
                for x in range(2):
                    k.V(lambda e, x=x, qk_v=qk_v, t1v=t1v, Cv=Cv: e.tensor_tensor(
                        out=t1v[:, :, x, :], in0=qk_v[:, :, x, :], in1=Cv[:, x, :].unsqueeze(1).to_broadcast([128, 8, 32]),
                        op=ALU.mult), [u, retC], [t1])
                    k.G(lambda e, x=x, qk_v=qk_v, t2v=t2v, Sv=Sv: e.tensor_tensor(
                        out=t2v[:, :, x, :], in0=qk_v[:, :, 1 - x, :], in1=Sv[:, x, :].unsqueeze(1).to_broadcast([128, 8, 32]),
                        op=ALU.mult), [u, retS], [t2])
                k.V(lambda e: e.tensor_tensor(out=rot[:], in0=t1[:], in1=t2[:], op=ALU.add), [t1, t2], [rot])
                v_r = vr[t % 2]
                for d_ in range(2):
                    eng = k.V if d_ == 0 else k.G
                    eng(lambda e, d_=d_, v_r=v_r: e.tensor_tensor(
                        out=v_r[:, d_, :, :].rearrange("p q (h d) -> p (q h) d", d=64),
                        in0=rot[:].rearrange("p (qh d) -> p qh d", d=64),
                        in1=DT[:, d_, :, :].rearrange("p q h -> p (q h)").unsqueeze(2).to_broadcast([128, 8, 64]),
                        op=ALU.mult), [rot, DT], [v_r])
                    k.G(lambda e, d_=d_, v_r=v_r, t=t: e.tensor_copy(out=KD[:, t, d_, :], in_=v_r[:, d_, 1, :]), [v_r], [KD])
                k.V(lambda e, u=u, t=t: e.tensor_copy(out=Vb[:, t, :], in_=u[:, 512:768]), [u], [Vb])
                k.A(lambda e, u=u, t=t: e.activation(out=SG[:, t, :], in_=u[:, 768:1024], func=AF.Silu), [u], [SG])
                p = ptr[t % 2]
                for d_ in range(2):
                    for qk in range(2):
                        for pr in range(2):
                            k.T(lambda e, p=p, v_r=v_r, d_=d_, qk=qk, pr=pr: e.transpose(
                                p[:, d_ * 4 + qk * 2 + pr, :], v_r[:, d_, qk, pr * 128:(pr + 1) * 128], identb[:]),
                                [v_r, identb], [p])
                k.A(lambda e, p=p, t=t: e.copy(out=RT[:, :, :, :, t * 128:(t + 1) * 128].rearrange("p a b c t -> p (a b c) t"),
                                               in_=p[:]), [p], [RT])
            for d_ in range(2):
                order = list(range(NT)) if d_ == 0 else [1, 0] + list(range(NT - 1, 1, -1))
                mk = 0 if d_ == 0 else 2
                St = Sst[d_]
                Sbb = Sb[d_]
                k.V(lambda e, St=St: e.memset(St[:], 0.0), [], [St])
                k.V(lambda e, Sbb=Sbb: e.memset(Sbb[:], 0.0), [], [Sbb])
                for ci, t in enumerate(order):
                    pss = pst[ci % 2]
                    for h in range(4):
                        pr, hh = h // 2, h % 2
                        k.T(lambda e, pss=pss, h=h, pr=pr, hh=hh, t=t, d_=d_: e.matmul(
                            pss[:, h, :], lhsT=RT[hh * 64:(hh + 1) * 64, d_, 1, pr, t * 128:(t + 1) * 128],
                            rhs=RT[hh * 64:(hh + 1) * 64, d_, 0, pr, t * 128:(t + 1) * 128], start=True, stop=True),
                            [RT], [pss])
                    stm = STm[ci % 2]
                    k.V(lambda e, stm=stm, pss=pss, mk=mk: e.tensor_tensor(
                        out=stm[:], in0=pss[:], in1=maskb[:, mk, :].unsqueeze(1).to_broadcast([128, 4, 128]), op=ALU.mult),
                        [pss, maskb], [stm])
                    po_ = pso[ci % 2]
                    for h in range(4):
                        pr, hh = h // 2, h % 2
                        k.T(lambda e, po_=po_, stm=stm, h=h, t=t: e.matmul(
                            po_[:, h, :], lhsT=stm[:, h, :], rhs=Vb[:, t, h * 64:(h + 1) * 64], start=True, stop=False),
                            [stm, Vb], [po_])
                        k.T(lambda e, po_=po_, h=h, pr=pr, hh=hh, t=t, d_=d_, Sbb=Sbb: e.matmul(
                            po_[:, h, :], lhsT=RT[hh * 64:(hh + 1) * 64, d_, 0, pr, t * 128:(t + 1) * 128],
                            rhs=Sbb[hh * 64:(hh + 1) * 64, pr, hh * 64:(hh + 1) * 64], start=False, stop=True),
                            [RT, Sbb], [po_])
                    if d_ == 0:
                        k.A(lambda e, po_=po_, t=t: e.copy(out=OA[:, t, :], in_=po_[:].rearrange("p h d -> p (h d)")),
                            [po_], [OA])
                    else:
                        k.V(lambda e, po_=po_, t=t: e.tensor_tensor(out=OA[:, t, :], in0=OA[:, t, :],
                                                                    in1=po_[:].rearrange("p h d -> p (h d)"), op=ALU.add),
                            [po_, OA], [OA])
                    if ci == NT - 1:
                        continue
                    pk = pkv[ci % 2]
                    for pr in range(2):
                        k.T(lambda e, pk=pk, pr=pr, t=t, d_=d_: e.matmul(
                            pk[:, pr, :], lhsT=KD[:, t, d_, pr * 128:(pr + 1) * 128], rhs=Vb[:, t, pr * 128:(pr + 1) * 128],
                            start=True, stop=True), [KD, Vb], [pk])
                    k.V(lambda e, pk=pk, St=St: e.tensor_tensor(out=tmp[:], in0=pk[:], in1=St[:], op=ALU.add), [pk, St], [tmp])
                    for pr in range(2):
                        k.V(lambda e, pr=pr, St=St, d_=d_: e.tensor_scalar(out=St[:, pr, :], in0=tmp[:, pr, :],
                                                                          scalar1=g128[:, d_, pr:pr + 1], scalar2=None,
                                                                          op0=ALU.mult), [tmp, g128], [St])
                        k.A(lambda e, pr=pr, Sbb=Sbb, d_=d_: e.activation(out=Sbb[:, pr, :], in_=tmp[:, pr, :], func=AF.Copy,
                                                                         scale=g128[:, d_, pr:pr + 1]), [tmp, g128], [Sbb])
            tiles = list(range(2, NT)) + ([] if last else [0, 1])
            for i_, t in enumerate(tiles):
                o3 = OA[:, t, :].rearrange("p (h d) -> p h d", d=64)
                k.V(lambda e, o3=o3: e.tensor_reduce(out=mean[:], in_=o3, axis=AX.X, op=ALU.add), [OA], [mean])
                k.V(lambda e: e.tensor_scalar(out=mean[:], in0=mean[:], scalar1=1.0 / 64, scalar2=None, op0=ALU.mult),
                    [mean], [mean])
                k.V(lambda e, o3=o3: e.tensor_tensor(out=cen[:].rearrange("p (h d) -> p h d", d=64), in0=o3,
                                                     in1=mean[:].unsqueeze(2).to_broadcast([128, 4, 64]), op=ALU.subtract),
                    [OA, mean], [cen])
                k.G(lambda e: e.tensor_tensor(out=sq[:], in0=cen[:], in1=cen[:], op=ALU.mult), [cen], [sq])
                k.V(lambda e: e.tensor_reduce(out=var[:], in_=sq[:].rearrange("p (h d) -> p h d", d=64), axis=AX.X,
                                              op=ALU.add), [sq], [var])
                k.V(lambda e: e.tensor_scalar(out=var[:], in0=var[:], scalar1=1.0 / 64, scalar2=EPS, op0=ALU.mult,
                                              op1=ALU.add), [var], [var])
                k.A(lambda e: e.sqrt(out=var[:], in_=var[:]), [var], [var])
                k.V(lambda e: e.reciprocal(out=var[:], in_=var[:]), [var], [var])
                r_o = ro[i_ % 2]
                k.V(lambda e, r_o=r_o: e.tensor_tensor(out=r_o[:].rearrange("p (h d) -> p h d", d=64),
                                                       in0=cen[:].rearrange("p (h d) -> p h d", d=64),
                                                       in1=var[:].unsqueeze(2).to_broadcast([128, 4, 64]), op=ALU.mult),
                    [cen, var], [r_o])
                k.G(lambda e, r_o=r_o: e.tensor_tensor(out=r_o[:], in0=r_o[:], in1=gn[:], op=ALU.mult), [r_o, gn], [r_o])
                k.V(lambda e, r_o=r_o, t=t: e.tensor_tensor(out=r_o[:], in0=r_o[:], in1=SG[:, t, :], op=ALU.mult),
                    [r_o, SG], [r_o])
                k.dma("act", lambda e, r_o=r_o, s=s, t=t: e.dma_start(
                    out=A["mixin_d"][s, t * 128:(t + 1) * 128, 512:768], in_=r_o[:]), reads=[r_o], writes=["mixin_d"])


def phase_conv(k, A, P, l, last):
    with k.phase():
        ident = P["ident"]
        identb = P["identb"]
        cw = k.sb("cw", [31, 256], F32)
        k.dma("sp", lambda e: e.dma_start(out=cw[:], in_=A["conv_w"][l]), writes=[cw])
        pw = k.ps("pw", [128, 2, 32], F32)
        for cc in range(2):
            k.T(lambda e, cc=cc: e.transpose(pw[:, cc, 0:31], cw[0:31, cc * 128:(cc + 1) * 128], ident[0:31, 0:31]),
                [cw, ident], [pw])
        cwT = k.sb("cwT", [128, 2, 32], F32)
        k.V(lambda e: e.tensor_copy(out=cwT[:, :, 0:31], in_=pw[:, :, 0:31]), [pw], [cwT])
        diag = k.sb("diag", [128, 2, 31, 128], BF16)
        for cc in range(2):
            for kk in range(31):
                eng = k.V if kk % 2 == 0 else k.G
                eng(lambda e, cc=cc, kk=kk: e.tensor_scalar(out=diag[:, cc, kk, :], in0=ident[:],
                                                            scalar1=cwT[:, cc, kk:kk + 1], scalar2=None, op0=ALU.mult),
                    [ident, cwT], [diag])
        vec = k.sb("vec", [3, 256], F32)
        k.dma("sp", lambda e: e.dma_start(out=vec[0:1, :], in_=A["conv_b"][l:l + 1, :]), writes=[vec])
        k.dma("sp", lambda e: e.dma_start(out=vec[1:2, :], in_=A["conv_norm_g"][l:l + 1, :]), writes=[vec])
        k.dma("sp", lambda e: e.dma_start(out=vec[2:3, :], in_=A["conv_norm_b"][l:l + 1, :]), writes=[vec])
        pv = k.ps("pv", [128, 2, 4], F32)
        for cc in range(2):
            k.T(lambda e, cc=cc: e.transpose(pv[:, cc, 0:3], vec[0:3, cc * 128:(cc + 1) * 128], ident[0:3, 0:3]),
                [vec, ident], [pv])
        vT = k.sb("vT", [128, 2, 4], F32)
        k.V(lambda e: e.tensor_copy(out=vT[:, :, 0:3], in_=pv[:, :, 0:3]), [pv], [vT])
        ones = k.sb("ones", [128, 128], F32R)
        onesf = k.sb("onesf", [128, 128], F32)
        k.V(lambda e: e.memset(onesf[:], 1.0 / 256), [], [onesf])
        k.V(lambda e: e.tensor_copy(out=ones[:], in_=onesf[:]), [onesf], [ones])

        LP = S + 30
        hp = [k.sb("hp%d" % i, [128, LP], BF16) for i in range(2)]
        val = [k.sb("val%d" % i, [128, S], F32) for i in range(2)]
        gt = [k.sb("gt%d" % i, [128, S], F32) for i in range(2)]
        cvo = [k.sb("cvo%d" % i, [128, 512], F32R) for i in range(2)]
        csq = [k.sb("csq%d" % i, [128, 512], F32R) for i in range(2)]
        pcv = [k.ps("pcv%d" % i, [128, 512], F32) for i in range(2)]
        pmean = k.ps("pmean", [128, 512], F32)
        pex2 = k.ps("pex2", [128, 512], F32)
        msb = k.sb("msb", [128, 512], F32)
        rsd = k.sb("rsd", [128, 512], F32)
        yv = [k.sb("yv%d" % i, [128, 512], F32) for i in range(2)]
        yo = [k.sb("yo%d" % i, [128, 512], F32) for i in range(2)]
        for s in range(NS):
            segs = [(C, S)] + ([] if last else [(0, C)])
            for (tok0, Lg) in segs:
                for cc in range(2):
                    k.dma("sp", lambda e, cc=cc, s=s, tok0=tok0, Lg=Lg: e.dma_start(
                        out=val[cc][:, 0:Lg], in_=A["cv_d"][s, cc * 128:(cc + 1) * 128, tok0:tok0 + Lg]),
                        reads=["cv_d"], writes=[val[cc]])
                    k.dma("act", lambda e, cc=cc, s=s, tok0=tok0, Lg=Lg: e.dma_start(
                        out=gt[cc][:, 0:Lg], in_=A["cv_d"][s, 256 + cc * 128: 256 + (cc + 1) * 128, tok0:tok0 + Lg]),
                        reads=["cv_d"], writes=[gt[cc]])
                    k.A(lambda e, cc=cc, Lg=Lg: e.activation(out=gt[cc][:, 0:Lg], in_=gt[cc][:, 0:Lg], func=AF.Sigmoid),
                        [gt[cc]], [gt[cc]])
                    k.G(lambda e, cc=cc: e.memset(hp[cc][:], 0.0), [], [hp[cc]])
                    k.V(lambda e, cc=cc, Lg=Lg: e.tensor_tensor(out=hp[cc][:, 15:15 + Lg], in0=val[cc][:, 0:Lg],
                                                               in1=gt[cc][:, 0:Lg], op=ALU.mult),
                        [val[cc], gt[cc]], [hp[cc]])
                nb = (Lg + 511) // 512
                for b in range(nb):
                    w = min(512, Lg - b * 512)
                    for cc in range(2):
                        p = pcv[cc]
                        for kk in range(31):
                            k.T(lambda e, p=p, cc=cc, kk=kk, b=b, w=w: e.matmul(
                                p[:, 0:w], lhsT=diag[:, cc, kk, :], rhs=hp[cc][:, b * 512 + kk: b * 512 + kk + w],
                                start=(kk == 0), stop=(kk == 30)), [diag, hp[cc]], [p])
                        k.A(lambda e, p=p, cc=cc, w=w: e.activation(out=cvo[cc][:, 0:w], in_=p[:, 0:w], func=AF.Identity,
                                                                   bias=vT[:, cc, 0:1], scale=1.0), [p, vT], [cvo[cc]])
                        k.V(lambda e, cc=cc, w=w: e.tensor_tensor(out=csq[cc][:, 0:w], in0=cvo[cc][:, 0:w].bitcast(F32),
                                                                 in1=cvo[cc][:, 0:w].bitcast(F32), op=ALU.mult),
                            [cvo[cc]], [csq[cc]])
                    for cc in range(2):
                        k.T(lambda e, cc=cc, w=w: e.matmul(pmean[:, 0:w], lhsT=ones[:], rhs=cvo[cc][:, 0:w],
                                                           start=(cc == 0), stop=(cc == 1)), [ones, cvo[cc]], [pmean])
                    for cc in range(2):
                        k.T(lambda e, cc=cc, w=w: e.matmul(pex2[:, 0:w], lhsT=ones[:], rhs=csq[cc][:, 0:w],
                                                           start=(cc == 0), stop=(cc == 1)), [ones, csq[cc]], [pex2])
                    k.A(lambda e, w=w: e.copy(out=msb[:, 0:w], in_=pmean[:, 0:w]), [pmean], [msb])
                    k.V(lambda e, w=w: e.tensor_tensor(out=rsd[:, 0:w], in0=msb[:, 0:w], in1=msb[:, 0:w], op=ALU.mult),
                        [msb], [rsd])
                    k.V(lambda e, w=w: e.tensor_tensor(out=rsd[:, 0:w], in0=pex2[:, 0:w], in1=rsd[:, 0:w], op=ALU.subtract),
                        [pex2, rsd], [rsd])
                    k.V(lambda e, w=w: e.tensor_scalar(out=rsd[:, 0:w], in0=rsd[:, 0:w], scalar1=EPS, scalar2=None,
                                                       op0=ALU.add), [rsd], [rsd])
                    k.A(lambda e, w=w: e.sqrt(out=rsd[:, 0:w], in_=rsd[:, 0:w]), [rsd], [rsd])
                    k.V(lambda e, w=w: e.reciprocal(out=rsd[:, 0:w], in_=rsd[:, 0:w]), [rsd], [rsd])
                    for cc in range(2):
                        k.G(lambda e, cc=cc, w=w: e.tensor_tensor(out=yv[cc][:, 0:w], in0=cvo[cc][:, 0:w].bitcast(F32),
                                                                 in1=msb[:, 0:w], op=ALU.subtract), [cvo[cc], msb], [yv[cc]])
                        k.V(lambda e, cc=cc, w=w: e.tensor_tensor(out=yv[cc][:, 0:w], in0=yv[cc][:, 0:w], in1=rsd[:, 0:w],
                                                                 op=ALU.mult), [yv[cc], rsd], [yv[cc]])
                        k.A(lambda e, cc=cc, w=w: e.activation(out=yo[cc][:, 0:w], in_=yv[cc][:, 0:w], func=AF.Silu,
                                                               scale=vT[:, cc, 1:2], bias=vT[:, cc, 2:3]),
                            [yv[cc], vT], [yo[cc]])
                        k.dma("act", lambda e, cc=cc, s=s, tok0=tok0, b=b, w=w: e.dma_start(
                            out=A["convT_d"][s, cc * 128:(cc + 1) * 128, tok0 + b * 512: tok0 + b * 512 + w],
                            in_=yo[cc][:, 0:w]), reads=[yo[cc]], writes=["convT_d"])


def phase_out(k, A, P, l, last, src_lat, src_ctx, dst_lat):
    with k.phase():
        ident = P["ident"]
        wout = k.sb("wout", [128, 8, D], F32R)
        wv = A["w_out"][l].rearrange("(kc p) f -> p kc f", p=128)
        for j in range(0, D, 512):
            k.dma("pool", lambda e, j=j: e.dma_start(out=wout[:, :, j:j + 512], in_=wv[:, :, j:j + 512]), writes=[wout])
        wr = k.sb("wr", [128, 8, E], F32)
        k.dma("sp", lambda e: e.dma_start(out=wr[:], in_=A["w_router"][l].rearrange("(kc p) e -> p kc e", p=128)),
              writes=[wr])
        nr = NS + (0 if last else 1)
        gta = [k.sb("gta%d" % r, [128, D], F32) for r in range(nr)]
        gsf = [k.sb("gsf%d" % r, [128, D], F32) for r in range(nr)]
        shf = [k.sb("shf%d" % r, [128, D], F32) for r in range(nr)]
        gf = k.sb("gf", [128, D], F32)
        k.dma("sp", lambda e: e.dma_start(out=gf[:], in_=A["g_ffn"][l, :].partition_broadcast(128)), writes=[gf])
        for r in range(nr):
            k.dma("sp", lambda e, r=r: e.dma_start(out=gta[r][:], in_=A["mod_d"][l, r, 2 * D:3 * D].partition_broadcast(128)),
                  reads=["mod_d"], writes=[gta[r]])
            k.dma("sp", lambda e, r=r: e.dma_start(out=shf[r][:], in_=A["mod_d"][l, r, 3 * D:4 * D].partition_broadcast(128)),
                  reads=["mod_d"], writes=[shf[r]])
            k.dma("sp", lambda e, r=r: e.dma_start(out=gsf[r][:], in_=A["mod_d"][l, r, 4 * D:5 * D].partition_broadcast(128)),
                  reads=["mod_d"], writes=[gsf[r]])
            k.V(lambda e, r=r: e.scalar_tensor_tensor(out=gsf[r][:], in0=gsf[r][:], scalar=1.0, in1=gf[:], op0=ALU.add,
                                                      op1=ALU.mult), [gsf[r], gf], [gsf[r]])
        mi = [k.sb("mi%d" % i, [128, 768], F32) for i in range(2)]
        mixT = [k.sb("mixT%d" % i, [128, 8, 128], F32R) for i in range(2)]
        xt = [k.sb("xt%d" % i, [128, D], F32) for i in range(2)]
        xm = [k.sb("xm%d" % i, [128, D], F32) for i in range(2)]
        hf = [k.sb("hf%d" % i, [128, D], F32) for i in range(2)]
        junk = k.sb("junk", [128, D], F32)
        ss = [k.sb("ss%d" % i, [128, 1], F32) for i in range(2)]
        rstd = [k.sb("rstd%d" % i, [128, 1], F32) for i in range(2)]
        hfT = [k.sb("hfT%d" % i, [128, 8, 128], F32) for i in range(2)]
        ptm = k.ps("ptm", [128, 6, 128], F32)
        py = [k.ps("py%d" % i, [128, 512], F32) for i in range(2)]
        pth = k.ps("pth", [128, 8, 128], F32)
        plg = k.ps("plg", [128, 16], F32)
        pat = k.ps("pat", [48, 128], F32)
        lmax = k.sb("lmax", [128, 1], F32)
        lsum = k.sb("lsum", [128, 1], F32)
        afftm = k.sb("afftm", [128, 48], F32)
        affT = k.sb("affT", [48, S + C], F32)
        k.V(lambda e: e.memset(afftm[:], 0.0), [], [afftm])
        ti = 0
        tiles = list(range(2, NT)) + ([] if last else [0, 1])
        for t in tiles:
            for s in range(NS):
                isctx = t < 2
                r = NS if isctx else s
                if isctx:
                    src = src_ctx[s * C + t * 128: s * C + (t + 1) * 128, :]
                    dst = A["res_d"][NLAT + s * C + t * 128: NLAT + s * C + (t + 1) * 128, :]
                    hrow = NLAT + s * C + t * 128
                else:
                    src = src_lat[s * S + (t - 2) * 128: s * S + (t - 1) * 128, :]
                    dst = dst_lat[s * S + (t - 2) * 128: s * S + (t - 1) * 128, :]
                    hrow = s * S + (t - 2) * 128
                m = mi[ti % 2]
                mt = mixT[ti % 2]
                x_t = xt[ti % 2]
                x_m = xm[ti % 2]
                h_f = hf[ti % 2]
                h_T = hfT[ti % 2]
                sst = ss[ti % 2]
                rs = rstd[ti % 2]
                k.dma("sp", lambda e, m=m, s=s, t=t: e.dma_start(out=m[:], in_=A["mixin_d"][s, t * 128:(t + 1) * 128, :]),
                      reads=["mixin_d"], writes=[m])
                k.dma("pool", lambda e, mt=mt, s=s, t=t: e.dma_start(
                    out=mt[:, 6:8, :], in_=A["convT_d"][s, :, t * 128:(t + 1) * 128].rearrange("(c p) t -> p c t", p=128)),
                    reads=["convT_d"], writes=[mt])
                k.dma("act", lambda e, x_t=x_t, src=src: e.dma_start(out=x_t[:], in_=src), writes=[x_t])
                for j in range(6):
                    k.T(lambda e, m=m, j=j: e.transpose(ptm[:, j, :], m[:, j * 128:(j + 1) * 128], ident[:]), [m, ident], [ptm])
                k.A(lambda e, mt=mt: e.copy(out=mt[:, 0:6, :], in_=ptm[:]), [ptm], [mt])
                for hh in range(2):
                    p = py[hh]
                    for kc in range(8):
                        k.T(lambda e, p=p, mt=mt, kc=kc, hh=hh: e.matmul(p[:], lhsT=mt[:, kc, :],
                                                                         rhs=wout[:, kc, hh * 512:(hh + 1) * 512],
                                                                         start=(kc == 0), stop=(kc == 7)), [mt, wout], [p])
                    k.V(lambda e, p=p, hh=hh, x_m=x_m, r=r: e.tensor_tensor(out=x_m[:, hh * 512:(hh + 1) * 512], in0=p[:],
                                                                           in1=gta[r][:, hh * 512:(hh + 1) * 512], op=ALU.mult),
                        [p, gta[r]], [x_m])
                k.G(lambda e, x_m=x_m, x_t=x_t: e.tensor_tensor(out=x_m[:], in0=x_m[:], in1=x_t[:], op=ALU.add),
                    [x_m, x_t], [x_m])
                k.dma("sp", lambda e, x_m=x_m, dst=dst: e.dma_start(out=dst, in_=x_m[:]), reads=[x_m], writes=["res"])
                rms_rstd(k, x_m[:], junk[:], sst, rs, D)
                k.V(lambda e, h_f=h_f, x_m=x_m, rs=rs, r=r: e.scalar_tensor_tensor(
                    out=h_f[:], in0=x_m[:], scalar=rs[:, 0:1], in1=gsf[r][:], op0=ALU.mult, op1=ALU.mult),
                    [x_m, rs, gsf[r]], [h_f])
                k.G(lambda e, h_f=h_f, r=r: e.tensor_tensor(out=h_f[:], in0=h_f[:], in1=shf[r][:], op=ALU.add),
                    [h_f, shf[r]], [h_f])
                k.dma("act", lambda e, h_f=h_f, hrow=hrow: e.dma_start(out=A["hf_d"][hrow:hrow + 128, :], in_=h_f[:]),
                      reads=[h_f], writes=["hf_d"])
                for kc in range(8):
                    k.T(lambda e, h_f=h_f, kc=kc: e.transpose(pth[:, kc, :], h_f[:, kc * 128:(kc + 1) * 128], ident[:]),
                        [h_f, ident], [pth])
                k.A(lambda e, h_T=h_T: e.copy(out=h_T[:], in_=pth[:]), [pth], [h_T])
                for kc in range(8):
                    k.T(lambda e, h_T=h_T, kc=kc: e.matmul(plg[:], lhsT=h_T[:, kc, :], rhs=wr[:, kc, :], start=(kc == 0),
                                                           stop=(kc == 7)), [h_T, wr], [plg])
                k.V(lambda e: e.tensor_reduce(out=lmax[:], in_=plg[:], axis=AX.X, op=ALU.max, negate=True), [plg], [lmax])
                k.A(lambda e, s=s: e.activation(out=afftm[:, s * 32:s * 32 + 16], in_=plg[:], func=AF.Exp, bias=lmax[:, 0:1],
                                                scale=1.0, accum_out=lsum[:, 0:1]), [plg, lmax], [afftm, lsum])
                k.V(lambda e: e.reciprocal(out=lsum[:], in_=lsum[:]), [lsum], [lsum])
                k.V(lambda e, s=s: e.tensor_scalar(out=afftm[:, s * 32:s * 32 + 16], in0=afftm[:, s * 32:s * 32 + 16],
                                                   scalar1=lsum[:, 0:1], scalar2=None, op0=ALU.mult), [afftm, lsum], [afftm])
                ti += 1
            k.T(lambda e: e.transpose(pat[:], afftm[:], ident[:]), [afftm, ident], [pat])
            k.A(lambda e, t=t: e.copy(out=affT[:, t * 128:(t + 1) * 128], in_=pat[:]), [pat], [affT])
        k.dma("sp", lambda e: e.dma_start(out=A["aff_d"], in_=affT[:, C:C + S]), reads=[affT], writes=["aff_d"])
        if not last:
            k.dma("sp", lambda e: e.dma_start(out=A["affc_d"], in_=affT[:, 0:C]), reads=[affT], writes=["affc_d"])


def phase_moe(k, A, P, l, last, dst_lat):
    with k.phase():
        ident = P["ident"]
        nctx = 0 if last else NS
        NSL = NS * CAPL + nctx * CAPC
        if last:
            cgroups = [(0, 512)]
        else:
            cgroups = [(0, 288), (288, 288)]
        idxT = k.sb("idxT", [128, 2, 48], I32)
        gT = k.sb("gTm", [128, 2, 48], F32)
        if not last:
            idcT = k.sb("idcT", [32, 48], I32)
            gcT = k.sb("gcT", [32, 48], F32)
        with k.phase():
            wa = k.sb("wa", [48, S], F32)
            wb = k.sb("wb", [48, S], F32)
            vals = k.sb("vals", [48, CAPL], F32)
            idx = k.sb("idx", [48, CAPL], U32)
            idxf = k.sb("idxf", [48, CAPL], F32)
            offs = k.sb("offs", [48, 1], F32)
            k.dma("sp", lambda e: e.dma_start(out=wa[:], in_=A["aff_d"]), reads=["aff_d"], writes=[wa])
            cur, oth = wa, wb
            for r in range(CAPL // 8):
                k.V(lambda e, cur=cur, r=r: e.max(out=vals[:, r * 8:(r + 1) * 8], in_=cur[:]), [cur], [vals])
                k.V(lambda e, cur=cur, r=r: e.max_index(out=idx[:, r * 8:(r + 1) * 8], in_max=vals[:, r * 8:(r + 1) * 8],
                                                        in_values=cur[:]), [cur, vals], [idx])
                if r < CAPL // 8 - 1:
                    k.V(lambda e, cur=cur, oth=oth, r=r: e.match_replace(out=oth[:], in_to_replace=vals[:, r * 8:(r + 1) * 8],
                                                                         in_values=cur[:], imm_value=-1.0), [cur, vals], [oth])
                    cur, oth = oth, cur
            k.V(lambda e: e.memset(offs[0:32, :], 0.0), [], [offs])
            k.V(lambda e: e.memset(offs[32:48, :], float(S)), [], [offs])
            k.V(lambda e: e.tensor_copy(out=idxf[:], in_=idx[:]), [idx], [idxf])
            k.V(lambda e: e.tensor_scalar(out=idxf[:], in0=idxf[:], scalar1=offs[:, 0:1], scalar2=None, op0=ALU.add),
                [idxf, offs], [idxf])
            pti = k.ps("pti", [128, 2, 48], F32)
            ptg = k.ps("ptg", [128, 2, 48], F32)
            for j in range(2):
                k.T(lambda e, j=j: e.transpose(pti[:, j, :], idxf[:, j * 128:(j + 1) * 128], ident[0:48, 0:48]), [idxf, ident], [pti])
                k.T(lambda e, j=j: e.transpose(ptg[:, j, :], vals[:, j * 128:(j + 1) * 128], ident[0:48, 0:48]), [vals, ident], [ptg])
            k.V(lambda e: e.tensor_copy(out=idxT[:], in_=pti[:]), [pti], [idxT])
            k.V(lambda e: e.tensor_copy(out=gT[:], in_=ptg[:]), [ptg], [gT])
            if not last:
                wc = k.sb("wc", [48, C], F32)
                wd_ = k.sb("wd_", [48, C], F32)
                valc = k.sb("valc", [48, CAPC], F32)
                idc = k.sb("idc", [48, CAPC], U32)
                idcf = k.sb("idcf", [48, CAPC], F32)
                offc = k.sb("offc", [48, 1], F32)
                k.dma("sp", lambda e: e.dma_start(out=wc[:], in_=A["affc_d"]), reads=["affc_d"], writes=[wc])
                cur, oth = wc, wd_
                for r in range(CAPC // 8):
                    k.V(lambda e, cur=cur, r=r: e.max(out=valc[:, r * 8:(r + 1) * 8], in_=cur[:]), [cur], [valc])
                    k.V(lambda e, cur=cur, r=r: e.max_index(out=idc[:, r * 8:(r + 1) * 8], in_max=valc[:, r * 8:(r + 1) * 8],
                                                            in_values=cur[:]), [cur, valc], [idc])
                    if r < CAPC // 8 - 1:
                        k.V(lambda e, cur=cur, oth=oth, r=r: e.match_replace(out=oth[:], in_to_replace=valc[:, r * 8:(r + 1) * 8],
                                                                             in_values=cur[:], imm_value=-1.0), [cur, valc], [oth])
                        cur, oth = oth, cur
                k.V(lambda e: e.memset(offc[0:32, :], float(NLAT)), [], [offc])
                k.V(lambda e: e.memset(offc[32:48, :], float(NLAT + C)), [], [offc])
                k.V(lambda e: e.tensor_copy(out=idcf[:], in_=idc[:]), [idc], [idcf])
                k.V(lambda e: e.tensor_scalar(out=idcf[:], in0=idcf[:], scalar1=offc[:, 0:1], scalar2=None, op0=ALU.add),
                    [idcf, offc], [idcf])
                ptic = k.ps("ptic", [32, 2, 48], F32)
                k.T(lambda e: e.transpose(ptic[:, 0, :], idcf[:, 0:32], ident[0:48, 0:48]), [idcf, ident], [ptic])
                k.T(lambda e: e.transpose(ptic[:, 1, :], valc[:, 0:32], ident[0:48, 0:48]), [valc, ident], [ptic])
                k.V(lambda e: e.tensor_copy(out=idcT[:], in_=ptic[:, 0, :]), [ptic], [idcT])
                k.V(lambda e: e.tensor_copy(out=gcT[:], in_=ptic[:, 1, :]), [ptic], [gcT])
        nr = NS + (0 if last else 1)
        gtf = [k.sb("gtf%d" % r, [128, D], F32) for r in range(nr)]
        for r in range(nr):
            k.dma("sp", lambda e, r=r: e.dma_start(out=gtf[r][:], in_=A["mod_d"][l, r, 5 * D:6 * D].partition_broadcast(128)),
                  reads=["mod_d"], writes=[gtf[r]])
        xe = [k.sb("xe%d" % i, [128, D], F32) for i in range(3)]
        xeT = [k.sb("xeT%d" % i, [128, 8, NSL], F32R) for i in range(2)]
        hT = k.sb("hT", [128, NFC, NSL], BF16)
        sg = [k.sb("sg%d" % i, [128, NSL], F32) for i in range(2)]
        wg = [k.sb("wg%d" % i, [128, 8, 256], F32R) for i in range(3)]
        wu = [k.sb("wu%d" % i, [128, 8, 256], F32R) for i in range(3)]
        wdn = k.sb("wdn", [128, NFC, D], BF16)
        ysb = [k.sb("ysb%d" % i, [128, D], F32) for i in range(2)]
        ncg = len(cgroups)
        pg = [k.ps("pg%d" % i, [128, 512], F32) for i in range(ncg)]
        pu = [k.ps("pu%d" % i, [128, 512], F32) for i in range(ncg)]
        pxt = k.ps("pxt", [128, 8, 128], F32)
        pyd = [k.ps("pyd%d" % i, [128, 512], F32) for i in range(8 - 2 * ncg - 2)]
        target = dst_lat
        npw = DFF // 256
        for ex in range(E):
            tiles = []
            for s in range(NS):
                for j in range(2):
                    tiles.append((128, s * 256 + j * 128, idxT[:, j, s * 32 + ex: s * 32 + ex + 1],
                                  gT[:, j, s * 32 + ex: s * 32 + ex + 1], s, "lat"))
            if not last:
                for s in range(NS):
                    tiles.append((32, NS * CAPL + s * CAPC, idcT[:, s * 32 + ex: s * 32 + ex + 1],
                                  gcT[:, s * 32 + ex: s * 32 + ex + 1], NS, "ctx"))
            xT = xeT[ex % 2]
            for ti, (rows, c0, iap, gap, r, kind) in enumerate(tiles):
                x_e = xe[ti % 3]
                k.dma("pool", lambda e, x_e=x_e, rows=rows, iap=iap: e.indirect_dma_start(
                    out=x_e[0:rows, :], out_offset=None, in_=A["hf_d"],
                    in_offset=bass.IndirectOffsetOnAxis(ap=iap[0:rows, :], axis=0)),
                    reads=["hf_d", idxT] + ([] if last else [idcT]), writes=[x_e])
                for kc in range(8):
                    k.T(lambda e, x_e=x_e, rows=rows, kc=kc: e.transpose(pxt[:, kc, 0:rows], x_e[0:rows, kc * 128:(kc + 1) * 128],
                                                                       ident[0:rows, 0:rows]), [x_e, ident], [pxt])
                eng = k.A if ti % 2 == 0 else k.V
                if ti % 2 == 0:
                    k.A(lambda e, xT=xT, rows=rows, c0=c0: e.copy(out=xT[:, :, c0:c0 + rows], in_=pxt[:, :, 0:rows]), [pxt], [xT])
                else:
                    k.V(lambda e, xT=xT, rows=rows, c0=c0: e.tensor_copy(out=xT[:, :, c0:c0 + rows], in_=pxt[:, :, 0:rows]),
                        [pxt], [xT])
            wgv = A["w_gate"][l, ex].rearrange("(kc p) f -> p kc f", p=128)
            wuv = A["w_up"][l, ex].rearrange("(kc p) f -> p kc f", p=128)
            wdv = A["w_down"][l, ex].rearrange("(fc p) d -> p fc d", p=128)
            for pi in range(npw):
                w_g = wg[pi % 3]
                w_u = wu[pi % 3]
                k.dma("pool", lambda e, w_g=w_g, pi=pi, wgv=wgv: e.dma_start(out=w_g[:], in_=wgv[:, :, pi * 256:(pi + 1) * 256]),
                      writes=[w_g])
                k.dma("pool", lambda e, w_u=w_u, pi=pi, wuv=wuv: e.dma_start(out=w_u[:], in_=wuv[:, :, pi * 256:(pi + 1) * 256]),
                      writes=[w_u])
                k.dma("pool", lambda e, pi=pi, wdv=wdv: e.dma_start(out=wdn[:, 2 * pi:2 * pi + 2, :], in_=wdv[:, 2 * pi:2 * pi + 2, :]),
                      writes=[("wdn", pi)], reads=[])
                for f2 in range(2):
                    fc = pi * 2 + f2
                    for (W, ps_list) in ((w_g, pg), (w_u, pu)):
                        for gi_, (g0, gw) in enumerate(cgroups):
                            p = ps_list[gi_]
                            for kc in range(8):
                                k.T(lambda e, p=p, W=W, kc=kc, f2=f2, g0=g0, gw=gw, xT=xT: e.matmul(
                                    p[:, 0:gw], lhsT=W[:, kc, f2 * 128:(f2 + 1) * 128], rhs=xT[:, kc, g0:g0 + gw],
                                    start=(kc == 0), stop=(kc == 7)), [W, xT], [p])
                    s_g = sg[fc % 2]
                    for gi_, (g0, gw) in enumerate(cgroups):
                        k.A(lambda e, s_g=s_g, gi_=gi_, g0=g0, gw=gw: e.activation(out=s_g[:, g0:g0 + gw], in_=pg[gi_][:, 0:gw],
                                                                                  func=AF.Silu), [pg[gi_]], [s_g])
                        k.V(lambda e, s_g=s_g, gi_=gi_, g0=g0, gw=gw, fc=fc: e.tensor_tensor(
                            out=hT[:, fc, g0:g0 + gw], in0=s_g[:, g0:g0 + gw], in1=pu[gi_][:, 0:gw], op=ALU.mult),
                            [s_g, pu[gi_]], [("hT", fc)])
            oi = 0
            for ti, (rows, c0, iap, gap, r, kind) in enumerate(tiles):
                y_s = ysb[ti % 2]
                for hh in range(2):
                    p = pyd[oi % len(pyd)]
                    oi += 1
                    for fc in range(NFC):
                        k.T(lambda e, p=p, rows=rows, c0=c0, fc=fc, hh=hh: e.matmul(
                            p[0:rows, :], lhsT=hT[:, fc, c0:c0 + rows], rhs=wdn[:, fc, hh * 512:(hh + 1) * 512],
                            start=(fc == 0), stop=(fc == NFC - 1)), [("hT", fc), ("wdn", fc // 2)], [p])
                    k.V(lambda e, p=p, rows=rows, y_s=y_s, hh=hh, gap=gap, r=r: e.scalar_tensor_tensor(
                        out=y_s[0:rows, hh * 512:(hh + 1) * 512], in0=p[0:rows, :], scalar=gap[0:rows, :],
                        in1=gtf[r][0:rows, hh * 512:(hh + 1) * 512], op0=ALU.mult, op1=ALU.mult),
                        [p, gT, gtf[r]] + ([] if last else [gcT]), [y_s])
                tgt = target if kind == "lat" else A["res_d"]
                k.dma("pool", lambda e, y_s=y_s, rows=rows, iap=iap, tgt=tgt: e.indirect_dma_start(
                    out=tgt, out_offset=bass.IndirectOffsetOnAxis(ap=iap[0:rows, :], axis=0), in_=y_s[0:rows, :],
                    in_offset=None, compute_op=ALU.add), reads=[y_s, idxT] + ([] if last else [idcT]), writes=["res"])


_NC_CACHE = {}


def kernel(**inputs):
    consts = host_consts()
    if "nc" not in _NC_CACHE:
        _NC_CACHE["nc"] = build_program()
    nc = _NC_CACHE["nc"]
    x = np.ascontiguousarray(inputs["x"], dtype=np.float32)
    c = np.ascontiguousarray(inputs["c"], dtype=np.float32)
    ctx = np.ascontiguousarray(inputs["ctx"], dtype=np.float32)
    shared = {"c_ctx": np.ascontiguousarray(inputs["c_ctx"], dtype=np.float32).reshape(1, D)}
    for n in WNAMES:
        shared[n] = np.ascontiguousarray(inputs[n], dtype=np.float32).reshape(WSHAPES[n])
    for n, v in consts.items():
        shared[n] = v
    in_maps = []
    for ci in range(NCORES):
        m = dict(shared)
        m["x"] = x[ci * NS:(ci + 1) * NS].reshape(NS * S, D)
        m["c"] = c[ci * NS:(ci + 1) * NS]
        m["ctx"] = ctx[ci * NS:(ci + 1) * NS].reshape(NS * C, D)
        in_maps.append(m)
    res = run_bass_kernel_spmd(nc, in_maps, core_ids=list(range(NCORES)))
    out = np.concatenate([r["y"].reshape(NS, S, D) for r in res.results], axis=0)
    return out.astype(np.float32)
```

```python
import os
import numpy as np
import concourse.bass as bass
import concourse.mybir as mybir
from concourse.bass_utils import run_bass_kernel_spmd
from contextlib import ExitStack, contextmanager

F32 = mybir.dt.float32
F32R = mybir.dt.float32r
BF16 = mybir.dt.bfloat16
U32 = mybir.dt.uint32
I32 = mybir.dt.int32
AF = mybir.ActivationFunctionType
ALU = mybir.AluOpType
AX = mybir.AxisListType

NCORES = 8
NS = 2
D = 1024
S = 2048
C = 256
NT = (S + C) // 128
L = 2
E = 16
DFF = 2816
NFC = DFF // 128
CAPL = 256
CAPC = 32
EPS = 1e-6
UC = 1792
NLAT = NS * S
NROW = NS * (S + C)


class KB:
    ENGS = ("pe", "dve", "act", "pool", "sp")

    def __init__(self, nc, ndma=24):
        self.nc = nc
        self.st = ExitStack()
        self.q = {e: [] for e in self.ENGS}
        self.cnt = {e: 0 for e in self.ENGS}
        self.sem = {e: self.st.enter_context(nc.semaphore("prog_" + e)) for e in self.ENGS}
        self.waited = {(c, p): 0 for c in self.ENGS for p in self.ENGS}
        self.waited_dma = {}
        self.lastw = {}
        self.readers = {}
        self.dma_sems = []
        self.ndma = {"sp": 0, "act": 0, "pool": 0}
        self.pool_of = {"sp": (0, 10), "act": (10, 8), "pool": (18, 14)}
        for i in range(32):
            s = self.st.enter_context(nc.semaphore("dma%d" % i))
            self.dma_sems.append([s, 0, None])
        self.cur = self.st

    def sb(self, name, shape, dt):
        self.uid = getattr(self, "uid", 0) + 1
        return self.cur.enter_context(self.nc.sbuf_tensor("%s_s%d" % (name, self.uid), shape, dt))

    def ps(self, name, shape, dt):
        self.uid = getattr(self, "uid", 0) + 1
        return self.cur.enter_context(self.nc.psum_tensor("%s_p%d" % (name, self.uid), shape, dt))

    @contextmanager
    def phase(self):
        prev = self.cur
        with ExitStack() as es:
            self.cur = es
            yield
            self.barrier()
        self.cur = prev

    def barrier(self):
        toks = [(p, self.cnt[p]) for p in self.ENGS if self.cnt[p] > 0]
        dtoks = [d[2] for d in self.dma_sems if d[2] is not None]
        for e in self.ENGS:
            for t in toks:
                if t[0] != e:
                    self._wait(e, t)
            for t in dtoks:
                self._wait(e, t)
        self.lastw = {}
        self.readers = {}

    def _key(self, x):
        if isinstance(x, (str, tuple)):
            return x
        return x.tensor.name if hasattr(x, "tensor") else x.name

    def _wait(self, eng, tok):
        if tok is None:
            return
        if tok[0] == "dma":
            _, i, val = tok
            if self.waited_dma.get((eng, i), 0) >= val:
                return
            self.waited_dma[(eng, i)] = val
            sem = self.dma_sems[i][0]
            self.q[eng].append(lambda e, sem=sem, val=val: e.wait_ge(sem, val))
            return
        p, c = tok
        if eng == "pe" and p == "pe" and not (PESYNC or getattr(self, "pesync", False)):
            return
        if self.waited[(eng, p)] >= c:
            return
        self.waited[(eng, p)] = c
        s = self.sem[p]
        self.q[eng].append(lambda e, s=s, c=c: e.wait_ge(s, c))

    def _deps(self, eng, rk, wk):
        for k in rk:
            self._wait(eng, self.lastw.get(k))
        for k in wk:
            self._wait(eng, self.lastw.get(k))
            for t in self.readers.get(k, ()):
                self._wait(eng, t)

    def _record(self, tok, rk, wk):
        for k in wk:
            self.lastw[k] = tok
            self.readers[k] = []
        for k in rk:
            self.readers.setdefault(k, []).append(tok)

    def op(self, eng, fn, reads=(), writes=()):
        rk = [self._key(r) for r in reads]
        wk = [self._key(w) for w in writes]
        self._deps(eng, rk, wk)
        self.cnt[eng] += 1
        tok = (eng, self.cnt[eng])
        s = self.sem[eng]
        self.q[eng].append(lambda e, fn=fn, s=s: fn(e).then_inc(s, 1))
        self._record(tok, rk, wk)
        return tok

    def V(self, fn, r=(), w=()):
        return self.op("dve", fn, r, w)

    def A(self, fn, r=(), w=()):
        return self.op("act", fn, r, w)

    def G(self, fn, r=(), w=()):
        return self.op("pool", fn, r, w)

    def T(self, fn, r=(), w=()):
        return self.op("pe", fn, r, w)

    def dma(self, eng, fn, reads=(), writes=()):
        rk = [self._key(r) for r in reads]
        wk = [self._key(w) for w in writes]
        self._deps(eng, rk, wk)
        base, n = self.pool_of[eng]
        i = base + self.ndma[eng] % n
        self.ndma[eng] += 1
        ent = self.dma_sems[i]
        if ent[2] is not None:
            self._wait(eng, ent[2])
        ent[1] += 16
        tok = ("dma", i, ent[1])
        ent[2] = tok
        sem = ent[0]
        self.q[eng].append(lambda e, fn=fn, sem=sem: fn(e).then_inc(sem, 16))
        self._record(tok, rk, wk)
        return tok

    def finish(self):
        nc = self.nc
        self.barrier()
        q = self.q
        with nc.Block() as block:
            @block.tensor
            def _(e):
                for f in q["pe"]:
                    f(e)

            @block.vector
            def _(e):
                for f in q["dve"]:
                    f(e)

            @block.scalar
            def _(e):
                for f in q["act"]:
                    f(e)

            @block.gpsimd
            def _(e):
                for f in q["pool"]:
                    f(e)

            @block.sync
            def _(e):
                for f in q["sp"]:
                    f(e)
        self.st.close()


def host_consts():
    f32 = np.float32
    half = 16
    inv = (10000.0 ** (-np.arange(half, dtype=f32) / half)).astype(f32)
    t = np.arange(S)
    rows = (t // 64).astype(f32)
    cols = (t % 64).astype(f32)
    ar = (rows[:, None] * inv[None, :]).astype(f32)
    ac = (cols[:, None] * inv[None, :]).astype(f32)
    attC = np.concatenate([np.cos(ar), np.cos(ar), np.cos(ac), np.cos(ac)], axis=1).astype(f32)
    attS = np.concatenate([-np.sin(ar), np.sin(ar), -np.sin(ac), np.sin(ac)], axis=1).astype(f32)
    half = 32
    inv = (10000.0 ** (-np.arange(half, dtype=f32) / half)).astype(f32)
    pos = np.arange(S + C).astype(f32)
    a = (pos[:, None] * inv[None, :]).astype(f32)
    retC = np.concatenate([np.cos(a), np.cos(a)], axis=1).astype(f32)
    retS = np.concatenate([-np.sin(a), np.sin(a)], axis=1).astype(f32)
    j = np.arange(128)[:, None]
    i = np.arange(128)[None, :]
    masks = np.stack([(j <= i), (j >= i), (j > i)], axis=0).astype(f32)
    ident = np.eye(128, dtype=f32)
    p = np.arange(128, dtype=f32)
    dcoef = np.stack([p + 1, -(p + 1), -p, p], axis=1).astype(f32)
    return {"attC": attC, "attS": attS, "retC": retC, "retS": retS, "masks": masks,
            "ident": ident, "dcoef": dcoef}


WNAMES = ["w_mod", "b_mod", "g_mix", "g_ffn", "w_in", "q_norm_g", "k_norm_g", "att_sink",
          "ret_decay_logit", "ret_norm_g", "conv_w", "conv_b", "conv_norm_g", "conv_norm_b",
          "w_out", "w_router", "w_gate", "w_up", "w_down"]
WSHAPES = {
    "w_mod": [L, D, 6 * D], "b_mod": [L, 6 * D], "g_mix": [L, D], "g_ffn": [L, D], "w_in": [L, D, 2304],
    "q_norm_g": [L, 64], "k_norm_g": [L, 64], "att_sink": [L, 8], "ret_decay_logit": [L, 8],
    "ret_norm_g": [L, 256], "conv_w": [L, 31, 256], "conv_b": [L, 256], "conv_norm_g": [L, 256],
    "conv_norm_b": [L, 256], "w_out": [L, D, D], "w_router": [L, D, E], "w_gate": [L, E, D, DFF],
    "w_up": [L, E, D, DFF], "w_down": [L, E, DFF, D],
}
CSHAPES = {"attC": [S, 64], "attS": [S, 64], "retC": [S + C, 64], "retS": [S + C, 64],
           "masks": [3, 128, 128], "ident": [128, 128], "dcoef": [128, 4]}

PESYNC = os.environ.get("MK_PESYNC", "") == "1"
RETSKIP = os.environ.get("MK_RETSKIP", "")
STOP = os.environ.get("MK_STOP", "")
DEBUG = os.environ.get("MK_DEBUG", "") == "1"


def build_program():
    nc = bass.Bass("TRN2", target_bir_lowering=False)
    A = {}
    A["x"] = nc.dram_tensor("x", [NS * S, D], F32, kind="ExternalInput").ap()
    A["c"] = nc.dram_tensor("c", [NS, D], F32, kind="ExternalInput").ap()
    A["ctx"] = nc.dram_tensor("ctx", [NS * C, D], F32, kind="ExternalInput").ap()
    A["c_ctx"] = nc.dram_tensor("c_ctx", [1, D], F32, kind="ExternalInput").ap()
    for n in WNAMES:
        A[n] = nc.dram_tensor(n, WSHAPES[n], F32, kind="ExternalInput").ap()
    for n, shp in CSHAPES.items():
        A[n] = nc.dram_tensor(n, shp, F32, kind="ExternalInput").ap()
    A["y"] = nc.dram_tensor("y", [NS * S, D], F32, kind="ExternalOutput").ap()
    dk = "ExternalOutput" if DEBUG else "Internal"
    A["mod_d"] = nc.dram_tensor("mod_d", [L, 4, 6 * D], F32, kind=dk).ap()
    A["u_d"] = nc.dram_tensor("u_d", [NS, S + C, UC], F32, kind=dk).ap()
    A["cv_d"] = nc.dram_tensor("cv_d", [NS, 512, S + C], F32, kind=dk).ap()
    A["mixin_d"] = nc.dram_tensor("mixin_d", [NS, S + C, 768], BF16, kind=dk).ap()
    A["convT_d"] = nc.dram_tensor("convT_d", [NS, 256, S + C], BF16, kind=dk).ap()
    A["res_d"] = nc.dram_tensor("res_d", [NROW, D], F32, kind=dk).ap()
    A["hf_d"] = nc.dram_tensor("hf_d", [NROW, D], F32, kind=dk).ap()
    A["aff_d"] = nc.dram_tensor("aff_d", [48, S], F32, kind=dk).ap()
    A["hfb_d"] = nc.dram_tensor("hfb_d", [NROW, D], BF16, kind=dk).ap()
    A["affc_d"] = nc.dram_tensor("affc_d", [48, C], F32, kind=dk).ap()

    k = KB(nc)
    P = {}
    P["ident"] = k.sb("ident", [128, 128], F32)
    P["identb"] = k.sb("identb", [128, 128], BF16)
    P["masks"] = k.sb("masks", [128, 3, 128], F32)
    P["modT"] = [k.sb("modT%d" % l, [128, 48, 4], F32) for l in range(L)]
    P["gT"] = k.sb("gT", [128, 8, 4], F32)
    P["gsA"] = [k.sb("gsA%d" % l, [128, 8, 4], F32) for l in range(L)]
    P["epsb"] = k.sb("epsb", [128, 1], F32)
    k.V(lambda e: e.memset(P["epsb"][:], EPS), [], [P["epsb"]])
    k.dma("sp", lambda e: e.dma_start(out=P["ident"][:], in_=A["ident"]), writes=[P["ident"]])
    k.dma("sp", lambda e: e.dma_start(out=P["masks"][:], in_=A["masks"].rearrange("m j i -> j m i")),
          writes=[P["masks"]])
    k.V(lambda e: e.tensor_copy(out=P["identb"][:], in_=P["ident"][:]), [P["ident"]], [P["identb"]])

    phase_mod(k, A, P)
    if STOP == "mod":
        k.finish()
        return nc
    for l in range(L):
        last = l == L - 1
        src_lat = A["x"] if l == 0 else A["res_d"][0:NLAT]
        src_ctx = A["ctx"] if l == 0 else A["res_d"][NLAT:NROW]
        dst_lat = A["y"] if last else A["res_d"][0:NLAT]
        phase_proj(k, A, P, l, src_lat, src_ctx)
        if STOP == "proj%d" % l:
            break
        phase_att(k, A, P, l, last)
        if STOP == "att%d" % l:
            break
        phase_ret(k, A, P, l, last)
        if STOP == "ret%d" % l:
            break
        phase_conv(k, A, P, l, last)
        if STOP == "conv%d" % l:
            break
        phase_out(k, A, P, l, last, src_lat, src_ctx, dst_lat)
        if STOP == "out%d" % l:
            break
        phase_moe(k, A, P, l, last, dst_lat)
        if STOP == "moe%d" % l:
            break
    k.finish()
    return nc


def phase_mod(k, A, P):
    with k.phase():
        cc = k.sb("cc", [4, D], F32)
        cs = k.sb("cs", [4, D], F32)
        csT = k.sb("csT", [128, 8, 4], F32R)
        gg = k.sb("gg", [4, D], F32)
        pt = k.ps("pt", [128, 8, 4], F32)
        pm = [k.ps("pm%d" % i, [4, 512], F32) for i in range(2)]
        ptm = k.ps("ptm", [128, 48, 4], F32)
        wm = [k.sb("wm%d" % i, [128, 8, 512], F32R) for i in range(2)]
        bm = k.sb("bm", [4, 6 * D], F32)
        modrow = k.sb("modrow", [4, 6 * D], F32)
        ident = P["ident"]
        k.V(lambda e: e.memset(cc[:], 0.0), [], [cc])
        k.dma("sp", lambda e: e.dma_start(out=cc[0:NS, :], in_=A["c"]), writes=[cc])
        k.dma("sp", lambda e: e.dma_start(out=cc[NS:NS + 1, :], in_=A["c_ctx"]), writes=[cc])
        k.A(lambda e: e.activation(out=cs[:], in_=cc[:], func=AF.Silu), [cc], [cs])
        for kc in range(8):
            k.T(lambda e, kc=kc: e.transpose(pt[:, kc, :], cs[0:4, kc * 128:(kc + 1) * 128], ident[0:4, 0:4]),
                [cs, ident], [pt])
        k.V(lambda e: e.tensor_copy(out=csT[:], in_=pt[:]), [pt], [csT])
        k.dma("sp", lambda e: e.dma_start(out=gg[0:2, :], in_=A["g_mix"]), writes=[gg])
        k.dma("sp", lambda e: e.dma_start(out=gg[2:4, :], in_=A["g_ffn"]), writes=[gg])
        for kc in range(8):
            k.T(lambda e, kc=kc: e.transpose(pt[:, kc, :], gg[0:4, kc * 128:(kc + 1) * 128], ident[0:4, 0:4]),
                [gg, ident], [pt])
        k.V(lambda e: e.tensor_copy(out=P["gT"][:], in_=pt[:]), [pt], [P["gT"]])
        for l in range(L):
            k.dma("sp", lambda e, l=l: e.dma_start(out=bm[:], in_=A["b_mod"][l, :].partition_broadcast(4)),
                  writes=[bm])
            wv = A["w_mod"][l].rearrange("(kc p) f -> p kc f", p=128)
            for j in range(12):
                w = wm[j % 2]
                k.dma("pool", lambda e, w=w, j=j, wv=wv: e.dma_start(out=w[:], in_=wv[:, :, j * 512:(j + 1) * 512]),
                      writes=[w])
                p = pm[j % 2]
                for kc in range(8):
                    k.T(lambda e, p=p, w=w, kc=kc: e.matmul(p[:], lhsT=csT[:, kc, :], rhs=w[:, kc, :],
                                                          start=(kc == 0), stop=(kc == 7)), [csT, w], [p])
                k.V(lambda e, p=p, j=j: e.tensor_tensor(out=modrow[:, j * 512:(j + 1) * 512], in0=p[:],
                                                        in1=bm[:, j * 512:(j + 1) * 512], op=ALU.add),
                    [p, bm], [modrow])
            k.dma("sp", lambda e, l=l: e.dma_start(out=A["mod_d"][l], in_=modrow[:]), reads=[modrow],
                  writes=["mod_d"])
            for j in range(48):
                k.T(lambda e, j=j: e.transpose(ptm[:, j, :], modrow[0:4, j * 128:(j + 1) * 128], ident[0:4, 0:4]),
                    [modrow, ident], [ptm])
            mT = P["modT"][l]
            k.V(lambda e, mT=mT: e.tensor_copy(out=mT[:], in_=ptm[:]), [ptm], [mT])
            gs = P["gsA"][l]
            k.V(lambda e, gs=gs, mT=mT: e.tensor_scalar(out=gs[:], in0=mT[:, 8:16, :], scalar1=1.0, scalar2=None,
                                                        op0=ALU.add), [mT], [gs])
            k.V(lambda e, gs=gs, l=l: e.tensor_tensor(out=gs[:], in0=gs[:],
                                                      in1=P["gT"][:, :, l:l + 1].to_broadcast([128, 8, 4]),
                                                      op=ALU.mult), [gs, P["gT"]], [gs])


def rms_rstd(k, xt, junk, ss, rstd, n):
    k.A(lambda e: e.activation(out=junk, in_=xt, func=AF.Square, accum_out=ss[:, 0:1]), [xt], [junk, ss])
    k.V(lambda e: e.tensor_scalar(out=ss[:, 0:1], in0=ss[:, 0:1], scalar1=1.0 / n, scalar2=EPS, op0=ALU.mult,
                                  op1=ALU.add), [ss], [ss])
    k.A(lambda e: e.sqrt(out=ss[:, 0:1], in_=ss[:, 0:1]), [ss], [ss])
    k.V(lambda e: e.reciprocal(out=rstd[:, 0:1], in_=ss[:, 0:1]), [ss], [rstd])


def phase_proj(k, A, P, l, src_lat, src_ctx):
    with k.phase():
        win = k.sb("win", [128, 8, 2304], BF16)
        wv = A["w_in"][l].rearrange("(kc p) f -> p kc f", p=128)
        for j in range(0, 2304, 384):
            k.dma("pool", lambda e, j=j: e.dma_start(out=win[:, :, j:j + 384], in_=wv[:, :, j:j + 384]),
                  writes=[("win", j)])
        winkeys = [("win", j) for j in range(0, 2304, 384)]
        NB = 8
        xts = [k.sb("xt%d" % i, [128, D], F32) for i in range(NB)]
        xn = [k.sb("xn%d" % i, [128, D], BF16) for i in range(NB)]
        junk = k.sb("junk", [128, D], F32)
        ss = [k.sb("ss%d" % i, [128, 1], F32) for i in range(NB)]
        rstd = [k.sb("rstd%d" % i, [128, 1], F32) for i in range(NB)]
        hT = [k.sb("hT%d" % i, [128, 8, 512], BF16) for i in range(2)]
        ptr = [k.ps("ptr%d" % i, [128, D], BF16) for i in range(2)]
        pu = [k.ps("pu%d" % i, [128, 512], F32) for i in range(3)]
        pc = [k.ps("pc%d" % i, [128, 512], F32) for i in range(2)]
        usb = [k.sb("usb%d" % i, [128, UC], F32) for i in range(2)]
        cvs = [k.sb("cvs%d" % i, [128, 4, 512], F32) for i in range(2)]
        identb = P["identb"]
        gs = P["gsA"][l]
        mT = P["modT"][l]
        groups = []
        for s in range(NS):
            groups.append((s, 0, 2, src_ctx[s * C:(s + 1) * C], NS))
            for g in range(4):
                groups.append((s, C + g * 512, 4, src_lat[s * S + g * 512: s * S + (g + 1) * 512], s))
        cnt = {"t": 0}

        def stats(gi):
            s, tok0, ntl, src, r = groups[gi]
            for t in range(ntl):
                i = (gi % 2) * 4 + t
                xt, xnn, sst, rs = xts[i], xn[i], ss[i], rstd[i]
                k.dma("sp", lambda e, xt=xt, src=src, t=t: e.dma_start(out=xt[:], in_=src[t * 128:(t + 1) * 128, :]),
                      writes=[xt])
                k.A(lambda e, xt=xt, sst=sst: e.activation(out=junk[:], in_=xt[:], func=AF.Square, accum_out=sst[:, 0:1]),
                    [xt], [junk, sst])
                k.A(lambda e, sst=sst: e.activation(out=sst[:, 0:1], in_=sst[:, 0:1], func=AF.Sqrt, scale=1.0 / D, bias=EPSB[:, 0:1]),
                    [sst, EPSB], [sst])
                k.V(lambda e, sst=sst, rs=rs: e.reciprocal(out=rs[:, 0:1], in_=sst[:, 0:1]), [sst], [rs])
                k.V(lambda e, xnn=xnn, xt=xt, rs=rs: e.tensor_scalar(out=xnn[:], in0=xt[:], scalar1=rs[:, 0:1],
                                                                   scalar2=None, op0=ALU.mult), [xt, rs], [xnn])

        def trans(gi):
            s, tok0, ntl, src, r = groups[gi]
            h = hT[gi % 2]
            for t in range(ntl):
                i = (gi % 2) * 4 + t
                xnn = xn[i]
                pt = ptr[cnt["t"] % 2]
                cnt["t"] += 1
                for kc in range(8):
                    k.T(lambda e, pt=pt, xnn=xnn, kc=kc: e.transpose(pt[:, kc * 128:(kc + 1) * 128],
                                                                   xnn[:, kc * 128:(kc + 1) * 128], identb[:]),
                        [xnn, identb], [pt])
                for kc in range(8):
                    if kc % 2 == 0:
                        k.A(lambda e, h=h, pt=pt, kc=kc, t=t, r=r: e.activation(
                            out=h[:, kc, t * 128:(t + 1) * 128], in_=pt[:, kc * 128:(kc + 1) * 128],
                            func=AF.Identity, scale=gs[:, kc, r:r + 1], bias=mT[:, kc, r:r + 1]),
                            [pt, gs, mT], [h])
                    else:
                        k.V(lambda e, h=h, pt=pt, kc=kc, t=t, r=r: e.tensor_scalar(
                            out=h[:, kc, t * 128:(t + 1) * 128], in0=pt[:, kc * 128:(kc + 1) * 128],
                            scalar1=gs[:, kc, r:r + 1], scalar2=mT[:, kc, r:r + 1], op0=ALU.mult, op1=ALU.add),
                            [pt, gs, mT], [h])

        def mm(gi):
            s, tok0, ntl, src, r = groups[gi]
            h = hT[gi % 2]
            ntok = ntl * 128
            ei = 0
            for t in range(ntl):
                ub = usb[t % 2]
                for ci, (c0, w) in enumerate([(0, 512), (512, 512), (1024, 512), (1536, 256)]):
                    p = pu[ei % 3]
                    for kc in range(8):
                        k.T(lambda e, p=p, h=h, kc=kc, t=t, c0=c0, w=w: e.matmul(
                            p[:, 0:w], lhsT=h[:, kc, t * 128:(t + 1) * 128], rhs=win[:, kc, c0:c0 + w],
                            start=(kc == 0), stop=(kc == 7)), [h] + winkeys, [p])
                    if ei % 2 == 0:
                        k.A(lambda e, ub=ub, p=p, c0=c0, w=w: e.copy(out=ub[:, c0:c0 + w], in_=p[:, 0:w]), [p], [ub])
                    else:
                        k.V(lambda e, ub=ub, p=p, c0=c0, w=w: e.tensor_copy(out=ub[:, c0:c0 + w], in_=p[:, 0:w]),
                            [p], [ub])
                    ei += 1
                k.dma("sp", lambda e, ub=ub, s=s, tok0=tok0, t=t: e.dma_start(
                    out=A["u_d"][s, tok0 + t * 128: tok0 + (t + 1) * 128, :], in_=ub[:]), reads=[ub], writes=["u_d"])
            cv = cvs[gi % 2]
            for j in range(4):
                p = pc[j % 2]
                for kc in range(8):
                    k.T(lambda e, p=p, h=h, kc=kc, j=j, ntok=ntok: e.matmul(
                        p[:, 0:ntok], lhsT=win[:, kc, UC + j * 128: UC + (j + 1) * 128], rhs=h[:, kc, 0:ntok],
                        start=(kc == 0), stop=(kc == 7)), [h] + winkeys, [p])
                if j % 2 == 0:
                    k.A(lambda e, cv=cv, p=p, j=j, ntok=ntok: e.copy(out=cv[:, j, 0:ntok], in_=p[:, 0:ntok]), [p], [cv])
                else:
                    k.V(lambda e, cv=cv, p=p, j=j, ntok=ntok: e.tensor_copy(out=cv[:, j, 0:ntok], in_=p[:, 0:ntok]),
                        [p], [cv])
            k.dma("act", lambda e, cv=cv, s=s, tok0=tok0, ntok=ntok: e.dma_start(
                out=A["cv_d"][s, :, tok0:tok0 + ntok].rearrange("(j p) t -> p j t", p=128), in_=cv[:, :, 0:ntok]),
                reads=[cv], writes=["cv_d"])

        EPSB = P["epsb"]
        ng = len(groups)
        stats(0)
        trans(0)
        for gi in range(ng):
            if gi + 1 < ng:
                stats(gi + 1)
            mm(gi)
            if gi + 1 < ng:
                trans(gi + 1)


def phase_att(k, A, P, l, last):
    with k.phase():
        ident = P["ident"]
        identb = P["identb"]
        attC = k.sb("attC", [128, 16, 64], F32)
        attS = k.sb("attS", [128, 16, 64], F32)
        k.dma("sp", lambda e: e.dma_start(out=attC[:], in_=A["attC"].rearrange("(t p) d -> p t d", p=128)), writes=[attC])
        k.dma("sp", lambda e: e.dma_start(out=attS[:], in_=A["attS"].rearrange("(t p) d -> p t d", p=128)), writes=[attS])
        gqk = k.sb("gqk", [128, 10, 64], F32)
        k.dma("sp", lambda e: e.dma_start(out=gqk[:, 0, :], in_=A["q_norm_g"][l, :].partition_broadcast(128)), writes=[gqk])
        k.dma("sp", lambda e: e.dma_start(out=gqk[:, 8, :], in_=A["k_norm_g"][l, :].partition_broadcast(128)), writes=[gqk])
        k.V(lambda e: e.tensor_scalar(out=gqk[:, 0, :], in0=gqk[:, 0, :], scalar1=0.125, scalar2=None, op0=ALU.mult),
            [gqk], [gqk])
        for h in range(1, 8):
            k.V(lambda e, h=h: e.tensor_copy(out=gqk[:, h, :], in_=gqk[:, 0, :]), [gqk], [gqk])
        k.V(lambda e: e.tensor_copy(out=gqk[:, 9, :], in_=gqk[:, 8, :]), [gqk], [gqk])
        sink = k.sb("sink", [128, 8], F32)
        k.dma("sp", lambda e: e.dma_start(out=sink[:], in_=A["att_sink"][l, :].partition_broadcast(128)), writes=[sink])
        k.A(lambda e: e.activation(out=sink[:], in_=sink[:], func=AF.Exp), [sink], [sink])
        maskb = k.sb("maskb", [128, 3, 128], BF16)
        k.V(lambda e: e.tensor_copy(out=maskb[:], in_=P["masks"][:]), [P["masks"]], [maskb])

        qT = k.sb("qT", [64, 8, NT * 128], BF16)
        kT = k.sb("kT", [64, 2, NT * 128], BF16)
        vx = k.sb("vx", [128, NT, 2, 65], BF16)
        k.V(lambda e: e.memset(vx[:], 1.0), [], [vx])
        uq = [k.sb("uq%d" % i, [128, 768], F32) for i in range(2)]
        sq = k.sb("sq", [128, 640], F32)
        ssq = k.sb("ssq", [128, 10], F32)
        qn = k.sb("qn", [128, 640], F32)
        t1 = k.sb("t1", [128, 640], F32)
        t2 = k.sb("t2", [128, 640], F32)
        qb = [k.sb("qb%d" % i, [128, 640], BF16) for i in range(2)]
        pq = [k.ps("pq%d" % i, [64, 8, 128], BF16) for i in range(2)]
        pkk = k.ps("pkk", [64, 2, 128], BF16)
        psc = [k.ps("psc%d" % i, [128, 512], F32) for i in range(2)]
        po = [k.ps("po%d" % i, [128, 4, 65], F32) for i in range(2)]
        Pm = [k.sb("Pm%d" % i, [128, 5, 512], BF16) for i in range(2)]
        den = k.sb("den", [128, 8], F32)
        ao = [k.sb("ao%d" % i, [128, 512], BF16) for i in range(2)]

        for s in range(NS):
            for t in range(NT):
                u = uq[t % 2]
                k.dma("sp", lambda e, u=u, s=s, t=t: e.dma_start(out=u[:], in_=A["u_d"][s, t * 128:(t + 1) * 128, 0:768]),
                      reads=["u_d"], writes=[u])
                k.G(lambda e, u=u: e.tensor_tensor(out=sq[:], in0=u[:, 0:640], in1=u[:, 0:640], op=ALU.mult), [u], [sq])
                k.V(lambda e: e.tensor_reduce(out=ssq[:], in_=sq[:].rearrange("p (h d) -> p h d", d=64), axis=AX.X,
                                              op=ALU.add), [sq], [ssq])
                k.V(lambda e: e.tensor_scalar(out=ssq[:], in0=ssq[:], scalar1=1.0 / 64, scalar2=EPS, op0=ALU.mult,
                                              op1=ALU.add), [ssq], [ssq])
                k.A(lambda e: e.sqrt(out=ssq[:], in_=ssq[:]), [ssq], [ssq])
                k.V(lambda e: e.reciprocal(out=ssq[:], in_=ssq[:]), [ssq], [ssq])
                k.V(lambda e, u=u: e.tensor_tensor(out=qn[:].rearrange("p (h d) -> p h d", d=64),
                                                   in0=u[:, 0:640].rearrange("p (h d) -> p h d", d=64),
                                                   in1=ssq[:].unsqueeze(2).to_broadcast([128, 10, 64]), op=ALU.mult),
                    [u, ssq], [qn])
                k.G(lambda e: e.tensor_tensor(out=qn[:], in0=qn[:], in1=gqk[:].rearrange("p h d -> p (h d)"), op=ALU.mult),
                    [qn, gqk], [qn])
                q_b = qb[t % 2]
                if t >= 2:
                    tl = t - 2
                    qv = qn[:].rearrange("p (h a x d) -> p h a x d", h=10, a=2, x=2, d=16)
                    t1v = t1[:].rearrange("p (h a x d) -> p h a x d", h=10, a=2, x=2, d=16)
                    t2v = t2[:].rearrange("p (h a x d) -> p h a x d", h=10, a=2, x=2, d=16)
                    Cv = attC[:, tl, :].rearrange("p (a x d) -> p a x d", a=2, x=2, d=16)
                    Sv = attS[:, tl, :].rearrange("p (a x d) -> p a x d", a=2, x=2, d=16)
                    for a in range(2):
                        for x in range(2):
                            k.V(lambda e, a=a, x=x, qv=qv, t1v=t1v, Cv=Cv: e.tensor_tensor(
                                out=t1v[:, :, a, x, :], in0=qv[:, :, a, x, :],
                                in1=Cv[:, a, x, :].unsqueeze(1).to_broadcast([128, 10, 16]), op=ALU.mult),
                                [qn, attC], [t1])
                            k.G(lambda e, a=a, x=x, qv=qv, t2v=t2v, Sv=Sv: e.tensor_tensor(
                                out=t2v[:, :, a, x, :], in0=qv[:, :, a, 1 - x, :],
                                in1=Sv[:, a, x, :].unsqueeze(1).to_broadcast([128, 10, 16]), op=ALU.mult),
                                [qn, attS], [t2])
                    k.V(lambda e, q_b=q_b: e.tensor_tensor(out=q_b[:], in0=t1[:], in1=t2[:], op=ALU.add), [t1, t2], [q_b])
                else:
                    k.V(lambda e, q_b=q_b: e.tensor_copy(out=q_b[:], in_=qn[:]), [qn], [q_b])
                k.G(lambda e, u=u, t=t: e.tensor_copy(out=vx[:, t, :, 0:64],
                                                      in_=u[:, 640:768].rearrange("p (h d) -> p h d", d=64)),
                    [u], [vx])
                p = pq[t % 2]
                for h in range(8):
                    k.T(lambda e, p=p, q_b=q_b, h=h: e.transpose(p[:, h, :], q_b[:, h * 64:(h + 1) * 64], identb[:]),
                        [q_b, identb], [p])
                for h in range(2):
                    k.T(lambda e, q_b=q_b, h=h: e.transpose(pkk[:, h, :], q_b[:, (8 + h) * 64:(9 + h) * 64], identb[:]),
                        [q_b, identb], [pkk])
                k.A(lambda e, p=p, t=t: e.copy(out=qT[:, :, t * 128:(t + 1) * 128], in_=p[:]), [p], [qT])
                k.V(lambda e, t=t: e.tensor_copy(out=kT[:, :, t * 128:(t + 1) * 128], in_=pkk[:]), [pkk], [kT])
            qblocks = list(range(2, NT)) + ([] if last else [0, 1])
            bi = 0
            for t in qblocks:
                if t >= 2:
                    kbl = []
                    if t - 1 >= 2:
                        kbl.append((t - 1, 1))
                    kbl.append((t, None))
                    if t + 1 < NT:
                        kbl.append((t + 1, 0))
                    kbl += [(0, None), (1, None)]
                else:
                    kbl = [(0, None), (1, None)]
                a_o = ao[bi % 2]
                for kh in range(2):
                    Pt = Pm[(bi * 2 + kh) % 2]
                    for bj, (kb_, mk) in enumerate(kbl):
                        ps = psc[bj % 2]
                        k.T(lambda e, ps=ps, kb_=kb_, kh=kh, t=t: e.matmul(
                            ps[:], lhsT=kT[:, kh, kb_ * 128:(kb_ + 1) * 128],
                            rhs=qT[:, kh * 4:(kh + 1) * 4, t * 128:(t + 1) * 128], start=True, stop=True),
                            [kT, qT], [ps])
                        k.A(lambda e, ps=ps, Pt=Pt, bj=bj: e.activation(out=Pt[:, bj, :], in_=ps[:], func=AF.Exp),
                            [ps], [Pt])
                        if mk is not None:
                            k.G(lambda e, Pt=Pt, bj=bj, mk=mk: e.tensor_tensor(
                                out=Pt[:, bj, :].rearrange("p (g q) -> p g q", g=4),
                                in0=Pt[:, bj, :].rearrange("p (g q) -> p g q", g=4),
                                in1=maskb[:, mk, :].unsqueeze(1).to_broadcast([128, 4, 128]), op=ALU.mult),
                                [Pt, maskb], [Pt])
                    pso = po[kh]
                    nb = len(kbl)
                    for g in range(4):
                        for bj, (kb_, mk) in enumerate(kbl):
                            k.T(lambda e, pso=pso, Pt=Pt, g=g, bj=bj, kb_=kb_, kh=kh, nb=nb: e.matmul(
                                pso[:, g, :], lhsT=Pt[:, bj, g * 128:(g + 1) * 128], rhs=vx[:, kb_, kh, :],
                                start=(bj == 0), stop=(bj == nb - 1)), [Pt, vx], [pso])
                    k.V(lambda e, pso=pso, kh=kh: e.tensor_tensor(out=den[:, kh * 4:(kh + 1) * 4], in0=pso[:, :, 64],
                                                                 in1=sink[:, kh * 4:(kh + 1) * 4], op=ALU.add),
                        [pso, sink], [den])
                    k.V(lambda e, kh=kh: e.reciprocal(out=den[:, kh * 4:(kh + 1) * 4], in_=den[:, kh * 4:(kh + 1) * 4]),
                        [den], [den])
                    k.V(lambda e, pso=pso, kh=kh, a_o=a_o: e.tensor_tensor(
                        out=a_o[:, kh * 256:(kh + 1) * 256].rearrange("p (g d) -> p g d", g=4),
                        in0=pso[:, :, 0:64], in1=den[:, kh * 4:(kh + 1) * 4].unsqueeze(2).to_broadcast([128, 4, 64]),
                        op=ALU.mult), [pso, den], [a_o])
                k.dma("act", lambda e, a_o=a_o, s=s, t=t: e.dma_start(
                    out=A["mixin_d"][s, t * 128:(t + 1) * 128, 0:512], in_=a_o[:]), reads=[a_o], writes=["mixin_d"])
                bi += 1


def phase_ret(k, A, P, l, last):
    k.pesync = True
    with k.phase():
        identb = P["identb"]
        retC = k.sb("retC", [128, NT, 64], F32)
        retS = k.sb("retS", [128, NT, 64], F32)
        k.dma("sp", lambda e: e.dma_start(out=retC[:], in_=A["retC"].rearrange("(t p) d -> p t d", p=128)), writes=[retC])
        k.dma("sp", lambda e: e.dma_start(out=retS[:], in_=A["retS"].rearrange("(t p) d -> p t d", p=128)), writes=[retS])
        lg = k.sb("lg", [128, 8], F32)
        k.dma("sp", lambda e: e.dma_start(out=lg[:], in_=A["ret_decay_logit"][l, :].partition_broadcast(128)), writes=[lg])
        k.A(lambda e: e.activation(out=lg[:], in_=lg[:], func=AF.Exp, scale=-1.0), [lg], [lg])
        k.A(lambda e: e.activation(out=lg[:], in_=lg[:], func=AF.Ln, bias=1.0), [lg], [lg])
        k.V(lambda e: e.tensor_scalar(out=lg[:], in0=lg[:], scalar1=-1.0, scalar2=None, op0=ALU.mult), [lg], [lg])
        dco = k.sb("dco", [128, 4], F32)
        k.dma("sp", lambda e: e.dma_start(out=dco[:], in_=A["dcoef"]), writes=[dco])
        DT = k.sb("DT", [128, 2, 2, 4], F32)
        for d_ in range(2):
            for qk in range(2):
                k.A(lambda e, d_=d_, qk=qk: e.activation(out=DT[:, d_, qk, :], in_=lg[:, d_ * 4:(d_ + 1) * 4], func=AF.Exp,
                                                         scale=dco[:, d_ * 2 + qk: d_ * 2 + qk + 1]), [lg, dco], [DT])
            k.V(lambda e, d_=d_: e.tensor_scalar(out=DT[:, d_, 1, :], in0=DT[:, d_, 1, :], scalar1=0.125, scalar2=None,
                                                 op0=ALU.mult), [DT], [DT])
        g128 = k.sb("g128", [128, 2, 2], F32)
        for d_ in range(2):
            for pr in range(2):
                for hh in range(2):
                    h = 2 * pr + hh
                    k.A(lambda e, d_=d_, pr=pr, hh=hh, h=h: e.activation(
                        out=g128[hh * 64:(hh + 1) * 64, d_, pr:pr + 1], in_=lg[hh * 64:(hh + 1) * 64, d_ * 4 + h: d_ * 4 + h + 1],
                        func=AF.Exp, scale=128.0), [lg], [g128])
        gn = k.sb("gn", [128, 256], F32)
        k.dma("sp", lambda e: e.dma_start(out=gn[:], in_=A["ret_norm_g"][l, :].partition_broadcast(128)), writes=[gn])
        maskb = k.sb("maskb", [128, 3, 128], BF16)
        k.V(lambda e: e.tensor_copy(out=maskb[:], in_=P["masks"][:]), [P["masks"]], [maskb])

        RT = k.sb("RT", [128, 2, 2, 2, NT * 128], BF16)
        KD = k.sb("KD", [128, NT, 2, 256], BF16)
        Vb = k.sb("Vb", [128, NT, 256], BF16)
        SG = k.sb("SG", [128, NT, 256], F32)
        OA = k.sb("OA", [128, NT, 256], F32)
        ur = [k.sb("ur%d" % i, [128, 1024], F32) for i in range(2)]
        t1 = k.sb("t1", [128, 512], F32)
        t2 = k.sb("t2", [128, 512], F32)
        rot = k.sb("rot", [128, 512], F32)
        vr = [k.sb("vr%d" % i, [128, 2, 2, 256], BF16) for i in range(2)]
        ptr = [k.ps("ptr%d" % i, [128, 8, 128], BF16) for i in range(2)]
        pst = [k.ps("pst%d" % i, [128, 4, 128], F32) for i in range(2)]
        pso = [k.ps("pso%d" % i, [128, 4, 64], F32) for i in range(2)]
        pkv = [k.ps("pkv%d" % i, [128, 2, 128], F32) for i in range(2)]
        STm = [k.sb("STm%d" % i, [128, 4, 128], BF16) for i in range(2)]
        Sst = [k.sb("Sst%d" % i, [128, 2, 128], F32) for i in range(2)]
        Sb = [k.sb("Sb%d" % i, [128, 2, 128], BF16) for i in range(2)]
        tmp = k.sb("tmp", [128, 2, 128], F32)
        mean = k.sb("mean", [128, 4], F32)
        cen = k.sb("cen", [128, 256], F32)
        sq = k.sb("sq", [128, 256], F32)
        var = k.sb("var", [128, 4], F32)
        ro = [k.sb("ro%d" % i, [128, 256], BF16) for i in range(2)]
        rof = k.sb("rof", [128, 256], F32)

        for s in range(NS):
            for t in range(NT):
                u = ur[t % 2]
                k.dma("sp", lambda e, u=u, s=s, t=t: e.dma_start(out=u[:], in_=A["u_d"][s, t * 128:(t + 1) * 128, 768:1792]),
                      reads=["u_d"], writes=[u])
                qk_v = u[:, 0:512].rearrange("p (h x d) -> p h x d", h=8, x=2, d=32)
                t1v = t1[:].rearrange("p (h x d) -> p h x d", h=8, x=2, d=32)
                t2v = t2[:].rearrange("p (h x d) -> p h x d", h=8, x=2, d=32)
                Cv = retC[:, t, :].rearrange("p (x d) -> p x d", x=2, d=32)
                Sv = retS[:, t, :].rearrange("p (x d) -> p x d", x=2, d=32)
                for x in range(2):
                    k.V(lambda e, x=x, qk_v=qk_v, t1v=t1v, Cv=Cv: e.tensor_tensor(
                        out=t1v[:, :, x, :], in0=qk_v[:, :, x, :], in1=Cv[:, x, :].unsqueeze(1).to_broadcast([128, 8, 32]),
                        op=ALU.mult), [u, retC], [t1])
                    k.G(lambda e, x=x, qk_v=qk_v, t2v=t2v, Sv=Sv: e.tensor_tensor(
                        out=t2v[:, :, x, :], in0=qk_v[:, :, 1 - x, :], in1=Sv[:, x, :].unsqueeze(1).to_broadcast([128, 8, 32]),
                        op=ALU.mult), [u, retS], [t2])
                k.V(lambda e: e.tensor_tensor(out=rot[:], in0=t1[:], in1=t2[:], op=ALU.add), [t1, t2], [rot])
                v_r = vr[t % 2]
                for d_ in range(2):
                    eng = k.V if d_ == 0 else k.G
                    eng(lambda e, d_=d_, v_r=v_r: e.tensor_tensor(
                        out=v_r[:, d_, :, :].rearrange("p q (h d) -> p (q h) d", d=64),
                        in0=rot[:].rearrange("p (qh d) -> p qh d", d=64),
                        in1=DT[:, d_, :, :].rearrange("p q h -> p (q h)").unsqueeze(2).to_broadcast([128, 8, 64]),
                        op=ALU.mult), [rot, DT], [v_r])
                    k.G(lambda e, d_=d_, v_r=v_r, t=t: e.tensor_copy(out=KD[:, t, d_, :], in_=v_r[:, d_, 1, :]), [v_r], [KD])
                k.V(lambda e, u=u, t=t: e.tensor_copy(out=Vb[:, t, :], in_=u[:, 512:768]), [u], [Vb])
                k.A(lambda e, u=u, t=t: e.activation(out=SG[:, t, :], in_=u[:, 768:1024], func=AF.Silu), [u], [SG])
                p = ptr[t % 2]
                for d_ in range(2):
                    for qk in range(2):
                        for pr in range(2):
                            k.T(lambda e, p=p, v_r=v_r, d_=d_, qk=qk, pr=pr: e.transpose(
                                p[:, d_ * 4 + qk * 2 + pr, :], v_r[:, d_, qk, pr * 128:(pr + 1) * 128], identb[:]),
                                [v_r, identb], [p])
                k.A(lambda e, p=p, t=t: e.copy(out=RT[:, :, :, :, t * 128:(t + 1) * 128].rearrange("p a b c t -> p (a b c) t"),
                                               in_=p[:]), [p], [RT])
            for d_ in range(2):
                order = list(range(NT)) if d_ == 0 else [1, 0] + list(range(NT - 1, 1, -1))
                mk = 0 if d_ == 0 else 2
                St = Sst[d_]
                Sbb = Sb[d_]
                k.V(lambda e, St=St: e.memset(St[:], 0.0), [], [St])
                k.V(lambda e, Sbb=Sbb: e.memset(Sbb[:], 0.0), [], [Sbb])
                for ci, t in enumerate(order):
                    pss = pst[ci % 2]
                    for h in range(4):
                        pr, hh = h // 2, h % 2
                        k.T(lambda e, pss=pss, h=h, pr=pr, hh=hh, t=t, d_=d_: e.matmul(
                            pss[:, h, :], lhsT=RT[hh * 64:(hh + 1) * 64, d_, 1, pr, t * 128:(t + 1) * 128],
                            rhs=RT[hh * 64:(hh + 1) * 64, d_, 0, pr, t * 128:(t + 1) * 128], start=True, stop=True),
                            [RT], [pss])
                    stm = STm[ci % 2]
                    k.V(lambda e, stm=stm, pss=pss, mk=mk: e.tensor_tensor(
                        out=stm[:], in0=pss[:], in1=maskb[:, mk, :].unsqueeze(1).to_broadcast([128, 4, 128]), op=ALU.mult),
                        [pss, maskb], [stm])
                    po_ = pso[ci % 2]
                    for h in range(4):
                        pr, hh = h // 2, h % 2
                        k.T(lambda e, po_=po_, stm=stm, h=h, t=t: e.matmul(
                            po_[:, h, :], lhsT=stm[:, h, :], rhs=Vb[:, t, h * 64:(h + 1) * 64], start=True, stop=False),
                            [stm, Vb], [po_])
                        k.T(lambda e, po_=po_, h=h, pr=pr, hh=hh, t=t, d_=d_, Sbb=Sbb: e.matmul(
                            po_[:, h, :], lhsT=RT[hh * 64:(hh + 1) * 64, d_, 0, pr, t * 128:(t + 1) * 128],
                            rhs=Sbb[hh * 64:(hh + 1) * 64, pr, hh * 64:(hh + 1) * 64], start=False, stop=True),
                            [RT, Sbb], [po_])
                    if d_ == 0:
                        k.A(lambda e, po_=po_, t=t: e.copy(out=OA[:, t, :], in_=po_[:].rearrange("p h d -> p (h d)")),
                            [po_], [OA])
                    else:
                        k.V(lambda e, po_=po_, t=t: e.tensor_tensor(out=OA[:, t, :], in0=OA[:, t, :],
                                                                    in1=po_[:].rearrange("p h d -> p (h d)"), op=ALU.add),
                            [po_, OA], [OA])
                    if ci == NT - 1:
                        continue
                    pk = pkv[ci % 2]
                    for pr in range(2):
                        k.T(lambda e, pk=pk, pr=pr, t=t, d_=d_: e.matmul(
                            pk[:, pr, :], lhsT=KD[:, t, d_, pr * 128:(pr + 1) * 128], rhs=Vb[:, t, pr * 128:(pr + 1) * 128],
                            start=True, stop=True), [KD, Vb], [pk])
                    k.V(lambda e, pk=pk, St=St: e.tensor_tensor(out=tmp[:], in0=pk[:], in1=St[:], op=ALU.add), [pk, St], [tmp])
                    for pr in range(2):
                        k.V(lambda e, pr=pr, St=St, d_=d_: e.tensor_scalar(out=St[:, pr, :], in0=tmp[:, pr, :],
                                                                          scalar1=g128[:, d_, pr:pr + 1], scalar2=None,
                                                                          op0=ALU.mult), [tmp, g128], [St])
                        k.A(lambda e, pr=pr, Sbb=Sbb, d_=d_: e.activation(out=Sbb[:, pr, :], in_=tmp[:, pr, :], func=AF.Copy,
                                                                         scale=g128[:, d_, pr:pr + 1]), [tmp, g128], [Sbb])
            tiles = list(range(2, NT)) + ([] if last else [0, 1])
            for i_, t in enumerate(tiles):
                o3 = OA[:, t, :].rearrange("p (h d) -> p h d", d=64)
                k.V(lambda e, o3=o3: e.tensor_reduce(out=mean[:], in_=o3, axis=AX.X, op=ALU.add), [OA], [mean])
                k.V(lambda e: e.tensor_scalar(out=mean[:], in0=mean[:], scalar1=1.0 / 64, scalar2=None, op0=ALU.mult),
                    [mean], [mean])
                k.V(lambda e, o3=o3: e.tensor_tensor(out=cen[:].rearrange("p (h d) -> p h d", d=64), in0=o3,
                                                     in1=mean[:].unsqueeze(2).to_broadcast([128, 4, 64]), op=ALU.subtract),
                    [OA, mean], [cen])
                k.G(lambda e: e.tensor_tensor(out=sq[:], in0=cen[:], in1=cen[:], op=ALU.mult), [cen], [sq])
                k.V(lambda e: e.tensor_reduce(out=var[:], in_=sq[:].rearrange("p (h d) -> p h d", d=64), axis=AX.X,
                                              op=ALU.add), [sq], [var])
                k.V(lambda e: e.tensor_scalar(out=var[:], in0=var[:], scalar1=1.0 / 64, scalar2=EPS, op0=ALU.mult,
                                              op1=ALU.add), [var], [var])
                k.A(lambda e: e.sqrt(out=var[:], in_=var[:]), [var], [var])
                k.V(lambda e: e.reciprocal(out=var[:], in_=var[:]), [var], [var])
                r_o = ro[i_ % 2]
                k.V(lambda e: e.tensor_tensor(out=rof[:].rearrange("p (h d) -> p h d", d=64),
                                              in0=cen[:].rearrange("p (h d) -> p h d", d=64),
                                              in1=var[:].unsqueeze(2).to_broadcast([128, 4, 64]), op=ALU.mult),
                    [cen, var], [rof])
                k.G(lambda e: e.tensor_tensor(out=rof[:], in0=rof[:], in1=gn[:], op=ALU.mult), [rof, gn], [rof])
                k.V(lambda e, r_o=r_o, t=t: e.tensor_tensor(out=r_o[:], in0=rof[:], in1=SG[:, t, :], op=ALU.mult),
                    [rof, SG], [r_o])
                k.dma("act", lambda e, r_o=r_o, s=s, t=t: e.dma_start(
                    out=A["mixin_d"][s, t * 128:(t + 1) * 128, 512:768], in_=r_o[:]), reads=[r_o], writes=["mixin_d"])
    k.pesync = False


def phase_conv(k, A, P, l, last):
    with k.phase():
        ident = P["ident"]
        identb = P["identb"]
        cw = k.sb("cw", [31, 256], F32)
        k.dma("sp", lambda e: e.dma_start(out=cw[:], in_=A["conv_w"][l]), writes=[cw])
        pw = k.ps("pw", [128, 2, 32], F32)
        for cc in range(2):
            k.T(lambda e, cc=cc: e.transpose(pw[:, cc, 0:31], cw[0:31, cc * 128:(cc + 1) * 128], ident[0:31, 0:31]),
                [cw, ident], [pw])
        cwT = k.sb("cwT", [128, 2, 32], F32)
        k.V(lambda e: e.tensor_copy(out=cwT[:, :, 0:31], in_=pw[:, :, 0:31]), [pw], [cwT])
        diag = k.sb("diag", [128, 2, 31, 128], BF16)
        for cc in range(2):
            for kk in range(31):
                eng = k.V if kk % 2 == 0 else k.G
                eng(lambda e, cc=cc, kk=kk: e.tensor_scalar(out=diag[:, cc, kk, :], in0=ident[:],
                                                            scalar1=cwT[:, cc, kk:kk + 1], scalar2=None, op0=ALU.mult),
                    [ident, cwT], [diag])
        vec = k.sb("vec", [3, 256], F32)
        k.dma("sp", lambda e: e.dma_start(out=vec[0:1, :], in_=A["conv_b"][l:l + 1, :]), writes=[vec])
        k.dma("sp", lambda e: e.dma_start(out=vec[1:2, :], in_=A["conv_norm_g"][l:l + 1, :]), writes=[vec])
        k.dma("sp", lambda e: e.dma_start(out=vec[2:3, :], in_=A["conv_norm_b"][l:l + 1, :]), writes=[vec])
        pv = k.ps("pv", [128, 2, 4], F32)
        for cc in range(2):
            k.T(lambda e, cc=cc: e.transpose(pv[:, cc, 0:3], vec[0:3, cc * 128:(cc + 1) * 128], ident[0:3, 0:3]),
                [vec, ident], [pv])
        vT = k.sb("vT", [128, 2, 4], F32)
        k.V(lambda e: e.tensor_copy(out=vT[:, :, 0:3], in_=pv[:, :, 0:3]), [pv], [vT])
        ones = k.sb("ones", [128, 128], F32R)
        onesf = k.sb("onesf", [128, 128], F32)
        k.V(lambda e: e.memset(onesf[:], 1.0 / 256), [], [onesf])
        k.V(lambda e: e.tensor_copy(out=ones[:], in_=onesf[:]), [onesf], [ones])

        LP = S + 30
        hp = [k.sb("hp%d" % i, [128, LP], BF16) for i in range(2)]
        val = [k.sb("val%d" % i, [128, S], F32) for i in range(2)]
        gt = [k.sb("gt%d" % i, [128, S], F32) for i in range(2)]
        cvo = [k.sb("cvo%d" % i, [128, 512], F32R) for i in range(2)]
        csq = [k.sb("csq%d" % i, [128, 512], F32R) for i in range(2)]
        pcv = [k.ps("pcv%d" % i, [128, 512], F32) for i in range(2)]
        pmean = k.ps("pmean", [128, 512], F32)
        pex2 = k.ps("pex2", [128, 512], F32)
        msb = k.sb("msb", [128, 512], F32)
        rsd = k.sb("rsd", [128, 512], F32)
        yv = [k.sb("yv%d" % i, [128, 512], F32) for i in range(2)]
        yo = [k.sb("yo%d" % i, [128, 512], BF16) for i in range(2)]
        for s in range(NS):
            segs = [(C, S)] + ([] if last else [(0, C)])
            for (tok0, Lg) in segs:
                for cc in range(2):
                    k.dma("sp", lambda e, cc=cc, s=s, tok0=tok0, Lg=Lg: e.dma_start(
                        out=val[cc][:, 0:Lg], in_=A["cv_d"][s, cc * 128:(cc + 1) * 128, tok0:tok0 + Lg]),
                        reads=["cv_d"], writes=[val[cc]])
                    k.dma("act", lambda e, cc=cc, s=s, tok0=tok0, Lg=Lg: e.dma_start(
                        out=gt[cc][:, 0:Lg], in_=A["cv_d"][s, 256 + cc * 128: 256 + (cc + 1) * 128, tok0:tok0 + Lg]),
                        reads=["cv_d"], writes=[gt[cc]])
                    k.A(lambda e, cc=cc, Lg=Lg: e.activation(out=gt[cc][:, 0:Lg], in_=gt[cc][:, 0:Lg], func=AF.Sigmoid),
                        [gt[cc]], [gt[cc]])
                    k.G(lambda e, cc=cc: e.memset(hp[cc][:], 0.0), [], [hp[cc]])
                    k.V(lambda e, cc=cc, Lg=Lg: e.tensor_tensor(out=hp[cc][:, 15:15 + Lg], in0=val[cc][:, 0:Lg],
                                                               in1=gt[cc][:, 0:Lg], op=ALU.mult),
                        [val[cc], gt[cc]], [hp[cc]])
                nb = (Lg + 511) // 512
                for b in range(nb):
                    w = min(512, Lg - b * 512)
                    for cc in range(2):
                        p = pcv[cc]
                        for kk in range(31):
                            k.T(lambda e, p=p, cc=cc, kk=kk, b=b, w=w: e.matmul(
                                p[:, 0:w], lhsT=diag[:, cc, kk, :], rhs=hp[cc][:, b * 512 + kk: b * 512 + kk + w],
                                start=(kk == 0), stop=(kk == 30)), [diag, hp[cc]], [p])
                        k.A(lambda e, p=p, cc=cc, w=w: e.activation(out=cvo[cc][:, 0:w], in_=p[:, 0:w], func=AF.Identity,
                                                                   bias=vT[:, cc, 0:1], scale=1.0), [p, vT], [cvo[cc]])
                        k.V(lambda e, cc=cc, w=w: e.tensor_tensor(out=csq[cc][:, 0:w], in0=cvo[cc][:, 0:w].bitcast(F32),
                                                                 in1=cvo[cc][:, 0:w].bitcast(F32), op=ALU.mult),
                            [cvo[cc]], [csq[cc]])
                    for cc in range(2):
                        k.T(lambda e, cc=cc, w=w: e.matmul(pmean[:, 0:w], lhsT=ones[:], rhs=cvo[cc][:, 0:w],
                                                           start=(cc == 0), stop=(cc == 1)), [ones, cvo[cc]], [pmean])
                    for cc in range(2):
                        k.T(lambda e, cc=cc, w=w: e.matmul(pex2[:, 0:w], lhsT=ones[:], rhs=csq[cc][:, 0:w],
                                                           start=(cc == 0), stop=(cc == 1)), [ones, csq[cc]], [pex2])
                    k.A(lambda e, w=w: e.copy(out=msb[:, 0:w], in_=pmean[:, 0:w]), [pmean], [msb])
                    k.V(lambda e, w=w: e.tensor_tensor(out=rsd[:, 0:w], in0=msb[:, 0:w], in1=msb[:, 0:w], op=ALU.mult),
                        [msb], [rsd])
                    k.V(lambda e, w=w: e.tensor_tensor(out=rsd[:, 0:w], in0=pex2[:, 0:w], in1=rsd[:, 0:w], op=ALU.subtract),
                        [pex2, rsd], [rsd])
                    k.V(lambda e, w=w: e.tensor_scalar(out=rsd[:, 0:w], in0=rsd[:, 0:w], scalar1=EPS, scalar2=None,
                                                       op0=ALU.add), [rsd], [rsd])
                    k.A(lambda e, w=w: e.sqrt(out=rsd[:, 0:w], in_=rsd[:, 0:w]), [rsd], [rsd])
                    k.V(lambda e, w=w: e.reciprocal(out=rsd[:, 0:w], in_=rsd[:, 0:w]), [rsd], [rsd])
                    for cc in range(2):
                        k.G(lambda e, cc=cc, w=w: e.tensor_tensor(out=yv[cc][:, 0:w], in0=cvo[cc][:, 0:w].bitcast(F32),
                                                                 in1=msb[:, 0:w], op=ALU.subtract), [cvo[cc], msb], [yv[cc]])
                        k.V(lambda e, cc=cc, w=w: e.tensor_tensor(out=yv[cc][:, 0:w], in0=yv[cc][:, 0:w], in1=rsd[:, 0:w],
                                                                 op=ALU.mult), [yv[cc], rsd], [yv[cc]])
                        k.A(lambda e, cc=cc, w=w: e.activation(out=yo[cc][:, 0:w], in_=yv[cc][:, 0:w], func=AF.Silu,
                                                               scale=vT[:, cc, 1:2], bias=vT[:, cc, 2:3]),
                            [yv[cc], vT], [yo[cc]])
                        k.dma("act", lambda e, cc=cc, s=s, tok0=tok0, b=b, w=w: e.dma_start(
                            out=A["convT_d"][s, cc * 128:(cc + 1) * 128, tok0 + b * 512: tok0 + b * 512 + w],
                            in_=yo[cc][:, 0:w]), reads=[yo[cc]], writes=["convT_d"])


def phase_out(k, A, P, l, last, src_lat, src_ctx, dst_lat):
    with k.phase():
        ident = P["ident"]
        identb = P["identb"]
        EPSB = P["epsb"]
        wout = k.sb("wout", [128, 8, D], BF16)
        wv = A["w_out"][l].rearrange("(kc p) f -> p kc f", p=128)
        for j in range(0, D, 512):
            k.dma("pool", lambda e, j=j: e.dma_start(out=wout[:, :, j:j + 512], in_=wv[:, :, j:j + 512]), writes=[("wout", j)])
        woutk = [("wout", 0), ("wout", 512)]
        wr = k.sb("wr", [128, 8, E], F32)
        k.dma("sp", lambda e: e.dma_start(out=wr[:], in_=A["w_router"][l].rearrange("(kc p) e -> p kc e", p=128)),
              writes=[wr])
        nr = NS + (0 if last else 1)
        gta = [k.sb("gta%d" % r, [128, D], F32) for r in range(nr)]
        gsf = [k.sb("gsf%d" % r, [128, D], F32) for r in range(nr)]
        shf = [k.sb("shf%d" % r, [128, D], F32) for r in range(nr)]
        gf = k.sb("gf", [128, D], F32)
        k.dma("sp", lambda e: e.dma_start(out=gf[:], in_=A["g_ffn"][l, :].partition_broadcast(128)), writes=[gf])
        for r in range(nr):
            k.dma("sp", lambda e, r=r: e.dma_start(out=gta[r][:], in_=A["mod_d"][l, r, 2 * D:3 * D].partition_broadcast(128)),
                  reads=["mod_d"], writes=[gta[r]])
            k.dma("sp", lambda e, r=r: e.dma_start(out=shf[r][:], in_=A["mod_d"][l, r, 3 * D:4 * D].partition_broadcast(128)),
                  reads=["mod_d"], writes=[shf[r]])
            k.dma("sp", lambda e, r=r: e.dma_start(out=gsf[r][:], in_=A["mod_d"][l, r, 4 * D:5 * D].partition_broadcast(128)),
                  reads=["mod_d"], writes=[gsf[r]])
            k.V(lambda e, r=r: e.scalar_tensor_tensor(out=gsf[r][:], in0=gsf[r][:], scalar=1.0, in1=gf[:], op0=ALU.add,
                                                      op1=ALU.mult), [gsf[r], gf], [gsf[r]])
        NB = 4
        mi = [k.sb("mi%d" % i, [128, 768], BF16) for i in range(NB)]
        mixT = [k.sb("mixT%d" % i, [128, 8, 128], BF16) for i in range(NB)]
        xt = [k.sb("xt%d" % i, [128, D], F32) for i in range(NB)]
        xm = [k.sb("xm%d" % i, [128, D], F32) for i in range(NB)]
        hf = [k.sb("hf%d" % i, [128, D], F32) for i in range(NB)]
        hfb = [k.sb("hfb%d" % i, [128, D], BF16) for i in range(2)]
        junk = k.sb("junk", [128, D], F32)
        ss = [k.sb("ss%d" % i, [128, 1], F32) for i in range(NB)]
        rstd = [k.sb("rstd%d" % i, [128, 1], F32) for i in range(NB)]
        hfT = [k.sb("hfT%d" % i, [128, 8, 128], F32) for i in range(2)]
        ptm = k.ps("ptm", [128, 6, 128], BF16)
        py = [k.ps("py%d" % i, [128, 512], F32) for i in range(2)]
        pth = k.ps("pth", [128, 8, 128], F32)
        plg = k.ps("plg", [128, 16], F32)
        pat = k.ps("pat", [48, 128], F32)
        lmax = k.sb("lmax", [128, 1], F32)
        lsum = k.sb("lsum", [128, 1], F32)
        afftm = [k.sb("afftm%d" % i, [128, 48], F32) for i in range(2)]
        affT = k.sb("affT", [48, S + C], F32)
        for a_ in afftm:
            k.V(lambda e, a_=a_: e.memset(a_[:], 0.0), [], [a_])
        tiles = list(range(2, NT)) + ([] if last else [0, 1])
        items = [(t, s) for t in tiles for s in range(NS)]

        def info(i):
            t, s = items[i]
            isctx = t < 2
            r = NS if isctx else s
            if isctx:
                src = src_ctx[s * C + t * 128: s * C + (t + 1) * 128, :]
                dst = A["res_d"][NLAT + s * C + t * 128: NLAT + s * C + (t + 1) * 128, :]
                hrow = NLAT + s * C + t * 128
            else:
                src = src_lat[s * S + (t - 2) * 128: s * S + (t - 1) * 128, :]
                dst = dst_lat[s * S + (t - 2) * 128: s * S + (t - 1) * 128, :]
                hrow = s * S + (t - 2) * 128
            return t, s, r, src, dst, hrow

        def stA(i):
            t, s, r, src, dst, hrow = info(i)
            m, mt, x_t, x_m = mi[i % NB], mixT[i % NB], xt[i % NB], xm[i % NB]
            k.dma("sp", lambda e: e.dma_start(out=m[:], in_=A["mixin_d"][s, t * 128:(t + 1) * 128, :]),
                  reads=["mixin_d"], writes=[m])
            k.dma("sp", lambda e: e.dma_start(
                out=mt[:, 6:8, :], in_=A["convT_d"][s, :, t * 128:(t + 1) * 128].rearrange("(c p) t -> p c t", p=128)),
                reads=["convT_d"], writes=[mt])
            k.dma("act", lambda e: e.dma_start(out=x_t[:], in_=src), writes=[x_t])
            for j in range(6):
                k.T(lambda e, j=j: e.transpose(ptm[:, j, :], m[:, j * 128:(j + 1) * 128], identb[:]), [m, identb], [ptm])
            k.A(lambda e: e.copy(out=mt[:, 0:6, :], in_=ptm[:]), [ptm], [mt])
            for hh in range(2):
                p = py[hh]
                for kc in range(8):
                    k.T(lambda e, p=p, kc=kc, hh=hh: e.matmul(p[:], lhsT=mt[:, kc, :], rhs=wout[:, kc, hh * 512:(hh + 1) * 512],
                                                              start=(kc == 0), stop=(kc == 7)), [mt] + woutk, [p])
                k.V(lambda e, p=p, hh=hh: e.tensor_tensor(out=x_m[:, hh * 512:(hh + 1) * 512], in0=p[:],
                                                          in1=gta[r][:, hh * 512:(hh + 1) * 512], op=ALU.mult),
                    [p, gta[r]], [x_m])
            k.G(lambda e: e.tensor_tensor(out=x_m[:], in0=x_m[:], in1=x_t[:], op=ALU.add), [x_m, x_t], [x_m])
            k.dma("sp", lambda e: e.dma_start(out=dst, in_=x_m[:]), reads=[x_m], writes=["res"])

        def stB(i):
            t, s, r, src, dst, hrow = info(i)
            x_m, h_f, sst, rs = xm[i % NB], hf[i % NB], ss[i % NB], rstd[i % NB]
            h_b = hfb[i % 2]
            k.A(lambda e: e.activation(out=junk[:], in_=x_m[:], func=AF.Square, accum_out=sst[:, 0:1]), [x_m], [junk, sst])
            k.A(lambda e: e.activation(out=sst[:, 0:1], in_=sst[:, 0:1], func=AF.Sqrt, scale=1.0 / D, bias=EPSB[:, 0:1]),
                [sst, EPSB], [sst])
            k.V(lambda e: e.reciprocal(out=rs[:, 0:1], in_=sst[:, 0:1]), [sst], [rs])
            k.V(lambda e: e.scalar_tensor_tensor(out=h_f[:], in0=x_m[:], scalar=rs[:, 0:1], in1=gsf[r][:], op0=ALU.mult,
                                                 op1=ALU.mult), [x_m, rs, gsf[r]], [h_f])
            k.G(lambda e: e.tensor_tensor(out=h_f[:], in0=h_f[:], in1=shf[r][:], op=ALU.add), [h_f, shf[r]], [h_f])
            k.A(lambda e: e.copy(out=h_b[:], in_=h_f[:]), [h_f], [h_b])
            k.dma("act", lambda e: e.dma_start(out=A["hfb_d"][hrow:hrow + 128, :], in_=h_b[:]), reads=[h_b], writes=["hfb_d"])

        def stC(i):
            t, s, r, src, dst, hrow = info(i)
            h_f, h_T = hf[i % NB], hfT[i % 2]
            af = afftm[(i // NS) % 2]
            for kc in range(8):
                k.T(lambda e, kc=kc: e.transpose(pth[:, kc, :], h_f[:, kc * 128:(kc + 1) * 128], ident[:]), [h_f, ident], [pth])
            k.A(lambda e: e.copy(out=h_T[:], in_=pth[:]), [pth], [h_T])
            for kc in range(8):
                k.T(lambda e, kc=kc: e.matmul(plg[:], lhsT=h_T[:, kc, :], rhs=wr[:, kc, :], start=(kc == 0), stop=(kc == 7)),
                    [h_T, wr], [plg])
            k.V(lambda e: e.tensor_reduce(out=lmax[:], in_=plg[:], axis=AX.X, op=ALU.max, negate=True), [plg], [lmax])
            k.A(lambda e: e.activation(out=af[:, s * 32:s * 32 + 16], in_=plg[:], func=AF.Exp, bias=lmax[:, 0:1],
                                       scale=1.0, accum_out=lsum[:, 0:1]), [plg, lmax], [af, lsum])
            k.V(lambda e: e.reciprocal(out=lsum[:], in_=lsum[:]), [lsum], [lsum])
            k.V(lambda e: e.tensor_scalar(out=af[:, s * 32:s * 32 + 16], in0=af[:, s * 32:s * 32 + 16],
                                          scalar1=lsum[:, 0:1], scalar2=None, op0=ALU.mult), [af, lsum], [af])
            if s == NS - 1:
                k.T(lambda e: e.transpose(pat[:], af[:], ident[:]), [af, ident], [pat])
                k.A(lambda e: e.copy(out=affT[:, t * 128:(t + 1) * 128], in_=pat[:]), [pat], [affT])

        n = len(items)
        for step in range(n + 2):
            if 0 <= step - 2 < n:
                stC(step - 2)
            if 0 <= step - 1 < n:
                stB(step - 1)
            if step < n:
                stA(step)
        k.dma("sp", lambda e: e.dma_start(out=A["aff_d"], in_=affT[:, C:C + S]), reads=[affT], writes=["aff_d"])
        if not last:
            k.dma("sp", lambda e: e.dma_start(out=A["affc_d"], in_=affT[:, 0:C]), reads=[affT], writes=["affc_d"])


def phase_moe(k, A, P, l, last, dst_lat):
    with k.phase():
        ident = P["ident"]
        nctx = 0 if last else NS
        NSL = NS * CAPL + nctx * CAPC
        if last:
            cgroups = [(0, 512)]
        else:
            cgroups = [(0, 288), (288, 288)]
        idxT = k.sb("idxT", [128, 2, 48], I32)
        gT = k.sb("gTm", [128, 2, 48], F32)
        if not last:
            idcT = k.sb("idcT", [32, 48], I32)
            gcT = k.sb("gcT", [32, 48], F32)
        with k.phase():
            wa = k.sb("wa", [48, S], F32)
            wb = k.sb("wb", [48, S], F32)
            vals = k.sb("vals", [48, CAPL], F32)
            idx = k.sb("idx", [48, CAPL], U32)
            idxf = k.sb("idxf", [48, CAPL], F32)
            offs = k.sb("offs", [48, 1], F32)
            k.dma("sp", lambda e: e.dma_start(out=wa[:], in_=A["aff_d"]), reads=["aff_d"], writes=[wa])
            cur, oth = wa, wb
            for r in range(CAPL // 8):
                k.V(lambda e, cur=cur, r=r: e.max(out=vals[:, r * 8:(r + 1) * 8], in_=cur[:]), [cur], [vals])
                k.V(lambda e, cur=cur, r=r: e.max_index(out=idx[:, r * 8:(r + 1) * 8], in_max=vals[:, r * 8:(r + 1) * 8],
                                                        in_values=cur[:]), [cur, vals], [idx])
                if r < CAPL // 8 - 1:
                    k.V(lambda e, cur=cur, oth=oth, r=r: e.match_replace(out=oth[:], in_to_replace=vals[:, r * 8:(r + 1) * 8],
                                                                         in_values=cur[:], imm_value=-1.0), [cur, vals], [oth])
                    cur, oth = oth, cur
            k.V(lambda e: e.memset(offs[0:32, :], 0.0), [], [offs])
            k.V(lambda e: e.memset(offs[32:48, :], float(S)), [], [offs])
            k.V(lambda e: e.tensor_copy(out=idxf[:], in_=idx[:]), [idx], [idxf])
            k.V(lambda e: e.tensor_scalar(out=idxf[:], in0=idxf[:], scalar1=offs[:, 0:1], scalar2=None, op0=ALU.add),
                [idxf, offs], [idxf])
            pti = k.ps("pti", [128, 2, 48], F32)
            ptg = k.ps("ptg", [128, 2, 48], F32)
            for j in range(2):
                k.T(lambda e, j=j: e.transpose(pti[:, j, :], idxf[:, j * 128:(j + 1) * 128], ident[0:48, 0:48]), [idxf, ident], [pti])
                k.T(lambda e, j=j: e.transpose(ptg[:, j, :], vals[:, j * 128:(j + 1) * 128], ident[0:48, 0:48]), [vals, ident], [ptg])
            k.V(lambda e: e.tensor_copy(out=idxT[:], in_=pti[:]), [pti], [idxT])
            k.V(lambda e: e.tensor_copy(out=gT[:], in_=ptg[:]), [ptg], [gT])
            if not last:
                wc = k.sb("wc", [48, C], F32)
                wd_ = k.sb("wd_", [48, C], F32)
                valc = k.sb("valc", [48, CAPC], F32)
                idc = k.sb("idc", [48, CAPC], U32)
                idcf = k.sb("idcf", [48, CAPC], F32)
                offc = k.sb("offc", [48, 1], F32)
                k.dma("sp", lambda e: e.dma_start(out=wc[:], in_=A["affc_d"]), reads=["affc_d"], writes=[wc])
                cur, oth = wc, wd_
                for r in range(CAPC // 8):
                    k.V(lambda e, cur=cur, r=r: e.max(out=valc[:, r * 8:(r + 1) * 8], in_=cur[:]), [cur], [valc])
                    k.V(lambda e, cur=cur, r=r: e.max_index(out=idc[:, r * 8:(r + 1) * 8], in_max=valc[:, r * 8:(r + 1) * 8],
                                                            in_values=cur[:]), [cur, valc], [idc])
                    if r < CAPC // 8 - 1:
                        k.V(lambda e, cur=cur, oth=oth, r=r: e.match_replace(out=oth[:], in_to_replace=valc[:, r * 8:(r + 1) * 8],
                                                                             in_values=cur[:], imm_value=-1.0), [cur, valc], [oth])
                        cur, oth = oth, cur
                k.V(lambda e: e.memset(offc[0:32, :], float(NLAT)), [], [offc])
                k.V(lambda e: e.memset(offc[32:48, :], float(NLAT + C)), [], [offc])
                k.V(lambda e: e.tensor_copy(out=idcf[:], in_=idc[:]), [idc], [idcf])
                k.V(lambda e: e.tensor_scalar(out=idcf[:], in0=idcf[:], scalar1=offc[:, 0:1], scalar2=None, op0=ALU.add),
                    [idcf, offc], [idcf])
                ptic = k.ps("ptic", [32, 2, 48], F32)
                k.T(lambda e: e.transpose(ptic[:, 0, :], idcf[:, 0:32], ident[0:48, 0:48]), [idcf, ident], [ptic])
                k.T(lambda e: e.transpose(ptic[:, 1, :], valc[:, 0:32], ident[0:48, 0:48]), [valc, ident], [ptic])
                k.V(lambda e: e.tensor_copy(out=idcT[:], in_=ptic[:, 0, :]), [ptic], [idcT])
                k.V(lambda e: e.tensor_copy(out=gcT[:], in_=ptic[:, 1, :]), [ptic], [gcT])
        identb = P["identb"]
        nr = NS + (0 if last else 1)
        gtf = [k.sb("gtf%d" % r, [128, D], F32) for r in range(nr)]
        for r in range(nr):
            k.dma("sp", lambda e, r=r: e.dma_start(out=gtf[r][:], in_=A["mod_d"][l, r, 5 * D:6 * D].partition_broadcast(128)),
                  reads=["mod_d"], writes=[gtf[r]])
        xe = [k.sb("xe%d" % i, [128, D], BF16) for i in range(3)]
        xeT = [k.sb("xeT%d" % i, [128, 8, NSL], BF16) for i in range(2)]
        hT = k.sb("hT", [128, NFC, NSL], BF16)
        sg = [k.sb("sg%d" % i, [128, NSL], F32) for i in range(2)]
        NW = 3
        NPRE = NW - 1
        wg = [k.sb("wg%d" % i, [128, 8, 512], BF16) for i in range(NW)]
        wu = [k.sb("wu%d" % i, [128, 8, 512], BF16) for i in range(NW)]
        wdn = k.sb("wdn", [128, NFC, D], BF16)
        ysb = [k.sb("ysb%d" % i, [128, D], F32) for i in range(2)]
        ncg = len(cgroups)
        pg = [k.ps("pg%d" % i, [128, 512], F32) for i in range(ncg)]
        pu = [k.ps("pu%d" % i, [128, 512], F32) for i in range(ncg)]
        pxt = k.ps("pxt", [128, 8, 128], BF16)
        pyd = [k.ps("pyd%d" % i, [128, 512], F32) for i in range(8 - 2 * ncg - 1)]
        target = dst_lat
        pieces = [(i * 512, min(512, DFF - i * 512)) for i in range((DFF + 511) // 512)]
        NP_ = len(pieces)
        cnt = {"y": 0}

        def tiles_of(ex):
            tl = []
            for s in range(NS):
                for j in range(2):
                    tl.append((128, s * 256 + j * 128, idxT[:, j, s * 32 + ex: s * 32 + ex + 1],
                               gT[:, j, s * 32 + ex: s * 32 + ex + 1], s, "lat"))
            if not last:
                for s in range(NS):
                    tl.append((32, NS * CAPL + s * CAPC, idcT[:, s * 32 + ex: s * 32 + ex + 1],
                               gcT[:, s * 32 + ex: s * 32 + ex + 1], NS, "ctx"))
            return tl

        idx_reads = [idxT] + ([] if last else [idcT])

        def gather_T(ex):
            xT = xeT[ex % 2]
            for ti, (rows, c0, iap, gap, r, kind) in enumerate(tiles_of(ex)):
                x_e = xe[ti % 3]
                k.dma("pool", lambda e, x_e=x_e, rows=rows, iap=iap: e.indirect_dma_start(
                    out=x_e[0:rows, :], out_offset=None, in_=A["hfb_d"],
                    in_offset=bass.IndirectOffsetOnAxis(ap=iap[0:rows, :], axis=0)),
                    reads=["hfb_d"] + idx_reads, writes=[x_e])
                for kc in range(8):
                    k.T(lambda e, x_e=x_e, rows=rows, kc=kc: e.transpose(pxt[:, kc, 0:rows], x_e[0:rows, kc * 128:(kc + 1) * 128],
                                                                       identb[0:rows, 0:rows]), [x_e, identb], [pxt])
                if ti % 2 == 0:
                    k.A(lambda e, xT=xT, rows=rows, c0=c0: e.copy(out=xT[:, :, c0:c0 + rows], in_=pxt[:, :, 0:rows]), [pxt], [xT])
                else:
                    k.V(lambda e, xT=xT, rows=rows, c0=c0: e.tensor_copy(out=xT[:, :, c0:c0 + rows], in_=pxt[:, :, 0:rows]),
                        [pxt], [xT])

        def load_gu(ex, pi):
            c0, w = pieces[pi]
            gi_ = (ex * NP_ + pi) % NW
            wgv = A["w_gate"][l, ex].rearrange("(kc p) f -> p kc f", p=128)
            wuv = A["w_up"][l, ex].rearrange("(kc p) f -> p kc f", p=128)
            k.dma("pool", lambda e: e.dma_start(out=wg[gi_][:, :, 0:w], in_=wgv[:, :, c0:c0 + w]), writes=[wg[gi_]])
            k.dma("pool", lambda e: e.dma_start(out=wu[gi_][:, :, 0:w], in_=wuv[:, :, c0:c0 + w]), writes=[wu[gi_]])

        def load_dn(ex, pi):
            c0, w = pieces[pi]
            f0, nf = c0 // 128, w // 128
            wdv = A["w_down"][l, ex].rearrange("(fc p) d -> p fc d", p=128)
            for f in range(f0, f0 + nf, 2):
                k.dma("pool", lambda e, f=f: e.dma_start(out=wdn[:, f:f + 2, :], in_=wdv[:, f:f + 2, :]),
                      writes=[("wdn", f // 2)])

        def gu(ex, pi):
            c0, w = pieces[pi]
            gi_ = (ex * NP_ + pi) % NW
            xT = xeT[ex % 2]
            for f2 in range(w // 128):
                fc = c0 // 128 + f2
                for (W, ps_list) in ((wg[gi_], pg), (wu[gi_], pu)):
                    for gj, (g0, gw) in enumerate(cgroups):
                        p = ps_list[gj]
                        for kc in range(8):
                            k.T(lambda e, p=p, W=W, kc=kc, f2=f2, g0=g0, gw=gw: e.matmul(
                                p[:, 0:gw], lhsT=W[:, kc, f2 * 128:(f2 + 1) * 128], rhs=xT[:, kc, g0:g0 + gw],
                                start=(kc == 0), stop=(kc == 7)), [W, xT], [p])
                s_g = sg[fc % 2]
                for gj, (g0, gw) in enumerate(cgroups):
                    k.A(lambda e, s_g=s_g, gj=gj, g0=g0, gw=gw: e.activation(out=s_g[:, g0:g0 + gw], in_=pg[gj][:, 0:gw],
                                                                            func=AF.Silu), [pg[gj]], [s_g])
                    k.V(lambda e, s_g=s_g, gj=gj, g0=g0, gw=gw, fc=fc: e.tensor_tensor(
                        out=hT[:, fc, g0:g0 + gw], in0=s_g[:, g0:g0 + gw], in1=pu[gj][:, 0:gw], op=ALU.mult),
                        [s_g, pu[gj]], [("hT", fc)])

        def down(ex):
            for ti, (rows, c0, iap, gap, r, kind) in enumerate(tiles_of(ex)):
                y_s = ysb[ti % 2]
                for hh in range(2):
                    p = pyd[cnt["y"] % len(pyd)]
                    cnt["y"] += 1
                    for fc in range(NFC):
                        k.T(lambda e, p=p, rows=rows, c0=c0, fc=fc, hh=hh: e.matmul(
                            p[0:rows, :], lhsT=hT[:, fc, c0:c0 + rows], rhs=wdn[:, fc, hh * 512:(hh + 1) * 512],
                            start=(fc == 0), stop=(fc == NFC - 1)), [("hT", fc), ("wdn", fc // 2)], [p])
                    k.V(lambda e, p=p, rows=rows, y_s=y_s, hh=hh, gap=gap, r=r: e.scalar_tensor_tensor(
                        out=y_s[0:rows, hh * 512:(hh + 1) * 512], in0=p[0:rows, :], scalar=gap[0:rows, :],
                        in1=gtf[r][0:rows, hh * 512:(hh + 1) * 512], op0=ALU.mult, op1=ALU.mult),
                        [p, gT, gtf[r]] + ([] if last else [gcT]), [y_s])
                tgt = target if kind == "lat" else A["res_d"]
                k.dma("pool", lambda e, y_s=y_s, rows=rows, iap=iap, tgt=tgt: e.indirect_dma_start(
                    out=tgt, out_offset=bass.IndirectOffsetOnAxis(ap=iap[0:rows, :], axis=0), in_=y_s[0:rows, :],
                    in_offset=None, compute_op=ALU.add), reads=[y_s] + idx_reads, writes=["res"])

        gather_T(0)
        for pi in range(NPRE):
            load_gu(0, pi)
        for ex in range(E):
            for pi in range(NP_):
                load_dn(ex, pi)
                gu(ex, pi)
                if pi + NPRE < NP_:
                    load_gu(ex, pi + NPRE)
            if ex + 1 < E:
                gather_T(ex + 1)
                for pi in range(NPRE):
                    load_gu(ex + 1, pi)
            down(ex)


_NC_CACHE = {}


def kernel(**inputs):
    consts = host_consts()
    if "nc" not in _NC_CACHE:
        _NC_CACHE["nc"] = build_program()
    nc = _NC_CACHE["nc"]
    x = np.ascontiguousarray(inputs["x"], dtype=np.float32)
    c = np.ascontiguousarray(inputs["c"], dtype=np.float32)
    ctx = np.ascontiguousarray(inputs["ctx"], dtype=np.float32)
    shared = {"c_ctx": np.ascontiguousarray(inputs["c_ctx"], dtype=np.float32).reshape(1, D)}
    for n in WNAMES:
        shared[n] = np.ascontiguousarray(inputs[n], dtype=np.float32).reshape(WSHAPES[n])
    for n, v in consts.items():
        shared[n] = v
    in_maps = []
    for ci in range(NCORES):
        m = dict(shared)
        m["x"] = x[ci * NS:(ci + 1) * NS].reshape(NS * S, D)
        m["c"] = c[ci * NS:(ci + 1) * NS]
        m["ctx"] = ctx[ci * NS:(ci + 1) * NS].reshape(NS * C, D)
        in_maps.append(m)
    res = run_bass_kernel_spmd(nc, in_maps, core_ids=list(range(NCORES)))
    out = np.concatenate([r["y"].reshape(NS, S, D) for r in res.results], axis=0)
    return out.astype(np.float32)
```

```python
import os
import numpy as np
import concourse.bass as bass
import concourse.mybir as mybir
from concourse.bass_utils import run_bass_kernel_spmd
from contextlib import ExitStack, contextmanager

F32 = mybir.dt.float32
F32R = mybir.dt.float32r
BF16 = mybir.dt.bfloat16
U32 = mybir.dt.uint32
I32 = mybir.dt.int32
AF = mybir.ActivationFunctionType
ALU = mybir.AluOpType
AX = mybir.AxisListType

NCORES = 8
NS = 2
D = 1024
S = 2048
C = 256
NT = (S + C) // 128
L = 2
E = 16
DFF = 2816
NFC = DFF // 128
CAPL = 256
CAPC = 32
EPS = 1e-6
UC = 1792
NLAT = NS * S
NROW = NS * (S + C)


class KB:
    ENGS = ("pe", "dve", "act", "pool", "sp")

    def __init__(self, nc, ndma=24):
        self.nc = nc
        self.st = ExitStack()
        self.q = {e: [] for e in self.ENGS}
        self.cnt = {e: 0 for e in self.ENGS}
        self.sem = {e: self.st.enter_context(nc.semaphore("prog_" + e)) for e in self.ENGS}
        self.waited = {(c, p): 0 for c in self.ENGS for p in self.ENGS}
        self.waited_dma = {}
        self.lastw = {}
        self.readers = {}
        self.dma_sems = []
        self.ndma = {"sp": 0, "act": 0, "pool": 0}
        self.pool_of = {"sp": (0, 10), "act": (10, 8), "pool": (18, 14)}
        for i in range(32):
            s = self.st.enter_context(nc.semaphore("dma%d" % i))
            self.dma_sems.append([s, 0, None])
        self.cur = self.st

    def sb(self, name, shape, dt):
        self.uid = getattr(self, "uid", 0) + 1
        return self.cur.enter_context(self.nc.sbuf_tensor("%s_s%d" % (name, self.uid), shape, dt))

    def ps(self, name, shape, dt):
        self.uid = getattr(self, "uid", 0) + 1
        return self.cur.enter_context(self.nc.psum_tensor("%s_p%d" % (name, self.uid), shape, dt))

    @contextmanager
    def phase(self):
        prev = self.cur
        with ExitStack() as es:
            self.cur = es
            yield
            self.barrier()
        self.cur = prev

    def barrier(self):
        toks = [(p, self.cnt[p]) for p in self.ENGS if self.cnt[p] > 0]
        dtoks = [d[2] for d in self.dma_sems if d[2] is not None]
        for e in self.ENGS:
            for t in toks:
                if t[0] != e:
                    self._wait(e, t)
            for t in dtoks:
                self._wait(e, t)
        self.lastw = {}
        self.readers = {}

    def _key(self, x):
        if isinstance(x, (str, tuple)):
            return x
        return x.tensor.name if hasattr(x, "tensor") else x.name

    def _wait(self, eng, tok):
        if tok is None:
            return
        if tok[0] == "dma":
            _, i, val = tok
            if self.waited_dma.get((eng, i), 0) >= val:
                return
            self.waited_dma[(eng, i)] = val
            sem = self.dma_sems[i][0]
            self.q[eng].append(lambda e, sem=sem, val=val: e.wait_ge(sem, val))
            return
        p, c = tok
        if eng == "pe" and p == "pe" and not (PESYNC or getattr(self, "pesync", False)):
            return
        if self.waited[(eng, p)] >= c:
            return
        self.waited[(eng, p)] = c
        s = self.sem[p]
        self.q[eng].append(lambda e, s=s, c=c: e.wait_ge(s, c))

    def _deps(self, eng, rk, wk):
        for k in rk:
            self._wait(eng, self.lastw.get(k))
        for k in wk:
            self._wait(eng, self.lastw.get(k))
            for t in self.readers.get(k, ()):
                self._wait(eng, t)

    def _record(self, tok, rk, wk):
        for k in wk:
            self.lastw[k] = tok
            self.readers[k] = []
        for k in rk:
            self.readers.setdefault(k, []).append(tok)

    def op(self, eng, fn, reads=(), writes=()):
        rk = [self._key(r) for r in reads]
        wk = [self._key(w) for w in writes]
        self._deps(eng, rk, wk)
        self.cnt[eng] += 1
        tok = (eng, self.cnt[eng])
        s = self.sem[eng]
        self.q[eng].append(lambda e, fn=fn, s=s: fn(e).then_inc(s, 1))
        self._record(tok, rk, wk)
        return tok

    def V(self, fn, r=(), w=()):
        return self.op("dve", fn, r, w)

    def A(self, fn, r=(), w=()):
        return self.op("act", fn, r, w)

    def G(self, fn, r=(), w=()):
        return self.op("pool", fn, r, w)

    def T(self, fn, r=(), w=()):
        return self.op("pe", fn, r, w)

    def dma(self, eng, fn, reads=(), writes=()):
        rk = [self._key(r) for r in reads]
        wk = [self._key(w) for w in writes]
        self._deps(eng, rk, wk)
        base, n = self.pool_of[eng]
        i = base + self.ndma[eng] % n
        self.ndma[eng] += 1
        ent = self.dma_sems[i]
        if ent[2] is not None:
            self._wait(eng, ent[2])
        ent[1] += 16
        tok = ("dma", i, ent[1])
        ent[2] = tok
        sem = ent[0]
        self.q[eng].append(lambda e, fn=fn, sem=sem: fn(e).then_inc(sem, 16))
        self._record(tok, rk, wk)
        return tok

    def finish(self):
        nc = self.nc
        self.barrier()
        q = self.q
        with nc.Block() as block:
            @block.tensor
            def _(e):
                for f in q["pe"]:
                    f(e)

            @block.vector
            def _(e):
                for f in q["dve"]:
                    f(e)

            @block.scalar
            def _(e):
                for f in q["act"]:
                    f(e)

            @block.gpsimd
            def _(e):
                for f in q["pool"]:
                    f(e)

            @block.sync
            def _(e):
                for f in q["sp"]:
                    f(e)
        self.st.close()


def host_consts():
    f32 = np.float32
    half = 16
    inv = (10000.0 ** (-np.arange(half, dtype=f32) / half)).astype(f32)
    t = np.arange(S)
    rows = (t // 64).astype(f32)
    cols = (t % 64).astype(f32)
    ar = (rows[:, None] * inv[None, :]).astype(f32)
    ac = (cols[:, None] * inv[None, :]).astype(f32)
    attC = np.concatenate([np.cos(ar), np.cos(ar), np.cos(ac), np.cos(ac)], axis=1).astype(f32)
    attS = np.concatenate([-np.sin(ar), np.sin(ar), -np.sin(ac), np.sin(ac)], axis=1).astype(f32)
    half = 32
    inv = (10000.0 ** (-np.arange(half, dtype=f32) / half)).astype(f32)
    pos = np.arange(S + C).astype(f32)
    a = (pos[:, None] * inv[None, :]).astype(f32)
    retC = np.concatenate([np.cos(a), np.cos(a)], axis=1).astype(f32)
    retS = np.concatenate([-np.sin(a), np.sin(a)], axis=1).astype(f32)
    j = np.arange(128)[:, None]
    i = np.arange(128)[None, :]
    masks = np.stack([(j <= i), (j >= i), (j > i)], axis=0).astype(f32)
    ident = np.eye(128, dtype=f32)
    p = np.arange(128, dtype=f32)
    dcoef = np.stack([p + 1, -(p + 1), -p, p], axis=1).astype(f32)
    return {"attC": attC, "attS": attS, "retC": retC, "retS": retS, "masks": masks,
            "ident": ident, "dcoef": dcoef}


WNAMES = ["w_mod", "b_mod", "g_mix", "g_ffn", "w_in", "q_norm_g", "k_norm_g", "att_sink",
          "ret_decay_logit", "ret_norm_g", "conv_w", "conv_b", "conv_norm_g", "conv_norm_b",
          "w_out", "w_router", "w_gate", "w_up", "w_down"]
WSHAPES = {
    "w_mod": [L, D, 6 * D], "b_mod": [L, 6 * D], "g_mix": [L, D], "g_ffn": [L, D], "w_in": [L, D, 2304],
    "q_norm_g": [L, 64], "k_norm_g": [L, 64], "att_sink": [L, 8], "ret_decay_logit": [L, 8],
    "ret_norm_g": [L, 256], "conv_w": [L, 31, 256], "conv_b": [L, 256], "conv_norm_g": [L, 256],
    "conv_norm_b": [L, 256], "w_out": [L, D, D], "w_router": [L, D, E], "w_gate": [L, E, D, DFF],
    "w_up": [L, E, D, DFF], "w_down": [L, E, DFF, D],
}
CSHAPES = {"attC": [S, 64], "attS": [S, 64], "retC": [S + C, 64], "retS": [S + C, 64],
           "masks": [3, 128, 128], "ident": [128, 128], "dcoef": [128, 4]}

PESYNC = os.environ.get("MK_PESYNC", "") == "1"
RETSKIP = os.environ.get("MK_RETSKIP", "")
STOP = os.environ.get("MK_STOP", "")
DEBUG = os.environ.get("MK_DEBUG", "") == "1"


def build_program():
    nc = bass.Bass("TRN2", target_bir_lowering=False)
    A = {}
    A["x"] = nc.dram_tensor("x", [NS * S, D], F32, kind="ExternalInput").ap()
    A["c"] = nc.dram_tensor("c", [NS, D], F32, kind="ExternalInput").ap()
    A["ctx"] = nc.dram_tensor("ctx", [NS * C, D], F32, kind="ExternalInput").ap()
    A["c_ctx"] = nc.dram_tensor("c_ctx", [1, D], F32, kind="ExternalInput").ap()
    for n in WNAMES:
        A[n] = nc.dram_tensor(n, WSHAPES[n], F32, kind="ExternalInput").ap()
    for n, shp in CSHAPES.items():
        A[n] = nc.dram_tensor(n, shp, F32, kind="ExternalInput").ap()
    A["y"] = nc.dram_tensor("y", [NS * S, D], F32, kind="ExternalOutput").ap()
    dk = "ExternalOutput" if DEBUG else "Internal"
    A["mod_d"] = nc.dram_tensor("mod_d", [L, 4, 6 * D], F32, kind=dk).ap()
    A["u_d"] = nc.dram_tensor("u_d", [NS, S + C, UC], F32, kind=dk).ap()
    A["cv_d"] = nc.dram_tensor("cv_d", [NS, 512, S + C], F32, kind=dk).ap()
    A["mixin_d"] = nc.dram_tensor("mixin_d", [NS, S + C, 768], BF16, kind=dk).ap()
    A["convT_d"] = nc.dram_tensor("convT_d", [NS, 256, S + C], BF16, kind=dk).ap()
    A["res_d"] = nc.dram_tensor("res_d", [NROW, D], F32, kind=dk).ap()
    A["hf_d"] = nc.dram_tensor("hf_d", [NROW, D], F32, kind=dk).ap()
    A["aff_d"] = nc.dram_tensor("aff_d", [48, S], F32, kind=dk).ap()
    A["hfb_d"] = nc.dram_tensor("hfb_d", [NROW, D], BF16, kind=dk).ap()
    A["affc_d"] = nc.dram_tensor("affc_d", [48, C], F32, kind=dk).ap()

    k = KB(nc)
    P = {}
    P["ident"] = k.sb("ident", [128, 128], F32)
    P["identb"] = k.sb("identb", [128, 128], BF16)
    P["masks"] = k.sb("masks", [128, 3, 128], F32)
    P["modT"] = [k.sb("modT%d" % l, [128, 48, 4], F32) for l in range(L)]
    P["gT"] = k.sb("gT", [128, 8, 4], F32)
    P["gsA"] = [k.sb("gsA%d" % l, [128, 8, 4], F32) for l in range(L)]
    P["epsb"] = k.sb("epsb", [128, 1], F32)
    k.V(lambda e: e.memset(P["epsb"][:], EPS), [], [P["epsb"]])
    k.dma("sp", lambda e: e.dma_start(out=P["ident"][:], in_=A["ident"]), writes=[P["ident"]])
    k.dma("sp", lambda e: e.dma_start(out=P["masks"][:], in_=A["masks"].rearrange("m j i -> j m i")),
          writes=[P["masks"]])
    k.V(lambda e: e.tensor_copy(out=P["identb"][:], in_=P["ident"][:]), [P["ident"]], [P["identb"]])

    phase_mod(k, A, P)
    if STOP == "mod":
        k.finish()
        return nc
    for l in range(L):
        last = l == L - 1
        src_lat = A["x"] if l == 0 else A["res_d"][0:NLAT]
        src_ctx = A["ctx"] if l == 0 else A["res_d"][NLAT:NROW]
        dst_lat = A["y"] if last else A["res_d"][0:NLAT]
        phase_proj(k, A, P, l, src_lat, src_ctx)
        if STOP == "proj%d" % l:
            break
        phase_att(k, A, P, l, last)
        if STOP == "att%d" % l:
            break
        phase_ret(k, A, P, l, last)
        if STOP == "ret%d" % l:
            break
        phase_conv(k, A, P, l, last)
        if STOP == "conv%d" % l:
            break
        phase_out(k, A, P, l, last, src_lat, src_ctx, dst_lat)
        if STOP == "out%d" % l:
            break
        phase_moe(k, A, P, l, last, dst_lat)
        if STOP == "moe%d" % l:
            break
    k.finish()
    return nc


def phase_mod(k, A, P):
    with k.phase():
        cc = k.sb("cc", [4, D], F32)
        cs = k.sb("cs", [4, D], F32)
        csT = k.sb("csT", [128, 8, 4], F32R)
        gg = k.sb("gg", [4, D], F32)
        pt = k.ps("pt", [128, 8, 4], F32)
        pm = [k.ps("pm%d" % i, [4, 512], F32) for i in range(2)]
        ptm = k.ps("ptm", [128, 48, 4], F32)
        wm = [k.sb("wm%d" % i, [128, 8, 512], F32R) for i in range(2)]
        bm = k.sb("bm", [4, 6 * D], F32)
        modrow = k.sb("modrow", [4, 6 * D], F32)
        ident = P["ident"]
        k.V(lambda e: e.memset(cc[:], 0.0), [], [cc])
        k.dma("sp", lambda e: e.dma_start(out=cc[0:NS, :], in_=A["c"]), writes=[cc])
        k.dma("sp", lambda e: e.dma_start(out=cc[NS:NS + 1, :], in_=A["c_ctx"]), writes=[cc])
        k.A(lambda e: e.activation(out=cs[:], in_=cc[:], func=AF.Silu), [cc], [cs])
        for kc in range(8):
            k.T(lambda e, kc=kc: e.transpose(pt[:, kc, :], cs[0:4, kc * 128:(kc + 1) * 128], ident[0:4, 0:4]),
                [cs, ident], [pt])
        k.V(lambda e: e.tensor_copy(out=csT[:], in_=pt[:]), [pt], [csT])
        k.dma("sp", lambda e: e.dma_start(out=gg[0:2, :], in_=A["g_mix"]), writes=[gg])
        k.dma("sp", lambda e: e.dma_start(out=gg[2:4, :], in_=A["g_ffn"]), writes=[gg])
        for kc in range(8):
            k.T(lambda e, kc=kc: e.transpose(pt[:, kc, :], gg[0:4, kc * 128:(kc + 1) * 128], ident[0:4, 0:4]),
                [gg, ident], [pt])
        k.V(lambda e: e.tensor_copy(out=P["gT"][:], in_=pt[:]), [pt], [P["gT"]])
        for l in range(L):
            k.dma("sp", lambda e, l=l: e.dma_start(out=bm[:], in_=A["b_mod"][l, :].partition_broadcast(4)),
                  writes=[bm])
            wv = A["w_mod"][l].rearrange("(kc p) f -> p kc f", p=128)
            for j in range(12):
                w = wm[j % 2]
                k.dma("pool", lambda e, w=w, j=j, wv=wv: e.dma_start(out=w[:], in_=wv[:, :, j * 512:(j + 1) * 512]),
                      writes=[w])
                p = pm[j % 2]
                for kc in range(8):
                    k.T(lambda e, p=p, w=w, kc=kc: e.matmul(p[:], lhsT=csT[:, kc, :], rhs=w[:, kc, :],
                                                          start=(kc == 0), stop=(kc == 7)), [csT, w], [p])
                k.V(lambda e, p=p, j=j: e.tensor_tensor(out=modrow[:, j * 512:(j + 1) * 512], in0=p[:],
                                                        in1=bm[:, j * 512:(j + 1) * 512], op=ALU.add),
                    [p, bm], [modrow])
            k.dma("sp", lambda e, l=l: e.dma_start(out=A["mod_d"][l], in_=modrow[:]), reads=[modrow],
                  writes=["mod_d"])
            for j in range(48):
                k.T(lambda e, j=j: e.transpose(ptm[:, j, :], modrow[0:4, j * 128:(j + 1) * 128], ident[0:4, 0:4]),
                    [modrow, ident], [ptm])
            mT = P["modT"][l]
            k.V(lambda e, mT=mT: e.tensor_copy(out=mT[:], in_=ptm[:]), [ptm], [mT])
            gs = P["gsA"][l]
            k.V(lambda e, gs=gs, mT=mT: e.tensor_scalar(out=gs[:], in0=mT[:, 8:16, :], scalar1=1.0, scalar2=None,
                                                        op0=ALU.add), [mT], [gs])
            k.V(lambda e, gs=gs, l=l: e.tensor_tensor(out=gs[:], in0=gs[:],
                                                      in1=P["gT"][:, :, l:l + 1].to_broadcast([128, 8, 4]),
                                                      op=ALU.mult), [gs, P["gT"]], [gs])


def rms_rstd(k, xt, junk, ss, rstd, n):
    k.A(lambda e: e.activation(out=junk, in_=xt, func=AF.Square, accum_out=ss[:, 0:1]), [xt], [junk, ss])
    k.V(lambda e: e.tensor_scalar(out=ss[:, 0:1], in0=ss[:, 0:1], scalar1=1.0 / n, scalar2=EPS, op0=ALU.mult,
                                  op1=ALU.add), [ss], [ss])
    k.A(lambda e: e.sqrt(out=ss[:, 0:1], in_=ss[:, 0:1]), [ss], [ss])
    k.V(lambda e: e.reciprocal(out=rstd[:, 0:1], in_=ss[:, 0:1]), [ss], [rstd])


def phase_proj(k, A, P, l, src_lat, src_ctx):
    with k.phase():
        win = k.sb("win", [128, 8, 2304], BF16)
        wv = A["w_in"][l].rearrange("(kc p) f -> p kc f", p=128)
        for j in range(0, 2304, 384):
            k.dma("pool", lambda e, j=j: e.dma_start(out=win[:, :, j:j + 384], in_=wv[:, :, j:j + 384]),
                  writes=[("win", j)])
        winkeys = [("win", j) for j in range(0, 2304, 384)]
        NB = 8
        xts = [k.sb("xt%d" % i, [128, D], F32) for i in range(NB)]
        xn = [k.sb("xn%d" % i, [128, D], BF16) for i in range(NB)]
        junk = k.sb("junk", [128, D], F32)
        ss = [k.sb("ss%d" % i, [128, 1], F32) for i in range(NB)]
        rstd = [k.sb("rstd%d" % i, [128, 1], F32) for i in range(NB)]
        hT = [k.sb("hT%d" % i, [128, 8, 512], BF16) for i in range(2)]
        ptr = [k.ps("ptr%d" % i, [128, D], BF16) for i in range(2)]
        pu = [k.ps("pu%d" % i, [128, 512], F32) for i in range(3)]
        pc = [k.ps("pc%d" % i, [128, 512], F32) for i in range(2)]
        usb = [k.sb("usb%d" % i, [128, UC], F32) for i in range(2)]
        cvs = [k.sb("cvs%d" % i, [128, 4, 512], F32) for i in range(2)]
        identb = P["identb"]
        gs = P["gsA"][l]
        mT = P["modT"][l]
        groups = []
        for s in range(NS):
            groups.append((s, 0, 2, src_ctx[s * C:(s + 1) * C], NS))
            for g in range(4):
                groups.append((s, C + g * 512, 4, src_lat[s * S + g * 512: s * S + (g + 1) * 512], s))
        cnt = {"t": 0}

        def stats(gi):
            s, tok0, ntl, src, r = groups[gi]
            for t in range(ntl):
                i = (gi % 2) * 4 + t
                xt, xnn, sst, rs = xts[i], xn[i], ss[i], rstd[i]
                k.dma("sp", lambda e, xt=xt, src=src, t=t: e.dma_start(out=xt[:], in_=src[t * 128:(t + 1) * 128, :]),
                      writes=[xt])
                k.A(lambda e, xt=xt, sst=sst: e.activation(out=junk[:], in_=xt[:], func=AF.Square, accum_out=sst[:, 0:1]),
                    [xt], [junk, sst])
                k.A(lambda e, sst=sst: e.activation(out=sst[:, 0:1], in_=sst[:, 0:1], func=AF.Ln, scale=1.0 / D, bias=EPSB[:, 0:1]),
                    [sst, EPSB], [sst])
                k.A(lambda e, sst=sst, rs=rs: e.activation(out=rs[:, 0:1], in_=sst[:, 0:1], func=AF.Exp, scale=-0.5), [sst], [rs])
                k.V(lambda e, xnn=xnn, xt=xt, rs=rs: e.tensor_scalar(out=xnn[:], in0=xt[:], scalar1=rs[:, 0:1],
                                                                   scalar2=None, op0=ALU.mult), [xt, rs], [xnn])

        def trans(gi):
            s, tok0, ntl, src, r = groups[gi]
            h = hT[gi % 2]
            for t in range(ntl):
                i = (gi % 2) * 4 + t
                xnn = xn[i]
                pt = ptr[cnt["t"] % 2]
                cnt["t"] += 1
                for kc in range(8):
                    k.T(lambda e, pt=pt, xnn=xnn, kc=kc: e.transpose(pt[:, kc * 128:(kc + 1) * 128],
                                                                   xnn[:, kc * 128:(kc + 1) * 128], identb[:]),
                        [xnn, identb], [pt])
                for kc in range(8):
                    if kc % 2 == 0:
                        k.A(lambda e, h=h, pt=pt, kc=kc, t=t, r=r: e.activation(
                            out=h[:, kc, t * 128:(t + 1) * 128], in_=pt[:, kc * 128:(kc + 1) * 128],
                            func=AF.Identity, scale=gs[:, kc, r:r + 1], bias=mT[:, kc, r:r + 1]),
                            [pt, gs, mT], [h])
                    else:
                        k.V(lambda e, h=h, pt=pt, kc=kc, t=t, r=r: e.tensor_scalar(
                            out=h[:, kc, t * 128:(t + 1) * 128], in0=pt[:, kc * 128:(kc + 1) * 128],
                            scalar1=gs[:, kc, r:r + 1], scalar2=mT[:, kc, r:r + 1], op0=ALU.mult, op1=ALU.add),
                            [pt, gs, mT], [h])

        def mm(gi):
            s, tok0, ntl, src, r = groups[gi]
            h = hT[gi % 2]
            ntok = ntl * 128
            ei = 0
            for t in range(ntl):
                ub = usb[t % 2]
                for ci, (c0, w) in enumerate([(0, 512), (512, 512), (1024, 512), (1536, 256)]):
                    p = pu[ei % 3]
                    for kc in range(8):
                        k.T(lambda e, p=p, h=h, kc=kc, t=t, c0=c0, w=w: e.matmul(
                            p[:, 0:w], lhsT=h[:, kc, t * 128:(t + 1) * 128], rhs=win[:, kc, c0:c0 + w],
                            start=(kc == 0), stop=(kc == 7)), [h] + winkeys, [p])
                    if ei % 2 == 0:
                        k.A(lambda e, ub=ub, p=p, c0=c0, w=w: e.copy(out=ub[:, c0:c0 + w], in_=p[:, 0:w]), [p], [ub])
                    else:
                        k.V(lambda e, ub=ub, p=p, c0=c0, w=w: e.tensor_copy(out=ub[:, c0:c0 + w], in_=p[:, 0:w]),
                            [p], [ub])
                    ei += 1
                k.dma("sp", lambda e, ub=ub, s=s, tok0=tok0, t=t: e.dma_start(
                    out=A["u_d"][s, tok0 + t * 128: tok0 + (t + 1) * 128, :], in_=ub[:]), reads=[ub], writes=["u_d"])
            cv = cvs[gi % 2]
            for j in range(4):
                p = pc[j % 2]
                for kc in range(8):
                    k.T(lambda e, p=p, h=h, kc=kc, j=j, ntok=ntok: e.matmul(
                        p[:, 0:ntok], lhsT=win[:, kc, UC + j * 128: UC + (j + 1) * 128], rhs=h[:, kc, 0:ntok],
                        start=(kc == 0), stop=(kc == 7)), [h] + winkeys, [p])
                if j % 2 == 0:
                    k.A(lambda e, cv=cv, p=p, j=j, ntok=ntok: e.copy(out=cv[:, j, 0:ntok], in_=p[:, 0:ntok]), [p], [cv])
                else:
                    k.V(lambda e, cv=cv, p=p, j=j, ntok=ntok: e.tensor_copy(out=cv[:, j, 0:ntok], in_=p[:, 0:ntok]),
                        [p], [cv])
            k.dma("act", lambda e, cv=cv, s=s, tok0=tok0, ntok=ntok: e.dma_start(
                out=A["cv_d"][s, :, tok0:tok0 + ntok].rearrange("(j p) t -> p j t", p=128), in_=cv[:, :, 0:ntok]),
                reads=[cv], writes=["cv_d"])

        EPSB = P["epsb"]
        ng = len(groups)
        stats(0)
        trans(0)
        for gi in range(ng):
            if gi + 1 < ng:
                stats(gi + 1)
            mm(gi)
            if gi + 1 < ng:
                trans(gi + 1)


def phase_att(k, A, P, l, last):
    with k.phase():
        ident = P["ident"]
        identb = P["identb"]
        attC = k.sb("attC", [128, 16, 64], F32)
        attS = k.sb("attS", [128, 16, 64], F32)
        k.dma("sp", lambda e: e.dma_start(out=attC[:], in_=A["attC"].rearrange("(t p) d -> p t d", p=128)), writes=[attC])
        k.dma("sp", lambda e: e.dma_start(out=attS[:], in_=A["attS"].rearrange("(t p) d -> p t d", p=128)), writes=[attS])
        gqk = k.sb("gqk", [128, 10, 64], F32)
        k.dma("sp", lambda e: e.dma_start(out=gqk[:, 0, :], in_=A["q_norm_g"][l, :].partition_broadcast(128)), writes=[gqk])
        k.dma("sp", lambda e: e.dma_start(out=gqk[:, 8, :], in_=A["k_norm_g"][l, :].partition_broadcast(128)), writes=[gqk])
        k.V(lambda e: e.tensor_scalar(out=gqk[:, 0, :], in0=gqk[:, 0, :], scalar1=0.125, scalar2=None, op0=ALU.mult),
            [gqk], [gqk])
        for h in range(1, 8):
            k.V(lambda e, h=h: e.tensor_copy(out=gqk[:, h, :], in_=gqk[:, 0, :]), [gqk], [gqk])
        k.V(lambda e: e.tensor_copy(out=gqk[:, 9, :], in_=gqk[:, 8, :]), [gqk], [gqk])
        sink = k.sb("sink", [128, 8], F32)
        k.dma("sp", lambda e: e.dma_start(out=sink[:], in_=A["att_sink"][l, :].partition_broadcast(128)), writes=[sink])
        k.A(lambda e: e.activation(out=sink[:], in_=sink[:], func=AF.Exp), [sink], [sink])
        maskb = k.sb("maskb", [128, 3, 128], BF16)
        k.V(lambda e: e.tensor_copy(out=maskb[:], in_=P["masks"][:]), [P["masks"]], [maskb])

        qT = k.sb("qT", [64, 8, NT * 128], BF16)
        kT = k.sb("kT", [64, 2, NT * 128], BF16)
        vx = k.sb("vx", [128, NT, 2, 65], BF16)
        k.V(lambda e: e.memset(vx[:], 1.0), [], [vx])
        uq = [k.sb("uq%d" % i, [128, 768], F32) for i in range(2)]
        sq = k.sb("sq", [128, 640], F32)
        ssq = k.sb("ssq", [128, 10], F32)
        qn = k.sb("qn", [128, 640], F32)
        t1 = k.sb("t1", [128, 640], F32)
        t2 = k.sb("t2", [128, 640], F32)
        qb = [k.sb("qb%d" % i, [128, 640], BF16) for i in range(2)]
        pq = [k.ps("pq%d" % i, [64, 8, 128], BF16) for i in range(2)]
        pkk = k.ps("pkk", [64, 2, 128], BF16)
        psc = [k.ps("psc%d" % i, [128, 512], F32) for i in range(2)]
        po = [k.ps("po%d" % i, [128, 4, 65], F32) for i in range(2)]
        Pm = [k.sb("Pm%d" % i, [128, 5, 512], BF16) for i in range(2)]
        den = k.sb("den", [128, 8], F32)
        ao = [k.sb("ao%d" % i, [128, 512], BF16) for i in range(2)]

        for s in range(NS):
            for t in range(NT):
                u = uq[t % 2]
                k.dma("sp", lambda e, u=u, s=s, t=t: e.dma_start(out=u[:], in_=A["u_d"][s, t * 128:(t + 1) * 128, 0:768]),
                      reads=["u_d"], writes=[u])
                k.G(lambda e, u=u: e.tensor_tensor(out=sq[:], in0=u[:, 0:640], in1=u[:, 0:640], op=ALU.mult), [u], [sq])
                k.V(lambda e: e.tensor_reduce(out=ssq[:], in_=sq[:].rearrange("p (h d) -> p h d", d=64), axis=AX.X,
                                              op=ALU.add), [sq], [ssq])
                k.V(lambda e: e.tensor_scalar(out=ssq[:], in0=ssq[:], scalar1=1.0 / 64, scalar2=EPS, op0=ALU.mult,
                                              op1=ALU.add), [ssq], [ssq])
                k.A(lambda e: e.sqrt(out=ssq[:], in_=ssq[:]), [ssq], [ssq])
                k.V(lambda e: e.reciprocal(out=ssq[:], in_=ssq[:]), [ssq], [ssq])
                k.V(lambda e, u=u: e.tensor_tensor(out=qn[:].rearrange("p (h d) -> p h d", d=64),
                                                   in0=u[:, 0:640].rearrange("p (h d) -> p h d", d=64),
                                                   in1=ssq[:].unsqueeze(2).to_broadcast([128, 10, 64]), op=ALU.mult),
                    [u, ssq], [qn])
                k.G(lambda e: e.tensor_tensor(out=qn[:], in0=qn[:], in1=gqk[:].rearrange("p h d -> p (h d)"), op=ALU.mult),
                    [qn, gqk], [qn])
                q_b = qb[t % 2]
                if t >= 2:
                    tl = t - 2
                    qv = qn[:].rearrange("p (h a x d) -> p h a x d", h=10, a=2, x=2, d=16)
                    t1v = t1[:].rearrange("p (h a x d) -> p h a x d", h=10, a=2, x=2, d=16)
                    t2v = t2[:].rearrange("p (h a x d) -> p h a x d", h=10, a=2, x=2, d=16)
                    Cv = attC[:, tl, :].rearrange("p (a x d) -> p a x d", a=2, x=2, d=16)
                    Sv = attS[:, tl, :].rearrange("p (a x d) -> p a x d", a=2, x=2, d=16)
                    for a in range(2):
                        for x in range(2):
                            k.V(lambda e, a=a, x=x, qv=qv, t1v=t1v, Cv=Cv: e.tensor_tensor(
                                out=t1v[:, :, a, x, :], in0=qv[:, :, a, x, :],
                                in1=Cv[:, a, x, :].unsqueeze(1).to_broadcast([128, 10, 16]), op=ALU.mult),
                                [qn, attC], [t1])
                            k.G(lambda e, a=a, x=x, qv=qv, t2v=t2v, Sv=Sv: e.tensor_tensor(
                                out=t2v[:, :, a, x, :], in0=qv[:, :, a, 1 - x, :],
                                in1=Sv[:, a, x, :].unsqueeze(1).to_broadcast([128, 10, 16]), op=ALU.mult),
                                [qn, attS], [t2])
                    k.V(lambda e, q_b=q_b: e.tensor_tensor(out=q_b[:], in0=t1[:], in1=t2[:], op=ALU.add), [t1, t2], [q_b])
                else:
                    k.V(lambda e, q_b=q_b: e.tensor_copy(out=q_b[:], in_=qn[:]), [qn], [q_b])
                k.G(lambda e, u=u, t=t: e.tensor_copy(out=vx[:, t, :, 0:64],
                                                      in_=u[:, 640:768].rearrange("p (h d) -> p h d", d=64)),
                    [u], [vx])
                p = pq[t % 2]
                for h in range(8):
                    k.T(lambda e, p=p, q_b=q_b, h=h: e.transpose(p[:, h, :], q_b[:, h * 64:(h + 1) * 64], identb[:]),
                        [q_b, identb], [p])
                for h in range(2):
                    k.T(lambda e, q_b=q_b, h=h: e.transpose(pkk[:, h, :], q_b[:, (8 + h) * 64:(9 + h) * 64], identb[:]),
                        [q_b, identb], [pkk])
                k.A(lambda e, p=p, t=t: e.copy(out=qT[:, :, t * 128:(t + 1) * 128], in_=p[:]), [p], [qT])
                k.V(lambda e, t=t: e.tensor_copy(out=kT[:, :, t * 128:(t + 1) * 128], in_=pkk[:]), [pkk], [kT])
            qblocks = list(range(2, NT)) + ([] if last else [0, 1])
            bi = 0
            for t in qblocks:
                if t >= 2:
                    kbl = []
                    if t - 1 >= 2:
                        kbl.append((t - 1, 1))
                    kbl.append((t, None))
                    if t + 1 < NT:
                        kbl.append((t + 1, 0))
                    kbl += [(0, None), (1, None)]
                else:
                    kbl = [(0, None), (1, None)]
                a_o = ao[bi % 2]
                for kh in range(2):
                    Pt = Pm[(bi * 2 + kh) % 2]
                    for bj, (kb_, mk) in enumerate(kbl):
                        ps = psc[bj % 2]
                        k.T(lambda e, ps=ps, kb_=kb_, kh=kh, t=t: e.matmul(
                            ps[:], lhsT=kT[:, kh, kb_ * 128:(kb_ + 1) * 128],
                            rhs=qT[:, kh * 4:(kh + 1) * 4, t * 128:(t + 1) * 128], start=True, stop=True),
                            [kT, qT], [ps])
                        k.A(lambda e, ps=ps, Pt=Pt, bj=bj: e.activation(out=Pt[:, bj, :], in_=ps[:], func=AF.Exp),
                            [ps], [Pt])
                        if mk is not None:
                            k.G(lambda e, Pt=Pt, bj=bj, mk=mk: e.tensor_tensor(
                                out=Pt[:, bj, :].rearrange("p (g q) -> p g q", g=4),
                                in0=Pt[:, bj, :].rearrange("p (g q) -> p g q", g=4),
                                in1=maskb[:, mk, :].unsqueeze(1).to_broadcast([128, 4, 128]), op=ALU.mult),
                                [Pt, maskb], [Pt])
                    pso = po[kh]
                    nb = len(kbl)
                    for g in range(4):
                        for bj, (kb_, mk) in enumerate(kbl):
                            k.T(lambda e, pso=pso, Pt=Pt, g=g, bj=bj, kb_=kb_, kh=kh, nb=nb: e.matmul(
                                pso[:, g, :], lhsT=Pt[:, bj, g * 128:(g + 1) * 128], rhs=vx[:, kb_, kh, :],
                                start=(bj == 0), stop=(bj == nb - 1)), [Pt, vx], [pso])
                    k.V(lambda e, pso=pso, kh=kh: e.tensor_tensor(out=den[:, kh * 4:(kh + 1) * 4], in0=pso[:, :, 64],
                                                                 in1=sink[:, kh * 4:(kh + 1) * 4], op=ALU.add),
                        [pso, sink], [den])
                    k.V(lambda e, kh=kh: e.reciprocal(out=den[:, kh * 4:(kh + 1) * 4], in_=den[:, kh * 4:(kh + 1) * 4]),
                        [den], [den])
                    k.V(lambda e, pso=pso, kh=kh, a_o=a_o: e.tensor_tensor(
                        out=a_o[:, kh * 256:(kh + 1) * 256].rearrange("p (g d) -> p g d", g=4),
                        in0=pso[:, :, 0:64], in1=den[:, kh * 4:(kh + 1) * 4].unsqueeze(2).to_broadcast([128, 4, 64]),
                        op=ALU.mult), [pso, den], [a_o])
                k.dma("act", lambda e, a_o=a_o, s=s, t=t: e.dma_start(
                    out=A["mixin_d"][s, t * 128:(t + 1) * 128, 0:512], in_=a_o[:]), reads=[a_o], writes=["mixin_d"])
                bi += 1


def phase_ret(k, A, P, l, last):
    k.pesync = True
    with k.phase():
        identb = P["identb"]
        retC = k.sb("retC", [128, NT, 64], F32)
        retS = k.sb("retS", [128, NT, 64], F32)
        k.dma("sp", lambda e: e.dma_start(out=retC[:], in_=A["retC"].rearrange("(t p) d -> p t d", p=128)), writes=[retC])
        k.dma("sp", lambda e: e.dma_start(out=retS[:], in_=A["retS"].rearrange("(t p) d -> p t d", p=128)), writes=[retS])
        lg = k.sb("lg", [128, 8], F32)
        k.dma("sp", lambda e: e.dma_start(out=lg[:], in_=A["ret_decay_logit"][l, :].partition_broadcast(128)), writes=[lg])
        k.A(lambda e: e.activation(out=lg[:], in_=lg[:], func=AF.Exp, scale=-1.0), [lg], [lg])
        k.A(lambda e: e.activation(out=lg[:], in_=lg[:], func=AF.Ln, bias=1.0), [lg], [lg])
        k.V(lambda e: e.tensor_scalar(out=lg[:], in0=lg[:], scalar1=-1.0, scalar2=None, op0=ALU.mult), [lg], [lg])
        dco = k.sb("dco", [128, 4], F32)
        k.dma("sp", lambda e: e.dma_start(out=dco[:], in_=A["dcoef"]), writes=[dco])
        DT = k.sb("DT", [128, 2, 2, 4], F32)
        for d_ in range(2):
            for qk in range(2):
                k.A(lambda e, d_=d_, qk=qk: e.activation(out=DT[:, d_, qk, :], in_=lg[:, d_ * 4:(d_ + 1) * 4], func=AF.Exp,
                                                         scale=dco[:, d_ * 2 + qk: d_ * 2 + qk + 1]), [lg, dco], [DT])
            k.V(lambda e, d_=d_: e.tensor_scalar(out=DT[:, d_, 1, :], in0=DT[:, d_, 1, :], scalar1=0.125, scalar2=None,
                                                 op0=ALU.mult), [DT], [DT])
        g128 = k.sb("g128", [128, 2, 2], F32)
        for d_ in range(2):
            for pr in range(2):
                for hh in range(2):
                    h = 2 * pr + hh
                    k.A(lambda e, d_=d_, pr=pr, hh=hh, h=h: e.activation(
                        out=g128[hh * 64:(hh + 1) * 64, d_, pr:pr + 1], in_=lg[hh * 64:(hh + 1) * 64, d_ * 4 + h: d_ * 4 + h + 1],
                        func=AF.Exp, scale=128.0), [lg], [g128])
        gn = k.sb("gn", [128, 256], F32)
        k.dma("sp", lambda e: e.dma_start(out=gn[:], in_=A["ret_norm_g"][l, :].partition_broadcast(128)), writes=[gn])
        maskb = k.sb("maskb", [128, 3, 128], BF16)
        k.V(lambda e: e.tensor_copy(out=maskb[:], in_=P["masks"][:]), [P["masks"]], [maskb])

        RT = k.sb("RT", [128, 2, 2, 2, NT * 128], BF16)
        KD = k.sb("KD", [128, NT, 2, 256], BF16)
        Vb = k.sb("Vb", [128, NT, 256], BF16)
        SG = k.sb("SG", [128, NT, 256], F32)
        OA = k.sb("OA", [128, NT, 256], F32)
        ur = [k.sb("ur%d" % i, [128, 1024], F32) for i in range(2)]
        t1 = k.sb("t1", [128, 512], F32)
        t2 = k.sb("t2", [128, 512], F32)
        rot = k.sb("rot", [128, 512], F32)
        vr = [k.sb("vr%d" % i, [128, 2, 2, 256], BF16) for i in range(2)]
        ptr = [k.ps("ptr%d" % i, [128, 8, 128], BF16) for i in range(2)]
        pst = [k.ps("pst%d" % i, [128, 4, 128], F32) for i in range(2)]
        pso = [k.ps("pso%d" % i, [128, 4, 64], F32) for i in range(2)]
        pkv = [k.ps("pkv%d" % i, [128, 2, 128], F32) for i in range(2)]
        STm = [k.sb("STm%d" % i, [128, 4, 128], BF16) for i in range(2)]
        Sst = [k.sb("Sst%d" % i, [128, 2, 128], F32) for i in range(2)]
        Sb = [k.sb("Sb%d" % i, [128, 2, 128], BF16) for i in range(2)]
        tmp = k.sb("tmp", [128, 2, 128], F32)
        mean = k.sb("mean", [128, 4], F32)
        cen = k.sb("cen", [128, 256], F32)
        sq = k.sb("sq", [128, 256], F32)
        var = k.sb("var", [128, 4], F32)
        ro = [k.sb("ro%d" % i, [128, 256], BF16) for i in range(2)]
        rof = k.sb("rof", [128, 256], F32)

        for s in range(NS):
            for t in range(NT):
                u = ur[t % 2]
                k.dma("sp", lambda e, u=u, s=s, t=t: e.dma_start(out=u[:], in_=A["u_d"][s, t * 128:(t + 1) * 128, 768:1792]),
                      reads=["u_d"], writes=[u])
                qk_v = u[:, 0:512].rearrange("p (h x d) -> p h x d", h=8, x=2, d=32)
                t1v = t1[:].rearrange("p (h x d) -> p h x d", h=8, x=2, d=32)
                t2v = t2[:].rearrange("p (h x d) -> p h x d", h=8, x=2, d=32)
                Cv = retC[:, t, :].rearrange("p (x d) -> p x d", x=2, d=32)
                Sv = retS[:, t, :].rearrange("p (x d) -> p x d", x=2, d=32)
                for x in range(2):
                    k.V(lambda e, x=x, qk_v=qk_v, t1v=t1v, Cv=Cv: e.tensor_tensor(
                        out=t1v[:, :, x, :], in0=qk_v[:, :, x, :], in1=Cv[:, x, :].unsqueeze(1).to_broadcast([128, 8, 32]),
                        op=ALU.mult), [u, retC], [t1])
                    k.G(lambda e, x=x, qk_v=qk_v, t2v=t2v, Sv=Sv: e.tensor_tensor(
                        out=t2v[:, :, x, :], in0=qk_v[:, :, 1 - x, :], in1=Sv[:, x, :].unsqueeze(1).to_broadcast([128, 8, 32]),
                        op=ALU.mult), [u, retS], [t2])
                k.V(lambda e: e.tensor_tensor(out=rot[:], in0=t1[:], in1=t2[:], op=ALU.add), [t1, t2], [rot])
                v_r = vr[t % 2]
                for d_ in range(2):
                    eng = k.V if d_ == 0 else k.G
                    eng(lambda e, d_=d_, v_r=v_r: e.tensor_tensor(
                        out=v_r[:, d_, :, :].rearrange("p q (h d) -> p (q h) d", d=64),
                        in0=rot[:].rearrange("p (qh d) -> p qh d", d=64),
                        in1=DT[:, d_, :, :].rearrange("p q h -> p (q h)").unsqueeze(2).to_broadcast([128, 8, 64]),
                        op=ALU.mult), [rot, DT], [v_r])
                    k.G(lambda e, d_=d_, v_r=v_r, t=t: e.tensor_copy(out=KD[:, t, d_, :], in_=v_r[:, d_, 1, :]), [v_r], [KD])
                k.V(lambda e, u=u, t=t: e.tensor_copy(out=Vb[:, t, :], in_=u[:, 512:768]), [u], [Vb])
                k.A(lambda e, u=u, t=t: e.activation(out=SG[:, t, :], in_=u[:, 768:1024], func=AF.Silu), [u], [SG])
                p = ptr[t % 2]
                for d_ in range(2):
                    for qk in range(2):
                        for pr in range(2):
                            k.T(lambda e, p=p, v_r=v_r, d_=d_, qk=qk, pr=pr: e.transpose(
                                p[:, d_ * 4 + qk * 2 + pr, :], v_r[:, d_, qk, pr * 128:(pr + 1) * 128], identb[:]),
                                [v_r, identb], [p])
                k.A(lambda e, p=p, t=t: e.copy(out=RT[:, :, :, :, t * 128:(t + 1) * 128].rearrange("p a b c t -> p (a b c) t"),
                                               in_=p[:]), [p], [RT])
            for d_ in range(2):
                order = list(range(NT)) if d_ == 0 else [1, 0] + list(range(NT - 1, 1, -1))
                mk = 0 if d_ == 0 else 2
                St = Sst[d_]
                Sbb = Sb[d_]
                k.V(lambda e, St=St: e.memset(St[:], 0.0), [], [St])
                k.V(lambda e, Sbb=Sbb: e.memset(Sbb[:], 0.0), [], [Sbb])
                for ci, t in enumerate(order):
                    pss = pst[ci % 2]
                    for h in range(4):
                        pr, hh = h // 2, h % 2
                        k.T(lambda e, pss=pss, h=h, pr=pr, hh=hh, t=t, d_=d_: e.matmul(
                            pss[:, h, :], lhsT=RT[hh * 64:(hh + 1) * 64, d_, 1, pr, t * 128:(t + 1) * 128],
                            rhs=RT[hh * 64:(hh + 1) * 64, d_, 0, pr, t * 128:(t + 1) * 128], start=True, stop=True),
                            [RT], [pss])
                    stm = STm[ci % 2]
                    k.V(lambda e, stm=stm, pss=pss, mk=mk: e.tensor_tensor(
                        out=stm[:], in0=pss[:], in1=maskb[:, mk, :].unsqueeze(1).to_broadcast([128, 4, 128]), op=ALU.mult),
                        [pss, maskb], [stm])
                    po_ = pso[ci % 2]
                    for h in range(4):
                        pr, hh = h // 2, h % 2
                        k.T(lambda e, po_=po_, stm=stm, h=h, t=t: e.matmul(
                            po_[:, h, :], lhsT=stm[:, h, :], rhs=Vb[:, t, h * 64:(h + 1) * 64], start=True, stop=False),
                            [stm, Vb], [po_])
                        k.T(lambda e, po_=po_, h=h, pr=pr, hh=hh, t=t, d_=d_, Sbb=Sbb: e.matmul(
                            po_[:, h, :], lhsT=RT[hh * 64:(hh + 1) * 64, d_, 0, pr, t * 128:(t + 1) * 128],
                            rhs=Sbb[hh * 64:(hh + 1) * 64, pr, hh * 64:(hh + 1) * 64], start=False, stop=True),
                            [RT, Sbb], [po_])
                    if d_ == 0:
                        k.A(lambda e, po_=po_, t=t: e.copy(out=OA[:, t, :], in_=po_[:].rearrange("p h d -> p (h d)")),
                            [po_], [OA])
                    else:
                        k.V(lambda e, po_=po_, t=t: e.tensor_tensor(out=OA[:, t, :], in0=OA[:, t, :],
                                                                    in1=po_[:].rearrange("p h d -> p (h d)"), op=ALU.add),
                            [po_, OA], [OA])
                    if ci == NT - 1:
                        continue
                    pk = pkv[ci % 2]
                    for pr in range(2):
                        k.T(lambda e, pk=pk, pr=pr, t=t, d_=d_: e.matmul(
                            pk[:, pr, :], lhsT=KD[:, t, d_, pr * 128:(pr + 1) * 128], rhs=Vb[:, t, pr * 128:(pr + 1) * 128],
                            start=True, stop=True), [KD, Vb], [pk])
                    k.V(lambda e, pk=pk, St=St: e.tensor_tensor(out=tmp[:], in0=pk[:], in1=St[:], op=ALU.add), [pk, St], [tmp])
                    for pr in range(2):
                        k.V(lambda e, pr=pr, St=St, d_=d_: e.tensor_scalar(out=St[:, pr, :], in0=tmp[:, pr, :],
                                                                          scalar1=g128[:, d_, pr:pr + 1], scalar2=None,
                                                                          op0=ALU.mult), [tmp, g128], [St])
                        k.A(lambda e, pr=pr, Sbb=Sbb, d_=d_: e.activation(out=Sbb[:, pr, :], in_=tmp[:, pr, :], func=AF.Copy,
                                                                         scale=g128[:, d_, pr:pr + 1]), [tmp, g128], [Sbb])
            tiles = list(range(2, NT)) + ([] if last else [0, 1])
            for i_, t in enumerate(tiles):
                o3 = OA[:, t, :].rearrange("p (h d) -> p h d", d=64)
                k.V(lambda e, o3=o3: e.tensor_reduce(out=mean[:], in_=o3, axis=AX.X, op=ALU.add), [OA], [mean])
                k.V(lambda e: e.tensor_scalar(out=mean[:], in0=mean[:], scalar1=1.0 / 64, scalar2=None, op0=ALU.mult),
                    [mean], [mean])
                k.V(lambda e, o3=o3: e.tensor_tensor(out=cen[:].rearrange("p (h d) -> p h d", d=64), in0=o3,
                                                     in1=mean[:].unsqueeze(2).to_broadcast([128, 4, 64]), op=ALU.subtract),
                    [OA, mean], [cen])
                k.G(lambda e: e.tensor_tensor(out=sq[:], in0=cen[:], in1=cen[:], op=ALU.mult), [cen], [sq])
                k.V(lambda e: e.tensor_reduce(out=var[:], in_=sq[:].rearrange("p (h d) -> p h d", d=64), axis=AX.X,
                                              op=ALU.add), [sq], [var])
                k.V(lambda e: e.tensor_scalar(out=var[:], in0=var[:], scalar1=1.0 / 64, scalar2=EPS, op0=ALU.mult,
                                              op1=ALU.add), [var], [var])
                k.A(lambda e: e.sqrt(out=var[:], in_=var[:]), [var], [var])
                k.V(lambda e: e.reciprocal(out=var[:], in_=var[:]), [var], [var])
                r_o = ro[i_ % 2]
                k.V(lambda e: e.tensor_tensor(out=rof[:].rearrange("p (h d) -> p h d", d=64),
                                              in0=cen[:].rearrange("p (h d) -> p h d", d=64),
                                              in1=var[:].unsqueeze(2).to_broadcast([128, 4, 64]), op=ALU.mult),
                    [cen, var], [rof])
                k.G(lambda e: e.tensor_tensor(out=rof[:], in0=rof[:], in1=gn[:], op=ALU.mult), [rof, gn], [rof])
                k.V(lambda e, r_o=r_o, t=t: e.tensor_tensor(out=r_o[:], in0=rof[:], in1=SG[:, t, :], op=ALU.mult),
                    [rof, SG], [r_o])
                k.dma("act", lambda e, r_o=r_o, s=s, t=t: e.dma_start(
                    out=A["mixin_d"][s, t * 128:(t + 1) * 128, 512:768], in_=r_o[:]), reads=[r_o], writes=["mixin_d"])
    k.pesync = False


def phase_conv(k, A, P, l, last):
    with k.phase():
        ident = P["ident"]
        identb = P["identb"]
        cw = k.sb("cw", [31, 256], F32)
        k.dma("sp", lambda e: e.dma_start(out=cw[:], in_=A["conv_w"][l]), writes=[cw])
        pw = k.ps("pw", [128, 2, 32], F32)
        for cc in range(2):
            k.T(lambda e, cc=cc: e.transpose(pw[:, cc, 0:31], cw[0:31, cc * 128:(cc + 1) * 128], ident[0:31, 0:31]),
                [cw, ident], [pw])
        cwT = k.sb("cwT", [128, 2, 32], F32)
        k.V(lambda e: e.tensor_copy(out=cwT[:, :, 0:31], in_=pw[:, :, 0:31]), [pw], [cwT])
        diag = k.sb("diag", [128, 2, 31, 128], BF16)
        for cc in range(2):
            for kk in range(31):
                eng = k.V if kk % 2 == 0 else k.G
                eng(lambda e, cc=cc, kk=kk: e.tensor_scalar(out=diag[:, cc, kk, :], in0=ident[:],
                                                            scalar1=cwT[:, cc, kk:kk + 1], scalar2=None, op0=ALU.mult),
                    [ident, cwT], [diag])
        vec = k.sb("vec", [3, 256], F32)
        k.dma("sp", lambda e: e.dma_start(out=vec[0:1, :], in_=A["conv_b"][l:l + 1, :]), writes=[vec])
        k.dma("sp", lambda e: e.dma_start(out=vec[1:2, :], in_=A["conv_norm_g"][l:l + 1, :]), writes=[vec])
        k.dma("sp", lambda e: e.dma_start(out=vec[2:3, :], in_=A["conv_norm_b"][l:l + 1, :]), writes=[vec])
        pv = k.ps("pv", [128, 2, 4], F32)
        for cc in range(2):
            k.T(lambda e, cc=cc: e.transpose(pv[:, cc, 0:3], vec[0:3, cc * 128:(cc + 1) * 128], ident[0:3, 0:3]),
                [vec, ident], [pv])
        vT = k.sb("vT", [128, 2, 4], F32)
        k.V(lambda e: e.tensor_copy(out=vT[:, :, 0:3], in_=pv[:, :, 0:3]), [pv], [vT])
        ones = k.sb("ones", [128, 128], F32R)
        onesf = k.sb("onesf", [128, 128], F32)
        k.V(lambda e: e.memset(onesf[:], 1.0 / 256), [], [onesf])
        k.V(lambda e: e.tensor_copy(out=ones[:], in_=onesf[:]), [onesf], [ones])

        LP = S + 30
        hp = [k.sb("hp%d" % i, [128, LP], BF16) for i in range(2)]
        val = [k.sb("val%d" % i, [128, S], F32) for i in range(2)]
        gt = [k.sb("gt%d" % i, [128, S], F32) for i in range(2)]
        cvo = [k.sb("cvo%d" % i, [128, 512], F32R) for i in range(2)]
        csq = [k.sb("csq%d" % i, [128, 512], F32R) for i in range(2)]
        pcv = [k.ps("pcv%d" % i, [128, 512], F32) for i in range(2)]
        pmean = k.ps("pmean", [128, 512], F32)
        pex2 = k.ps("pex2", [128, 512], F32)
        msb = k.sb("msb", [128, 512], F32)
        rsd = k.sb("rsd", [128, 512], F32)
        yv = [k.sb("yv%d" % i, [128, 512], F32) for i in range(2)]
        yo = [k.sb("yo%d" % i, [128, 512], BF16) for i in range(2)]
        for s in range(NS):
            segs = [(C, S)] + ([] if last else [(0, C)])
            for (tok0, Lg) in segs:
                for cc in range(2):
                    k.dma("sp", lambda e, cc=cc, s=s, tok0=tok0, Lg=Lg: e.dma_start(
                        out=val[cc][:, 0:Lg], in_=A["cv_d"][s, cc * 128:(cc + 1) * 128, tok0:tok0 + Lg]),
                        reads=["cv_d"], writes=[val[cc]])
                    k.dma("act", lambda e, cc=cc, s=s, tok0=tok0, Lg=Lg: e.dma_start(
                        out=gt[cc][:, 0:Lg], in_=A["cv_d"][s, 256 + cc * 128: 256 + (cc + 1) * 128, tok0:tok0 + Lg]),
                        reads=["cv_d"], writes=[gt[cc]])
                    k.A(lambda e, cc=cc, Lg=Lg: e.activation(out=gt[cc][:, 0:Lg], in_=gt[cc][:, 0:Lg], func=AF.Sigmoid),
                        [gt[cc]], [gt[cc]])
                    k.G(lambda e, cc=cc: e.memset(hp[cc][:], 0.0), [], [hp[cc]])
                    k.V(lambda e, cc=cc, Lg=Lg: e.tensor_tensor(out=hp[cc][:, 15:15 + Lg], in0=val[cc][:, 0:Lg],
                                                               in1=gt[cc][:, 0:Lg], op=ALU.mult),
                        [val[cc], gt[cc]], [hp[cc]])
                nb = (Lg + 511) // 512
                for b in range(nb):
                    w = min(512, Lg - b * 512)
                    for cc in range(2):
                        p = pcv[cc]
                        for kk in range(31):
                            k.T(lambda e, p=p, cc=cc, kk=kk, b=b, w=w: e.matmul(
                                p[:, 0:w], lhsT=diag[:, cc, kk, :], rhs=hp[cc][:, b * 512 + kk: b * 512 + kk + w],
                                start=(kk == 0), stop=(kk == 30)), [diag, hp[cc]], [p])
                        k.A(lambda e, p=p, cc=cc, w=w: e.activation(out=cvo[cc][:, 0:w], in_=p[:, 0:w], func=AF.Identity,
                                                                   bias=vT[:, cc, 0:1], scale=1.0), [p, vT], [cvo[cc]])
                        k.V(lambda e, cc=cc, w=w: e.tensor_tensor(out=csq[cc][:, 0:w], in0=cvo[cc][:, 0:w].bitcast(F32),
                                                                 in1=cvo[cc][:, 0:w].bitcast(F32), op=ALU.mult),
                            [cvo[cc]], [csq[cc]])
                    for cc in range(2):
                        k.T(lambda e, cc=cc, w=w: e.matmul(pmean[:, 0:w], lhsT=ones[:], rhs=cvo[cc][:, 0:w],
                                                           start=(cc == 0), stop=(cc == 1)), [ones, cvo[cc]], [pmean])
                    for cc in range(2):
                        k.T(lambda e, cc=cc, w=w: e.matmul(pex2[:, 0:w], lhsT=ones[:], rhs=csq[cc][:, 0:w],
                                                           start=(cc == 0), stop=(cc == 1)), [ones, csq[cc]], [pex2])
                    k.A(lambda e, w=w: e.copy(out=msb[:, 0:w], in_=pmean[:, 0:w]), [pmean], [msb])
                    k.V(lambda e, w=w: e.tensor_tensor(out=rsd[:, 0:w], in0=msb[:, 0:w], in1=msb[:, 0:w], op=ALU.mult),
                        [msb], [rsd])
                    k.V(lambda e, w=w: e.tensor_tensor(out=rsd[:, 0:w], in0=pex2[:, 0:w], in1=rsd[:, 0:w], op=ALU.subtract),
                        [pex2, rsd], [rsd])
                    k.V(lambda e, w=w: e.tensor_scalar(out=rsd[:, 0:w], in0=rsd[:, 0:w], scalar1=EPS, scalar2=None,
                                                       op0=ALU.add), [rsd], [rsd])
                    k.A(lambda e, w=w: e.sqrt(out=rsd[:, 0:w], in_=rsd[:, 0:w]), [rsd], [rsd])
                    k.V(lambda e, w=w: e.reciprocal(out=rsd[:, 0:w], in_=rsd[:, 0:w]), [rsd], [rsd])
                    for cc in range(2):
                        k.G(lambda e, cc=cc, w=w: e.tensor_tensor(out=yv[cc][:, 0:w], in0=cvo[cc][:, 0:w].bitcast(F32),
                                                                 in1=msb[:, 0:w], op=ALU.subtract), [cvo[cc], msb], [yv[cc]])
                        k.V(lambda e, cc=cc, w=w: e.tensor_tensor(out=yv[cc][:, 0:w], in0=yv[cc][:, 0:w], in1=rsd[:, 0:w],
                                                                 op=ALU.mult), [yv[cc], rsd], [yv[cc]])
                        k.A(lambda e, cc=cc, w=w: e.activation(out=yo[cc][:, 0:w], in_=yv[cc][:, 0:w], func=AF.Silu,
                                                               scale=vT[:, cc, 1:2], bias=vT[:, cc, 2:3]),
                            [yv[cc], vT], [yo[cc]])
                        k.dma("act", lambda e, cc=cc, s=s, tok0=tok0, b=b, w=w: e.dma_start(
                            out=A["convT_d"][s, cc * 128:(cc + 1) * 128, tok0 + b * 512: tok0 + b * 512 + w],
                            in_=yo[cc][:, 0:w]), reads=[yo[cc]], writes=["convT_d"])


def phase_out(k, A, P, l, last, src_lat, src_ctx, dst_lat):
    with k.phase():
        ident = P["ident"]
        identb = P["identb"]
        EPSB = P["epsb"]
        wout = k.sb("wout", [128, 8, D], BF16)
        wv = A["w_out"][l].rearrange("(kc p) f -> p kc f", p=128)
        for j in range(0, D, 512):
            k.dma("pool", lambda e, j=j: e.dma_start(out=wout[:, :, j:j + 512], in_=wv[:, :, j:j + 512]), writes=[("wout", j)])
        woutk = [("wout", 0), ("wout", 512)]
        wr = k.sb("wr", [128, 8, E], F32)
        k.dma("sp", lambda e: e.dma_start(out=wr[:], in_=A["w_router"][l].rearrange("(kc p) e -> p kc e", p=128)),
              writes=[wr])
        nr = NS + (0 if last else 1)
        gta = [k.sb("gta%d" % r, [128, D], F32) for r in range(nr)]
        gsf = [k.sb("gsf%d" % r, [128, D], F32) for r in range(nr)]
        shf = [k.sb("shf%d" % r, [128, D], F32) for r in range(nr)]
        gf = k.sb("gf", [128, D], F32)
        k.dma("sp", lambda e: e.dma_start(out=gf[:], in_=A["g_ffn"][l, :].partition_broadcast(128)), writes=[gf])
        for r in range(nr):
            k.dma("sp", lambda e, r=r: e.dma_start(out=gta[r][:], in_=A["mod_d"][l, r, 2 * D:3 * D].partition_broadcast(128)),
                  reads=["mod_d"], writes=[gta[r]])
            k.dma("sp", lambda e, r=r: e.dma_start(out=shf[r][:], in_=A["mod_d"][l, r, 3 * D:4 * D].partition_broadcast(128)),
                  reads=["mod_d"], writes=[shf[r]])
            k.dma("sp", lambda e, r=r: e.dma_start(out=gsf[r][:], in_=A["mod_d"][l, r, 4 * D:5 * D].partition_broadcast(128)),
                  reads=["mod_d"], writes=[gsf[r]])
            k.V(lambda e, r=r: e.scalar_tensor_tensor(out=gsf[r][:], in0=gsf[r][:], scalar=1.0, in1=gf[:], op0=ALU.add,
                                                      op1=ALU.mult), [gsf[r], gf], [gsf[r]])
        NB = 6
        mi = [k.sb("mi%d" % i, [128, 768], BF16) for i in range(3)]
        mixT = [k.sb("mixT%d" % i, [128, 8, 128], BF16) for i in range(4)]
        xt = [k.sb("xt%d" % i, [128, D], F32) for i in range(5)]
        xm = [k.sb("xm%d" % i, [128, D], F32) for i in range(5)]
        hf = [k.sb("hf%d" % i, [128, D], F32) for i in range(4)]
        hfb = [k.sb("hfb%d" % i, [128, D], BF16) for i in range(2)]
        junk = k.sb("junk", [128, D], BF16)
        ss = [k.sb("ss%d" % i, [128, 1], F32) for i in range(NB)]
        rstd = [k.sb("rstd%d" % i, [128, 1], F32) for i in range(NB)]
        hfT = [k.sb("hfT%d" % i, [128, 8, 128], F32) for i in range(2)]
        ptm = k.ps("ptm", [128, 6, 128], BF16)
        py = [k.ps("py%d" % i, [128, 512], F32) for i in range(2)]
        pth = k.ps("pth", [128, 8, 128], F32)
        plg = k.ps("plg", [128, 16], F32)
        pat = k.ps("pat", [48, 128], F32)
        lmax = [k.sb("lmax%d" % i, [128, 1], F32) for i in range(2)]
        lsum = [k.sb("lsum%d" % i, [128, 1], F32) for i in range(2)]
        afftm = [k.sb("afftm%d" % i, [128, 48], F32) for i in range(3)]
        affT = k.sb("affT", [48, S + C], F32)
        for a_ in afftm:
            k.V(lambda e, a_=a_: e.memset(a_[:], 0.0), [], [a_])
        tiles = list(range(2, NT)) + ([] if last else [0, 1])
        items = [(t, s) for t in tiles for s in range(NS)]

        def info(i):
            t, s = items[i]
            isctx = t < 2
            r = NS if isctx else s
            if isctx:
                src = src_ctx[s * C + t * 128: s * C + (t + 1) * 128, :]
                dst = A["res_d"][NLAT + s * C + t * 128: NLAT + s * C + (t + 1) * 128, :]
                hrow = NLAT + s * C + t * 128
            else:
                src = src_lat[s * S + (t - 2) * 128: s * S + (t - 1) * 128, :]
                dst = dst_lat[s * S + (t - 2) * 128: s * S + (t - 1) * 128, :]
                hrow = s * S + (t - 2) * 128
            return t, s, r, src, dst, hrow

        NX, NM, NH = 5, 4, 4

        def stL(i):
            t, s, r, src, dst, hrow = info(i)
            m, mt, x_t = mi[i % 3], mixT[i % NM], xt[i % NX]
            k.dma("sp", lambda e: e.dma_start(out=m[:], in_=A["mixin_d"][s, t * 128:(t + 1) * 128, :]),
                  reads=["mixin_d"], writes=[m])
            k.dma("sp", lambda e: e.dma_start(
                out=mt[:, 6:8, :], in_=A["convT_d"][s, :, t * 128:(t + 1) * 128].rearrange("(c p) t -> p c t", p=128)),
                reads=["convT_d"], writes=[mt])
            k.dma("act", lambda e: e.dma_start(out=x_t[:], in_=src), writes=[x_t])

        def stA1(i):
            m, mt = mi[i % 3], mixT[i % NM]
            for j in range(6):
                k.T(lambda e, j=j: e.transpose(ptm[:, j, :], m[:, j * 128:(j + 1) * 128], identb[:]), [m, identb], [ptm])
            k.A(lambda e: e.copy(out=mt[:, 0:6, :], in_=ptm[:]), [ptm], [mt])

        def stA2(i):
            t, s, r, src, dst, hrow = info(i)
            mt, x_m = mixT[i % NM], xm[i % NX]
            for hh in range(2):
                p = py[hh]
                for kc in range(8):
                    k.T(lambda e, p=p, kc=kc, hh=hh: e.matmul(p[:], lhsT=mt[:, kc, :], rhs=wout[:, kc, hh * 512:(hh + 1) * 512],
                                                              start=(kc == 0), stop=(kc == 7)), [mt] + woutk, [p])
                k.V(lambda e, p=p, hh=hh: e.tensor_tensor(out=x_m[:, hh * 512:(hh + 1) * 512], in0=p[:],
                                                          in1=gta[r][:, hh * 512:(hh + 1) * 512], op=ALU.mult),
                    [p, gta[r]], [x_m])

        def stA3(i):
            t, s, r, src, dst, hrow = info(i)
            x_t, x_m = xt[i % NX], xm[i % NX]
            k.G(lambda e: e.tensor_tensor(out=x_m[:], in0=x_m[:], in1=x_t[:], op=ALU.add), [x_m, x_t], [x_m])
            k.dma("sp", lambda e: e.dma_start(out=dst, in_=x_m[:]), reads=[x_m], writes=["res"])

        def stB1(i):
            x_m, sst, rs = xm[i % NX], ss[i % NX], rstd[i % NX]
            k.A(lambda e: e.activation(out=junk[:], in_=x_m[:], func=AF.Square, accum_out=sst[:, 0:1]), [x_m], [junk, sst])
            k.A(lambda e: e.activation(out=sst[:, 0:1], in_=sst[:, 0:1], func=AF.Ln, scale=1.0 / D, bias=EPSB[:, 0:1]),
                [sst, EPSB], [sst])
            k.A(lambda e: e.activation(out=rs[:, 0:1], in_=sst[:, 0:1], func=AF.Exp, scale=-0.5), [sst], [rs])

        def stB2(i):
            t, s, r, src, dst, hrow = info(i)
            x_m, h_f, rs = xm[i % NX], hf[i % NH], rstd[i % NX]
            k.V(lambda e: e.scalar_tensor_tensor(out=h_f[:], in0=x_m[:], scalar=rs[:, 0:1], in1=gsf[r][:], op0=ALU.mult,
                                                 op1=ALU.mult), [x_m, rs, gsf[r]], [h_f])

        def stB3(i):
            t, s, r, src, dst, hrow = info(i)
            h_f = hf[i % NH]
            k.G(lambda e: e.tensor_tensor(out=h_f[:], in0=h_f[:], in1=shf[r][:], op=ALU.add), [h_f, shf[r]], [h_f])

        def stB4(i):
            t, s, r, src, dst, hrow = info(i)
            h_f, h_b = hf[i % NH], hfb[i % 2]
            for kc in range(8):
                k.T(lambda e, kc=kc: e.transpose(pth[:, kc, :], h_f[:, kc * 128:(kc + 1) * 128], ident[:]), [h_f, ident], [pth])
            k.A(lambda e: e.copy(out=h_b[:], in_=h_f[:]), [h_f], [h_b])
            k.dma("act", lambda e: e.dma_start(out=A["hfb_d"][hrow:hrow + 128, :], in_=h_b[:]), reads=[h_b], writes=["hfb_d"])

        def stC1(i):
            h_T = hfT[i % 2]
            k.A(lambda e: e.copy(out=h_T[:], in_=pth[:]), [pth], [h_T])

        def stC2(i):
            h_T = hfT[i % 2]
            for kc in range(8):
                k.T(lambda e, kc=kc: e.matmul(plg[:], lhsT=h_T[:, kc, :], rhs=wr[:, kc, :], start=(kc == 0), stop=(kc == 7)),
                    [h_T, wr], [plg])
            lm = lmax[i % 2]
            k.V(lambda e: e.tensor_reduce(out=lm[:], in_=plg[:], axis=AX.X, op=ALU.max, negate=True), [plg], [lm])

        def stC3(i):
            t, s, r, src, dst, hrow = info(i)
            af = afftm[(i // NS) % 3]
            lm, ls = lmax[i % 2], lsum[i % 2]
            k.A(lambda e: e.activation(out=af[:, s * 32:s * 32 + 16], in_=plg[:], func=AF.Exp, bias=lm[:, 0:1],
                                       scale=1.0, accum_out=ls[:, 0:1]), [plg, lm], [af, ls])

        def stC4(i):
            t, s, r, src, dst, hrow = info(i)
            af = afftm[(i // NS) % 3]
            ls = lsum[i % 2]
            k.V(lambda e: e.reciprocal(out=ls[:], in_=ls[:]), [ls], [ls])
            k.V(lambda e: e.tensor_scalar(out=af[:, s * 32:s * 32 + 16], in0=af[:, s * 32:s * 32 + 16],
                                          scalar1=ls[:, 0:1], scalar2=None, op0=ALU.mult), [af, ls], [af])
            if s == NS - 1:
                k.T(lambda e: e.transpose(pat[:], af[:], ident[:]), [af, ident], [pat])

        def stC5(i):
            t, s, r, src, dst, hrow = info(i)
            if s == NS - 1:
                k.A(lambda e: e.copy(out=affT[:, t * 128:(t + 1) * 128], in_=pat[:]), [pat], [affT])

        stages = [stL, stA1, stA2, stA3, stB1, stB2, stB3, stB4, stC1, stC2, stC3, stC4, stC5]
        n = len(items)
        ns = len(stages)
        for step in range(n + ns - 1):
            for j in range(ns - 1, -1, -1):
                i = step - j
                if 0 <= i < n:
                    stages[j](i)
        k.dma("sp", lambda e: e.dma_start(out=A["aff_d"], in_=affT[:, C:C + S]), reads=[affT], writes=["aff_d"])
        if not last:
            k.dma("sp", lambda e: e.dma_start(out=A["affc_d"], in_=affT[:, 0:C]), reads=[affT], writes=["affc_d"])


def phase_moe(k, A, P, l, last, dst_lat):
    with k.phase():
        ident = P["ident"]
        nctx = 0 if last else NS
        NSL = NS * CAPL + nctx * CAPC
        if last:
            cgroups = [(0, 512)]
        else:
            cgroups = [(0, 288), (288, 288)]
        idxT = k.sb("idxT", [128, 2, 48], I32)
        gT = k.sb("gTm", [128, 2, 48], F32)
        if not last:
            idcT = k.sb("idcT", [32, 48], I32)
            gcT = k.sb("gcT", [32, 48], F32)
        with k.phase():
            wa = k.sb("wa", [48, S], F32)
            wb = k.sb("wb", [48, S], F32)
            vals = k.sb("vals", [48, CAPL], F32)
            idx = k.sb("idx", [48, CAPL], U32)
            idxf = k.sb("idxf", [48, CAPL], F32)
            offs = k.sb("offs", [48, 1], F32)
            k.dma("sp", lambda e: e.dma_start(out=wa[:], in_=A["aff_d"]), reads=["aff_d"], writes=[wa])
            cur, oth = wa, wb
            for r in range(CAPL // 8):
                k.V(lambda e, cur=cur, r=r: e.max(out=vals[:, r * 8:(r + 1) * 8], in_=cur[:]), [cur], [vals])
                k.V(lambda e, cur=cur, r=r: e.max_index(out=idx[:, r * 8:(r + 1) * 8], in_max=vals[:, r * 8:(r + 1) * 8],
                                                        in_values=cur[:]), [cur, vals], [idx])
                if r < CAPL // 8 - 1:
                    k.V(lambda e, cur=cur, oth=oth, r=r: e.match_replace(out=oth[:], in_to_replace=vals[:, r * 8:(r + 1) * 8],
                                                                         in_values=cur[:], imm_value=-1.0), [cur, vals], [oth])
                    cur, oth = oth, cur
            k.V(lambda e: e.memset(offs[0:32, :], 0.0), [], [offs])
            k.V(lambda e: e.memset(offs[32:48, :], float(S)), [], [offs])
            k.V(lambda e: e.tensor_copy(out=idxf[:], in_=idx[:]), [idx], [idxf])
            k.V(lambda e: e.tensor_scalar(out=idxf[:], in0=idxf[:], scalar1=offs[:, 0:1], scalar2=None, op0=ALU.add),
                [idxf, offs], [idxf])
            pti = k.ps("pti", [128, 2, 48], F32)
            ptg = k.ps("ptg", [128, 2, 48], F32)
            for j in range(2):
                k.T(lambda e, j=j: e.transpose(pti[:, j, :], idxf[:, j * 128:(j + 1) * 128], ident[0:48, 0:48]), [idxf, ident], [pti])
                k.T(lambda e, j=j: e.transpose(ptg[:, j, :], vals[:, j * 128:(j + 1) * 128], ident[0:48, 0:48]), [vals, ident], [ptg])
            k.V(lambda e: e.tensor_copy(out=idxT[:], in_=pti[:]), [pti], [idxT])
            k.V(lambda e: e.tensor_copy(out=gT[:], in_=ptg[:]), [ptg], [gT])
            if not last:
                wc = k.sb("wc", [48, C], F32)
                wd_ = k.sb("wd_", [48, C], F32)
                valc = k.sb("valc", [48, CAPC], F32)
                idc = k.sb("idc", [48, CAPC], U32)
                idcf = k.sb("idcf", [48, CAPC], F32)
                offc = k.sb("offc", [48, 1], F32)
                k.dma("sp", lambda e: e.dma_start(out=wc[:], in_=A["affc_d"]), reads=["affc_d"], writes=[wc])
                cur, oth = wc, wd_
                for r in range(CAPC // 8):
                    k.V(lambda e, cur=cur, r=r: e.max(out=valc[:, r * 8:(r + 1) * 8], in_=cur[:]), [cur], [valc])
                    k.V(lambda e, cur=cur, r=r: e.max_index(out=idc[:, r * 8:(r + 1) * 8], in_max=valc[:, r * 8:(r + 1) * 8],
                                                            in_values=cur[:]), [cur, valc], [idc])
                    if r < CAPC // 8 - 1:
                        k.V(lambda e, cur=cur, oth=oth, r=r: e.match_replace(out=oth[:], in_to_replace=valc[:, r * 8:(r + 1) * 8],
                                                                             in_values=cur[:], imm_value=-1.0), [cur, valc], [oth])
                        cur, oth = oth, cur
                k.V(lambda e: e.memset(offc[0:32, :], float(NLAT)), [], [offc])
                k.V(lambda e: e.memset(offc[32:48, :], float(NLAT + C)), [], [offc])
                k.V(lambda e: e.tensor_copy(out=idcf[:], in_=idc[:]), [idc], [idcf])
                k.V(lambda e: e.tensor_scalar(out=idcf[:], in0=idcf[:], scalar1=offc[:, 0:1], scalar2=None, op0=ALU.add),
                    [idcf, offc], [idcf])
                ptic = k.ps("ptic", [32, 2, 48], F32)
                k.T(lambda e: e.transpose(ptic[:, 0, :], idcf[:, 0:32], ident[0:48, 0:48]), [idcf, ident], [ptic])
                k.T(lambda e: e.transpose(ptic[:, 1, :], valc[:, 0:32], ident[0:48, 0:48]), [valc, ident], [ptic])
                k.V(lambda e: e.tensor_copy(out=idcT[:], in_=ptic[:, 0, :]), [ptic], [idcT])
                k.V(lambda e: e.tensor_copy(out=gcT[:], in_=ptic[:, 1, :]), [ptic], [gcT])
        identb = P["identb"]
        nr = NS + (0 if last else 1)
        gtf = [k.sb("gtf%d" % r, [128, D], F32) for r in range(nr)]
        for r in range(nr):
            k.dma("sp", lambda e, r=r: e.dma_start(out=gtf[r][:], in_=A["mod_d"][l, r, 5 * D:6 * D].partition_broadcast(128)),
                  reads=["mod_d"], writes=[gtf[r]])
        xe = [k.sb("xe%d" % i, [128, D], BF16) for i in range(3)]
        xeT = [k.sb("xeT%d" % i, [128, 8, NSL], BF16) for i in range(2)]
        hT = k.sb("hT", [128, NFC, NSL], BF16)
        sg = [k.sb("sg%d" % i, [128, NSL], F32) for i in range(2)]
        NW = 3
        NPRE = NW - 1
        wg = [k.sb("wg%d" % i, [128, 8, 512], BF16) for i in range(NW)]
        wu = [k.sb("wu%d" % i, [128, 8, 512], BF16) for i in range(NW)]
        wdn = k.sb("wdn", [128, NFC, D], BF16)
        ysb = [k.sb("ysb%d" % i, [128, D], F32) for i in range(2)]
        ncg = len(cgroups)
        pg = [k.ps("pg%d" % i, [128, 512], F32) for i in range(ncg)]
        pu = [k.ps("pu%d" % i, [128, 512], F32) for i in range(ncg)]
        pxt = k.ps("pxt", [128, 8, 128], BF16)
        pyd = [k.ps("pyd%d" % i, [128, 512], F32) for i in range(8 - 2 * ncg - 1)]
        target = dst_lat
        pieces = [(i * 512, min(512, DFF - i * 512)) for i in range((DFF + 511) // 512)]
        NP_ = len(pieces)
        cnt = {"y": 0}

        def tiles_of(ex):
            tl = []
            for s in range(NS):
                for j in range(2):
                    tl.append((128, s * 256 + j * 128, idxT[:, j, s * 32 + ex: s * 32 + ex + 1],
                               gT[:, j, s * 32 + ex: s * 32 + ex + 1], s, "lat"))
            if not last:
                for s in range(NS):
                    tl.append((32, NS * CAPL + s * CAPC, idcT[:, s * 32 + ex: s * 32 + ex + 1],
                               gcT[:, s * 32 + ex: s * 32 + ex + 1], NS, "ctx"))
            return tl

        idx_reads = [idxT] + ([] if last else [idcT])

        def gather_T(ex):
            xT = xeT[ex % 2]
            for ti, (rows, c0, iap, gap, r, kind) in enumerate(tiles_of(ex)):
                x_e = xe[ti % 3]
                k.dma("pool", lambda e, x_e=x_e, rows=rows, iap=iap: e.indirect_dma_start(
                    out=x_e[0:rows, :], out_offset=None, in_=A["hfb_d"],
                    in_offset=bass.IndirectOffsetOnAxis(ap=iap[0:rows, :], axis=0)),
                    reads=["hfb_d"] + idx_reads, writes=[x_e])
                for kc in range(8):
                    k.T(lambda e, x_e=x_e, rows=rows, kc=kc: e.transpose(pxt[:, kc, 0:rows], x_e[0:rows, kc * 128:(kc + 1) * 128],
                                                                       identb[0:rows, 0:rows]), [x_e, identb], [pxt])
                if ti % 2 == 0:
                    k.A(lambda e, xT=xT, rows=rows, c0=c0: e.copy(out=xT[:, :, c0:c0 + rows], in_=pxt[:, :, 0:rows]), [pxt], [xT])
                else:
                    k.V(lambda e, xT=xT, rows=rows, c0=c0: e.tensor_copy(out=xT[:, :, c0:c0 + rows], in_=pxt[:, :, 0:rows]),
                        [pxt], [xT])

        def load_gu(ex, pi):
            c0, w = pieces[pi]
            gi_ = (ex * NP_ + pi) % NW
            wgv = A["w_gate"][l, ex].rearrange("(kc p) f -> p kc f", p=128)
            wuv = A["w_up"][l, ex].rearrange("(kc p) f -> p kc f", p=128)
            k.dma("pool", lambda e: e.dma_start(out=wg[gi_][:, :, 0:w], in_=wgv[:, :, c0:c0 + w]), writes=[wg[gi_]])
            k.dma("pool", lambda e: e.dma_start(out=wu[gi_][:, :, 0:w], in_=wuv[:, :, c0:c0 + w]), writes=[wu[gi_]])

        def load_dn(ex, pi):
            c0, w = pieces[pi]
            f0, nf = c0 // 128, w // 128
            wdv = A["w_down"][l, ex].rearrange("(fc p) d -> p fc d", p=128)
            for f in range(f0, f0 + nf, 2):
                k.dma("pool", lambda e, f=f: e.dma_start(out=wdn[:, f:f + 2, :], in_=wdv[:, f:f + 2, :]),
                      writes=[("wdn", f // 2)])

        def gu(ex, pi):
            c0, w = pieces[pi]
            gi_ = (ex * NP_ + pi) % NW
            xT = xeT[ex % 2]
            for f2 in range(w // 128):
                fc = c0 // 128 + f2
                for (W, ps_list) in ((wg[gi_], pg), (wu[gi_], pu)):
                    for gj, (g0, gw) in enumerate(cgroups):
                        p = ps_list[gj]
                        for kc in range(8):
                            k.T(lambda e, p=p, W=W, kc=kc, f2=f2, g0=g0, gw=gw: e.matmul(
                                p[:, 0:gw], lhsT=W[:, kc, f2 * 128:(f2 + 1) * 128], rhs=xT[:, kc, g0:g0 + gw],
                                start=(kc == 0), stop=(kc == 7)), [W, xT], [p])
                s_g = sg[fc % 2]
                for gj, (g0, gw) in enumerate(cgroups):
                    k.A(lambda e, s_g=s_g, gj=gj, g0=g0, gw=gw: e.activation(out=s_g[:, g0:g0 + gw], in_=pg[gj][:, 0:gw],
                                                                            func=AF.Silu), [pg[gj]], [s_g])
                    k.V(lambda e, s_g=s_g, gj=gj, g0=g0, gw=gw, fc=fc: e.tensor_tensor(
                        out=hT[:, fc, g0:g0 + gw], in0=s_g[:, g0:g0 + gw], in1=pu[gj][:, 0:gw], op=ALU.mult),
                        [s_g, pu[gj]], [("hT", fc)])

        def down(ex):
            for ti, (rows, c0, iap, gap, r, kind) in enumerate(tiles_of(ex)):
                y_s = ysb[ti % 2]
                for hh in range(2):
                    p = pyd[cnt["y"] % len(pyd)]
                    cnt["y"] += 1
                    for fc in range(NFC):
                        k.T(lambda e, p=p, rows=rows, c0=c0, fc=fc, hh=hh: e.matmul(
                            p[0:rows, :], lhsT=hT[:, fc, c0:c0 + rows], rhs=wdn[:, fc, hh * 512:(hh + 1) * 512],
                            start=(fc == 0), stop=(fc == NFC - 1)), [("hT", fc), ("wdn", fc // 2)], [p])
                    k.V(lambda e, p=p, rows=rows, y_s=y_s, hh=hh, gap=gap, r=r: e.scalar_tensor_tensor(
                        out=y_s[0:rows, hh * 512:(hh + 1) * 512], in0=p[0:rows, :], scalar=gap[0:rows, :],
                        in1=gtf[r][0:rows, hh * 512:(hh + 1) * 512], op0=ALU.mult, op1=ALU.mult),
                        [p, gT, gtf[r]] + ([] if last else [gcT]), [y_s])
                tgt = target if kind == "lat" else A["res_d"]
                k.dma("pool", lambda e, y_s=y_s, rows=rows, iap=iap, tgt=tgt: e.indirect_dma_start(
                    out=tgt, out_offset=bass.IndirectOffsetOnAxis(ap=iap[0:rows, :], axis=0), in_=y_s[0:rows, :],
                    in_offset=None, compute_op=ALU.add), reads=[y_s] + idx_reads, writes=["res"])

        gather_T(0)
        for pi in range(NPRE):
            load_gu(0, pi)
        for ex in range(E):
            for pi in range(NP_):
                load_dn(ex, pi)
                gu(ex, pi)
                if pi + NPRE < NP_:
                    load_gu(ex, pi + NPRE)
            if ex + 1 < E:
                gather_T(ex + 1)
                for pi in range(NPRE):
                    load_gu(ex + 1, pi)
            down(ex)


_NC_CACHE = {}


def kernel(**inputs):
    consts = host_consts()
    if "nc" not in _NC_CACHE:
        _NC_CACHE["nc"] = build_program()
    nc = _NC_CACHE["nc"]
    x = np.ascontiguousarray(inputs["x"], dtype=np.float32)
    c = np.ascontiguousarray(inputs["c"], dtype=np.float32)
    ctx = np.ascontiguousarray(inputs["ctx"], dtype=np.float32)
    shared = {"c_ctx": np.ascontiguousarray(inputs["c_ctx"], dtype=np.float32).reshape(1, D)}
    for n in WNAMES:
        shared[n] = np.ascontiguousarray(inputs[n], dtype=np.float32).reshape(WSHAPES[n])
    for n, v in consts.items():
        shared[n] = v
    in_maps = []
    for ci in range(NCORES):
        m = dict(shared)
        m["x"] = x[ci * NS:(ci + 1) * NS].reshape(NS * S, D)
        m["c"] = c[ci * NS:(ci + 1) * NS]
        m["ctx"] = ctx[ci * NS:(ci + 1) * NS].reshape(NS * C, D)
        in_maps.append(m)
    res = run_bass_kernel_spmd(nc, in_maps, core_ids=list(range(NCORES)))
    out = np.concatenate([r["y"].reshape(NS, S, D) for r in res.results], axis=0)
    return out.astype(np.float32)
```

```python
import os
import numpy as np
import concourse.bass as bass
import concourse.mybir as mybir
from concourse.bass_utils import run_bass_kernel_spmd
from contextlib import ExitStack, contextmanager

F32 = mybir.dt.float32
F32R = mybir.dt.float32r
BF16 = mybir.dt.bfloat16
U32 = mybir.dt.uint32
I32 = mybir.dt.int32
AF = mybir.ActivationFunctionType
ALU = mybir.AluOpType
AX = mybir.AxisListType

NCORES = 8
NS = 2
D = 1024
S = 2048
C = 256
NT = (S + C) // 128
L = 2
E = 16
DFF = 2816
NFC = DFF // 128
CAPL = 256
CAPC = 32
EPS = 1e-6
UC = 1792
NLAT = NS * S
NROW = NS * (S + C)


class KB:
    ENGS = ("pe", "dve", "act", "pool", "sp")

    def __init__(self, nc, ndma=24):
        self.nc = nc
        self.st = ExitStack()
        self.q = {e: [] for e in self.ENGS}
        self.cnt = {e: 0 for e in self.ENGS}
        self.sem = {e: self.st.enter_context(nc.semaphore("prog_" + e)) for e in self.ENGS}
        self.waited = {(c, p): 0 for c in self.ENGS for p in self.ENGS}
        self.waited_dma = {}
        self.lastw = {}
        self.readers = {}
        self.dma_sems = []
        self.ndma = {"sp": 0, "act": 0, "pool": 0}
        self.pool_of = {"sp": (0, 10), "act": (10, 8), "pool": (18, 14)}
        for i in range(32):
            s = self.st.enter_context(nc.semaphore("dma%d" % i))
            self.dma_sems.append([s, 0, None])
        self.cur = self.st

    def sb(self, name, shape, dt):
        self.uid = getattr(self, "uid", 0) + 1
        return self.cur.enter_context(self.nc.sbuf_tensor("%s_s%d" % (name, self.uid), shape, dt))

    def ps(self, name, shape, dt):
        self.uid = getattr(self, "uid", 0) + 1
        return self.cur.enter_context(self.nc.psum_tensor("%s_p%d" % (name, self.uid), shape, dt))

    @contextmanager
    def phase(self):
        prev = self.cur
        with ExitStack() as es:
            self.cur = es
            yield
            self.barrier()
        self.cur = prev

    def barrier(self):
        toks = [(p, self.cnt[p]) for p in self.ENGS if self.cnt[p] > 0]
        dtoks = [d[2] for d in self.dma_sems if d[2] is not None]
        for e in self.ENGS:
            for t in toks:
                if t[0] != e:
                    self._wait(e, t)
            for t in dtoks:
                self._wait(e, t)
        self.lastw = {}
        self.readers = {}

    def _key(self, x):
        if isinstance(x, (str, tuple)):
            return x
        return x.tensor.name if hasattr(x, "tensor") else x.name

    def _wait(self, eng, tok):
        if tok is None:
            return
        if tok[0] == "dma":
            _, i, val = tok
            if self.waited_dma.get((eng, i), 0) >= val:
                return
            self.waited_dma[(eng, i)] = val
            sem = self.dma_sems[i][0]
            self.q[eng].append(lambda e, sem=sem, val=val: e.wait_ge(sem, val))
            return
        p, c = tok
        if eng == "pe" and p == "pe" and not (PESYNC or getattr(self, "pesync", False)):
            return
        if self.waited[(eng, p)] >= c:
            return
        self.waited[(eng, p)] = c
        s = self.sem[p]
        self.q[eng].append(lambda e, s=s, c=c: e.wait_ge(s, c))

    def _deps(self, eng, rk, wk):
        for k in rk:
            self._wait(eng, self.lastw.get(k))
        for k in wk:
            self._wait(eng, self.lastw.get(k))
            for t in self.readers.get(k, ()):
                self._wait(eng, t)

    def _record(self, tok, rk, wk):
        for k in wk:
            self.lastw[k] = tok
            self.readers[k] = []
        for k in rk:
            self.readers.setdefault(k, []).append(tok)

    def op(self, eng, fn, reads=(), writes=()):
        rk = [self._key(r) for r in reads]
        wk = [self._key(w) for w in writes]
        self._deps(eng, rk, wk)
        self.cnt[eng] += 1
        tok = (eng, self.cnt[eng])
        s = self.sem[eng]
        self.q[eng].append(lambda e, fn=fn, s=s: fn(e).then_inc(s, 1))
        self._record(tok, rk, wk)
        return tok

    def V(self, fn, r=(), w=()):
        return self.op("dve", fn, r, w)

    def A(self, fn, r=(), w=()):
        return self.op("act", fn, r, w)

    def G(self, fn, r=(), w=()):
        return self.op("pool", fn, r, w)

    def T(self, fn, r=(), w=()):
        return self.op("pe", fn, r, w)

    def dma(self, eng, fn, reads=(), writes=()):
        rk = [self._key(r) for r in reads]
        wk = [self._key(w) for w in writes]
        self._deps(eng, rk, wk)
        base, n = self.pool_of[eng]
        i = base + self.ndma[eng] % n
        self.ndma[eng] += 1
        ent = self.dma_sems[i]
        if ent[2] is not None:
            self._wait(eng, ent[2])
        ent[1] += 16
        tok = ("dma", i, ent[1])
        ent[2] = tok
        sem = ent[0]
        self.q[eng].append(lambda e, fn=fn, sem=sem: fn(e).then_inc(sem, 16))
        self._record(tok, rk, wk)
        return tok

    def finish(self):
        nc = self.nc
        self.barrier()
        q = self.q
        with nc.Block() as block:
            @block.tensor
            def _(e):
                for f in q["pe"]:
                    f(e)

            @block.vector
            def _(e):
                for f in q["dve"]:
                    f(e)

            @block.scalar
            def _(e):
                for f in q["act"]:
                    f(e)

            @block.gpsimd
            def _(e):
                for f in q["pool"]:
                    f(e)

            @block.sync
            def _(e):
                for f in q["sp"]:
                    f(e)
        self.st.close()


def host_consts():
    f32 = np.float32
    half = 16
    inv = (10000.0 ** (-np.arange(half, dtype=f32) / half)).astype(f32)
    t = np.arange(S)
    rows = (t // 64).astype(f32)
    cols = (t % 64).astype(f32)
    ar = (rows[:, None] * inv[None, :]).astype(f32)
    ac = (cols[:, None] * inv[None, :]).astype(f32)
    attC = np.concatenate([np.cos(ar), np.cos(ar), np.cos(ac), np.cos(ac)], axis=1).astype(f32)
    attS = np.concatenate([-np.sin(ar), np.sin(ar), -np.sin(ac), np.sin(ac)], axis=1).astype(f32)
    half = 32
    inv = (10000.0 ** (-np.arange(half, dtype=f32) / half)).astype(f32)
    pos = np.arange(S + C).astype(f32)
    a = (pos[:, None] * inv[None, :]).astype(f32)
    retC = np.concatenate([np.cos(a), np.cos(a)], axis=1).astype(f32)
    retS = np.concatenate([-np.sin(a), np.sin(a)], axis=1).astype(f32)
    j = np.arange(128)[:, None]
    i = np.arange(128)[None, :]
    masks = np.stack([(j <= i), (j >= i), (j > i)], axis=0).astype(f32)
    ident = np.eye(128, dtype=f32)
    p = np.arange(128, dtype=f32)
    dcoef = np.stack([p + 1, -(p + 1), -p, p], axis=1).astype(f32)
    return {"attC": attC, "attS": attS, "retC": retC, "retS": retS, "masks": masks,
            "ident": ident, "dcoef": dcoef}


WNAMES = ["w_mod", "b_mod", "g_mix", "g_ffn", "w_in", "q_norm_g", "k_norm_g", "att_sink",
          "ret_decay_logit", "ret_norm_g", "conv_w", "conv_b", "conv_norm_g", "conv_norm_b",
          "w_out", "w_router", "w_gate", "w_up", "w_down"]
WSHAPES = {
    "w_mod": [L, D, 6 * D], "b_mod": [L, 6 * D], "g_mix": [L, D], "g_ffn": [L, D], "w_in": [L, D, 2304],
    "q_norm_g": [L, 64], "k_norm_g": [L, 64], "att_sink": [L, 8], "ret_decay_logit": [L, 8],
    "ret_norm_g": [L, 256], "conv_w": [L, 31, 256], "conv_b": [L, 256], "conv_norm_g": [L, 256],
    "conv_norm_b": [L, 256], "w_out": [L, D, D], "w_router": [L, D, E], "w_gate": [L, E, D, DFF],
    "w_up": [L, E, D, DFF], "w_down": [L, E, DFF, D],
}
CSHAPES = {"attC": [S, 64], "attS": [S, 64], "retC": [S + C, 64], "retS": [S + C, 64],
           "masks": [3, 128, 128], "ident": [128, 128], "dcoef": [128, 4]}

PESYNC = os.environ.get("MK_PESYNC", "") == "1"
RETSKIP = os.environ.get("MK_RETSKIP", "")
STOP = os.environ.get("MK_STOP", "")
DEBUG = os.environ.get("MK_DEBUG", "") == "1"


def build_program():
    nc = bass.Bass("TRN2", target_bir_lowering=False)
    A = {}
    A["x"] = nc.dram_tensor("x", [NS * S, D], F32, kind="ExternalInput").ap()
    A["c"] = nc.dram_tensor("c", [NS, D], F32, kind="ExternalInput").ap()
    A["ctx"] = nc.dram_tensor("ctx", [NS * C, D], F32, kind="ExternalInput").ap()
    A["c_ctx"] = nc.dram_tensor("c_ctx", [1, D], F32, kind="ExternalInput").ap()
    for n in WNAMES:
        A[n] = nc.dram_tensor(n, WSHAPES[n], F32, kind="ExternalInput").ap()
    for n, shp in CSHAPES.items():
        A[n] = nc.dram_tensor(n, shp, F32, kind="ExternalInput").ap()
    A["y"] = nc.dram_tensor("y", [NS * S, D], F32, kind="ExternalOutput").ap()
    dk = "ExternalOutput" if DEBUG else "Internal"
    A["mod_d"] = nc.dram_tensor("mod_d", [L, 4, 6 * D], F32, kind=dk).ap()
    A["u_d"] = nc.dram_tensor("u_d", [NS, S + C, UC], F32, kind=dk).ap()
    A["cv_d"] = nc.dram_tensor("cv_d", [NS, 512, S + C], F32, kind=dk).ap()
    A["mixin_d"] = nc.dram_tensor("mixin_d", [NS, S + C, 768], BF16, kind=dk).ap()
    A["convT_d"] = nc.dram_tensor("convT_d", [NS, 256, S + C], BF16, kind=dk).ap()
    A["res_d"] = nc.dram_tensor("res_d", [NROW, D], F32, kind=dk).ap()
    A["hf_d"] = nc.dram_tensor("hf_d", [NROW, D], F32, kind=dk).ap()
    A["aff_d"] = nc.dram_tensor("aff_d", [48, S], F32, kind=dk).ap()
    A["hfb_d"] = nc.dram_tensor("hfb_d", [NROW, D], BF16, kind=dk).ap()
    A["affc_d"] = nc.dram_tensor("affc_d", [48, C], F32, kind=dk).ap()

    k = KB(nc)
    P = {}
    P["ident"] = k.sb("ident", [128, 128], F32)
    P["identb"] = k.sb("identb", [128, 128], BF16)
    P["masks"] = k.sb("masks", [128, 3, 128], F32)
    P["modT"] = [k.sb("modT%d" % l, [128, 48, 4], F32) for l in range(L)]
    P["gT"] = k.sb("gT", [128, 8, 4], F32)
    P["gsA"] = [k.sb("gsA%d" % l, [128, 8, 4], F32) for l in range(L)]
    P["epsb"] = k.sb("epsb", [128, 1], F32)
    k.V(lambda e: e.memset(P["epsb"][:], EPS), [], [P["epsb"]])
    k.dma("sp", lambda e: e.dma_start(out=P["ident"][:], in_=A["ident"]), writes=[P["ident"]])
    k.dma("sp", lambda e: e.dma_start(out=P["masks"][:], in_=A["masks"].rearrange("m j i -> j m i")),
          writes=[P["masks"]])
    k.V(lambda e: e.tensor_copy(out=P["identb"][:], in_=P["ident"][:]), [P["ident"]], [P["identb"]])

    phase_mod(k, A, P)
    if STOP == "mod":
        k.finish()
        return nc
    for l in range(L):
        last = l == L - 1
        src_lat = A["x"] if l == 0 else A["res_d"][0:NLAT]
        src_ctx = A["ctx"] if l == 0 else A["res_d"][NLAT:NROW]
        dst_lat = A["y"] if last else A["res_d"][0:NLAT]
        phase_proj(k, A, P, l, src_lat, src_ctx)
        if STOP == "proj%d" % l:
            break
        phase_att(k, A, P, l, last)
        if STOP == "att%d" % l:
            break
        phase_ret(k, A, P, l, last)
        if STOP == "ret%d" % l:
            break
        phase_conv(k, A, P, l, last)
        if STOP == "conv%d" % l:
            break
        phase_out(k, A, P, l, last, src_lat, src_ctx, dst_lat)
        if STOP == "out%d" % l:
            break
        phase_moe(k, A, P, l, last, dst_lat)
        if STOP == "moe%d" % l:
            break
    k.finish()
    return nc


def phase_mod(k, A, P):
    with k.phase():
        cc = k.sb("cc", [4, D], F32)
        cs = k.sb("cs", [4, D], F32)
        csT = k.sb("csT", [128, 8, 4], F32R)
        gg = k.sb("gg", [4, D], F32)
        pt = k.ps("pt", [128, 8, 4], F32)
        pm = [k.ps("pm%d" % i, [4, 512], F32) for i in range(2)]
        ptm = k.ps("ptm", [128, 48, 4], F32)
        wm = [k.sb("wm%d" % i, [128, 8, 512], F32R) for i in range(2)]
        bm = k.sb("bm", [4, 6 * D], F32)
        modrow = k.sb("modrow", [4, 6 * D], F32)
        ident = P["ident"]
        k.V(lambda e: e.memset(cc[:], 0.0), [], [cc])
        k.dma("sp", lambda e: e.dma_start(out=cc[0:NS, :], in_=A["c"]), writes=[cc])
        k.dma("sp", lambda e: e.dma_start(out=cc[NS:NS + 1, :], in_=A["c_ctx"]), writes=[cc])
        k.A(lambda e: e.activation(out=cs[:], in_=cc[:], func=AF.Silu), [cc], [cs])
        for kc in range(8):
            k.T(lambda e, kc=kc: e.transpose(pt[:, kc, :], cs[0:4, kc * 128:(kc + 1) * 128], ident[0:4, 0:4]),
                [cs, ident], [pt])
        k.V(lambda e: e.tensor_copy(out=csT[:], in_=pt[:]), [pt], [csT])
        k.dma("sp", lambda e: e.dma_start(out=gg[0:2, :], in_=A["g_mix"]), writes=[gg])
        k.dma("sp", lambda e: e.dma_start(out=gg[2:4, :], in_=A["g_ffn"]), writes=[gg])
        for kc in range(8):
            k.T(lambda e, kc=kc: e.transpose(pt[:, kc, :], gg[0:4, kc * 128:(kc + 1) * 128], ident[0:4, 0:4]),
                [gg, ident], [pt])
        k.V(lambda e: e.tensor_copy(out=P["gT"][:], in_=pt[:]), [pt], [P["gT"]])
        for l in range(L):
            k.dma("sp", lambda e, l=l: e.dma_start(out=bm[:], in_=A["b_mod"][l, :].partition_broadcast(4)),
                  writes=[bm])
            wv = A["w_mod"][l].rearrange("(kc p) f -> p kc f", p=128)
            for j in range(12):
                w = wm[j % 2]
                k.dma("pool", lambda e, w=w, j=j, wv=wv: e.dma_start(out=w[:], in_=wv[:, :, j * 512:(j + 1) * 512]),
                      writes=[w])
                p = pm[j % 2]
                for kc in range(8):
                    k.T(lambda e, p=p, w=w, kc=kc: e.matmul(p[:], lhsT=csT[:, kc, :], rhs=w[:, kc, :],
                                                          start=(kc == 0), stop=(kc == 7)), [csT, w], [p])
                k.V(lambda e, p=p, j=j: e.tensor_tensor(out=modrow[:, j * 512:(j + 1) * 512], in0=p[:],
                                                        in1=bm[:, j * 512:(j + 1) * 512], op=ALU.add),
                    [p, bm], [modrow])
            k.dma("sp", lambda e, l=l: e.dma_start(out=A["mod_d"][l], in_=modrow[:]), reads=[modrow],
                  writes=["mod_d"])
            for j in range(48):
                k.T(lambda e, j=j: e.transpose(ptm[:, j, :], modrow[0:4, j * 128:(j + 1) * 128], ident[0:4, 0:4]),
                    [modrow, ident], [ptm])
            mT = P["modT"][l]
            k.V(lambda e, mT=mT: e.tensor_copy(out=mT[:], in_=ptm[:]), [ptm], [mT])
            gs = P["gsA"][l]
            k.V(lambda e, gs=gs, mT=mT: e.tensor_scalar(out=gs[:], in0=mT[:, 8:16, :], scalar1=1.0, scalar2=None,
                                                        op0=ALU.add), [mT], [gs])
            k.V(lambda e, gs=gs, l=l: e.tensor_tensor(out=gs[:], in0=gs[:],
                                                      in1=P["gT"][:, :, l:l + 1].to_broadcast([128, 8, 4]),
                                                      op=ALU.mult), [gs, P["gT"]], [gs])


def rms_rstd(k, xt, junk, ss, rstd, n):
    k.A(lambda e: e.activation(out=junk, in_=xt, func=AF.Square, accum_out=ss[:, 0:1]), [xt], [junk, ss])
    k.V(lambda e: e.tensor_scalar(out=ss[:, 0:1], in0=ss[:, 0:1], scalar1=1.0 / n, scalar2=EPS, op0=ALU.mult,
                                  op1=ALU.add), [ss], [ss])
    k.A(lambda e: e.sqrt(out=ss[:, 0:1], in_=ss[:, 0:1]), [ss], [ss])
    k.V(lambda e: e.reciprocal(out=rstd[:, 0:1], in_=ss[:, 0:1]), [ss], [rstd])


def phase_proj(k, A, P, l, src_lat, src_ctx):
    with k.phase():
        win = k.sb("win", [128, 8, 2304], BF16)
        wv = A["w_in"][l].rearrange("(kc p) f -> p kc f", p=128)
        for j in range(0, 2304, 384):
            k.dma("pool", lambda e, j=j: e.dma_start(out=win[:, :, j:j + 384], in_=wv[:, :, j:j + 384]),
                  writes=[("win", j)])
        winkeys = [("win", j) for j in range(0, 2304, 384)]
        NB = 8
        xts = [k.sb("xt%d" % i, [128, D], F32) for i in range(NB)]
        xn = [k.sb("xn%d" % i, [128, D], BF16) for i in range(NB)]
        junk = k.sb("junk", [128, D], F32)
        ss = [k.sb("ss%d" % i, [128, 1], F32) for i in range(NB)]
        rstd = [k.sb("rstd%d" % i, [128, 1], F32) for i in range(NB)]
        hT = [k.sb("hT%d" % i, [128, 8, 512], BF16) for i in range(2)]
        ptr = [k.ps("ptr%d" % i, [128, D], BF16) for i in range(2)]
        pu = [k.ps("pu%d" % i, [128, 512], F32) for i in range(3)]
        pc = [k.ps("pc%d" % i, [128, 512], F32) for i in range(2)]
        usb = [k.sb("usb%d" % i, [128, UC], F32) for i in range(2)]
        cvs = [k.sb("cvs%d" % i, [128, 4, 512], F32) for i in range(2)]
        identb = P["identb"]
        gs = P["gsA"][l]
        mT = P["modT"][l]
        groups = []
        for s in range(NS):
            groups.append((s, 0, 2, src_ctx[s * C:(s + 1) * C], NS))
            for g in range(4):
                groups.append((s, C + g * 512, 4, src_lat[s * S + g * 512: s * S + (g + 1) * 512], s))
        cnt = {"t": 0}

        def stats(gi):
            s, tok0, ntl, src, r = groups[gi]
            for t in range(ntl):
                i = (gi % 2) * 4 + t
                xt, xnn, sst, rs = xts[i], xn[i], ss[i], rstd[i]
                k.dma("sp", lambda e, xt=xt, src=src, t=t: e.dma_start(out=xt[:], in_=src[t * 128:(t + 1) * 128, :]),
                      writes=[xt])
                k.A(lambda e, xt=xt, sst=sst: e.activation(out=junk[:], in_=xt[:], func=AF.Square, accum_out=sst[:, 0:1]),
                    [xt], [junk, sst])
                k.A(lambda e, sst=sst: e.activation(out=sst[:, 0:1], in_=sst[:, 0:1], func=AF.Ln, scale=1.0 / D, bias=EPSB[:, 0:1]),
                    [sst, EPSB], [sst])
                k.A(lambda e, sst=sst, rs=rs: e.activation(out=rs[:, 0:1], in_=sst[:, 0:1], func=AF.Exp, scale=-0.5), [sst], [rs])
                k.V(lambda e, xnn=xnn, xt=xt, rs=rs: e.tensor_scalar(out=xnn[:], in0=xt[:], scalar1=rs[:, 0:1],
                                                                   scalar2=None, op0=ALU.mult), [xt, rs], [xnn])

        def trans(gi):
            s, tok0, ntl, src, r = groups[gi]
            h = hT[gi % 2]
            for t in range(ntl):
                i = (gi % 2) * 4 + t
                xnn = xn[i]
                pt = ptr[cnt["t"] % 2]
                cnt["t"] += 1
                for kc in range(8):
                    k.T(lambda e, pt=pt, xnn=xnn, kc=kc: e.transpose(pt[:, kc * 128:(kc + 1) * 128],
                                                                   xnn[:, kc * 128:(kc + 1) * 128], identb[:]),
                        [xnn, identb], [pt])
                for kc in range(8):
                    if kc % 2 == 0:
                        k.A(lambda e, h=h, pt=pt, kc=kc, t=t, r=r: e.activation(
                            out=h[:, kc, t * 128:(t + 1) * 128], in_=pt[:, kc * 128:(kc + 1) * 128],
                            func=AF.Identity, scale=gs[:, kc, r:r + 1], bias=mT[:, kc, r:r + 1]),
                            [pt, gs, mT], [h])
                    else:
                        k.V(lambda e, h=h, pt=pt, kc=kc, t=t, r=r: e.tensor_scalar(
                            out=h[:, kc, t * 128:(t + 1) * 128], in0=pt[:, kc * 128:(kc + 1) * 128],
                            scalar1=gs[:, kc, r:r + 1], scalar2=mT[:, kc, r:r + 1], op0=ALU.mult, op1=ALU.add),
                            [pt, gs, mT], [h])

        def mm(gi):
            s, tok0, ntl, src, r = groups[gi]
            h = hT[gi % 2]
            ntok = ntl * 128
            ei = 0
            for t in range(ntl):
                ub = usb[t % 2]
                for ci, (c0, w) in enumerate([(0, 512), (512, 512), (1024, 512), (1536, 256)]):
                    p = pu[ei % 3]
                    for kc in range(8):
                        k.T(lambda e, p=p, h=h, kc=kc, t=t, c0=c0, w=w: e.matmul(
                            p[:, 0:w], lhsT=h[:, kc, t * 128:(t + 1) * 128], rhs=win[:, kc, c0:c0 + w],
                            start=(kc == 0), stop=(kc == 7)), [h] + winkeys, [p])
                    if ei % 2 == 0:
                        k.A(lambda e, ub=ub, p=p, c0=c0, w=w: e.copy(out=ub[:, c0:c0 + w], in_=p[:, 0:w]), [p], [ub])
                    else:
                        k.V(lambda e, ub=ub, p=p, c0=c0, w=w: e.tensor_copy(out=ub[:, c0:c0 + w], in_=p[:, 0:w]),
                            [p], [ub])
                    ei += 1
                k.dma("sp", lambda e, ub=ub, s=s, tok0=tok0, t=t: e.dma_start(
                    out=A["u_d"][s, tok0 + t * 128: tok0 + (t + 1) * 128, :], in_=ub[:]), reads=[ub], writes=["u_d"])
            cv = cvs[gi % 2]
            for j in range(4):
                p = pc[j % 2]
                for kc in range(8):
                    k.T(lambda e, p=p, h=h, kc=kc, j=j, ntok=ntok: e.matmul(
                        p[:, 0:ntok], lhsT=win[:, kc, UC + j * 128: UC + (j + 1) * 128], rhs=h[:, kc, 0:ntok],
                        start=(kc == 0), stop=(kc == 7)), [h] + winkeys, [p])
                if j % 2 == 0:
                    k.A(lambda e, cv=cv, p=p, j=j, ntok=ntok: e.copy(out=cv[:, j, 0:ntok], in_=p[:, 0:ntok]), [p], [cv])
                else:
                    k.V(lambda e, cv=cv, p=p, j=j, ntok=ntok: e.tensor_copy(out=cv[:, j, 0:ntok], in_=p[:, 0:ntok]),
                        [p], [cv])
            k.dma("act", lambda e, cv=cv, s=s, tok0=tok0, ntok=ntok: e.dma_start(
                out=A["cv_d"][s, :, tok0:tok0 + ntok].rearrange("(j p) t -> p j t", p=128), in_=cv[:, :, 0:ntok]),
                reads=[cv], writes=["cv_d"])

        EPSB = P["epsb"]
        ng = len(groups)
        stats(0)
        trans(0)
        for gi in range(ng):
            if gi + 1 < ng:
                stats(gi + 1)
            mm(gi)
            if gi + 1 < ng:
                trans(gi + 1)


def run_pipe(n, stages):
    ns = len(stages)
    for step in range(n + ns - 1):
        for j in range(ns - 1, -1, -1):
            i = step - j
            if 0 <= i < n:
                stages[j](i)


def phase_att(k, A, P, l, last):
    with k.phase():
        identb = P["identb"]
        EPSB = P["epsb"]
        attC = k.sb("attC", [128, 16, 64], F32)
        attS = k.sb("attS", [128, 16, 64], F32)
        k.dma("sp", lambda e: e.dma_start(out=attC[:], in_=A["attC"].rearrange("(t p) d -> p t d", p=128)), writes=[attC])
        k.dma("sp", lambda e: e.dma_start(out=attS[:], in_=A["attS"].rearrange("(t p) d -> p t d", p=128)), writes=[attS])
        gqk = k.sb("gqk", [128, 10, 64], F32)
        k.dma("sp", lambda e: e.dma_start(out=gqk[:, 0, :], in_=A["q_norm_g"][l, :].partition_broadcast(128)), writes=[gqk])
        k.dma("sp", lambda e: e.dma_start(out=gqk[:, 8, :], in_=A["k_norm_g"][l, :].partition_broadcast(128)), writes=[gqk])
        k.V(lambda e: e.tensor_scalar(out=gqk[:, 0, :], in0=gqk[:, 0, :], scalar1=0.125, scalar2=None, op0=ALU.mult),
            [gqk], [gqk])
        for h in range(1, 8):
            k.V(lambda e, h=h: e.tensor_copy(out=gqk[:, h, :], in_=gqk[:, 0, :]), [gqk], [gqk])
        k.V(lambda e: e.tensor_copy(out=gqk[:, 9, :], in_=gqk[:, 8, :]), [gqk], [gqk])
        sink = k.sb("sink", [128, 8], F32)
        k.dma("sp", lambda e: e.dma_start(out=sink[:], in_=A["att_sink"][l, :].partition_broadcast(128)), writes=[sink])
        k.A(lambda e: e.activation(out=sink[:], in_=sink[:], func=AF.Exp), [sink], [sink])
        maskb = k.sb("maskb", [128, 3, 128], BF16)
        k.V(lambda e: e.tensor_copy(out=maskb[:], in_=P["masks"][:]), [P["masks"]], [maskb])

        qT = k.sb("qT", [64, 8, NT * 128], BF16)
        kT = k.sb("kT", [64, 2, NT * 128], BF16)
        vx = k.sb("vx", [128, NT, 2, 65], BF16)
        k.V(lambda e: e.memset(vx[:], 1.0), [], [vx])
        NU, NQ = 7, 5
        uq = [k.sb("uq%d" % i, [128, 768], F32) for i in range(NU)]
        sq = [k.sb("sq%d" % i, [128, 640], F32) for i in range(2)]
        ssq = [k.sb("ssq%d" % i, [128, 10], F32) for i in range(4)]
        qn = [k.sb("qn%d" % i, [128, 640], F32) for i in range(NQ)]
        t1 = [k.sb("t1%d" % i, [128, 640], F32) for i in range(2)]
        t2 = [k.sb("t2%d" % i, [128, 640], F32) for i in range(2)]
        qb = [k.sb("qb%d" % i, [128, 640], BF16) for i in range(3)]
        pq = [k.ps("pq%d" % i, [64, 8, 128], BF16) for i in range(2)]
        pkk = k.ps("pkk", [64, 2, 128], BF16)
        psc = [k.ps("psc%d" % i, [128, 512], F32) for i in range(3)]
        po = [k.ps("po%d" % i, [128, 4, 65], F32) for i in range(2)]
        Pm = [k.sb("Pm%d" % i, [128, 5, 512], BF16) for i in range(3)]
        den = [k.sb("den%d" % i, [128, 4], F32) for i in range(2)]
        ao = [k.sb("ao%d" % i, [128, 512], BF16) for i in range(2)]
        cnt = {"sc": 0}

        for s in range(NS):
            def p0(i, s=s):
                u = uq[i % NU]
                k.dma("sp", lambda e: e.dma_start(out=u[:], in_=A["u_d"][s, i * 128:(i + 1) * 128, 0:768]),
                      reads=["u_d"], writes=[u])

            def p1(i):
                u, q2 = uq[i % NU], sq[i % 2]
                k.G(lambda e: e.tensor_tensor(out=q2[:], in0=u[:, 0:640], in1=u[:, 0:640], op=ALU.mult), [u], [q2])

            def p2(i):
                q2, sv = sq[i % 2], ssq[i % 4]
                k.V(lambda e: e.tensor_reduce(out=sv[:], in_=q2[:].rearrange("p (h d) -> p h d", d=64), axis=AX.X,
                                              op=ALU.add), [q2], [sv])

            def p3(i):
                sv = ssq[i % 4]
                k.A(lambda e: e.activation(out=sv[:], in_=sv[:], func=AF.Ln, scale=1.0 / 64, bias=EPSB[:, 0:1]), [sv, EPSB], [sv])
                k.A(lambda e: e.activation(out=sv[:], in_=sv[:], func=AF.Exp, scale=-0.5), [sv], [sv])

            def p4(i):
                u, sv, q = uq[i % NU], ssq[i % 4], qn[i % NQ]
                k.V(lambda e: e.tensor_tensor(out=q[:].rearrange("p (h d) -> p h d", d=64),
                                              in0=u[:, 0:640].rearrange("p (h d) -> p h d", d=64),
                                              in1=sv[:].unsqueeze(2).to_broadcast([128, 10, 64]), op=ALU.mult), [u, sv], [q])

            def p5(i):
                u, q = uq[i % NU], qn[i % NQ]
                k.G(lambda e: e.tensor_tensor(out=q[:], in0=q[:], in1=gqk[:].rearrange("p h d -> p (h d)"), op=ALU.mult),
                    [q, gqk], [q])
                k.G(lambda e: e.tensor_copy(out=vx[:, i, :, 0:64], in_=u[:, 640:768].rearrange("p (h d) -> p h d", d=64)),
                    [u], [vx])

            def p6(i):
                if i < 2:
                    return
                q, a1, a2 = qn[i % NQ], t1[i % 2], t2[i % 2]
                tl = i - 2
                qv = q[:].rearrange("p (h a x d) -> p h a x d", h=10, a=2, x=2, d=16)
                t1v = a1[:].rearrange("p (h a x d) -> p h a x d", h=10, a=2, x=2, d=16)
                t2v = a2[:].rearrange("p (h a x d) -> p h a x d", h=10, a=2, x=2, d=16)
                Cv = attC[:, tl, :].rearrange("p (a x d) -> p a x d", a=2, x=2, d=16)
                Sv = attS[:, tl, :].rearrange("p (a x d) -> p a x d", a=2, x=2, d=16)
                for a in range(2):
                    for x in range(2):
                        k.V(lambda e, a=a, x=x: e.tensor_tensor(
                            out=t1v[:, :, a, x, :], in0=qv[:, :, a, x, :],
                            in1=Cv[:, a, x, :].unsqueeze(1).to_broadcast([128, 10, 16]), op=ALU.mult), [q, attC], [a1])
                        k.G(lambda e, a=a, x=x: e.tensor_tensor(
                            out=t2v[:, :, a, x, :], in0=qv[:, :, a, 1 - x, :],
                            in1=Sv[:, a, x, :].unsqueeze(1).to_broadcast([128, 10, 16]), op=ALU.mult), [q, attS], [a2])

            def p7(i):
                q, a1, a2, q_b = qn[i % NQ], t1[i % 2], t2[i % 2], qb[i % 3]
                if i >= 2:
                    k.V(lambda e: e.tensor_tensor(out=q_b[:], in0=a1[:], in1=a2[:], op=ALU.add), [a1, a2], [q_b])
                else:
                    k.V(lambda e: e.tensor_copy(out=q_b[:], in_=q[:]), [q], [q_b])

            def p8(i):
                q_b, p = qb[i % 3], pq[i % 2]
                for h in range(8):
                    k.T(lambda e, h=h: e.transpose(p[:, h, :], q_b[:, h * 64:(h + 1) * 64], identb[:]), [q_b, identb], [p])
                for h in range(2):
                    k.T(lambda e, h=h: e.transpose(pkk[:, h, :], q_b[:, (8 + h) * 64:(9 + h) * 64], identb[:]),
                        [q_b, identb], [pkk])

            def p9(i):
                p = pq[i % 2]
                k.A(lambda e: e.copy(out=qT[:, :, i * 128:(i + 1) * 128], in_=p[:]), [p], [("qT", i)])
                k.V(lambda e: e.tensor_copy(out=kT[:, :, i * 128:(i + 1) * 128], in_=pkk[:]), [pkk], [("kT", i)])

            run_pipe(NT, [p0, p1, p2, p3, p4, p5, p6, p7, p8, p9])

            qblocks = list(range(2, NT)) + ([] if last else [0, 1])
            items = [(t, kh) for t in qblocks for kh in range(2)]

            def kblocks(t):
                if t >= 2:
                    kbl = []
                    if t - 1 >= 2:
                        kbl.append((t - 1, 1))
                    kbl.append((t, None))
                    if t + 1 < NT:
                        kbl.append((t + 1, 0))
                    return kbl + [(0, None), (1, None)]
                return [(0, None), (1, None)]

            def b0(i):
                t, kh = items[i]
                Pt = Pm[i % 3]
                for bj, (kb_, mk) in enumerate(kblocks(t)):
                    ps = psc[cnt["sc"] % 3]
                    cnt["sc"] += 1
                    k.T(lambda e, ps=ps, kb_=kb_: e.matmul(
                        ps[:], lhsT=kT[:, kh, kb_ * 128:(kb_ + 1) * 128],
                        rhs=qT[:, kh * 4:(kh + 1) * 4, t * 128:(t + 1) * 128], start=True, stop=True),
                        [("kT", kb_), ("qT", t)], [ps])
                    k.A(lambda e, ps=ps, bj=bj: e.activation(out=Pt[:, bj, :], in_=ps[:], func=AF.Exp), [ps], [(Pt.name, bj)])
                    if mk is not None:
                        k.G(lambda e, bj=bj, mk=mk: e.tensor_tensor(
                            out=Pt[:, bj, :].rearrange("p (g q) -> p g q", g=4),
                            in0=Pt[:, bj, :].rearrange("p (g q) -> p g q", g=4),
                            in1=maskb[:, mk, :].unsqueeze(1).to_broadcast([128, 4, 128]), op=ALU.mult),
                            [(Pt.name, bj), maskb], [(Pt.name, bj)])

            def b1(i):
                t, kh = items[i]
                Pt = Pm[i % 3]
                pso = po[i % 2]
                kbl = kblocks(t)
                nb = len(kbl)
                for g in range(4):
                    for bj, (kb_, mk) in enumerate(kbl):
                        k.T(lambda e, g=g, bj=bj, kb_=kb_: e.matmul(
                            pso[:, g, :], lhsT=Pt[:, bj, g * 128:(g + 1) * 128], rhs=vx[:, kb_, kh, :],
                            start=(bj == 0), stop=(bj == nb - 1)), [(Pt.name, bj), vx], [pso])

            def b2(i, s=s):
                t, kh = items[i]
                pso = po[i % 2]
                dn = den[i % 2]
                a_o = ao[(i // 2) % 2]
                k.V(lambda e: e.tensor_tensor(out=dn[:], in0=pso[:, :, 64], in1=sink[:, kh * 4:(kh + 1) * 4], op=ALU.add),
                    [pso, sink], [dn])
                k.V(lambda e: e.reciprocal(out=dn[:], in_=dn[:]), [dn], [dn])
                k.V(lambda e: e.tensor_tensor(
                    out=a_o[:, kh * 256:(kh + 1) * 256].rearrange("p (g d) -> p g d", g=4),
                    in0=pso[:, :, 0:64], in1=dn[:].unsqueeze(2).to_broadcast([128, 4, 64]), op=ALU.mult), [pso, dn], [a_o])
                if kh == 1:
                    k.dma("act", lambda e: e.dma_start(out=A["mixin_d"][s, t * 128:(t + 1) * 128, 0:512], in_=a_o[:]),
                          reads=[a_o], writes=["mixin_d"])

            run_pipe(len(items), [b0, b1, b2])


def phase_ret(k, A, P, l, last):
    with k.phase():
        identb = P["identb"]
        retC = k.sb("retC", [128, NT, 64], F32)
        retS = k.sb("retS", [128, NT, 64], F32)
        k.dma("sp", lambda e: e.dma_start(out=retC[:], in_=A["retC"].rearrange("(t p) d -> p t d", p=128)), writes=[retC])
        k.dma("sp", lambda e: e.dma_start(out=retS[:], in_=A["retS"].rearrange("(t p) d -> p t d", p=128)), writes=[retS])
        lg = k.sb("lg", [128, 8], F32)
        k.dma("sp", lambda e: e.dma_start(out=lg[:], in_=A["ret_decay_logit"][l, :].partition_broadcast(128)), writes=[lg])
        k.A(lambda e: e.activation(out=lg[:], in_=lg[:], func=AF.Exp, scale=-1.0), [lg], [lg])
        k.A(lambda e: e.activation(out=lg[:], in_=lg[:], func=AF.Ln, bias=1.0), [lg], [lg])
        k.V(lambda e: e.tensor_scalar(out=lg[:], in0=lg[:], scalar1=-1.0, scalar2=None, op0=ALU.mult), [lg], [lg])
        dco = k.sb("dco", [128, 4], F32)
        k.dma("sp", lambda e: e.dma_start(out=dco[:], in_=A["dcoef"]), writes=[dco])
        DT = k.sb("DT", [128, 2, 2, 4], F32)
        for d_ in range(2):
            for qk in range(2):
                k.A(lambda e, d_=d_, qk=qk: e.activation(out=DT[:, d_, qk, :], in_=lg[:, d_ * 4:(d_ + 1) * 4], func=AF.Exp,
                                                         scale=dco[:, d_ * 2 + qk: d_ * 2 + qk + 1]), [lg, dco], [DT])
            k.V(lambda e, d_=d_: e.tensor_scalar(out=DT[:, d_, 1, :], in0=DT[:, d_, 1, :], scalar1=0.125, scalar2=None,
                                                 op0=ALU.mult), [DT], [DT])
        g128 = k.sb("g128", [128, 2, 2], F32)
        for d_ in range(2):
            for pr in range(2):
                for hh in range(2):
                    h = 2 * pr + hh
                    k.A(lambda e, d_=d_, pr=pr, hh=hh, h=h: e.activation(
                        out=g128[hh * 64:(hh + 1) * 64, d_, pr:pr + 1], in_=lg[hh * 64:(hh + 1) * 64, d_ * 4 + h: d_ * 4 + h + 1],
                        func=AF.Exp, scale=128.0), [lg], [g128])
        gn = k.sb("gn", [128, 256], F32)
        k.dma("sp", lambda e: e.dma_start(out=gn[:], in_=A["ret_norm_g"][l, :].partition_broadcast(128)), writes=[gn])
        maskb = k.sb("maskb", [128, 3, 128], BF16)
        k.V(lambda e: e.tensor_copy(out=maskb[:], in_=P["masks"][:]), [P["masks"]], [maskb])

        EPSB = P["epsb"]
        RT = k.sb("RT", [128, 2, 2, 2, NT * 128], BF16)
        KD = k.sb("KD", [128, NT, 2, 256], BF16)
        Vb = k.sb("Vb", [128, NT, 256], BF16)
        SG = k.sb("SG", [128, NT, 256], BF16)
        OA = k.sb("OA", [128, NT, 256], F32)
        ur = [k.sb("ur%d" % i, [128, 1024], F32) for i in range(3)]
        t1 = [k.sb("t1%d" % i, [128, 512], F32) for i in range(2)]
        t2 = [k.sb("t2%d" % i, [128, 512], F32) for i in range(2)]
        rot = [k.sb("rot%d" % i, [128, 512], F32) for i in range(3)]
        vr = [k.sb("vr%d" % i, [128, 2, 2, 256], BF16) for i in range(4)]
        ptr = [k.ps("ptr%d" % i, [128, 8, 128], BF16) for i in range(1)]
        pssA = [k.ps("pssA%d" % i, [128, 512], F32) for i in range(2)]
        pssB = [k.ps("pssB%d" % i, [128, 512], F32) for i in range(2)]
        pso = [k.ps("pso%d" % i, [128, 4, 64], F32) for i in range(2)]
        pkvt = k.ps("pkvt", [128, 2, 2, 128], F32)
        STm = [k.sb("STm%d" % i, [128, 4, 128], BF16) for i in range(2)]
        Sst = [k.sb("Sst%d" % i, [128, 2, 128], F32) for i in range(2)]
        Sb = [k.sb("Sb%d" % i, [128, 2, 128], BF16) for i in range(2)]
        tmp = [k.sb("tmp%d" % i, [128, 2, 128], F32) for i in range(2)]
        osum = [k.sb("osum%d" % i, [128, 256], F32) for i in range(3)]
        mean = [k.sb("mean%d" % i, [128, 4], F32) for i in range(3)]
        cen = [k.sb("cen%d" % i, [128, 256], F32) for i in range(4)]
        sq = [k.sb("sq%d" % i, [128, 256], F32) for i in range(2)]
        var = [k.sb("var%d" % i, [128, 4], F32) for i in range(3)]
        rof = [k.sb("rof%d" % i, [128, 256], F32) for i in range(3)]
        ro = [k.sb("ro%d" % i, [128, 256], BF16) for i in range(2)]

        for s in range(NS):
            def r0(i, s=s):
                u = ur[i % 3]
                k.dma("sp", lambda e: e.dma_start(out=u[:], in_=A["u_d"][s, i * 128:(i + 1) * 128, 768:1792]),
                      reads=["u_d"], writes=[u])

            def r1(i):
                u, a1, a2 = ur[i % 3], t1[i % 2], t2[i % 2]
                qk_v = u[:, 0:512].rearrange("p (h x d) -> p h x d", h=8, x=2, d=32)
                t1v = a1[:].rearrange("p (h x d) -> p h x d", h=8, x=2, d=32)
                t2v = a2[:].rearrange("p (h x d) -> p h x d", h=8, x=2, d=32)
                Cv = retC[:, i, :].rearrange("p (x d) -> p x d", x=2, d=32)
                Sv = retS[:, i, :].rearrange("p (x d) -> p x d", x=2, d=32)
                for x in range(2):
                    k.V(lambda e, x=x: e.tensor_tensor(
                        out=t1v[:, :, x, :], in0=qk_v[:, :, x, :], in1=Cv[:, x, :].unsqueeze(1).to_broadcast([128, 8, 32]),
                        op=ALU.mult), [u, retC], [a1])
                    k.G(lambda e, x=x: e.tensor_tensor(
                        out=t2v[:, :, x, :], in0=qk_v[:, :, 1 - x, :], in1=Sv[:, x, :].unsqueeze(1).to_broadcast([128, 8, 32]),
                        op=ALU.mult), [u, retS], [a2])
                k.V(lambda e: e.tensor_copy(out=Vb[:, i, :], in_=u[:, 512:768]), [u], [("Vb", i)])
                k.A(lambda e: e.activation(out=SG[:, i, :], in_=u[:, 768:1024], func=AF.Silu), [u], [("SG", i)])

            def r2(i):
                a1, a2, rt_ = t1[i % 2], t2[i % 2], rot[i % 3]
                k.V(lambda e: e.tensor_tensor(out=rt_[:], in0=a1[:], in1=a2[:], op=ALU.add), [a1, a2], [rt_])

            def r3(i):
                rt_, v_r = rot[i % 3], vr[i % 4]
                for d_ in range(2):
                    eng = k.V if d_ == 0 else k.G
                    eng(lambda e, d_=d_: e.tensor_tensor(
                        out=v_r[:, d_, :, :].rearrange("p q (h d) -> p (q h) d", d=64),
                        in0=rt_[:].rearrange("p (qh d) -> p qh d", d=64),
                        in1=DT[:, d_, :, :].rearrange("p q h -> p (q h)").unsqueeze(2).to_broadcast([128, 8, 64]),
                        op=ALU.mult), [rt_, DT], [(v_r.name, d_)])

            def r4(i):
                v_r, p = vr[i % 4], ptr[0]
                for d_ in range(2):
                    k.G(lambda e, d_=d_: e.tensor_copy(out=KD[:, i, d_, :], in_=v_r[:, d_, 1, :]), [(v_r.name, d_)], [("KD", i)])
                for d_ in range(2):
                    for qk in range(2):
                        for pr in range(2):
                            k.T(lambda e, d_=d_, qk=qk, pr=pr: e.transpose(
                                p[:, d_ * 4 + qk * 2 + pr, :], v_r[:, d_, qk, pr * 128:(pr + 1) * 128], identb[:]),
                                [(v_r.name, d_), identb], [p])

            def r5(i):
                p = ptr[0]
                k.A(lambda e: e.copy(out=RT[:, :, :, :, i * 128:(i + 1) * 128].rearrange("p a b c t -> p (a b c) t"),
                                     in_=p[:]), [p], [("RT", i)])

            run_pipe(NT, [r0, r1, r2, r3, r4, r5])

            orders = [list(range(NT)), [1, 0] + list(range(NT - 1, 1, -1))]
            for d_ in range(2):
                k.V(lambda e, d_=d_: e.memset(Sst[d_][:], 0.0), [], [Sst[d_]])
                k.V(lambda e, d_=d_: e.memset(Sb[d_][:], 0.0), [], [Sb[d_]])
            oak = [("OA", t_) for t_ in range(NT)]
            k.G(lambda e: e.memset(OA[:], 0.0), oak, oak)
            for ci in range(NT):
                tt = [orders[0][ci], orders[1][ci]]
                for d_ in range(2):
                    t = tt[d_]
                    for h in (0, 2, 1, 3):
                        pr, hh = h // 2, h % 2
                        pss = pssA[d_] if hh == 0 else pssB[d_]
                        k.T(lambda e, pss=pss, pr=pr, hh=hh, t=t, d_=d_: e.matmul(
                            pss[:, pr * 128:(pr + 1) * 128], lhsT=RT[hh * 64:(hh + 1) * 64, d_, 1, pr, t * 128:(t + 1) * 128],
                            rhs=RT[hh * 64:(hh + 1) * 64, d_, 0, pr, t * 128:(t + 1) * 128], start=True, stop=True),
                            [("RT", t)], [pss])
                for d_ in range(2):
                    mk = 0 if d_ == 0 else 2
                    stm = STm[d_]
                    sv = stm[:].rearrange("p (pr hh) j -> p pr hh j", hh=2)
                    for hh, pss in ((0, pssA[d_]), (1, pssB[d_])):
                        k.V(lambda e, hh=hh, pss=pss, mk=mk, sv=sv: e.tensor_tensor(
                            out=sv[:, :, hh, :], in0=pss[:, 0:256].rearrange("p (pr j) -> p pr j", pr=2),
                            in1=maskb[:, mk, :].unsqueeze(1).to_broadcast([128, 2, 128]), op=ALU.mult),
                            [pss, maskb], [(stm.name, hh)])
                for d_ in range(2):
                    t = tt[d_]
                    stm, po_, Sbb = STm[d_], pso[d_], Sb[d_]
                    for h in range(4):
                        pr, hh = h // 2, h % 2
                        k.T(lambda e, h=h, t=t, stm=stm, po_=po_: e.matmul(
                            po_[:, h, :], lhsT=stm[:, h, :], rhs=Vb[:, t, h * 64:(h + 1) * 64], start=True, stop=False),
                            [(stm.name, hh), ("Vb", t)], [po_])
                        k.T(lambda e, h=h, pr=pr, hh=hh, t=t, d_=d_, Sbb=Sbb, po_=po_: e.matmul(
                            po_[:, h, :], lhsT=RT[hh * 64:(hh + 1) * 64, d_, 0, pr, t * 128:(t + 1) * 128],
                            rhs=Sbb[hh * 64:(hh + 1) * 64, pr, hh * 64:(hh + 1) * 64], start=False, stop=True),
                            [("RT", t), Sbb], [po_])
                    if ci < NT - 1:
                        for pr in range(2):
                            k.T(lambda e, pr=pr, t=t, d_=d_: e.matmul(
                                pkvt[:, d_, pr, :], lhsT=KD[:, t, d_, pr * 128:(pr + 1) * 128], rhs=Vb[:, t, pr * 128:(pr + 1) * 128],
                                start=True, stop=True), [("KD", t), ("Vb", t)], [("pkv", d_)])
                for d_ in range(2):
                    t = tt[d_]
                    po_, St, Sbb, tm = pso[d_], Sst[d_], Sb[d_], tmp[d_]
                    k.V(lambda e, po_=po_, t=t: e.tensor_tensor(out=OA[:, t, :], in0=OA[:, t, :],
                                                                in1=po_[:].rearrange("p h d -> p (h d)"), op=ALU.add),
                        [po_, ("OA", t)], [("OA", t)])
                    if ci < NT - 1:
                        k.V(lambda e, St=St, tm=tm, d_=d_: e.tensor_tensor(out=tm[:], in0=pkvt[:, d_, :, :], in1=St[:], op=ALU.add),
                            [("pkv", d_), St], [tm])
                        for pr in range(2):
                            k.V(lambda e, pr=pr, St=St, d_=d_, tm=tm: e.tensor_scalar(
                                out=St[:, pr, :], in0=tm[:, pr, :], scalar1=g128[:, d_, pr:pr + 1], scalar2=None,
                                op0=ALU.mult), [tm, g128], [St])
                            k.A(lambda e, pr=pr, Sbb=Sbb, d_=d_, tm=tm: e.activation(
                                out=Sbb[:, pr, :], in_=tm[:, pr, :], func=AF.Copy, scale=g128[:, d_, pr:pr + 1]),
                                [tm, g128], [Sbb])

            tiles = list(range(2, NT)) + ([] if last else [0, 1])

            def g0(i):
                t = tiles[i]
                os_, mn = osum[i % 3], mean[i % 3]
                k.G(lambda e: e.tensor_copy(out=os_[:], in_=OA[:, t, :]), [("OA", t)], [os_])
                k.V(lambda e: e.tensor_reduce(out=mn[:], in_=os_[:].rearrange("p (h d) -> p h d", d=64), axis=AX.X,
                                              op=ALU.add), [os_], [mn])
                k.V(lambda e: e.tensor_scalar(out=mn[:], in0=mn[:], scalar1=1.0 / 64, scalar2=None, op0=ALU.mult), [mn], [mn])

            def g1(i):
                os_, mn, cn, s2 = osum[i % 3], mean[i % 3], cen[i % 4], sq[i % 2]
                k.V(lambda e: e.tensor_tensor(out=cn[:].rearrange("p (h d) -> p h d", d=64),
                                              in0=os_[:].rearrange("p (h d) -> p h d", d=64),
                                              in1=mn[:].unsqueeze(2).to_broadcast([128, 4, 64]), op=ALU.subtract),
                    [os_, mn], [cn])
                k.G(lambda e: e.tensor_tensor(out=s2[:], in0=cn[:], in1=cn[:], op=ALU.mult), [cn], [s2])

            def g2(i):
                s2, vv = sq[i % 2], var[i % 3]
                k.V(lambda e: e.tensor_reduce(out=vv[:], in_=s2[:].rearrange("p (h d) -> p h d", d=64), axis=AX.X,
                                              op=ALU.add), [s2], [vv])
                k.A(lambda e: e.activation(out=vv[:], in_=vv[:], func=AF.Ln, scale=1.0 / 64, bias=EPSB[:, 0:1]), [vv, EPSB], [vv])
                k.A(lambda e: e.activation(out=vv[:], in_=vv[:], func=AF.Exp, scale=-0.5), [vv], [vv])

            def g3(i):
                cn, vv, rf = cen[i % 4], var[i % 3], rof[i % 3]
                k.V(lambda e: e.tensor_tensor(out=rf[:].rearrange("p (h d) -> p h d", d=64),
                                              in0=cn[:].rearrange("p (h d) -> p h d", d=64),
                                              in1=vv[:].unsqueeze(2).to_broadcast([128, 4, 64]), op=ALU.mult), [cn, vv], [rf])
                k.G(lambda e: e.tensor_tensor(out=rf[:], in0=rf[:], in1=gn[:], op=ALU.mult), [rf, gn], [rf])

            def g4(i, s=s):
                t = tiles[i]
                rf, r_o = rof[i % 3], ro[i % 2]
                k.V(lambda e: e.tensor_tensor(out=r_o[:], in0=rf[:], in1=SG[:, t, :], op=ALU.mult), [rf, ("SG", t)], [r_o])
                k.dma("act", lambda e: e.dma_start(out=A["mixin_d"][s, t * 128:(t + 1) * 128, 512:768], in_=r_o[:]),
                      reads=[r_o], writes=["mixin_d"])

            run_pipe(len(tiles), [g0, g1, g2, g3, g4])


def phase_conv(k, A, P, l, last):
    with k.phase():
        ident = P["ident"]
        EPSB = P["epsb"]
        cwT = k.sb("cwT", [128, 2, 32], F32)
        vT = k.sb("vT", [128, 2, 4], F32)
        with k.phase():
            cw = k.sb("cw", [31, 256], F32)
            k.dma("sp", lambda e: e.dma_start(out=cw[:], in_=A["conv_w"][l]), writes=[cw])
            pw = k.ps("pw", [128, 2, 32], F32)
            for cc in range(2):
                k.T(lambda e, cc=cc: e.transpose(pw[:, cc, 0:31], cw[0:31, cc * 128:(cc + 1) * 128], ident[0:31, 0:31]),
                    [cw, ident], [pw])
            k.V(lambda e: e.tensor_copy(out=cwT[:, :, 0:31], in_=pw[:, :, 0:31]), [pw], [cwT])
            vec = k.sb("vec", [3, 256], F32)
            k.dma("sp", lambda e: e.dma_start(out=vec[0:1, :], in_=A["conv_b"][l:l + 1, :]), writes=[vec])
            k.dma("sp", lambda e: e.dma_start(out=vec[1:2, :], in_=A["conv_norm_g"][l:l + 1, :]), writes=[vec])
            k.dma("sp", lambda e: e.dma_start(out=vec[2:3, :], in_=A["conv_norm_b"][l:l + 1, :]), writes=[vec])
            pv = k.ps("pv", [128, 2, 4], F32)
            for cc in range(2):
                k.T(lambda e, cc=cc: e.transpose(pv[:, cc, 0:3], vec[0:3, cc * 128:(cc + 1) * 128], ident[0:3, 0:3]),
                    [vec, ident], [pv])
            k.V(lambda e: e.tensor_copy(out=vT[:, :, 0:3], in_=pv[:, :, 0:3]), [pv], [vT])
        diag = k.sb("diag", [128, 2, 31, 128], BF16)
        for cc in range(2):
            for kk in range(31):
                eng = k.V if kk % 2 == 0 else k.G
                eng(lambda e, cc=cc, kk=kk: e.tensor_scalar(out=diag[:, cc, kk, :], in0=ident[:],
                                                            scalar1=cwT[:, cc, kk:kk + 1], scalar2=None, op0=ALU.mult),
                    [ident, cwT], [diag])
        ones = k.sb("ones", [128, 128], F32R)
        onesf = k.sb("onesf", [128, 128], F32)
        k.V(lambda e: e.memset(onesf[:], 1.0 / 256), [], [onesf])
        k.V(lambda e: e.tensor_copy(out=ones[:], in_=onesf[:]), [onesf], [ones])

        LP = S + 30
        hp = [[k.sb("hp%d_%d" % (j, cc), [128, LP], BF16) for cc in range(2)] for j in range(2)]
        val = [[k.sb("val%d_%d" % (j, cc), [128, S], F32) for cc in range(2)] for j in range(2)]
        gt = [[k.sb("gt%d_%d" % (j, cc), [128, S], F32) for cc in range(2)] for j in range(2)]
        cvo = [[k.sb("cvo%d_%d" % (j, cc), [128, 512], F32R) for cc in range(2)] for j in range(5)]
        csq = [[k.sb("csq%d_%d" % (j, cc), [128, 512], F32R) for cc in range(2)] for j in range(2)]
        pcv = [[k.ps("pcv%d_%d" % (j, cc), [128, 512], F32) for cc in range(2)] for j in range(2)]
        pmean = [k.ps("pmean%d" % j, [128, 512], F32) for j in range(2)]
        pex2 = [k.ps("pex2%d" % j, [128, 512], F32) for j in range(2)]
        msb = [k.sb("msb%d" % j, [128, 512], F32) for j in range(3)]
        rsd = [k.sb("rsd%d" % j, [128, 512], F32) for j in range(3)]
        yv = [[k.sb("yv%d_%d" % (j, cc), [128, 512], F32) for cc in range(2)] for j in range(2)]
        yo = [[k.sb("yo%d_%d" % (j, cc), [128, 512], BF16) for cc in range(2)] for j in range(2)]
        segs = []
        for s in range(NS):
            segs.append((s, C, S))
            if not last:
                segs.append((s, 0, C))
        items = []
        for si, (s, tok0, Lg) in enumerate(segs):
            nb = (Lg + 511) // 512
            for b in range(nb):
                items.append((si, s, tok0, Lg, b, min(512, Lg - b * 512)))

        def pre(i):
            si, s, tok0, Lg, b, w = items[i]
            if b != 0:
                return
            j = si % 2
            for cc in range(2):
                k.dma("sp", lambda e, cc=cc: e.dma_start(
                    out=val[j][cc][:, 0:Lg], in_=A["cv_d"][s, cc * 128:(cc + 1) * 128, tok0:tok0 + Lg]),
                    reads=["cv_d"], writes=[val[j][cc]])
                k.dma("act", lambda e, cc=cc: e.dma_start(
                    out=gt[j][cc][:, 0:Lg], in_=A["cv_d"][s, 256 + cc * 128: 256 + (cc + 1) * 128, tok0:tok0 + Lg]),
                    reads=["cv_d"], writes=[gt[j][cc]])
                k.A(lambda e, cc=cc: e.activation(out=gt[j][cc][:, 0:Lg], in_=gt[j][cc][:, 0:Lg], func=AF.Sigmoid),
                    [gt[j][cc]], [gt[j][cc]])
                k.G(lambda e, cc=cc: e.memset(hp[j][cc][:], 0.0), [], [hp[j][cc]])
                k.V(lambda e, cc=cc: e.tensor_tensor(out=hp[j][cc][:, 15:15 + Lg], in0=val[j][cc][:, 0:Lg],
                                                     in1=gt[j][cc][:, 0:Lg], op=ALU.mult),
                    [val[j][cc], gt[j][cc]], [hp[j][cc]])

        def nop(i):
            return

        def c0(i):
            si, s, tok0, Lg, b, w = items[i]
            j = si % 2
            for cc in range(2):
                p = pcv[i % 2][cc]
                for kk in range(31):
                    k.T(lambda e, p=p, cc=cc, kk=kk: e.matmul(
                        p[:, 0:w], lhsT=diag[:, cc, kk, :], rhs=hp[j][cc][:, b * 512 + kk: b * 512 + kk + w],
                        start=(kk == 0), stop=(kk == 30)), [diag, hp[j][cc]], [p])

        def c1(i):
            si, s, tok0, Lg, b, w = items[i]
            for cc in range(2):
                p, co, cq = pcv[i % 2][cc], cvo[i % 5][cc], csq[i % 2][cc]
                k.A(lambda e, p=p, co=co, cc=cc: e.activation(out=co[:, 0:w], in_=p[:, 0:w], func=AF.Identity,
                                                             bias=vT[:, cc, 0:1], scale=1.0), [p, vT], [co])
                k.V(lambda e, co=co, cq=cq: e.tensor_tensor(out=cq[:, 0:w], in0=co[:, 0:w].bitcast(F32),
                                                            in1=co[:, 0:w].bitcast(F32), op=ALU.mult), [co], [cq])

        def c2(i):
            si, s, tok0, Lg, b, w = items[i]
            pm_, px_ = pmean[i % 2], pex2[i % 2]
            for cc in range(2):
                k.T(lambda e, cc=cc: e.matmul(pm_[:, 0:w], lhsT=ones[:], rhs=cvo[i % 5][cc][:, 0:w],
                                              start=(cc == 0), stop=(cc == 1)), [ones, cvo[i % 5][cc]], [pm_])
            for cc in range(2):
                k.T(lambda e, cc=cc: e.matmul(px_[:, 0:w], lhsT=ones[:], rhs=csq[i % 2][cc][:, 0:w],
                                              start=(cc == 0), stop=(cc == 1)), [ones, csq[i % 2][cc]], [px_])

        def c3(i):
            si, s, tok0, Lg, b, w = items[i]
            pm_, px_, ms, rs = pmean[i % 2], pex2[i % 2], msb[i % 3], rsd[i % 3]
            k.A(lambda e: e.copy(out=ms[:, 0:w], in_=pm_[:, 0:w]), [pm_], [ms])
            k.V(lambda e: e.tensor_tensor(out=rs[:, 0:w], in0=ms[:, 0:w], in1=ms[:, 0:w], op=ALU.mult), [ms], [rs])
            k.V(lambda e: e.tensor_tensor(out=rs[:, 0:w], in0=px_[:, 0:w], in1=rs[:, 0:w], op=ALU.subtract), [px_, rs], [rs])

        def c4(i):
            si, s, tok0, Lg, b, w = items[i]
            rs = rsd[i % 3]
            k.A(lambda e: e.activation(out=rs[:, 0:w], in_=rs[:, 0:w], func=AF.Ln, bias=EPSB[:, 0:1], scale=1.0), [rs, EPSB], [rs])
            k.A(lambda e: e.activation(out=rs[:, 0:w], in_=rs[:, 0:w], func=AF.Exp, scale=-0.5), [rs], [rs])

        def c5(i):
            si, s, tok0, Lg, b, w = items[i]
            ms, rs = msb[i % 3], rsd[i % 3]
            for cc in range(2):
                co, y = cvo[i % 5][cc], yv[i % 2][cc]
                k.G(lambda e, co=co, y=y: e.tensor_tensor(out=y[:, 0:w], in0=co[:, 0:w].bitcast(F32), in1=ms[:, 0:w],
                                                          op=ALU.subtract), [co, ms], [y])
                k.V(lambda e, y=y: e.tensor_tensor(out=y[:, 0:w], in0=y[:, 0:w], in1=rs[:, 0:w], op=ALU.mult), [y, rs], [y])

        def c6(i):
            si, s, tok0, Lg, b, w = items[i]
            for cc in range(2):
                y, o_ = yv[i % 2][cc], yo[i % 2][cc]
                k.A(lambda e, y=y, o_=o_, cc=cc: e.activation(out=o_[:, 0:w], in_=y[:, 0:w], func=AF.Silu,
                                                             scale=vT[:, cc, 1:2], bias=vT[:, cc, 2:3]), [y, vT], [o_])
                k.dma("act", lambda e, cc=cc, o_=o_: e.dma_start(
                    out=A["convT_d"][s, cc * 128:(cc + 1) * 128, tok0 + b * 512: tok0 + b * 512 + w],
                    in_=o_[:, 0:w]), reads=[o_], writes=["convT_d"])

        run_pipe(len(items), [pre, nop, c0, c1, c2, c3, c4, c5, c6])


def phase_out(k, A, P, l, last, src_lat, src_ctx, dst_lat):
    with k.phase():
        ident = P["ident"]
        identb = P["identb"]
        EPSB = P["epsb"]
        wout = k.sb("wout", [128, 8, D], BF16)
        wv = A["w_out"][l].rearrange("(kc p) f -> p kc f", p=128)
        for j in range(0, D, 512):
            k.dma("pool", lambda e, j=j: e.dma_start(out=wout[:, :, j:j + 512], in_=wv[:, :, j:j + 512]), writes=[("wout", j)])
        woutk = [("wout", 0), ("wout", 512)]
        wr = k.sb("wr", [128, 8, E], F32)
        k.dma("sp", lambda e: e.dma_start(out=wr[:], in_=A["w_router"][l].rearrange("(kc p) e -> p kc e", p=128)),
              writes=[wr])
        nr = NS + (0 if last else 1)
        gta = [k.sb("gta%d" % r, [128, D], F32) for r in range(nr)]
        gsf = [k.sb("gsf%d" % r, [128, D], F32) for r in range(nr)]
        shf = [k.sb("shf%d" % r, [128, D], F32) for r in range(nr)]
        gf = k.sb("gf", [128, D], F32)
        k.dma("sp", lambda e: e.dma_start(out=gf[:], in_=A["g_ffn"][l, :].partition_broadcast(128)), writes=[gf])
        for r in range(nr):
            k.dma("sp", lambda e, r=r: e.dma_start(out=gta[r][:], in_=A["mod_d"][l, r, 2 * D:3 * D].partition_broadcast(128)),
                  reads=["mod_d"], writes=[gta[r]])
            k.dma("sp", lambda e, r=r: e.dma_start(out=shf[r][:], in_=A["mod_d"][l, r, 3 * D:4 * D].partition_broadcast(128)),
                  reads=["mod_d"], writes=[shf[r]])
            k.dma("sp", lambda e, r=r: e.dma_start(out=gsf[r][:], in_=A["mod_d"][l, r, 4 * D:5 * D].partition_broadcast(128)),
                  reads=["mod_d"], writes=[gsf[r]])
            k.V(lambda e, r=r: e.scalar_tensor_tensor(out=gsf[r][:], in0=gsf[r][:], scalar=1.0, in1=gf[:], op0=ALU.add,
                                                      op1=ALU.mult), [gsf[r], gf], [gsf[r]])
        NB = 6
        mi = [k.sb("mi%d" % i, [128, 768], BF16) for i in range(3)]
        mixT = [k.sb("mixT%d" % i, [128, 8, 128], BF16) for i in range(4)]
        xt = [k.sb("xt%d" % i, [128, D], F32) for i in range(5)]
        xm = [k.sb("xm%d" % i, [128, D], F32) for i in range(5)]
        hf = [k.sb("hf%d" % i, [128, D], F32) for i in range(4)]
        hfb = [k.sb("hfb%d" % i, [128, D], BF16) for i in range(2)]
        junk = k.sb("junk", [128, D], BF16)
        ss = [k.sb("ss%d" % i, [128, 1], F32) for i in range(NB)]
        rstd = [k.sb("rstd%d" % i, [128, 1], F32) for i in range(NB)]
        hfT = [k.sb("hfT%d" % i, [128, 8, 128], F32) for i in range(2)]
        ptm = k.ps("ptm", [128, 6, 128], BF16)
        py = [k.ps("py%d" % i, [128, 512], F32) for i in range(2)]
        pth = k.ps("pth", [128, 8, 128], F32)
        plg = k.ps("plg", [128, 16], F32)
        pat = k.ps("pat", [48, 128], F32)
        lmax = [k.sb("lmax%d" % i, [128, 1], F32) for i in range(2)]
        lsum = [k.sb("lsum%d" % i, [128, 1], F32) for i in range(2)]
        afftm = [k.sb("afftm%d" % i, [128, 48], F32) for i in range(3)]
        affT = k.sb("affT", [48, S + C], F32)
        for a_ in afftm:
            k.V(lambda e, a_=a_: e.memset(a_[:], 0.0), [], [a_])
        tiles = list(range(2, NT)) + ([] if last else [0, 1])
        items = [(t, s) for t in tiles for s in range(NS)]

        def info(i):
            t, s = items[i]
            isctx = t < 2
            r = NS if isctx else s
            if isctx:
                src = src_ctx[s * C + t * 128: s * C + (t + 1) * 128, :]
                dst = A["res_d"][NLAT + s * C + t * 128: NLAT + s * C + (t + 1) * 128, :]
                hrow = NLAT + s * C + t * 128
            else:
                src = src_lat[s * S + (t - 2) * 128: s * S + (t - 1) * 128, :]
                dst = dst_lat[s * S + (t - 2) * 128: s * S + (t - 1) * 128, :]
                hrow = s * S + (t - 2) * 128
            return t, s, r, src, dst, hrow

        NX, NM, NH = 5, 4, 4

        def stL(i):
            t, s, r, src, dst, hrow = info(i)
            m, mt, x_t = mi[i % 3], mixT[i % NM], xt[i % NX]
            k.dma("sp", lambda e: e.dma_start(out=m[:], in_=A["mixin_d"][s, t * 128:(t + 1) * 128, :]),
                  reads=["mixin_d"], writes=[m])
            k.dma("sp", lambda e: e.dma_start(
                out=mt[:, 6:8, :], in_=A["convT_d"][s, :, t * 128:(t + 1) * 128].rearrange("(c p) t -> p c t", p=128)),
                reads=["convT_d"], writes=[mt])
            k.dma("act", lambda e: e.dma_start(out=x_t[:], in_=src), writes=[x_t])

        def stA1(i):
            m, mt = mi[i % 3], mixT[i % NM]
            for j in range(6):
                k.T(lambda e, j=j: e.transpose(ptm[:, j, :], m[:, j * 128:(j + 1) * 128], identb[:]), [m, identb], [ptm])
            k.A(lambda e: e.copy(out=mt[:, 0:6, :], in_=ptm[:]), [ptm], [mt])

        def stA2(i):
            t, s, r, src, dst, hrow = info(i)
            mt, x_m = mixT[i % NM], xm[i % NX]
            for hh in range(2):
                p = py[hh]
                for kc in range(8):
                    k.T(lambda e, p=p, kc=kc, hh=hh: e.matmul(p[:], lhsT=mt[:, kc, :], rhs=wout[:, kc, hh * 512:(hh + 1) * 512],
                                                              start=(kc == 0), stop=(kc == 7)), [mt] + woutk, [p])
                k.V(lambda e, p=p, hh=hh: e.tensor_tensor(out=x_m[:, hh * 512:(hh + 1) * 512], in0=p[:],
                                                          in1=gta[r][:, hh * 512:(hh + 1) * 512], op=ALU.mult),
                    [p, gta[r]], [x_m])

        def stA3(i):
            t, s, r, src, dst, hrow = info(i)
            x_t, x_m = xt[i % NX], xm[i % NX]
            k.G(lambda e: e.tensor_tensor(out=x_m[:], in0=x_m[:], in1=x_t[:], op=ALU.add), [x_m, x_t], [x_m])
            k.dma("sp", lambda e: e.dma_start(out=dst, in_=x_m[:]), reads=[x_m], writes=["res"])

        def stB1(i):
            x_m, sst, rs = xm[i % NX], ss[i % NX], rstd[i % NX]
            k.A(lambda e: e.activation(out=junk[:], in_=x_m[:], func=AF.Square, accum_out=sst[:, 0:1]), [x_m], [junk, sst])
            k.A(lambda e: e.activation(out=sst[:, 0:1], in_=sst[:, 0:1], func=AF.Ln, scale=1.0 / D, bias=EPSB[:, 0:1]),
                [sst, EPSB], [sst])
            k.A(lambda e: e.activation(out=rs[:, 0:1], in_=sst[:, 0:1], func=AF.Exp, scale=-0.5), [sst], [rs])

        def stB2(i):
            t, s, r, src, dst, hrow = info(i)
            x_m, h_f, rs = xm[i % NX], hf[i % NH], rstd[i % NX]
            k.V(lambda e: e.scalar_tensor_tensor(out=h_f[:], in0=x_m[:], scalar=rs[:, 0:1], in1=gsf[r][:], op0=ALU.mult,
                                                 op1=ALU.mult), [x_m, rs, gsf[r]], [h_f])

        def stB3(i):
            t, s, r, src, dst, hrow = info(i)
            h_f = hf[i % NH]
            k.G(lambda e: e.tensor_tensor(out=h_f[:], in0=h_f[:], in1=shf[r][:], op=ALU.add), [h_f, shf[r]], [h_f])

        def stB4(i):
            t, s, r, src, dst, hrow = info(i)
            h_f, h_b = hf[i % NH], hfb[i % 2]
            for kc in range(8):
                k.T(lambda e, kc=kc: e.transpose(pth[:, kc, :], h_f[:, kc * 128:(kc + 1) * 128], ident[:]), [h_f, ident], [pth])
            k.A(lambda e: e.copy(out=h_b[:], in_=h_f[:]), [h_f], [h_b])
            k.dma("act", lambda e: e.dma_start(out=A["hfb_d"][hrow:hrow + 128, :], in_=h_b[:]), reads=[h_b], writes=["hfb_d"])

        def stC1(i):
            h_T = hfT[i % 2]
            k.A(lambda e: e.copy(out=h_T[:], in_=pth[:]), [pth], [h_T])

        def stC2(i):
            h_T = hfT[i % 2]
            for kc in range(8):
                k.T(lambda e, kc=kc: e.matmul(plg[:], lhsT=h_T[:, kc, :], rhs=wr[:, kc, :], start=(kc == 0), stop=(kc == 7)),
                    [h_T, wr], [plg])
            lm = lmax[i % 2]
            k.V(lambda e: e.tensor_reduce(out=lm[:], in_=plg[:], axis=AX.X, op=ALU.max, negate=True), [plg], [lm])

        def stC3(i):
            t, s, r, src, dst, hrow = info(i)
            af = afftm[(i // NS) % 3]
            lm, ls = lmax[i % 2], lsum[i % 2]
            k.A(lambda e: e.activation(out=af[:, s * 32:s * 32 + 16], in_=plg[:], func=AF.Exp, bias=lm[:, 0:1],
                                       scale=1.0, accum_out=ls[:, 0:1]), [plg, lm], [af, ls])

        def stC4(i):
            t, s, r, src, dst, hrow = info(i)
            af = afftm[(i // NS) % 3]
            ls = lsum[i % 2]
            k.V(lambda e: e.reciprocal(out=ls[:], in_=ls[:]), [ls], [ls])
            k.V(lambda e: e.tensor_scalar(out=af[:, s * 32:s * 32 + 16], in0=af[:, s * 32:s * 32 + 16],
                                          scalar1=ls[:, 0:1], scalar2=None, op0=ALU.mult), [af, ls], [af])
            if s == NS - 1:
                k.T(lambda e: e.transpose(pat[:], af[:], ident[:]), [af, ident], [pat])

        def stC5(i):
            t, s, r, src, dst, hrow = info(i)
            if s == NS - 1:
                k.A(lambda e: e.copy(out=affT[:, t * 128:(t + 1) * 128], in_=pat[:]), [pat], [affT])

        stages = [stL, stA1, stA2, stA3, stB1, stB2, stB3, stB4, stC1, stC2, stC3, stC4, stC5]
        n = len(items)
        ns = len(stages)
        for step in range(n + ns - 1):
            for j in range(ns - 1, -1, -1):
                i = step - j
                if 0 <= i < n:
                    stages[j](i)
        k.dma("sp", lambda e: e.dma_start(out=A["aff_d"], in_=affT[:, C:C + S]), reads=[affT], writes=["aff_d"])
        if not last:
            k.dma("sp", lambda e: e.dma_start(out=A["affc_d"], in_=affT[:, 0:C]), reads=[affT], writes=["affc_d"])


def phase_moe(k, A, P, l, last, dst_lat):
    with k.phase():
        ident = P["ident"]
        nctx = 0 if last else NS
        NSL = NS * CAPL + nctx * CAPC
        if last:
            cgroups = [(0, 512)]
        else:
            cgroups = [(0, 288), (288, 288)]
        idxT = k.sb("idxT", [128, 2, 48], I32)
        gT = k.sb("gTm", [128, 2, 48], F32)
        if not last:
            idcT = k.sb("idcT", [32, 48], I32)
            gcT = k.sb("gcT", [32, 48], F32)
        with k.phase():
            wa = k.sb("wa", [48, S], F32)
            wb = k.sb("wb", [48, S], F32)
            vals = k.sb("vals", [48, CAPL], F32)
            idx = k.sb("idx", [48, CAPL], U32)
            idxf = k.sb("idxf", [48, CAPL], F32)
            offs = k.sb("offs", [48, 1], F32)
            k.dma("sp", lambda e: e.dma_start(out=wa[:], in_=A["aff_d"]), reads=["aff_d"], writes=[wa])
            cur, oth = wa, wb
            for r in range(CAPL // 8):
                k.V(lambda e, cur=cur, r=r: e.max(out=vals[:, r * 8:(r + 1) * 8], in_=cur[:]), [cur], [vals])
                k.V(lambda e, cur=cur, r=r: e.max_index(out=idx[:, r * 8:(r + 1) * 8], in_max=vals[:, r * 8:(r + 1) * 8],
                                                        in_values=cur[:]), [cur, vals], [idx])
                if r < CAPL // 8 - 1:
                    k.V(lambda e, cur=cur, oth=oth, r=r: e.match_replace(out=oth[:], in_to_replace=vals[:, r * 8:(r + 1) * 8],
                                                                         in_values=cur[:], imm_value=-1.0), [cur, vals], [oth])
                    cur, oth = oth, cur
            k.V(lambda e: e.memset(offs[0:32, :], 0.0), [], [offs])
            k.V(lambda e: e.memset(offs[32:48, :], float(S)), [], [offs])
            k.V(lambda e: e.tensor_copy(out=idxf[:], in_=idx[:]), [idx], [idxf])
            k.V(lambda e: e.tensor_scalar(out=idxf[:], in0=idxf[:], scalar1=offs[:, 0:1], scalar2=None, op0=ALU.add),
                [idxf, offs], [idxf])
            pti = k.ps("pti", [128, 2, 48], F32)
            ptg = k.ps("ptg", [128, 2, 48], F32)
            for j in range(2):
                k.T(lambda e, j=j: e.transpose(pti[:, j, :], idxf[:, j * 128:(j + 1) * 128], ident[0:48, 0:48]), [idxf, ident], [pti])
                k.T(lambda e, j=j: e.transpose(ptg[:, j, :], vals[:, j * 128:(j + 1) * 128], ident[0:48, 0:48]), [vals, ident], [ptg])
            k.V(lambda e: e.tensor_copy(out=idxT[:], in_=pti[:]), [pti], [idxT])
            k.V(lambda e: e.tensor_copy(out=gT[:], in_=ptg[:]), [ptg], [gT])
            if not last:
                wc = k.sb("wc", [48, C], F32)
                wd_ = k.sb("wd_", [48, C], F32)
                valc = k.sb("valc", [48, CAPC], F32)
                idc = k.sb("idc", [48, CAPC], U32)
                idcf = k.sb("idcf", [48, CAPC], F32)
                offc = k.sb("offc", [48, 1], F32)
                k.dma("sp", lambda e: e.dma_start(out=wc[:], in_=A["affc_d"]), reads=["affc_d"], writes=[wc])
                cur, oth = wc, wd_
                for r in range(CAPC // 8):
                    k.V(lambda e, cur=cur, r=r: e.max(out=valc[:, r * 8:(r + 1) * 8], in_=cur[:]), [cur], [valc])
                    k.V(lambda e, cur=cur, r=r: e.max_index(out=idc[:, r * 8:(r + 1) * 8], in_max=valc[:, r * 8:(r + 1) * 8],
                                                            in_values=cur[:]), [cur, valc], [idc])
                    if r < CAPC // 8 - 1:
                        k.V(lambda e, cur=cur, oth=oth, r=r: e.match_replace(out=oth[:], in_to_replace=valc[:, r * 8:(r + 1) * 8],
                                                                             in_values=cur[:], imm_value=-1.0), [cur, valc], [oth])
                        cur, oth = oth, cur
                k.V(lambda e: e.memset(offc[0:32, :], float(NLAT)), [], [offc])
                k.V(lambda e: e.memset(offc[32:48, :], float(NLAT + C)), [], [offc])
                k.V(lambda e: e.tensor_copy(out=idcf[:], in_=idc[:]), [idc], [idcf])
                k.V(lambda e: e.tensor_scalar(out=idcf[:], in0=idcf[:], scalar1=offc[:, 0:1], scalar2=None, op0=ALU.add),
                    [idcf, offc], [idcf])
                ptic = k.ps("ptic", [32, 2, 48], F32)
                k.T(lambda e: e.transpose(ptic[:, 0, :], idcf[:, 0:32], ident[0:48, 0:48]), [idcf, ident], [ptic])
                k.T(lambda e: e.transpose(ptic[:, 1, :], valc[:, 0:32], ident[0:48, 0:48]), [valc, ident], [ptic])
                k.V(lambda e: e.tensor_copy(out=idcT[:], in_=ptic[:, 0, :]), [ptic], [idcT])
                k.V(lambda e: e.tensor_copy(out=gcT[:], in_=ptic[:, 1, :]), [ptic], [gcT])
        identb = P["identb"]
        nr = NS + (0 if last else 1)
        gtf = [k.sb("gtf%d" % r, [128, D], F32) for r in range(nr)]
        for r in range(nr):
            k.dma("sp", lambda e, r=r: e.dma_start(out=gtf[r][:], in_=A["mod_d"][l, r, 5 * D:6 * D].partition_broadcast(128)),
                  reads=["mod_d"], writes=[gtf[r]])
        xe = [k.sb("xe%d" % i, [128, D], BF16) for i in range(3)]
        xeT = [k.sb("xeT%d" % i, [128, 8, NSL], BF16) for i in range(2)]
        hT = k.sb("hT", [128, NFC, NSL], BF16)
        sg = [k.sb("sg%d" % i, [128, NSL], F32) for i in range(2)]
        NW = 3
        NPRE = NW - 1
        wg = [k.sb("wg%d" % i, [128, 8, 512], BF16) for i in range(NW)]
        wu = [k.sb("wu%d" % i, [128, 8, 512], BF16) for i in range(NW)]
        wdn = k.sb("wdn", [128, NFC, D], BF16)
        ysb = [k.sb("ysb%d" % i, [128, D], F32) for i in range(2)]
        ncg = len(cgroups)
        pg = [k.ps("pg%d" % i, [128, 512], F32) for i in range(ncg)]
        pu = [k.ps("pu%d" % i, [128, 512], F32) for i in range(ncg)]
        pxt = k.ps("pxt", [128, 8, 128], BF16)
        pyd = [k.ps("pyd%d" % i, [128, 512], F32) for i in range(8 - 2 * ncg - 1)]
        target = dst_lat
        pieces = [(i * 512, min(512, DFF - i * 512)) for i in range((DFF + 511) // 512)]
        NP_ = len(pieces)
        cnt = {"y": 0}

        def tiles_of(ex):
            tl = []
            for s in range(NS):
                for j in range(2):
                    tl.append((128, s * 256 + j * 128, idxT[:, j, s * 32 + ex: s * 32 + ex + 1],
                               gT[:, j, s * 32 + ex: s * 32 + ex + 1], s, "lat"))
            if not last:
                for s in range(NS):
                    tl.append((32, NS * CAPL + s * CAPC, idcT[:, s * 32 + ex: s * 32 + ex + 1],
                               gcT[:, s * 32 + ex: s * 32 + ex + 1], NS, "ctx"))
            return tl

        idx_reads = [idxT] + ([] if last else [idcT])

        def gather_T(ex):
            xT = xeT[ex % 2]
            for ti, (rows, c0, iap, gap, r, kind) in enumerate(tiles_of(ex)):
                x_e = xe[ti % 3]
                k.dma("pool", lambda e, x_e=x_e, rows=rows, iap=iap: e.indirect_dma_start(
                    out=x_e[0:rows, :], out_offset=None, in_=A["hfb_d"],
                    in_offset=bass.IndirectOffsetOnAxis(ap=iap[0:rows, :], axis=0)),
                    reads=["hfb_d"] + idx_reads, writes=[x_e])
                for kc in range(8):
                    k.T(lambda e, x_e=x_e, rows=rows, kc=kc: e.transpose(pxt[:, kc, 0:rows], x_e[0:rows, kc * 128:(kc + 1) * 128],
                                                                       identb[0:rows, 0:rows]), [x_e, identb], [pxt])
                if ti % 2 == 0:
                    k.A(lambda e, xT=xT, rows=rows, c0=c0: e.copy(out=xT[:, :, c0:c0 + rows], in_=pxt[:, :, 0:rows]), [pxt], [xT])
                else:
                    k.V(lambda e, xT=xT, rows=rows, c0=c0: e.tensor_copy(out=xT[:, :, c0:c0 + rows], in_=pxt[:, :, 0:rows]),
                        [pxt], [xT])

        def load_gu(ex, pi):
            c0, w = pieces[pi]
            gi_ = (ex * NP_ + pi) % NW
            wgv = A["w_gate"][l, ex].rearrange("(kc p) f -> p kc f", p=128)
            wuv = A["w_up"][l, ex].rearrange("(kc p) f -> p kc f", p=128)
            k.dma("pool", lambda e: e.dma_start(out=wg[gi_][:, :, 0:w], in_=wgv[:, :, c0:c0 + w]), writes=[wg[gi_]])
            k.dma("pool", lambda e: e.dma_start(out=wu[gi_][:, :, 0:w], in_=wuv[:, :, c0:c0 + w]), writes=[wu[gi_]])

        def load_dn(ex, pi):
            c0, w = pieces[pi]
            f0, nf = c0 // 128, w // 128
            wdv = A["w_down"][l, ex].rearrange("(fc p) d -> p fc d", p=128)
            for f in range(f0, f0 + nf, 2):
                k.dma("pool", lambda e, f=f: e.dma_start(out=wdn[:, f:f + 2, :], in_=wdv[:, f:f + 2, :]),
                      writes=[("wdn", f // 2)])

        def gu(ex, pi):
            c0, w = pieces[pi]
            gi_ = (ex * NP_ + pi) % NW
            xT = xeT[ex % 2]
            for f2 in range(w // 128):
                fc = c0 // 128 + f2
                for (W, ps_list) in ((wg[gi_], pg), (wu[gi_], pu)):
                    for gj, (g0, gw) in enumerate(cgroups):
                        p = ps_list[gj]
                        for kc in range(8):
                            k.T(lambda e, p=p, W=W, kc=kc, f2=f2, g0=g0, gw=gw: e.matmul(
                                p[:, 0:gw], lhsT=W[:, kc, f2 * 128:(f2 + 1) * 128], rhs=xT[:, kc, g0:g0 + gw],
                                start=(kc == 0), stop=(kc == 7)), [W, xT], [p])
                s_g = sg[fc % 2]
                for gj, (g0, gw) in enumerate(cgroups):
                    k.A(lambda e, s_g=s_g, gj=gj, g0=g0, gw=gw: e.activation(out=s_g[:, g0:g0 + gw], in_=pg[gj][:, 0:gw],
                                                                            func=AF.Silu), [pg[gj]], [s_g])
                    k.V(lambda e, s_g=s_g, gj=gj, g0=g0, gw=gw, fc=fc: e.tensor_tensor(
                        out=hT[:, fc, g0:g0 + gw], in0=s_g[:, g0:g0 + gw], in1=pu[gj][:, 0:gw], op=ALU.mult),
                        [s_g, pu[gj]], [("hT", fc)])

        def down(ex):
            for ti, (rows, c0, iap, gap, r, kind) in enumerate(tiles_of(ex)):
                y_s = ysb[ti % 2]
                for hh in range(2):
                    p = pyd[cnt["y"] % len(pyd)]
                    cnt["y"] += 1
                    for fc in range(NFC):
                        k.T(lambda e, p=p, rows=rows, c0=c0, fc=fc, hh=hh: e.matmul(
                            p[0:rows, :], lhsT=hT[:, fc, c0:c0 + rows], rhs=wdn[:, fc, hh * 512:(hh + 1) * 512],
                            start=(fc == 0), stop=(fc == NFC - 1)), [("hT", fc), ("wdn", fc // 2)], [p])
                    k.V(lambda e, p=p, rows=rows, y_s=y_s, hh=hh, gap=gap, r=r: e.scalar_tensor_tensor(
                        out=y_s[0:rows, hh * 512:(hh + 1) * 512], in0=p[0:rows, :], scalar=gap[0:rows, :],
                        in1=gtf[r][0:rows, hh * 512:(hh + 1) * 512], op0=ALU.mult, op1=ALU.mult),
                        [p, gT, gtf[r]] + ([] if last else [gcT]), [y_s])
                tgt = target if kind == "lat" else A["res_d"]
                k.dma("pool", lambda e, y_s=y_s, rows=rows, iap=iap, tgt=tgt: e.indirect_dma_start(
                    out=tgt, out_offset=bass.IndirectOffsetOnAxis(ap=iap[0:rows, :], axis=0), in_=y_s[0:rows, :],
                    in_offset=None, compute_op=ALU.add), reads=[y_s] + idx_reads, writes=["res"])

        gather_T(0)
        for pi in range(NPRE):
            load_gu(0, pi)
        for ex in range(E):
            for pi in range(NP_):
                load_dn(ex, pi)
                gu(ex, pi)
                if pi + NPRE < NP_:
                    load_gu(ex, pi + NPRE)
            if ex + 1 < E:
                gather_T(ex + 1)
                for pi in range(NPRE):
                    load_gu(ex + 1, pi)
            down(ex)


_NC_CACHE = {}


def kernel(**inputs):
    consts = host_consts()
    if "nc" not in _NC_CACHE:
        _NC_CACHE["nc"] = build_program()
    nc = _NC_CACHE["nc"]
    x = np.ascontiguousarray(inputs["x"], dtype=np.float32)
    c = np.ascontiguousarray(inputs["c"], dtype=np.float32)
    ctx = np.ascontiguousarray(inputs["ctx"], dtype=np.float32)
    shared = {"c_ctx": np.ascontiguousarray(inputs["c_ctx"], dtype=np.float32).reshape(1, D)}
    for n in WNAMES:
        shared[n] = np.ascontiguousarray(inputs[n], dtype=np.float32).reshape(WSHAPES[n])
    for n, v in consts.items():
        shared[n] = v
    in_maps = []
    for ci in range(NCORES):
        m = dict(shared)
        m["x"] = x[ci * NS:(ci + 1) * NS].reshape(NS * S, D)
        m["c"] = c[ci * NS:(ci + 1) * NS]
        m["ctx"] = ctx[ci * NS:(ci + 1) * NS].reshape(NS * C, D)
        in_maps.append(m)
    res = run_bass_kernel_spmd(nc, in_maps, core_ids=list(range(NCORES)))
    out = np.concatenate([r["y"].reshape(NS, S, D) for r in res.results], axis=0)
    return out.astype(np.float32)
```

```python
import os
import numpy as np
import concourse.bass as bass
import concourse.mybir as mybir
from concourse.bass_utils import run_bass_kernel_spmd
from contextlib import ExitStack, contextmanager

F32 = mybir.dt.float32
F32R = mybir.dt.float32r
BF16 = mybir.dt.bfloat16
U32 = mybir.dt.uint32
I32 = mybir.dt.int32
AF = mybir.ActivationFunctionType
ALU = mybir.AluOpType
AX = mybir.AxisListType

NCORES = 8
NS = 2
D = 1024
S = 2048
C = 256
NT = (S + C) // 128
L = 2
E = 16
DFF = 2816
NFC = DFF // 128
CAPL = 256
CAPC = 32
EPS = 1e-6
UC = 1792
NLAT = NS * S
NROW = NS * (S + C)


class KB:
    ENGS = ("pe", "dve", "act", "pool", "sp")

    def __init__(self, nc, ndma=24):
        self.nc = nc
        self.st = ExitStack()
        self.q = {e: [] for e in self.ENGS}
        self.cnt = {e: 0 for e in self.ENGS}
        self.sem = {e: self.st.enter_context(nc.semaphore("prog_" + e)) for e in self.ENGS}
        self.waited = {(c, p): 0 for c in self.ENGS for p in self.ENGS}
        self.waited_dma = {}
        self.lastw = {}
        self.readers = {}
        self.dma_sems = []
        self.ndma = {"sp": 0, "act": 0, "pool": 0}
        self.pool_of = {"sp": (0, 10), "act": (10, 8), "pool": (18, 14)}
        for i in range(32):
            s = self.st.enter_context(nc.semaphore("dma%d" % i))
            self.dma_sems.append([s, 0, None])
        self.cur = self.st

    def sb(self, name, shape, dt):
        self.uid = getattr(self, "uid", 0) + 1
        return self.cur.enter_context(self.nc.sbuf_tensor("%s_s%d" % (name, self.uid), shape, dt))

    def ps(self, name, shape, dt):
        self.uid = getattr(self, "uid", 0) + 1
        return self.cur.enter_context(self.nc.psum_tensor("%s_p%d" % (name, self.uid), shape, dt))

    @contextmanager
    def phase(self):
        prev = self.cur
        with ExitStack() as es:
            self.cur = es
            yield
            self.barrier()
        self.cur = prev

    def barrier(self):
        toks = [(p, self.cnt[p]) for p in self.ENGS if self.cnt[p] > 0]
        dtoks = [d[2] for d in self.dma_sems if d[2] is not None]
        for e in self.ENGS:
            for t in toks:
                if t[0] != e:
                    self._wait(e, t)
            for t in dtoks:
                self._wait(e, t)
        self.lastw = {}
        self.readers = {}

    def _key(self, x):
        if isinstance(x, (str, tuple)):
            return x
        return x.tensor.name if hasattr(x, "tensor") else x.name

    def _wait(self, eng, tok):
        if tok is None:
            return
        if tok[0] == "dma":
            _, i, val = tok
            if self.waited_dma.get((eng, i), 0) >= val:
                return
            self.waited_dma[(eng, i)] = val
            sem = self.dma_sems[i][0]
            self.q[eng].append(lambda e, sem=sem, val=val: e.wait_ge(sem, val))
            return
        p, c = tok
        if eng == "pe" and p == "pe" and not (PESYNC or getattr(self, "pesync", False)):
            return
        if self.waited[(eng, p)] >= c:
            return
        self.waited[(eng, p)] = c
        s = self.sem[p]
        self.q[eng].append(lambda e, s=s, c=c: e.wait_ge(s, c))

    def _deps(self, eng, rk, wk):
        for k in rk:
            self._wait(eng, self.lastw.get(k))
        for k in wk:
            self._wait(eng, self.lastw.get(k))
            for t in self.readers.get(k, ()):
                self._wait(eng, t)

    def _record(self, tok, rk, wk):
        for k in wk:
            self.lastw[k] = tok
            self.readers[k] = []
        for k in rk:
            self.readers.setdefault(k, []).append(tok)

    def op(self, eng, fn, reads=(), writes=()):
        rk = [self._key(r) for r in reads]
        wk = [self._key(w) for w in writes]
        self._deps(eng, rk, wk)
        self.cnt[eng] += 1
        tok = (eng, self.cnt[eng])
        s = self.sem[eng]
        self.q[eng].append(lambda e, fn=fn, s=s: fn(e).then_inc(s, 1))
        self._record(tok, rk, wk)
        return tok

    def V(self, fn, r=(), w=()):
        return self.op("dve", fn, r, w)

    def A(self, fn, r=(), w=()):
        return self.op("act", fn, r, w)

    def G(self, fn, r=(), w=()):
        return self.op("pool", fn, r, w)

    def T(self, fn, r=(), w=()):
        return self.op("pe", fn, r, w)

    def dma(self, eng, fn, reads=(), writes=()):
        rk = [self._key(r) for r in reads]
        wk = [self._key(w) for w in writes]
        self._deps(eng, rk, wk)
        base, n = self.pool_of[eng]
        i = base + self.ndma[eng] % n
        self.ndma[eng] += 1
        ent = self.dma_sems[i]
        if ent[2] is not None:
            self._wait(eng, ent[2])
        ent[1] += 16
        tok = ("dma", i, ent[1])
        ent[2] = tok
        sem = ent[0]
        self.q[eng].append(lambda e, fn=fn, sem=sem: fn(e).then_inc(sem, 16))
        self._record(tok, rk, wk)
        return tok

    def finish(self):
        nc = self.nc
        self.barrier()
        q = self.q
        with nc.Block() as block:
            @block.tensor
            def _(e):
                for f in q["pe"]:
                    f(e)

            @block.vector
            def _(e):
                for f in q["dve"]:
                    f(e)

            @block.scalar
            def _(e):
                for f in q["act"]:
                    f(e)

            @block.gpsimd
            def _(e):
                for f in q["pool"]:
                    f(e)

            @block.sync
            def _(e):
                for f in q["sp"]:
                    f(e)
        self.st.close()


def host_consts():
    f32 = np.float32
    half = 16
    inv = (10000.0 ** (-np.arange(half, dtype=f32) / half)).astype(f32)
    t = np.arange(S)
    rows = (t // 64).astype(f32)
    cols = (t % 64).astype(f32)
    ar = (rows[:, None] * inv[None, :]).astype(f32)
    ac = (cols[:, None] * inv[None, :]).astype(f32)
    attC = np.concatenate([np.cos(ar), np.cos(ar), np.cos(ac), np.cos(ac)], axis=1).astype(f32)
    attS = np.concatenate([-np.sin(ar), np.sin(ar), -np.sin(ac), np.sin(ac)], axis=1).astype(f32)
    half = 32
    inv = (10000.0 ** (-np.arange(half, dtype=f32) / half)).astype(f32)
    pos = np.arange(S + C).astype(f32)
    a = (pos[:, None] * inv[None, :]).astype(f32)
    retC = np.concatenate([np.cos(a), np.cos(a)], axis=1).astype(f32)
    retS = np.concatenate([-np.sin(a), np.sin(a)], axis=1).astype(f32)
    j = np.arange(128)[:, None]
    i = np.arange(128)[None, :]
    masks = np.stack([(j <= i), (j >= i), (j > i)], axis=0).astype(f32)
    ident = np.eye(128, dtype=f32)
    p = np.arange(128, dtype=f32)
    dcoef = np.stack([p + 1, -(p + 1), -p, p], axis=1).astype(f32)
    return {"attC": attC, "attS": attS, "retC": retC, "retS": retS, "masks": masks,
            "ident": ident, "dcoef": dcoef}


WNAMES = ["w_mod", "b_mod", "g_mix", "g_ffn", "w_in", "q_norm_g", "k_norm_g", "att_sink",
          "ret_decay_logit", "ret_norm_g", "conv_w", "conv_b", "conv_norm_g", "conv_norm_b",
          "w_out", "w_router", "w_gate", "w_up", "w_down"]
WSHAPES = {
    "w_mod": [L, D, 6 * D], "b_mod": [L, 6 * D], "g_mix": [L, D], "g_ffn": [L, D], "w_in": [L, D, 2304],
    "q_norm_g": [L, 64], "k_norm_g": [L, 64], "att_sink": [L, 8], "ret_decay_logit": [L, 8],
    "ret_norm_g": [L, 256], "conv_w": [L, 31, 256], "conv_b": [L, 256], "conv_norm_g": [L, 256],
    "conv_norm_b": [L, 256], "w_out": [L, D, D], "w_router": [L, D, E], "w_gate": [L, E, D, DFF],
    "w_up": [L, E, D, DFF], "w_down": [L, E, DFF, D],
}
CSHAPES = {"attC": [S, 64], "attS": [S, 64], "retC": [S + C, 64], "retS": [S + C, 64],
           "masks": [3, 128, 128], "ident": [128, 128], "dcoef": [128, 4]}

PESYNC = os.environ.get("MK_PESYNC", "") == "1"
RETSKIP = os.environ.get("MK_RETSKIP", "")
STOP = os.environ.get("MK_STOP", "")
DEBUG = os.environ.get("MK_DEBUG", "") == "1"


def build_program():
    nc = bass.Bass("TRN2", target_bir_lowering=False)
    A = {}
    A["x"] = nc.dram_tensor("x", [NS * S, D], F32, kind="ExternalInput").ap()
    A["c"] = nc.dram_tensor("c", [NS, D], F32, kind="ExternalInput").ap()
    A["ctx"] = nc.dram_tensor("ctx", [NS * C, D], F32, kind="ExternalInput").ap()
    A["c_ctx"] = nc.dram_tensor("c_ctx", [1, D], F32, kind="ExternalInput").ap()
    for n in WNAMES:
        A[n] = nc.dram_tensor(n, WSHAPES[n], F32, kind="ExternalInput").ap()
    for n, shp in CSHAPES.items():
        A[n] = nc.dram_tensor(n, shp, F32, kind="ExternalInput").ap()
    A["y"] = nc.dram_tensor("y", [NS * S, D], F32, kind="ExternalOutput").ap()
    dk = "ExternalOutput" if DEBUG else "Internal"
    A["mod_d"] = nc.dram_tensor("mod_d", [L, 4, 6 * D], F32, kind=dk).ap()
    A["u_d"] = nc.dram_tensor("u_d", [NS, S + C, UC], F32, kind=dk).ap()
    A["cv_d"] = nc.dram_tensor("cv_d", [NS, 512, S + C], F32, kind=dk).ap()
    A["mixin_d"] = nc.dram_tensor("mixin_d", [NS, S + C, 768], BF16, kind=dk).ap()
    A["convT_d"] = nc.dram_tensor("convT_d", [NS, 256, S + C], BF16, kind=dk).ap()
    A["res_d"] = nc.dram_tensor("res_d", [NROW, D], F32, kind=dk).ap()
    A["hf_d"] = nc.dram_tensor("hf_d", [NROW, D], F32, kind=dk).ap()
    A["aff_d"] = nc.dram_tensor("aff_d", [48, S], F32, kind=dk).ap()
    A["hfb_d"] = nc.dram_tensor("hfb_d", [NROW, D], BF16, kind=dk).ap()
    A["affc_d"] = nc.dram_tensor("affc_d", [48, C], F32, kind=dk).ap()

    k = KB(nc)
    P = {}
    P["ident"] = k.sb("ident", [128, 128], F32)
    P["identb"] = k.sb("identb", [128, 128], BF16)
    P["masks"] = k.sb("masks", [128, 3, 128], F32)
    P["modT"] = [k.sb("modT%d" % l, [128, 48, 4], F32) for l in range(L)]
    P["gT"] = k.sb("gT", [128, 8, 4], F32)
    P["gsA"] = [k.sb("gsA%d" % l, [128, 8, 4], F32) for l in range(L)]
    P["epsb"] = k.sb("epsb", [128, 1], F32)
    k.V(lambda e: e.memset(P["epsb"][:], EPS), [], [P["epsb"]])
    k.dma("sp", lambda e: e.dma_start(out=P["ident"][:], in_=A["ident"]), writes=[P["ident"]])
    k.dma("sp", lambda e: e.dma_start(out=P["masks"][:], in_=A["masks"].rearrange("m j i -> j m i")),
          writes=[P["masks"]])
    k.V(lambda e: e.tensor_copy(out=P["identb"][:], in_=P["ident"][:]), [P["ident"]], [P["identb"]])

    phase_mod(k, A, P)
    if STOP == "mod":
        k.finish()
        return nc
    for l in range(L):
        last = l == L - 1
        src_lat = A["x"] if l == 0 else A["res_d"][0:NLAT]
        src_ctx = A["ctx"] if l == 0 else A["res_d"][NLAT:NROW]
        dst_lat = A["y"] if last else A["res_d"][0:NLAT]
        phase_proj(k, A, P, l, src_lat, src_ctx)
        if STOP == "proj%d" % l:
            break
        phase_att(k, A, P, l, last)
        if STOP == "att%d" % l:
            break
        phase_ret(k, A, P, l, last)
        if STOP == "ret%d" % l:
            break
        phase_conv(k, A, P, l, last)
        if STOP == "conv%d" % l:
            break
        phase_out(k, A, P, l, last, src_lat, src_ctx, dst_lat)
        if STOP == "out%d" % l:
            break
        phase_moe(k, A, P, l, last, dst_lat)
        if STOP == "moe%d" % l:
            break
    k.finish()
    return nc


def phase_mod(k, A, P):
    with k.phase():
        cc = k.sb("cc", [4, D], F32)
        cs = k.sb("cs", [4, D], F32)
        csT = k.sb("csT", [128, 8, 4], BF16)
        gg = k.sb("gg", [4, D], F32)
        pt = k.ps("pt", [128, 8, 4], F32)
        pm = [k.ps("pm%d" % i, [4, 512], F32) for i in range(2)]
        ptm = k.ps("ptm", [128, 48, 4], F32)
        wm = [k.sb("wm%d" % i, [128, 8, 512], BF16) for i in range(3)]
        bm = k.sb("bm", [4, 6 * D], F32)
        modrow = k.sb("modrow", [4, 6 * D], F32)
        ident = P["ident"]
        k.V(lambda e: e.memset(cc[:], 0.0), [], [cc])
        k.dma("sp", lambda e: e.dma_start(out=cc[0:NS, :], in_=A["c"]), writes=[cc])
        k.dma("sp", lambda e: e.dma_start(out=cc[NS:NS + 1, :], in_=A["c_ctx"]), writes=[cc])
        k.A(lambda e: e.activation(out=cs[:], in_=cc[:], func=AF.Silu), [cc], [cs])
        for kc in range(8):
            k.T(lambda e, kc=kc: e.transpose(pt[:, kc, :], cs[0:4, kc * 128:(kc + 1) * 128], ident[0:4, 0:4]),
                [cs, ident], [pt])
        k.V(lambda e: e.tensor_copy(out=csT[:], in_=pt[:]), [pt], [csT])
        k.dma("sp", lambda e: e.dma_start(out=gg[0:2, :], in_=A["g_mix"]), writes=[gg])
        k.dma("sp", lambda e: e.dma_start(out=gg[2:4, :], in_=A["g_ffn"]), writes=[gg])
        for kc in range(8):
            k.T(lambda e, kc=kc: e.transpose(pt[:, kc, :], gg[0:4, kc * 128:(kc + 1) * 128], ident[0:4, 0:4]),
                [gg, ident], [pt])
        k.V(lambda e: e.tensor_copy(out=P["gT"][:], in_=pt[:]), [pt], [P["gT"]])
        for l in range(L):
            k.dma("sp", lambda e, l=l: e.dma_start(out=bm[:], in_=A["b_mod"][l, :].partition_broadcast(4)),
                  writes=[bm])
            wv = A["w_mod"][l].rearrange("(kc p) f -> p kc f", p=128)
            for j in range(12):
                w = wm[(l * 12 + j) % 3]
                k.dma("pool", lambda e, w=w, j=j, wv=wv: e.dma_start(out=w[:], in_=wv[:, :, j * 512:(j + 1) * 512]),
                      writes=[w])
                p = pm[j % 2]
                for kc in range(8):
                    k.T(lambda e, p=p, w=w, kc=kc: e.matmul(p[:], lhsT=csT[:, kc, :], rhs=w[:, kc, :],
                                                          start=(kc == 0), stop=(kc == 7)), [csT, w], [p])
                k.V(lambda e, p=p, j=j: e.tensor_tensor(out=modrow[:, j * 512:(j + 1) * 512], in0=p[:],
                                                        in1=bm[:, j * 512:(j + 1) * 512], op=ALU.add),
                    [p, bm], [modrow])
            k.dma("sp", lambda e, l=l: e.dma_start(out=A["mod_d"][l], in_=modrow[:]), reads=[modrow],
                  writes=["mod_d"])
            for j in range(48):
                k.T(lambda e, j=j: e.transpose(ptm[:, j, :], modrow[0:4, j * 128:(j + 1) * 128], ident[0:4, 0:4]),
                    [modrow, ident], [ptm])
            mT = P["modT"][l]
            k.V(lambda e, mT=mT: e.tensor_copy(out=mT[:], in_=ptm[:]), [ptm], [mT])
            gs = P["gsA"][l]
            k.V(lambda e, gs=gs, mT=mT: e.tensor_scalar(out=gs[:], in0=mT[:, 8:16, :], scalar1=1.0, scalar2=None,
                                                        op0=ALU.add), [mT], [gs])
            k.V(lambda e, gs=gs, l=l: e.tensor_tensor(out=gs[:], in0=gs[:],
                                                      in1=P["gT"][:, :, l:l + 1].to_broadcast([128, 8, 4]),
                                                      op=ALU.mult), [gs, P["gT"]], [gs])


def rms_rstd(k, xt, junk, ss, rstd, n):
    k.A(lambda e: e.activation(out=junk, in_=xt, func=AF.Square, accum_out=ss[:, 0:1]), [xt], [junk, ss])
    k.V(lambda e: e.tensor_scalar(out=ss[:, 0:1], in0=ss[:, 0:1], scalar1=1.0 / n, scalar2=EPS, op0=ALU.mult,
                                  op1=ALU.add), [ss], [ss])
    k.A(lambda e: e.sqrt(out=ss[:, 0:1], in_=ss[:, 0:1]), [ss], [ss])
    k.V(lambda e: e.reciprocal(out=rstd[:, 0:1], in_=ss[:, 0:1]), [ss], [rstd])


def phase_proj(k, A, P, l, src_lat, src_ctx):
    with k.phase():
        win = k.sb("win", [128, 8, 2304], BF16)
        wv = A["w_in"][l].rearrange("(kc p) f -> p kc f", p=128)
        for j in range(0, 2304, 384):
            k.dma("pool", lambda e, j=j: e.dma_start(out=win[:, :, j:j + 384], in_=wv[:, :, j:j + 384]),
                  writes=[("win", j)])
        winkeys = [("win", j) for j in range(0, 2304, 384)]
        NB = 8
        xts = [k.sb("xt%d" % i, [128, D], F32) for i in range(NB)]
        xn = [k.sb("xn%d" % i, [128, D], BF16) for i in range(NB)]
        junk = k.sb("junk", [128, D], F32)
        ss = [k.sb("ss%d" % i, [128, 1], F32) for i in range(NB)]
        rstd = [k.sb("rstd%d" % i, [128, 1], F32) for i in range(NB)]
        hT = [k.sb("hT%d" % i, [128, 8, 512], BF16) for i in range(2)]
        ptr = [k.ps("ptr%d" % i, [128, D], BF16) for i in range(2)]
        pu = [k.ps("pu%d" % i, [128, 512], F32) for i in range(3)]
        pc = [k.ps("pc%d" % i, [128, 512], F32) for i in range(2)]
        usb = [k.sb("usb%d" % i, [128, UC], F32) for i in range(2)]
        cvs = [k.sb("cvs%d" % i, [128, 4, 512], F32) for i in range(2)]
        identb = P["identb"]
        gs = P["gsA"][l]
        mT = P["modT"][l]
        groups = []
        for s in range(NS):
            groups.append((s, 0, 2, src_ctx[s * C:(s + 1) * C], NS))
            for g in range(4):
                groups.append((s, C + g * 512, 4, src_lat[s * S + g * 512: s * S + (g + 1) * 512], s))
        cnt = {"t": 0, "e": 0}

        def stats(gi):
            s, tok0, ntl, src, r = groups[gi]
            for t in range(ntl):
                i = (gi % 2) * 4 + t
                xt, xnn, sst, rs = xts[i], xn[i], ss[i], rstd[i]
                k.dma("sp", lambda e, xt=xt, src=src, t=t: e.dma_start(out=xt[:], in_=src[t * 128:(t + 1) * 128, :]),
                      writes=[xt])
                k.A(lambda e, xt=xt, sst=sst: e.activation(out=junk[:], in_=xt[:], func=AF.Square, accum_out=sst[:, 0:1]),
                    [xt], [junk, sst])
                k.A(lambda e, sst=sst: e.activation(out=sst[:, 0:1], in_=sst[:, 0:1], func=AF.Ln, scale=1.0 / D, bias=EPSB[:, 0:1]),
                    [sst, EPSB], [sst])
                k.A(lambda e, sst=sst, rs=rs: e.activation(out=rs[:, 0:1], in_=sst[:, 0:1], func=AF.Exp, scale=-0.5), [sst], [rs])
                k.V(lambda e, xnn=xnn, xt=xt, rs=rs: e.tensor_scalar(out=xnn[:], in0=xt[:], scalar1=rs[:, 0:1],
                                                                   scalar2=None, op0=ALU.mult), [xt, rs], [xnn])

        def trans_tile(gi, t):
            s, tok0, ntl, src, r = groups[gi]
            h = hT[gi % 2]
            if True:
                i = (gi % 2) * 4 + t
                xnn = xn[i]
                pt = ptr[cnt["t"] % 2]
                cnt["t"] += 1
                for kc in range(8):
                    k.T(lambda e, pt=pt, xnn=xnn, kc=kc: e.transpose(pt[:, kc * 128:(kc + 1) * 128],
                                                                   xnn[:, kc * 128:(kc + 1) * 128], identb[:]),
                        [xnn, identb], [pt])
                for kc in range(8):
                    if kc % 2 == 0:
                        k.A(lambda e, h=h, pt=pt, kc=kc, t=t, r=r: e.activation(
                            out=h[:, kc, t * 128:(t + 1) * 128], in_=pt[:, kc * 128:(kc + 1) * 128],
                            func=AF.Identity, scale=gs[:, kc, r:r + 1], bias=mT[:, kc, r:r + 1]),
                            [pt, gs, mT], [h])
                    else:
                        k.V(lambda e, h=h, pt=pt, kc=kc, t=t, r=r: e.tensor_scalar(
                            out=h[:, kc, t * 128:(t + 1) * 128], in0=pt[:, kc * 128:(kc + 1) * 128],
                            scalar1=gs[:, kc, r:r + 1], scalar2=mT[:, kc, r:r + 1], op0=ALU.mult, op1=ALU.add),
                            [pt, gs, mT], [h])

        def mm_tile(gi, t):
            s, tok0, ntl, src, r = groups[gi]
            h = hT[gi % 2]
            ei = cnt["e"]
            if True:
                ub = usb[t % 2]
                for ci, (c0, w) in enumerate([(0, 512), (512, 512), (1024, 512), (1536, 256)]):
                    p = pu[ei % 3]
                    for kc in range(8):
                        k.T(lambda e, p=p, h=h, kc=kc, t=t, c0=c0, w=w: e.matmul(
                            p[:, 0:w], lhsT=h[:, kc, t * 128:(t + 1) * 128], rhs=win[:, kc, c0:c0 + w],
                            start=(kc == 0), stop=(kc == 7)), [h] + winkeys, [p])
                    if ei % 2 == 0:
                        k.A(lambda e, ub=ub, p=p, c0=c0, w=w: e.copy(out=ub[:, c0:c0 + w], in_=p[:, 0:w]), [p], [ub])
                    else:
                        k.V(lambda e, ub=ub, p=p, c0=c0, w=w: e.tensor_copy(out=ub[:, c0:c0 + w], in_=p[:, 0:w]),
                            [p], [ub])
                    ei += 1
                k.dma("sp", lambda e, ub=ub, s=s, tok0=tok0, t=t: e.dma_start(
                    out=A["u_d"][s, tok0 + t * 128: tok0 + (t + 1) * 128, :], in_=ub[:]), reads=[ub], writes=["u_d"])
            cnt["e"] = ei

        def mm_conv(gi):
            s, tok0, ntl, src, r = groups[gi]
            h = hT[gi % 2]
            ntok = ntl * 128
            cv = cvs[gi % 2]
            for j in range(4):
                p = pc[j % 2]
                for kc in range(8):
                    k.T(lambda e, p=p, h=h, kc=kc, j=j, ntok=ntok: e.matmul(
                        p[:, 0:ntok], lhsT=win[:, kc, UC + j * 128: UC + (j + 1) * 128], rhs=h[:, kc, 0:ntok],
                        start=(kc == 0), stop=(kc == 7)), [h] + winkeys, [p])
                if j % 2 == 0:
                    k.A(lambda e, cv=cv, p=p, j=j, ntok=ntok: e.copy(out=cv[:, j, 0:ntok], in_=p[:, 0:ntok]), [p], [cv])
                else:
                    k.V(lambda e, cv=cv, p=p, j=j, ntok=ntok: e.tensor_copy(out=cv[:, j, 0:ntok], in_=p[:, 0:ntok]),
                        [p], [cv])
            k.dma("act", lambda e, cv=cv, s=s, tok0=tok0, ntok=ntok: e.dma_start(
                out=A["cv_d"][s, :, tok0:tok0 + ntok].rearrange("(j p) t -> p j t", p=128), in_=cv[:, :, 0:ntok]),
                reads=[cv], writes=["cv_d"])

        EPSB = P["epsb"]
        ng = len(groups)
        stats(0)
        for t in range(groups[0][2]):
            trans_tile(0, t)
        for gi in range(ng):
            if gi + 1 < ng:
                stats(gi + 1)
            n_cur = groups[gi][2]
            n_nxt = groups[gi + 1][2] if gi + 1 < ng else 0
            for t in range(max(n_cur, n_nxt)):
                if t < n_cur:
                    mm_tile(gi, t)
                if t < n_nxt:
                    trans_tile(gi + 1, t)
            mm_conv(gi)


def run_pipe(n, stages):
    ns = len(stages)
    for step in range(n + ns - 1):
        for j in range(ns - 1, -1, -1):
            i = step - j
            if 0 <= i < n:
                stages[j](i)


def phase_att(k, A, P, l, last):
    with k.phase():
        identb = P["identb"]
        EPSB = P["epsb"]
        attC = k.sb("attC", [128, 16, 64], F32)
        attS = k.sb("attS", [128, 16, 64], F32)
        k.dma("sp", lambda e: e.dma_start(out=attC[:], in_=A["attC"].rearrange("(t p) d -> p t d", p=128)), writes=[attC])
        k.dma("sp", lambda e: e.dma_start(out=attS[:], in_=A["attS"].rearrange("(t p) d -> p t d", p=128)), writes=[attS])
        gqk = k.sb("gqk", [128, 10, 64], F32)
        k.dma("sp", lambda e: e.dma_start(out=gqk[:, 0, :], in_=A["q_norm_g"][l, :].partition_broadcast(128)), writes=[gqk])
        k.dma("sp", lambda e: e.dma_start(out=gqk[:, 8, :], in_=A["k_norm_g"][l, :].partition_broadcast(128)), writes=[gqk])
        k.V(lambda e: e.tensor_scalar(out=gqk[:, 0, :], in0=gqk[:, 0, :], scalar1=0.125, scalar2=None, op0=ALU.mult),
            [gqk], [gqk])
        for h in range(1, 8):
            k.V(lambda e, h=h: e.tensor_copy(out=gqk[:, h, :], in_=gqk[:, 0, :]), [gqk], [gqk])
        k.V(lambda e: e.tensor_copy(out=gqk[:, 9, :], in_=gqk[:, 8, :]), [gqk], [gqk])
        sink = k.sb("sink", [128, 8], F32)
        k.dma("sp", lambda e: e.dma_start(out=sink[:], in_=A["att_sink"][l, :].partition_broadcast(128)), writes=[sink])
        k.A(lambda e: e.activation(out=sink[:], in_=sink[:], func=AF.Exp), [sink], [sink])
        maskb = k.sb("maskb", [128, 3, 128], BF16)
        k.V(lambda e: e.tensor_copy(out=maskb[:], in_=P["masks"][:]), [P["masks"]], [maskb])

        qT = k.sb("qT", [64, 8, NT * 128], BF16)
        kT = k.sb("kT", [64, 2, NT * 128], BF16)
        vx = k.sb("vx", [128, NT, 2, 65], BF16)
        k.V(lambda e: e.memset(vx[:], 1.0), [], [vx])
        NU, NQ = 7, 5
        uq = [k.sb("uq%d" % i, [128, 768], F32) for i in range(NU)]
        sq = [k.sb("sq%d" % i, [128, 640], F32) for i in range(2)]
        ssq = [k.sb("ssq%d" % i, [128, 10], F32) for i in range(4)]
        qn = [k.sb("qn%d" % i, [128, 640], F32) for i in range(NQ)]
        t1 = [k.sb("t1%d" % i, [128, 640], F32) for i in range(2)]
        t2 = [k.sb("t2%d" % i, [128, 640], F32) for i in range(2)]
        qb = [k.sb("qb%d" % i, [128, 640], BF16) for i in range(3)]
        pq = [k.ps("pq%d" % i, [64, 8, 128], BF16) for i in range(2)]
        pkk = k.ps("pkk", [64, 2, 128], BF16)
        psc = [k.ps("psc%d" % i, [128, 512], F32) for i in range(3)]
        po = [k.ps("po%d" % i, [128, 4, 65], F32) for i in range(2)]
        Pm = [k.sb("Pm%d" % i, [128, 5, 512], BF16) for i in range(3)]
        den = [k.sb("den%d" % i, [128, 4], F32) for i in range(2)]
        ao = [k.sb("ao%d" % i, [128, 512], BF16) for i in range(2)]
        cnt = {"sc": 0}

        for s in range(NS):
            def p0(i, s=s):
                u = uq[i % NU]
                k.dma("sp", lambda e: e.dma_start(out=u[:], in_=A["u_d"][s, i * 128:(i + 1) * 128, 0:768]),
                      reads=["u_d"], writes=[u])

            def p1(i):
                u, q2 = uq[i % NU], sq[i % 2]
                k.G(lambda e: e.tensor_tensor(out=q2[:], in0=u[:, 0:640], in1=u[:, 0:640], op=ALU.mult), [u], [q2])

            def p2(i):
                q2, sv = sq[i % 2], ssq[i % 4]
                k.V(lambda e: e.tensor_reduce(out=sv[:], in_=q2[:].rearrange("p (h d) -> p h d", d=64), axis=AX.X,
                                              op=ALU.add), [q2], [sv])

            def p3(i):
                sv = ssq[i % 4]
                k.A(lambda e: e.activation(out=sv[:], in_=sv[:], func=AF.Ln, scale=1.0 / 64, bias=EPSB[:, 0:1]), [sv, EPSB], [sv])
                k.A(lambda e: e.activation(out=sv[:], in_=sv[:], func=AF.Exp, scale=-0.5), [sv], [sv])

            def p4(i):
                u, sv, q = uq[i % NU], ssq[i % 4], qn[i % NQ]
                k.V(lambda e: e.tensor_tensor(out=q[:].rearrange("p (h d) -> p h d", d=64),
                                              in0=u[:, 0:640].rearrange("p (h d) -> p h d", d=64),
                                              in1=sv[:].unsqueeze(2).to_broadcast([128, 10, 64]), op=ALU.mult), [u, sv], [q])

            def p5(i):
                u, q = uq[i % NU], qn[i % NQ]
                k.G(lambda e: e.tensor_tensor(out=q[:], in0=q[:], in1=gqk[:].rearrange("p h d -> p (h d)"), op=ALU.mult),
                    [q, gqk], [q])
                k.G(lambda e: e.tensor_copy(out=vx[:, i, :, 0:64], in_=u[:, 640:768].rearrange("p (h d) -> p h d", d=64)),
                    [u], [vx])

            def p6(i):
                if i < 2:
                    return
                q, a1, a2 = qn[i % NQ], t1[i % 2], t2[i % 2]
                tl = i - 2
                qv = q[:].rearrange("p (h a x d) -> p h a x d", h=10, a=2, x=2, d=16)
                t1v = a1[:].rearrange("p (h a x d) -> p h a x d", h=10, a=2, x=2, d=16)
                t2v = a2[:].rearrange("p (h a x d) -> p h a x d", h=10, a=2, x=2, d=16)
                Cv = attC[:, tl, :].rearrange("p (a x d) -> p a x d", a=2, x=2, d=16)
                Sv = attS[:, tl, :].rearrange("p (a x d) -> p a x d", a=2, x=2, d=16)
                k.V(lambda e: e.tensor_tensor(
                    out=a1[:].rearrange("p (h d) -> p h d", d=64), in0=q[:].rearrange("p (h d) -> p h d", d=64),
                    in1=attC[:, tl, :].unsqueeze(1).to_broadcast([128, 10, 64]), op=ALU.mult), [q, attC], [a1])
                qv = q[:].rearrange("p (h a x d) -> p h a x d", h=10, a=2, x=2, d=16)
                t2v = a2[:].rearrange("p (h a x d) -> p h a x d", h=10, a=2, x=2, d=16)
                Sv = attS[:, tl, :].rearrange("p (a x d) -> p a x d", a=2, x=2, d=16)
                for a in range(2):
                    for x in range(2):
                        k.G(lambda e, a=a, x=x: e.tensor_tensor(
                            out=t2v[:, :, a, x, :], in0=qv[:, :, a, 1 - x, :],
                            in1=Sv[:, a, x, :].unsqueeze(1).to_broadcast([128, 10, 16]), op=ALU.mult), [q, attS], [a2])

            def p7(i):
                q, a1, a2, q_b = qn[i % NQ], t1[i % 2], t2[i % 2], qb[i % 3]
                if i >= 2:
                    k.V(lambda e: e.tensor_tensor(out=q_b[:], in0=a1[:], in1=a2[:], op=ALU.add), [a1, a2], [q_b])
                else:
                    k.V(lambda e: e.tensor_copy(out=q_b[:], in_=q[:]), [q], [q_b])

            def p8(i):
                q_b, p = qb[i % 3], pq[i % 2]
                for h in range(8):
                    k.T(lambda e, h=h: e.transpose(p[:, h, :], q_b[:, h * 64:(h + 1) * 64], identb[:]), [q_b, identb], [p])
                for h in range(2):
                    k.T(lambda e, h=h: e.transpose(pkk[:, h, :], q_b[:, (8 + h) * 64:(9 + h) * 64], identb[:]),
                        [q_b, identb], [pkk])

            def p9(i):
                p = pq[i % 2]
                k.A(lambda e: e.copy(out=qT[:, :, i * 128:(i + 1) * 128], in_=p[:]), [p], [("qT", i)])
                k.V(lambda e: e.tensor_copy(out=kT[:, :, i * 128:(i + 1) * 128], in_=pkk[:]), [pkk], [("kT", i)])

            run_pipe(NT, [p0, p1, p2, p3, p4, p5, p6, p7, p8, p9])

            qblocks = list(range(2, NT)) + ([] if last else [0, 1])
            items = [(t, kh) for t in qblocks for kh in range(2)]

            def kblocks(t):
                if t >= 2:
                    kbl = []
                    if t - 1 >= 2:
                        kbl.append((t - 1, 1))
                    kbl.append((t, None))
                    if t + 1 < NT:
                        kbl.append((t + 1, 0))
                    return kbl + [(0, None), (1, None)]
                return [(0, None), (1, None)]

            def b0(i):
                t, kh = items[i]
                Pt = Pm[i % 3]
                for bj, (kb_, mk) in enumerate(kblocks(t)):
                    ps = psc[cnt["sc"] % 3]
                    cnt["sc"] += 1
                    k.T(lambda e, ps=ps, kb_=kb_: e.matmul(
                        ps[:], lhsT=kT[:, kh, kb_ * 128:(kb_ + 1) * 128],
                        rhs=qT[:, kh * 4:(kh + 1) * 4, t * 128:(t + 1) * 128], start=True, stop=True),
                        [("kT", kb_), ("qT", t)], [ps])
                    k.A(lambda e, ps=ps, bj=bj: e.activation(out=Pt[:, bj, :], in_=ps[:], func=AF.Exp), [ps], [(Pt.name, bj)])
                    if mk is not None:
                        k.G(lambda e, bj=bj, mk=mk: e.tensor_tensor(
                            out=Pt[:, bj, :].rearrange("p (g q) -> p g q", g=4),
                            in0=Pt[:, bj, :].rearrange("p (g q) -> p g q", g=4),
                            in1=maskb[:, mk, :].unsqueeze(1).to_broadcast([128, 4, 128]), op=ALU.mult),
                            [(Pt.name, bj), maskb], [(Pt.name, bj)])

            def b1(i):
                t, kh = items[i]
                Pt = Pm[i % 3]
                pso = po[i % 2]
                kbl = kblocks(t)
                nb = len(kbl)
                for g in range(4):
                    for bj, (kb_, mk) in enumerate(kbl):
                        k.T(lambda e, g=g, bj=bj, kb_=kb_: e.matmul(
                            pso[:, g, :], lhsT=Pt[:, bj, g * 128:(g + 1) * 128], rhs=vx[:, kb_, kh, :],
                            start=(bj == 0), stop=(bj == nb - 1)), [(Pt.name, bj), vx], [pso])

            def b2(i, s=s):
                t, kh = items[i]
                pso = po[i % 2]
                dn = den[i % 2]
                a_o = ao[(i // 2) % 2]
                k.V(lambda e: e.tensor_tensor(out=dn[:], in0=pso[:, :, 64], in1=sink[:, kh * 4:(kh + 1) * 4], op=ALU.add),
                    [pso, sink], [dn])
                k.V(lambda e: e.reciprocal(out=dn[:], in_=dn[:]), [dn], [dn])
                k.V(lambda e: e.tensor_tensor(
                    out=a_o[:, kh * 256:(kh + 1) * 256].rearrange("p (g d) -> p g d", g=4),
                    in0=pso[:, :, 0:64], in1=dn[:].unsqueeze(2).to_broadcast([128, 4, 64]), op=ALU.mult), [pso, dn], [a_o])
                if kh == 1:
                    k.dma("act", lambda e: e.dma_start(out=A["mixin_d"][s, t * 128:(t + 1) * 128, 0:512], in_=a_o[:]),
                          reads=[a_o], writes=["mixin_d"])

            run_pipe(len(items), [b0, b1, b2])


def phase_ret(k, A, P, l, last):
    with k.phase():
        identb = P["identb"]
        retC = k.sb("retC", [128, NT, 64], F32)
        retS = k.sb("retS", [128, NT, 64], F32)
        k.dma("sp", lambda e: e.dma_start(out=retC[:], in_=A["retC"].rearrange("(t p) d -> p t d", p=128)), writes=[retC])
        k.dma("sp", lambda e: e.dma_start(out=retS[:], in_=A["retS"].rearrange("(t p) d -> p t d", p=128)), writes=[retS])
        lg = k.sb("lg", [128, 8], F32)
        k.dma("sp", lambda e: e.dma_start(out=lg[:], in_=A["ret_decay_logit"][l, :].partition_broadcast(128)), writes=[lg])
        k.A(lambda e: e.activation(out=lg[:], in_=lg[:], func=AF.Exp, scale=-1.0), [lg], [lg])
        k.A(lambda e: e.activation(out=lg[:], in_=lg[:], func=AF.Ln, bias=1.0), [lg], [lg])
        k.V(lambda e: e.tensor_scalar(out=lg[:], in0=lg[:], scalar1=-1.0, scalar2=None, op0=ALU.mult), [lg], [lg])
        dco = k.sb("dco", [128, 4], F32)
        k.dma("sp", lambda e: e.dma_start(out=dco[:], in_=A["dcoef"]), writes=[dco])
        DT = k.sb("DT", [128, 2, 2, 4], F32)
        for d_ in range(2):
            for qk in range(2):
                k.A(lambda e, d_=d_, qk=qk: e.activation(out=DT[:, d_, qk, :], in_=lg[:, d_ * 4:(d_ + 1) * 4], func=AF.Exp,
                                                         scale=dco[:, d_ * 2 + qk: d_ * 2 + qk + 1]), [lg, dco], [DT])
            k.V(lambda e, d_=d_: e.tensor_scalar(out=DT[:, d_, 1, :], in0=DT[:, d_, 1, :], scalar1=0.125, scalar2=None,
                                                 op0=ALU.mult), [DT], [DT])
        g128 = k.sb("g128", [128, 2, 2], F32)
        for d_ in range(2):
            for pr in range(2):
                for hh in range(2):
                    h = 2 * pr + hh
                    k.A(lambda e, d_=d_, pr=pr, hh=hh, h=h: e.activation(
                        out=g128[hh * 64:(hh + 1) * 64, d_, pr:pr + 1], in_=lg[hh * 64:(hh + 1) * 64, d_ * 4 + h: d_ * 4 + h + 1],
                        func=AF.Exp, scale=128.0), [lg], [g128])
        gn = k.sb("gn", [128, 256], F32)
        k.dma("sp", lambda e: e.dma_start(out=gn[:], in_=A["ret_norm_g"][l, :].partition_broadcast(128)), writes=[gn])
        maskb = k.sb("maskb", [128, 3, 128], BF16)
        k.V(lambda e: e.tensor_copy(out=maskb[:], in_=P["masks"][:]), [P["masks"]], [maskb])

        EPSB = P["epsb"]
        RT = k.sb("RT", [128, 2, 2, 2, NT * 128], BF16)
        KD = k.sb("KD", [128, NT, 2, 256], BF16)
        Vb = k.sb("Vb", [128, NT, 256], BF16)
        SG = k.sb("SG", [128, NT, 256], BF16)
        OA = k.sb("OA", [128, NT, 256], F32)
        ur = [k.sb("ur%d" % i, [128, 1024], F32) for i in range(3)]
        t1 = [k.sb("t1%d" % i, [128, 512], F32) for i in range(2)]
        t2 = [k.sb("t2%d" % i, [128, 512], F32) for i in range(2)]
        rot = [k.sb("rot%d" % i, [128, 512], F32) for i in range(3)]
        vr = [k.sb("vr%d" % i, [128, 2, 2, 256], BF16) for i in range(4)]
        ptr = [k.ps("ptr%d" % i, [128, 8, 128], BF16) for i in range(1)]
        pssA = [k.ps("pssA%d" % i, [128, 512], F32) for i in range(2)]
        pssB = [k.ps("pssB%d" % i, [128, 512], F32) for i in range(2)]
        pso = [k.ps("pso%d" % i, [128, 4, 64], F32) for i in range(2)]
        pkvt = k.ps("pkvt", [128, 2, 2, 128], F32)
        STm = [k.sb("STm%d" % i, [128, 4, 128], BF16) for i in range(2)]
        Sst = [k.sb("Sst%d" % i, [128, 2, 128], F32) for i in range(2)]
        Sb = [k.sb("Sb%d" % i, [128, 2, 128], BF16) for i in range(2)]
        tmp = [k.sb("tmp%d" % i, [128, 2, 128], F32) for i in range(2)]
        osum = [k.sb("osum%d" % i, [128, 256], F32) for i in range(3)]
        mean = [k.sb("mean%d" % i, [128, 4], F32) for i in range(3)]
        cen = [k.sb("cen%d" % i, [128, 256], F32) for i in range(4)]
        sq = [k.sb("sq%d" % i, [128, 256], F32) for i in range(2)]
        var = [k.sb("var%d" % i, [128, 4], F32) for i in range(3)]
        rof = [k.sb("rof%d" % i, [128, 256], F32) for i in range(3)]
        ro = [k.sb("ro%d" % i, [128, 256], BF16) for i in range(2)]

        for s in range(NS):
            def r0(i, s=s):
                u = ur[i % 3]
                k.dma("sp", lambda e: e.dma_start(out=u[:], in_=A["u_d"][s, i * 128:(i + 1) * 128, 768:1792]),
                      reads=["u_d"], writes=[u])

            def r1(i):
                u, a1, a2 = ur[i % 3], t1[i % 2], t2[i % 2]
                qk_v = u[:, 0:512].rearrange("p (h x d) -> p h x d", h=8, x=2, d=32)
                t1v = a1[:].rearrange("p (h x d) -> p h x d", h=8, x=2, d=32)
                t2v = a2[:].rearrange("p (h x d) -> p h x d", h=8, x=2, d=32)
                Cv = retC[:, i, :].rearrange("p (x d) -> p x d", x=2, d=32)
                Sv = retS[:, i, :].rearrange("p (x d) -> p x d", x=2, d=32)
                for x in range(2):
                    k.V(lambda e, x=x: e.tensor_tensor(
                        out=t1v[:, :, x, :], in0=qk_v[:, :, x, :], in1=Cv[:, x, :].unsqueeze(1).to_broadcast([128, 8, 32]),
                        op=ALU.mult), [u, retC], [a1])
                    k.G(lambda e, x=x: e.tensor_tensor(
                        out=t2v[:, :, x, :], in0=qk_v[:, :, 1 - x, :], in1=Sv[:, x, :].unsqueeze(1).to_broadcast([128, 8, 32]),
                        op=ALU.mult), [u, retS], [a2])
                k.V(lambda e: e.tensor_copy(out=Vb[:, i, :], in_=u[:, 512:768]), [u], [("Vb", i)])
                k.A(lambda e: e.activation(out=SG[:, i, :], in_=u[:, 768:1024], func=AF.Silu), [u], [("SG", i)])

            def r2(i):
                a1, a2, rt_ = t1[i % 2], t2[i % 2], rot[i % 3]
                k.V(lambda e: e.tensor_tensor(out=rt_[:], in0=a1[:], in1=a2[:], op=ALU.add), [a1, a2], [rt_])

            def r3(i):
                rt_, v_r = rot[i % 3], vr[i % 4]
                for d_ in range(2):
                    eng = k.V if d_ == 0 else k.G
                    eng(lambda e, d_=d_: e.tensor_tensor(
                        out=v_r[:, d_, :, :].rearrange("p q (h d) -> p (q h) d", d=64),
                        in0=rt_[:].rearrange("p (qh d) -> p qh d", d=64),
                        in1=DT[:, d_, :, :].rearrange("p q h -> p (q h)").unsqueeze(2).to_broadcast([128, 8, 64]),
                        op=ALU.mult), [rt_, DT], [(v_r.name, d_)])

            def r4(i):
                v_r, p = vr[i % 4], ptr[0]
                for d_ in range(2):
                    k.G(lambda e, d_=d_: e.tensor_copy(out=KD[:, i, d_, :], in_=v_r[:, d_, 1, :]), [(v_r.name, d_)], [("KD", i)])
                for d_ in range(2):
                    for qk in range(2):
                        for pr in range(2):
                            k.T(lambda e, d_=d_, qk=qk, pr=pr: e.transpose(
                                p[:, d_ * 4 + qk * 2 + pr, :], v_r[:, d_, qk, pr * 128:(pr + 1) * 128], identb[:]),
                                [(v_r.name, d_), identb], [p])

            def r5(i):
                p = ptr[0]
                k.A(lambda e: e.copy(out=RT[:, :, :, :, i * 128:(i + 1) * 128].rearrange("p a b c t -> p (a b c) t"),
                                     in_=p[:]), [p], [("RT", i)])

            run_pipe(NT, [r0, r1, r2, r3, r4, r5])

            orders = [list(range(NT)), [1, 0] + list(range(NT - 1, 1, -1))]
            for d_ in range(2):
                k.V(lambda e, d_=d_: e.memset(Sst[d_][:], 0.0), [], [Sst[d_]])
                k.V(lambda e, d_=d_: e.memset(Sb[d_][:], 0.0), [], [Sb[d_]])
            oak = [("OA", t_) for t_ in range(NT)]
            k.G(lambda e: e.memset(OA[:], 0.0), oak, oak)
            for ci in range(NT):
                tt = [orders[0][ci], orders[1][ci]]
                for d_ in range(2):
                    t = tt[d_]
                    for h in (0, 2, 1, 3):
                        pr, hh = h // 2, h % 2
                        pss = pssA[d_] if hh == 0 else pssB[d_]
                        k.T(lambda e, pss=pss, pr=pr, hh=hh, t=t, d_=d_: e.matmul(
                            pss[:, pr * 128:(pr + 1) * 128], lhsT=RT[hh * 64:(hh + 1) * 64, d_, 1, pr, t * 128:(t + 1) * 128],
                            rhs=RT[hh * 64:(hh + 1) * 64, d_, 0, pr, t * 128:(t + 1) * 128], start=True, stop=True),
                            [("RT", t)], [pss])
                for d_ in range(2):
                    mk = 0 if d_ == 0 else 2
                    stm = STm[d_]
                    sv = stm[:].rearrange("p (pr hh) j -> p pr hh j", hh=2)
                    for hh, pss in ((0, pssA[d_]), (1, pssB[d_])):
                        k.V(lambda e, hh=hh, pss=pss, mk=mk, sv=sv: e.tensor_tensor(
                            out=sv[:, :, hh, :], in0=pss[:, 0:256].rearrange("p (pr j) -> p pr j", pr=2),
                            in1=maskb[:, mk, :].unsqueeze(1).to_broadcast([128, 2, 128]), op=ALU.mult),
                            [pss, maskb], [(stm.name, hh)])
                for d_ in range(2):
                    t = tt[d_]
                    stm, po_, Sbb = STm[d_], pso[d_], Sb[d_]
                    for h in range(4):
                        pr, hh = h // 2, h % 2
                        k.T(lambda e, h=h, t=t, stm=stm, po_=po_: e.matmul(
                            po_[:, h, :], lhsT=stm[:, h, :], rhs=Vb[:, t, h * 64:(h + 1) * 64], start=True, stop=False),
                            [(stm.name, hh), ("Vb", t)], [po_])
                        k.T(lambda e, h=h, pr=pr, hh=hh, t=t, d_=d_, Sbb=Sbb, po_=po_: e.matmul(
                            po_[:, h, :], lhsT=RT[hh * 64:(hh + 1) * 64, d_, 0, pr, t * 128:(t + 1) * 128],
                            rhs=Sbb[hh * 64:(hh + 1) * 64, pr, hh * 64:(hh + 1) * 64], start=False, stop=True),
                            [("RT", t), Sbb], [po_])
                    if ci < NT - 1:
                        for pr in range(2):
                            k.T(lambda e, pr=pr, t=t, d_=d_: e.matmul(
                                pkvt[:, d_, pr, :], lhsT=KD[:, t, d_, pr * 128:(pr + 1) * 128], rhs=Vb[:, t, pr * 128:(pr + 1) * 128],
                                start=True, stop=True), [("KD", t), ("Vb", t)], [("pkv", d_)])
                for d_ in range(2):
                    t = tt[d_]
                    po_, St, Sbb, tm = pso[d_], Sst[d_], Sb[d_], tmp[d_]
                    k.V(lambda e, po_=po_, t=t: e.tensor_tensor(out=OA[:, t, :], in0=OA[:, t, :],
                                                                in1=po_[:].rearrange("p h d -> p (h d)"), op=ALU.add),
                        [po_, ("OA", t)], [("OA", t)])
                    if ci < NT - 1:
                        k.V(lambda e, St=St, tm=tm, d_=d_: e.tensor_tensor(out=tm[:], in0=pkvt[:, d_, :, :], in1=St[:], op=ALU.add),
                            [("pkv", d_), St], [tm])
                        for pr in range(2):
                            k.V(lambda e, pr=pr, St=St, d_=d_, tm=tm: e.tensor_scalar(
                                out=St[:, pr, :], in0=tm[:, pr, :], scalar1=g128[:, d_, pr:pr + 1], scalar2=None,
                                op0=ALU.mult), [tm, g128], [St])
                            k.A(lambda e, pr=pr, Sbb=Sbb, d_=d_, tm=tm: e.activation(
                                out=Sbb[:, pr, :], in_=tm[:, pr, :], func=AF.Copy, scale=g128[:, d_, pr:pr + 1]),
                                [tm, g128], [Sbb])

            tiles = list(range(2, NT)) + ([] if last else [0, 1])

            def g0(i):
                t = tiles[i]
                os_, mn = osum[i % 3], mean[i % 3]
                k.G(lambda e: e.tensor_copy(out=os_[:], in_=OA[:, t, :]), [("OA", t)], [os_])
                k.V(lambda e: e.tensor_reduce(out=mn[:], in_=os_[:].rearrange("p (h d) -> p h d", d=64), axis=AX.X,
                                              op=ALU.add), [os_], [mn])
                k.V(lambda e: e.tensor_scalar(out=mn[:], in0=mn[:], scalar1=1.0 / 64, scalar2=None, op0=ALU.mult), [mn], [mn])

            def g1(i):
                os_, mn, cn, s2 = osum[i % 3], mean[i % 3], cen[i % 4], sq[i % 2]
                k.V(lambda e: e.tensor_tensor(out=cn[:].rearrange("p (h d) -> p h d", d=64),
                                              in0=os_[:].rearrange("p (h d) -> p h d", d=64),
                                              in1=mn[:].unsqueeze(2).to_broadcast([128, 4, 64]), op=ALU.subtract),
                    [os_, mn], [cn])
                k.G(lambda e: e.tensor_tensor(out=s2[:], in0=cn[:], in1=cn[:], op=ALU.mult), [cn], [s2])

            def g2(i):
                s2, vv = sq[i % 2], var[i % 3]
                k.V(lambda e: e.tensor_reduce(out=vv[:], in_=s2[:].rearrange("p (h d) -> p h d", d=64), axis=AX.X,
                                              op=ALU.add), [s2], [vv])
                k.A(lambda e: e.activation(out=vv[:], in_=vv[:], func=AF.Ln, scale=1.0 / 64, bias=EPSB[:, 0:1]), [vv, EPSB], [vv])
                k.A(lambda e: e.activation(out=vv[:], in_=vv[:], func=AF.Exp, scale=-0.5), [vv], [vv])

            def g3(i):
                cn, vv, rf = cen[i % 4], var[i % 3], rof[i % 3]
                k.V(lambda e: e.tensor_tensor(out=rf[:].rearrange("p (h d) -> p h d", d=64),
                                              in0=cn[:].rearrange("p (h d) -> p h d", d=64),
                                              in1=vv[:].unsqueeze(2).to_broadcast([128, 4, 64]), op=ALU.mult), [cn, vv], [rf])
                k.G(lambda e: e.tensor_tensor(out=rf[:], in0=rf[:], in1=gn[:], op=ALU.mult), [rf, gn], [rf])

            def g4(i, s=s):
                t = tiles[i]
                rf, r_o = rof[i % 3], ro[i % 2]
                k.V(lambda e: e.tensor_tensor(out=r_o[:], in0=rf[:], in1=SG[:, t, :], op=ALU.mult), [rf, ("SG", t)], [r_o])
                k.dma("act", lambda e: e.dma_start(out=A["mixin_d"][s, t * 128:(t + 1) * 128, 512:768], in_=r_o[:]),
                      reads=[r_o], writes=["mixin_d"])

            run_pipe(len(tiles), [g0, g1, g2, g3, g4])


def phase_conv(k, A, P, l, last):
    with k.phase():
        ident = P["ident"]
        EPSB = P["epsb"]
        cwT = k.sb("cwT", [128, 2, 32], F32)
        vT = k.sb("vT", [128, 2, 4], F32)
        with k.phase():
            cw = k.sb("cw", [31, 256], F32)
            k.dma("sp", lambda e: e.dma_start(out=cw[:], in_=A["conv_w"][l]), writes=[cw])
            pw = k.ps("pw", [128, 2, 32], F32)
            for cc in range(2):
                k.T(lambda e, cc=cc: e.transpose(pw[:, cc, 0:31], cw[0:31, cc * 128:(cc + 1) * 128], ident[0:31, 0:31]),
                    [cw, ident], [pw])
            k.V(lambda e: e.tensor_copy(out=cwT[:, :, 0:31], in_=pw[:, :, 0:31]), [pw], [cwT])
            vec = k.sb("vec", [3, 256], F32)
            k.dma("sp", lambda e: e.dma_start(out=vec[0:1, :], in_=A["conv_b"][l:l + 1, :]), writes=[vec])
            k.dma("sp", lambda e: e.dma_start(out=vec[1:2, :], in_=A["conv_norm_g"][l:l + 1, :]), writes=[vec])
            k.dma("sp", lambda e: e.dma_start(out=vec[2:3, :], in_=A["conv_norm_b"][l:l + 1, :]), writes=[vec])
            pv = k.ps("pv", [128, 2, 4], F32)
            for cc in range(2):
                k.T(lambda e, cc=cc: e.transpose(pv[:, cc, 0:3], vec[0:3, cc * 128:(cc + 1) * 128], ident[0:3, 0:3]),
                    [vec, ident], [pv])
            k.V(lambda e: e.tensor_copy(out=vT[:, :, 0:3], in_=pv[:, :, 0:3]), [pv], [vT])
        diag = k.sb("diag", [128, 2, 31, 128], BF16)
        for cc in range(2):
            for kk in range(31):
                eng = k.V if kk % 2 == 0 else k.G
                eng(lambda e, cc=cc, kk=kk: e.tensor_scalar(out=diag[:, cc, kk, :], in0=ident[:],
                                                            scalar1=cwT[:, cc, kk:kk + 1], scalar2=None, op0=ALU.mult),
                    [ident, cwT], [diag])
        ones = k.sb("ones", [128, 128], F32R)
        onesf = k.sb("onesf", [128, 128], F32)
        k.V(lambda e: e.memset(onesf[:], 1.0 / 256), [], [onesf])
        k.V(lambda e: e.tensor_copy(out=ones[:], in_=onesf[:]), [onesf], [ones])

        LP = S + 30
        hp = [[k.sb("hp%d_%d" % (j, cc), [128, LP], BF16) for cc in range(2)] for j in range(2)]
        val = [[k.sb("val%d_%d" % (j, cc), [128, S], F32) for cc in range(2)] for j in range(2)]
        gt = [[k.sb("gt%d_%d" % (j, cc), [128, S], F32) for cc in range(2)] for j in range(2)]
        cvo = [[k.sb("cvo%d_%d" % (j, cc), [128, 512], F32R) for cc in range(2)] for j in range(5)]
        csq = [[k.sb("csq%d_%d" % (j, cc), [128, 512], F32R) for cc in range(2)] for j in range(2)]
        pcv = [[k.ps("pcv%d_%d" % (j, cc), [128, 512], F32) for cc in range(2)] for j in range(2)]
        pmean = [k.ps("pmean%d" % j, [128, 512], F32) for j in range(2)]
        pex2 = [k.ps("pex2%d" % j, [128, 512], F32) for j in range(2)]
        msb = [k.sb("msb%d" % j, [128, 512], F32) for j in range(3)]
        rsd = [k.sb("rsd%d" % j, [128, 512], F32) for j in range(3)]
        yv = [[k.sb("yv%d_%d" % (j, cc), [128, 512], F32) for cc in range(2)] for j in range(2)]
        yo = [[k.sb("yo%d_%d" % (j, cc), [128, 512], BF16) for cc in range(2)] for j in range(2)]
        segs = []
        for s in range(NS):
            segs.append((s, C, S))
            if not last:
                segs.append((s, 0, C))
        items = []
        for si, (s, tok0, Lg) in enumerate(segs):
            nb = (Lg + 511) // 512
            for b in range(nb):
                items.append((si, s, tok0, Lg, b, min(512, Lg - b * 512)))

        def pre(i):
            si, s, tok0, Lg, b, w = items[i]
            if b != 0:
                return
            j = si % 2
            for cc in range(2):
                k.dma("sp", lambda e, cc=cc: e.dma_start(
                    out=val[j][cc][:, 0:Lg], in_=A["cv_d"][s, cc * 128:(cc + 1) * 128, tok0:tok0 + Lg]),
                    reads=["cv_d"], writes=[val[j][cc]])
                k.dma("act", lambda e, cc=cc: e.dma_start(
                    out=gt[j][cc][:, 0:Lg], in_=A["cv_d"][s, 256 + cc * 128: 256 + (cc + 1) * 128, tok0:tok0 + Lg]),
                    reads=["cv_d"], writes=[gt[j][cc]])
                k.A(lambda e, cc=cc: e.activation(out=gt[j][cc][:, 0:Lg], in_=gt[j][cc][:, 0:Lg], func=AF.Sigmoid),
                    [gt[j][cc]], [gt[j][cc]])
                k.G(lambda e, cc=cc: e.memset(hp[j][cc][:], 0.0), [], [hp[j][cc]])
                k.V(lambda e, cc=cc: e.tensor_tensor(out=hp[j][cc][:, 15:15 + Lg], in0=val[j][cc][:, 0:Lg],
                                                     in1=gt[j][cc][:, 0:Lg], op=ALU.mult),
                    [val[j][cc], gt[j][cc]], [hp[j][cc]])

        def nop(i):
            return

        def c0(i):
            si, s, tok0, Lg, b, w = items[i]
            j = si % 2
            for cc in range(2):
                p = pcv[i % 2][cc]
                for kk in range(31):
                    k.T(lambda e, p=p, cc=cc, kk=kk: e.matmul(
                        p[:, 0:w], lhsT=diag[:, cc, kk, :], rhs=hp[j][cc][:, b * 512 + kk: b * 512 + kk + w],
                        start=(kk == 0), stop=(kk == 30)), [diag, hp[j][cc]], [p])

        def c1(i):
            si, s, tok0, Lg, b, w = items[i]
            for cc in range(2):
                p, co, cq = pcv[i % 2][cc], cvo[i % 5][cc], csq[i % 2][cc]
                k.A(lambda e, p=p, co=co, cc=cc: e.activation(out=co[:, 0:w], in_=p[:, 0:w], func=AF.Identity,
                                                             bias=vT[:, cc, 0:1], scale=1.0), [p, vT], [co])
                k.V(lambda e, co=co, cq=cq: e.tensor_tensor(out=cq[:, 0:w], in0=co[:, 0:w].bitcast(F32),
                                                            in1=co[:, 0:w].bitcast(F32), op=ALU.mult), [co], [cq])

        def c2(i):
            si, s, tok0, Lg, b, w = items[i]
            pm_, px_ = pmean[i % 2], pex2[i % 2]
            for cc in range(2):
                k.T(lambda e, cc=cc: e.matmul(pm_[:, 0:w], lhsT=ones[:], rhs=cvo[i % 5][cc][:, 0:w],
                                              start=(cc == 0), stop=(cc == 1)), [ones, cvo[i % 5][cc]], [pm_])
            for cc in range(2):
                k.T(lambda e, cc=cc: e.matmul(px_[:, 0:w], lhsT=ones[:], rhs=csq[i % 2][cc][:, 0:w],
                                              start=(cc == 0), stop=(cc == 1)), [ones, csq[i % 2][cc]], [px_])

        def c3(i):
            si, s, tok0, Lg, b, w = items[i]
            pm_, px_, ms, rs = pmean[i % 2], pex2[i % 2], msb[i % 3], rsd[i % 3]
            k.A(lambda e: e.copy(out=ms[:, 0:w], in_=pm_[:, 0:w]), [pm_], [ms])
            k.V(lambda e: e.tensor_tensor(out=rs[:, 0:w], in0=ms[:, 0:w], in1=ms[:, 0:w], op=ALU.mult), [ms], [rs])
            k.V(lambda e: e.tensor_tensor(out=rs[:, 0:w], in0=px_[:, 0:w], in1=rs[:, 0:w], op=ALU.subtract), [px_, rs], [rs])

        def c4(i):
            si, s, tok0, Lg, b, w = items[i]
            rs = rsd[i % 3]
            k.A(lambda e: e.activation(out=rs[:, 0:w], in_=rs[:, 0:w], func=AF.Ln, bias=EPSB[:, 0:1], scale=1.0), [rs, EPSB], [rs])
            k.A(lambda e: e.activation(out=rs[:, 0:w], in_=rs[:, 0:w], func=AF.Exp, scale=-0.5), [rs], [rs])

        def c5(i):
            si, s, tok0, Lg, b, w = items[i]
            ms, rs = msb[i % 3], rsd[i % 3]
            for cc in range(2):
                co, y = cvo[i % 5][cc], yv[i % 2][cc]
                k.G(lambda e, co=co, y=y: e.tensor_tensor(out=y[:, 0:w], in0=co[:, 0:w].bitcast(F32), in1=ms[:, 0:w],
                                                          op=ALU.subtract), [co, ms], [y])
                k.V(lambda e, y=y: e.tensor_tensor(out=y[:, 0:w], in0=y[:, 0:w], in1=rs[:, 0:w], op=ALU.mult), [y, rs], [y])

        def c6(i):
            si, s, tok0, Lg, b, w = items[i]
            for cc in range(2):
                y, o_ = yv[i % 2][cc], yo[i % 2][cc]
                k.A(lambda e, y=y, o_=o_, cc=cc: e.activation(out=o_[:, 0:w], in_=y[:, 0:w], func=AF.Silu,
                                                             scale=vT[:, cc, 1:2], bias=vT[:, cc, 2:3]), [y, vT], [o_])
                k.dma("act", lambda e, cc=cc, o_=o_: e.dma_start(
                    out=A["convT_d"][s, cc * 128:(cc + 1) * 128, tok0 + b * 512: tok0 + b * 512 + w],
                    in_=o_[:, 0:w]), reads=[o_], writes=["convT_d"])

        run_pipe(len(items), [pre, nop, c0, c1, c2, c3, c4, c5, c6])


def phase_out(k, A, P, l, last, src_lat, src_ctx, dst_lat):
    with k.phase():
        ident = P["ident"]
        identb = P["identb"]
        EPSB = P["epsb"]
        wout = k.sb("wout", [128, 8, D], BF16)
        wv = A["w_out"][l].rearrange("(kc p) f -> p kc f", p=128)
        for j in range(0, D, 512):
            k.dma("pool", lambda e, j=j: e.dma_start(out=wout[:, :, j:j + 512], in_=wv[:, :, j:j + 512]), writes=[("wout", j)])
        woutk = [("wout", 0), ("wout", 512)]
        wr = k.sb("wr", [128, 8, E], F32)
        k.dma("sp", lambda e: e.dma_start(out=wr[:], in_=A["w_router"][l].rearrange("(kc p) e -> p kc e", p=128)),
              writes=[wr])
        nr = NS + (0 if last else 1)
        gta = [k.sb("gta%d" % r, [128, D], F32) for r in range(nr)]
        gsf = [k.sb("gsf%d" % r, [128, D], F32) for r in range(nr)]
        shf = [k.sb("shf%d" % r, [128, D], F32) for r in range(nr)]
        gf = k.sb("gf", [128, D], F32)
        k.dma("sp", lambda e: e.dma_start(out=gf[:], in_=A["g_ffn"][l, :].partition_broadcast(128)), writes=[gf])
        for r in range(nr):
            k.dma("sp", lambda e, r=r: e.dma_start(out=gta[r][:], in_=A["mod_d"][l, r, 2 * D:3 * D].partition_broadcast(128)),
                  reads=["mod_d"], writes=[gta[r]])
            k.dma("sp", lambda e, r=r: e.dma_start(out=shf[r][:], in_=A["mod_d"][l, r, 3 * D:4 * D].partition_broadcast(128)),
                  reads=["mod_d"], writes=[shf[r]])
            k.dma("sp", lambda e, r=r: e.dma_start(out=gsf[r][:], in_=A["mod_d"][l, r, 4 * D:5 * D].partition_broadcast(128)),
                  reads=["mod_d"], writes=[gsf[r]])
            k.V(lambda e, r=r: e.scalar_tensor_tensor(out=gsf[r][:], in0=gsf[r][:], scalar=1.0, in1=gf[:], op0=ALU.add,
                                                      op1=ALU.mult), [gsf[r], gf], [gsf[r]])
        NB = 6
        mi = [k.sb("mi%d" % i, [128, 768], BF16) for i in range(3)]
        mixT = [k.sb("mixT%d" % i, [128, 8, 128], BF16) for i in range(4)]
        xt = [k.sb("xt%d" % i, [128, D], F32) for i in range(5)]
        xm = [k.sb("xm%d" % i, [128, D], F32) for i in range(5)]
        hf = [k.sb("hf%d" % i, [128, D], F32) for i in range(4)]
        hfb = [k.sb("hfb%d" % i, [128, D], BF16) for i in range(2)]
        junk = k.sb("junk", [128, D], BF16)
        ss = [k.sb("ss%d" % i, [128, 1], F32) for i in range(NB)]
        rstd = [k.sb("rstd%d" % i, [128, 1], F32) for i in range(NB)]
        hfT = [k.sb("hfT%d" % i, [128, 8, 128], F32) for i in range(2)]
        ptm = k.ps("ptm", [128, 6, 128], BF16)
        py = [k.ps("py%d" % i, [128, 512], F32) for i in range(2)]
        pth = k.ps("pth", [128, 8, 128], F32)
        plg = k.ps("plg", [128, 16], F32)
        pat = k.ps("pat", [48, 128], F32)
        lmax = [k.sb("lmax%d" % i, [128, 1], F32) for i in range(2)]
        lsum = [k.sb("lsum%d" % i, [128, 1], F32) for i in range(2)]
        afftm = [k.sb("afftm%d" % i, [128, 48], F32) for i in range(3)]
        affT = k.sb("affT", [48, S + C], F32)
        for a_ in afftm:
            k.V(lambda e, a_=a_: e.memset(a_[:], 0.0), [], [a_])
        tiles = list(range(2, NT)) + ([] if last else [0, 1])
        items = [(t, s) for t in tiles for s in range(NS)]

        def info(i):
            t, s = items[i]
            isctx = t < 2
            r = NS if isctx else s
            if isctx:
                src = src_ctx[s * C + t * 128: s * C + (t + 1) * 128, :]
                dst = A["res_d"][NLAT + s * C + t * 128: NLAT + s * C + (t + 1) * 128, :]
                hrow = NLAT + s * C + t * 128
            else:
                src = src_lat[s * S + (t - 2) * 128: s * S + (t - 1) * 128, :]
                dst = dst_lat[s * S + (t - 2) * 128: s * S + (t - 1) * 128, :]
                hrow = s * S + (t - 2) * 128
            return t, s, r, src, dst, hrow

        NX, NM, NH = 5, 4, 4

        def stL(i):
            t, s, r, src, dst, hrow = info(i)
            m, mt, x_t = mi[i % 3], mixT[i % NM], xt[i % NX]
            k.dma("sp", lambda e: e.dma_start(out=m[:], in_=A["mixin_d"][s, t * 128:(t + 1) * 128, :]),
                  reads=["mixin_d"], writes=[m])
            k.dma("sp", lambda e: e.dma_start(
                out=mt[:, 6:8, :], in_=A["convT_d"][s, :, t * 128:(t + 1) * 128].rearrange("(c p) t -> p c t", p=128)),
                reads=["convT_d"], writes=[mt])
            k.dma("act", lambda e: e.dma_start(out=x_t[:], in_=src), writes=[x_t])

        def stA1(i):
            m, mt = mi[i % 3], mixT[i % NM]
            for j in range(6):
                k.T(lambda e, j=j: e.transpose(ptm[:, j, :], m[:, j * 128:(j + 1) * 128], identb[:]), [m, identb], [ptm])
            k.A(lambda e: e.copy(out=mt[:, 0:6, :], in_=ptm[:]), [ptm], [mt])

        def stA2(i):
            t, s, r, src, dst, hrow = info(i)
            mt, x_m = mixT[i % NM], xm[i % NX]
            for hh in range(2):
                p = py[hh]
                for kc in range(8):
                    k.T(lambda e, p=p, kc=kc, hh=hh: e.matmul(p[:], lhsT=mt[:, kc, :], rhs=wout[:, kc, hh * 512:(hh + 1) * 512],
                                                              start=(kc == 0), stop=(kc == 7)), [mt] + woutk, [p])
                k.V(lambda e, p=p, hh=hh: e.tensor_tensor(out=x_m[:, hh * 512:(hh + 1) * 512], in0=p[:],
                                                          in1=gta[r][:, hh * 512:(hh + 1) * 512], op=ALU.mult),
                    [p, gta[r]], [x_m])

        def stA3(i):
            t, s, r, src, dst, hrow = info(i)
            x_t, x_m = xt[i % NX], xm[i % NX]
            k.G(lambda e: e.tensor_tensor(out=x_m[:], in0=x_m[:], in1=x_t[:], op=ALU.add), [x_m, x_t], [x_m])
            k.dma("sp", lambda e: e.dma_start(out=dst, in_=x_m[:]), reads=[x_m], writes=["res"])

        def stB1(i):
            x_m, sst, rs = xm[i % NX], ss[i % NX], rstd[i % NX]
            k.A(lambda e: e.activation(out=junk[:], in_=x_m[:], func=AF.Square, accum_out=sst[:, 0:1]), [x_m], [junk, sst])
            k.A(lambda e: e.activation(out=sst[:, 0:1], in_=sst[:, 0:1], func=AF.Ln, scale=1.0 / D, bias=EPSB[:, 0:1]),
                [sst, EPSB], [sst])
            k.A(lambda e: e.activation(out=rs[:, 0:1], in_=sst[:, 0:1], func=AF.Exp, scale=-0.5), [sst], [rs])

        def stB2(i):
            t, s, r, src, dst, hrow = info(i)
            x_m, h_f, rs = xm[i % NX], hf[i % NH], rstd[i % NX]
            k.V(lambda e: e.scalar_tensor_tensor(out=h_f[:], in0=x_m[:], scalar=rs[:, 0:1], in1=gsf[r][:], op0=ALU.mult,
                                                 op1=ALU.mult), [x_m, rs, gsf[r]], [h_f])

        def stB3(i):
            t, s, r, src, dst, hrow = info(i)
            h_f = hf[i % NH]
            k.G(lambda e: e.tensor_tensor(out=h_f[:], in0=h_f[:], in1=shf[r][:], op=ALU.add), [h_f, shf[r]], [h_f])

        def stB4(i):
            t, s, r, src, dst, hrow = info(i)
            h_f, h_b = hf[i % NH], hfb[i % 2]
            for kc in range(8):
                k.T(lambda e, kc=kc: e.transpose(pth[:, kc, :], h_f[:, kc * 128:(kc + 1) * 128], ident[:]), [h_f, ident], [pth])
            k.A(lambda e: e.copy(out=h_b[:], in_=h_f[:]), [h_f], [h_b])
            k.dma("act", lambda e: e.dma_start(out=A["hfb_d"][hrow:hrow + 128, :], in_=h_b[:]), reads=[h_b], writes=["hfb_d"])

        def stC1(i):
            h_T = hfT[i % 2]
            k.A(lambda e: e.copy(out=h_T[:], in_=pth[:]), [pth], [h_T])

        def stC2(i):
            h_T = hfT[i % 2]
            for kc in range(8):
                k.T(lambda e, kc=kc: e.matmul(plg[:], lhsT=h_T[:, kc, :], rhs=wr[:, kc, :], start=(kc == 0), stop=(kc == 7)),
                    [h_T, wr], [plg])
            lm = lmax[i % 2]
            k.V(lambda e: e.tensor_reduce(out=lm[:], in_=plg[:], axis=AX.X, op=ALU.max, negate=True), [plg], [lm])

        def stC3(i):
            t, s, r, src, dst, hrow = info(i)
            af = afftm[(i // NS) % 3]
            lm, ls = lmax[i % 2], lsum[i % 2]
            k.A(lambda e: e.activation(out=af[:, s * 32:s * 32 + 16], in_=plg[:], func=AF.Exp, bias=lm[:, 0:1],
                                       scale=1.0, accum_out=ls[:, 0:1]), [plg, lm], [af, ls])

        def stC4(i):
            t, s, r, src, dst, hrow = info(i)
            af = afftm[(i // NS) % 3]
            ls = lsum[i % 2]
            k.V(lambda e: e.reciprocal(out=ls[:], in_=ls[:]), [ls], [ls])
            k.V(lambda e: e.tensor_scalar(out=af[:, s * 32:s * 32 + 16], in0=af[:, s * 32:s * 32 + 16],
                                          scalar1=ls[:, 0:1], scalar2=None, op0=ALU.mult), [af, ls], [af])
            if s == NS - 1:
                k.T(lambda e: e.transpose(pat[:], af[:], ident[:]), [af, ident], [pat])

        def stC5(i):
            t, s, r, src, dst, hrow = info(i)
            if s == NS - 1:
                k.A(lambda e: e.copy(out=affT[:, t * 128:(t + 1) * 128], in_=pat[:]), [pat], [affT])

        stages = [stL, stA1, stA2, stA3, stB1, stB2, stB3, stB4, stC1, stC2, stC3, stC4, stC5]
        n = len(items)
        ns = len(stages)
        for step in range(n + ns - 1):
            for j in range(ns - 1, -1, -1):
                i = step - j
                if 0 <= i < n:
                    stages[j](i)
        k.dma("sp", lambda e: e.dma_start(out=A["aff_d"], in_=affT[:, C:C + S]), reads=[affT], writes=["aff_d"])
        if not last:
            k.dma("sp", lambda e: e.dma_start(out=A["affc_d"], in_=affT[:, 0:C]), reads=[affT], writes=["affc_d"])


def phase_moe(k, A, P, l, last, dst_lat):
    with k.phase():
        ident = P["ident"]
        nctx = 0 if last else NS
        NSL = NS * CAPL + nctx * CAPC
        if last:
            cgroups = [(0, 512)]
        else:
            cgroups = [(0, 288), (288, 288)]
        idxT = k.sb("idxT", [128, 2, 48], I32)
        gT = k.sb("gTm", [128, 2, 48], F32)
        if not last:
            idcT = k.sb("idcT", [32, 48], I32)
            gcT = k.sb("gcT", [32, 48], F32)
        with k.phase():
            wa = k.sb("wa", [48, S], F32)
            wb = k.sb("wb", [48, S], F32)
            vals = k.sb("vals", [48, CAPL], F32)
            idx = k.sb("idx", [48, CAPL], U32)
            idxf = k.sb("idxf", [48, CAPL], F32)
            offs = k.sb("offs", [48, 1], F32)
            k.dma("sp", lambda e: e.dma_start(out=wa[:], in_=A["aff_d"]), reads=["aff_d"], writes=[wa])
            cur, oth = wa, wb
            for r in range(CAPL // 8):
                k.V(lambda e, cur=cur, r=r: e.max(out=vals[:, r * 8:(r + 1) * 8], in_=cur[:]), [cur], [vals])
                k.V(lambda e, cur=cur, r=r: e.max_index(out=idx[:, r * 8:(r + 1) * 8], in_max=vals[:, r * 8:(r + 1) * 8],
                                                        in_values=cur[:]), [cur, vals], [idx])
                if r < CAPL // 8 - 1:
                    k.V(lambda e, cur=cur, oth=oth, r=r: e.match_replace(out=oth[:], in_to_replace=vals[:, r * 8:(r + 1) * 8],
                                                                         in_values=cur[:], imm_value=-1.0), [cur, vals], [oth])
                    cur, oth = oth, cur
            k.V(lambda e: e.memset(offs[0:32, :], 0.0), [], [offs])
            k.V(lambda e: e.memset(offs[32:48, :], float(S)), [], [offs])
            k.V(lambda e: e.tensor_copy(out=idxf[:], in_=idx[:]), [idx], [idxf])
            k.V(lambda e: e.tensor_scalar(out=idxf[:], in0=idxf[:], scalar1=offs[:, 0:1], scalar2=None, op0=ALU.add),
                [idxf, offs], [idxf])
            pti = k.ps("pti", [128, 2, 48], F32)
            ptg = k.ps("ptg", [128, 2, 48], F32)
            for j in range(2):
                k.T(lambda e, j=j: e.transpose(pti[:, j, :], idxf[:, j * 128:(j + 1) * 128], ident[0:48, 0:48]), [idxf, ident], [pti])
                k.T(lambda e, j=j: e.transpose(ptg[:, j, :], vals[:, j * 128:(j + 1) * 128], ident[0:48, 0:48]), [vals, ident], [ptg])
            k.V(lambda e: e.tensor_copy(out=idxT[:], in_=pti[:]), [pti], [idxT])
            k.V(lambda e: e.tensor_copy(out=gT[:], in_=ptg[:]), [ptg], [gT])
            if not last:
                wc = k.sb("wc", [48, C], F32)
                wd_ = k.sb("wd_", [48, C], F32)
                valc = k.sb("valc", [48, CAPC], F32)
                idc = k.sb("idc", [48, CAPC], U32)
                idcf = k.sb("idcf", [48, CAPC], F32)
                offc = k.sb("offc", [48, 1], F32)
                k.dma("sp", lambda e: e.dma_start(out=wc[:], in_=A["affc_d"]), reads=["affc_d"], writes=[wc])
                cur, oth = wc, wd_
                for r in range(CAPC // 8):
                    k.V(lambda e, cur=cur, r=r: e.max(out=valc[:, r * 8:(r + 1) * 8], in_=cur[:]), [cur], [valc])
                    k.V(lambda e, cur=cur, r=r: e.max_index(out=idc[:, r * 8:(r + 1) * 8], in_max=valc[:, r * 8:(r + 1) * 8],
                                                            in_values=cur[:]), [cur, valc], [idc])
                    if r < CAPC // 8 - 1:
                        k.V(lambda e, cur=cur, oth=oth, r=r: e.match_replace(out=oth[:], in_to_replace=valc[:, r * 8:(r + 1) * 8],
                                                                             in_values=cur[:], imm_value=-1.0), [cur, valc], [oth])
                        cur, oth = oth, cur
                k.V(lambda e: e.memset(offc[0:32, :], float(NLAT)), [], [offc])
                k.V(lambda e: e.memset(offc[32:48, :], float(NLAT + C)), [], [offc])
                k.V(lambda e: e.tensor_copy(out=idcf[:], in_=idc[:]), [idc], [idcf])
                k.V(lambda e: e.tensor_scalar(out=idcf[:], in0=idcf[:], scalar1=offc[:, 0:1], scalar2=None, op0=ALU.add),
                    [idcf, offc], [idcf])
                ptic = k.ps("ptic", [32, 2, 48], F32)
                k.T(lambda e: e.transpose(ptic[:, 0, :], idcf[:, 0:32], ident[0:48, 0:48]), [idcf, ident], [ptic])
                k.T(lambda e: e.transpose(ptic[:, 1, :], valc[:, 0:32], ident[0:48, 0:48]), [valc, ident], [ptic])
                k.V(lambda e: e.tensor_copy(out=idcT[:], in_=ptic[:, 0, :]), [ptic], [idcT])
                k.V(lambda e: e.tensor_copy(out=gcT[:], in_=ptic[:, 1, :]), [ptic], [gcT])
        identb = P["identb"]
        nr = NS + (0 if last else 1)
        gtf = [k.sb("gtf%d" % r, [128, D], F32) for r in range(nr)]
        for r in range(nr):
            k.dma("sp", lambda e, r=r: e.dma_start(out=gtf[r][:], in_=A["mod_d"][l, r, 5 * D:6 * D].partition_broadcast(128)),
                  reads=["mod_d"], writes=[gtf[r]])
        xe = [k.sb("xe%d" % i, [128, D], BF16) for i in range(3)]
        xeT = [k.sb("xeT%d" % i, [128, 8, NSL], BF16) for i in range(2)]
        hT = k.sb("hT", [128, NFC, NSL], BF16)
        sg = [k.sb("sg%d" % i, [128, NSL], F32) for i in range(2)]
        NW = 3
        NPRE = NW - 1
        wg = [k.sb("wg%d" % i, [128, 8, 512], BF16) for i in range(NW)]
        wu = [k.sb("wu%d" % i, [128, 8, 512], BF16) for i in range(NW)]
        wdn = k.sb("wdn", [128, NFC, D], BF16)
        ysb = [k.sb("ysb%d" % i, [128, D], F32) for i in range(2)]
        ncg = len(cgroups)
        pg = [k.ps("pg%d" % i, [128, 512], F32) for i in range(ncg)]
        pu = [k.ps("pu%d" % i, [128, 512], F32) for i in range(ncg)]
        pxt = k.ps("pxt", [128, 8, 128], BF16)
        pyd = [k.ps("pyd%d" % i, [128, 512], F32) for i in range(8 - 2 * ncg - 1)]
        target = dst_lat
        pieces = [(i * 512, min(512, DFF - i * 512)) for i in range((DFF + 511) // 512)]
        NP_ = len(pieces)
        cnt = {"y": 0}

        def tiles_of(ex):
            tl = []
            for s in range(NS):
                for j in range(2):
                    tl.append((128, s * 256 + j * 128, idxT[:, j, s * 32 + ex: s * 32 + ex + 1],
                               gT[:, j, s * 32 + ex: s * 32 + ex + 1], s, "lat"))
            if not last:
                for s in range(NS):
                    tl.append((32, NS * CAPL + s * CAPC, idcT[:, s * 32 + ex: s * 32 + ex + 1],
                               gcT[:, s * 32 + ex: s * 32 + ex + 1], NS, "ctx"))
            return tl

        idx_reads = [idxT] + ([] if last else [idcT])

        def gather_T(ex):
            xT = xeT[ex % 2]
            for ti, (rows, c0, iap, gap, r, kind) in enumerate(tiles_of(ex)):
                x_e = xe[ti % 3]
                k.dma("pool", lambda e, x_e=x_e, rows=rows, iap=iap: e.indirect_dma_start(
                    out=x_e[0:rows, :], out_offset=None, in_=A["hfb_d"],
                    in_offset=bass.IndirectOffsetOnAxis(ap=iap[0:rows, :], axis=0)),
                    reads=["hfb_d"] + idx_reads, writes=[x_e])
                for kc in range(8):
                    k.T(lambda e, x_e=x_e, rows=rows, kc=kc: e.transpose(pxt[:, kc, 0:rows], x_e[0:rows, kc * 128:(kc + 1) * 128],
                                                                       identb[0:rows, 0:rows]), [x_e, identb], [pxt])
                if ti % 2 == 0:
                    k.A(lambda e, xT=xT, rows=rows, c0=c0: e.copy(out=xT[:, :, c0:c0 + rows], in_=pxt[:, :, 0:rows]), [pxt], [xT])
                else:
                    k.V(lambda e, xT=xT, rows=rows, c0=c0: e.tensor_copy(out=xT[:, :, c0:c0 + rows], in_=pxt[:, :, 0:rows]),
                        [pxt], [xT])

        def load_gu(ex, pi):
            c0, w = pieces[pi]
            gi_ = (ex * NP_ + pi) % NW
            wgv = A["w_gate"][l, ex].rearrange("(kc p) f -> p kc f", p=128)
            wuv = A["w_up"][l, ex].rearrange("(kc p) f -> p kc f", p=128)
            k.dma("pool", lambda e: e.dma_start(out=wg[gi_][:, :, 0:w], in_=wgv[:, :, c0:c0 + w]), writes=[wg[gi_]])
            k.dma("pool", lambda e: e.dma_start(out=wu[gi_][:, :, 0:w], in_=wuv[:, :, c0:c0 + w]), writes=[wu[gi_]])

        def load_dn(ex, pi):
            c0, w = pieces[pi]
            f0, nf = c0 // 128, w // 128
            wdv = A["w_down"][l, ex].rearrange("(fc p) d -> p fc d", p=128)
            for f in range(f0, f0 + nf, 2):
                k.dma("pool", lambda e, f=f: e.dma_start(out=wdn[:, f:f + 2, :], in_=wdv[:, f:f + 2, :]),
                      writes=[("wdn", f // 2)])

        def gu(ex, pi):
            c0, w = pieces[pi]
            gi_ = (ex * NP_ + pi) % NW
            xT = xeT[ex % 2]
            for f2 in range(w // 128):
                fc = c0 // 128 + f2
                for (W, ps_list) in ((wg[gi_], pg), (wu[gi_], pu)):
                    for gj, (g0, gw) in enumerate(cgroups):
                        p = ps_list[gj]
                        for kc in range(8):
                            k.T(lambda e, p=p, W=W, kc=kc, f2=f2, g0=g0, gw=gw: e.matmul(
                                p[:, 0:gw], lhsT=W[:, kc, f2 * 128:(f2 + 1) * 128], rhs=xT[:, kc, g0:g0 + gw],
                                start=(kc == 0), stop=(kc == 7)), [W, xT], [p])
                s_g = sg[fc % 2]
                for gj, (g0, gw) in enumerate(cgroups):
                    k.A(lambda e, s_g=s_g, gj=gj, g0=g0, gw=gw: e.activation(out=s_g[:, g0:g0 + gw], in_=pg[gj][:, 0:gw],
                                                                            func=AF.Silu), [pg[gj]], [s_g])
                    k.V(lambda e, s_g=s_g, gj=gj, g0=g0, gw=gw, fc=fc: e.tensor_tensor(
                        out=hT[:, fc, g0:g0 + gw], in0=s_g[:, g0:g0 + gw], in1=pu[gj][:, 0:gw], op=ALU.mult),
                        [s_g, pu[gj]], [("hT", fc)])

        def down(ex):
            for ti, (rows, c0, iap, gap, r, kind) in enumerate(tiles_of(ex)):
                y_s = ysb[ti % 2]
                for hh in range(2):
                    p = pyd[cnt["y"] % len(pyd)]
                    cnt["y"] += 1
                    for fc in range(NFC):
                        k.T(lambda e, p=p, rows=rows, c0=c0, fc=fc, hh=hh: e.matmul(
                            p[0:rows, :], lhsT=hT[:, fc, c0:c0 + rows], rhs=wdn[:, fc, hh * 512:(hh + 1) * 512],
                            start=(fc == 0), stop=(fc == NFC - 1)), [("hT", fc), ("wdn", fc // 2)], [p])
                    k.V(lambda e, p=p, rows=rows, y_s=y_s, hh=hh, gap=gap, r=r: e.scalar_tensor_tensor(
                        out=y_s[0:rows, hh * 512:(hh + 1) * 512], in0=p[0:rows, :], scalar=gap[0:rows, :],
                        in1=gtf[r][0:rows, hh * 512:(hh + 1) * 512], op0=ALU.mult, op1=ALU.mult),
                        [p, gT, gtf[r]] + ([] if last else [gcT]), [y_s])
                tgt = target if kind == "lat" else A["res_d"]
                k.dma("pool", lambda e, y_s=y_s, rows=rows, iap=iap, tgt=tgt: e.indirect_dma_start(
                    out=tgt, out_offset=bass.IndirectOffsetOnAxis(ap=iap[0:rows, :], axis=0), in_=y_s[0:rows, :],
                    in_offset=None, compute_op=ALU.add), reads=[y_s] + idx_reads, writes=["res"])

        gather_T(0)
        for pi in range(NPRE):
            load_gu(0, pi)
        for ex in range(E):
            for pi in range(NP_):
                load_dn(ex, pi)
                gu(ex, pi)
                if pi + NPRE < NP_:
                    load_gu(ex, pi + NPRE)
            if ex + 1 < E:
                gather_T(ex + 1)
                for pi in range(NPRE):
                    load_gu(ex + 1, pi)
            down(ex)


_NC_CACHE = {}


def kernel(**inputs):
    consts = host_consts()
    if "nc" not in _NC_CACHE:
        _NC_CACHE["nc"] = build_program()
    nc = _NC_CACHE["nc"]
    x = np.ascontiguousarray(inputs["x"], dtype=np.float32)
    c = np.ascontiguousarray(inputs["c"], dtype=np.float32)
    ctx = np.ascontiguousarray(inputs["ctx"], dtype=np.float32)
    shared = {"c_ctx": np.ascontiguousarray(inputs["c_ctx"], dtype=np.float32).reshape(1, D)}
    for n in WNAMES:
        shared[n] = np.ascontiguousarray(inputs[n], dtype=np.float32).reshape(WSHAPES[n])
    for n, v in consts.items():
        shared[n] = v
    in_maps = []
    for ci in range(NCORES):
        m = dict(shared)
        m["x"] = x[ci * NS:(ci + 1) * NS].reshape(NS * S, D)
        m["c"] = c[ci * NS:(ci + 1) * NS]
        m["ctx"] = ctx[ci * NS:(ci + 1) * NS].reshape(NS * C, D)
        in_maps.append(m)
    res = run_bass_kernel_spmd(nc, in_maps, core_ids=list(range(NCORES)))
    out = np.concatenate([r["y"].reshape(NS, S, D) for r in res.results], axis=0)
    return out.astype(np.float32)
```

```python
import os
import numpy as np
import concourse.bass as bass
import concourse.mybir as mybir
from concourse.bass_utils import run_bass_kernel_spmd
from contextlib import ExitStack, contextmanager

F32 = mybir.dt.float32
F32R = mybir.dt.float32r
BF16 = mybir.dt.bfloat16
U32 = mybir.dt.uint32
I32 = mybir.dt.int32
AF = mybir.ActivationFunctionType
ALU = mybir.AluOpType
AX = mybir.AxisListType

NCORES = 8
NS = 2
D = 1024
S = 2048
C = 256
NT = (S + C) // 128
L = 2
E = 16
DFF = 2816
NFC = DFF // 128
CAPL = 256
CAPC = 32
EPS = 1e-6
UC = 1792
NLAT = NS * S
NROW = NS * (S + C)


class KB:
    ENGS = ("pe", "dve", "act", "pool", "sp")

    def __init__(self, nc, ndma=24):
        self.nc = nc
        self.st = ExitStack()
        self.q = {e: [] for e in self.ENGS}
        self.cnt = {e: 0 for e in self.ENGS}
        self.sem = {e: self.st.enter_context(nc.semaphore("prog_" + e)) for e in self.ENGS}
        self.waited = {(c, p): 0 for c in self.ENGS for p in self.ENGS}
        self.waited_dma = {}
        self.lastw = {}
        self.readers = {}
        self.dma_sems = []
        self.ndma = {"sp": 0, "act": 0, "pool": 0}
        self.pool_of = {"sp": (0, 10), "act": (10, 8), "pool": (18, 14)}
        for i in range(32):
            s = self.st.enter_context(nc.semaphore("dma%d" % i))
            self.dma_sems.append([s, 0, None])
        self.cur = self.st

    def sb(self, name, shape, dt):
        self.uid = getattr(self, "uid", 0) + 1
        return self.cur.enter_context(self.nc.sbuf_tensor("%s_s%d" % (name, self.uid), shape, dt))

    def ps(self, name, shape, dt):
        self.uid = getattr(self, "uid", 0) + 1
        return self.cur.enter_context(self.nc.psum_tensor("%s_p%d" % (name, self.uid), shape, dt))

    @contextmanager
    def phase(self):
        prev = self.cur
        with ExitStack() as es:
            self.cur = es
            yield
            self.barrier()
        self.cur = prev

    def barrier(self):
        toks = [(p, self.cnt[p]) for p in self.ENGS if self.cnt[p] > 0]
        dtoks = [d[2] for d in self.dma_sems if d[2] is not None]
        for e in self.ENGS:
            for t in toks:
                if t[0] != e:
                    self._wait(e, t)
            for t in dtoks:
                self._wait(e, t)
        self.lastw = {}
        self.readers = {}

    def _key(self, x):
        if isinstance(x, (str, tuple)):
            return x
        return x.tensor.name if hasattr(x, "tensor") else x.name

    def _wait(self, eng, tok):
        if tok is None:
            return
        if tok[0] == "dma":
            _, i, val = tok
            if self.waited_dma.get((eng, i), 0) >= val:
                return
            self.waited_dma[(eng, i)] = val
            sem = self.dma_sems[i][0]
            self.q[eng].append(lambda e, sem=sem, val=val: e.wait_ge(sem, val))
            return
        p, c = tok
        if eng == "pe" and p == "pe" and not (PESYNC or getattr(self, "pesync", False)):
            return
        if self.waited[(eng, p)] >= c:
            return
        self.waited[(eng, p)] = c
        s = self.sem[p]
        self.q[eng].append(lambda e, s=s, c=c: e.wait_ge(s, c))

    def _deps(self, eng, rk, wk):
        for k in rk:
            self._wait(eng, self.lastw.get(k))
        for k in wk:
            self._wait(eng, self.lastw.get(k))
            for t in self.readers.get(k, ()):
                self._wait(eng, t)

    def _record(self, tok, rk, wk):
        for k in wk:
            self.lastw[k] = tok
            self.readers[k] = []
        for k in rk:
            self.readers.setdefault(k, []).append(tok)

    def op(self, eng, fn, reads=(), writes=()):
        rk = [self._key(r) for r in reads]
        wk = [self._key(w) for w in writes]
        self._deps(eng, rk, wk)
        self.cnt[eng] += 1
        tok = (eng, self.cnt[eng])
        s = self.sem[eng]
        self.q[eng].append(lambda e, fn=fn, s=s: fn(e).then_inc(s, 1))
        self._record(tok, rk, wk)
        return tok

    def V(self, fn, r=(), w=()):
        return self.op("dve", fn, r, w)

    def A(self, fn, r=(), w=()):
        return self.op("act", fn, r, w)

    def G(self, fn, r=(), w=()):
        return self.op("pool", fn, r, w)

    def T(self, fn, r=(), w=()):
        return self.op("pe", fn, r, w)

    def dma(self, eng, fn, reads=(), writes=()):
        rk = [self._key(r) for r in reads]
        wk = [self._key(w) for w in writes]
        self._deps(eng, rk, wk)
        base, n = self.pool_of[eng]
        i = base + self.ndma[eng] % n
        self.ndma[eng] += 1
        ent = self.dma_sems[i]
        if ent[2] is not None:
            self._wait(eng, ent[2])
        ent[1] += 16
        tok = ("dma", i, ent[1])
        ent[2] = tok
        sem = ent[0]
        self.q[eng].append(lambda e, fn=fn, sem=sem: fn(e).then_inc(sem, 16))
        self._record(tok, rk, wk)
        return tok

    def finish(self):
        nc = self.nc
        self.barrier()
        q = self.q
        with nc.Block() as block:
            @block.tensor
            def _(e):
                for f in q["pe"]:
                    f(e)

            @block.vector
            def _(e):
                for f in q["dve"]:
                    f(e)

            @block.scalar
            def _(e):
                for f in q["act"]:
                    f(e)

            @block.gpsimd
            def _(e):
                for f in q["pool"]:
                    f(e)

            @block.sync
            def _(e):
                for f in q["sp"]:
                    f(e)
        self.st.close()


def host_consts():
    f32 = np.float32
    half = 16
    inv = (10000.0 ** (-np.arange(half, dtype=f32) / half)).astype(f32)
    t = np.arange(S)
    rows = (t // 64).astype(f32)
    cols = (t % 64).astype(f32)
    ar = (rows[:, None] * inv[None, :]).astype(f32)
    ac = (cols[:, None] * inv[None, :]).astype(f32)
    attC = np.concatenate([np.cos(ar), np.cos(ar), np.cos(ac), np.cos(ac)], axis=1).astype(f32)
    attS = np.concatenate([-np.sin(ar), np.sin(ar), -np.sin(ac), np.sin(ac)], axis=1).astype(f32)
    half = 32
    inv = (10000.0 ** (-np.arange(half, dtype=f32) / half)).astype(f32)
    pos = np.arange(S + C).astype(f32)
    a = (pos[:, None] * inv[None, :]).astype(f32)
    retC = np.concatenate([np.cos(a), np.cos(a)], axis=1).astype(f32)
    retS = np.concatenate([-np.sin(a), np.sin(a)], axis=1).astype(f32)
    j = np.arange(128)[:, None]
    i = np.arange(128)[None, :]
    masks = np.stack([(j <= i), (j >= i), (j > i)], axis=0).astype(f32)
    ident = np.eye(128, dtype=f32)
    p = np.arange(128, dtype=f32)
    dcoef = np.stack([p + 1, -(p + 1), -p, p], axis=1).astype(f32)
    return {"attC": attC, "attS": attS, "retC": retC, "retS": retS, "masks": masks,
            "ident": ident, "dcoef": dcoef}


WNAMES = ["w_mod", "b_mod", "g_mix", "g_ffn", "w_in", "q_norm_g", "k_norm_g", "att_sink",
          "ret_decay_logit", "ret_norm_g", "conv_w", "conv_b", "conv_norm_g", "conv_norm_b",
          "w_out", "w_router", "w_gate", "w_up", "w_down"]
WSHAPES = {
    "w_mod": [L, D, 6 * D], "b_mod": [L, 6 * D], "g_mix": [L, D], "g_ffn": [L, D], "w_in": [L, D, 2304],
    "q_norm_g": [L, 64], "k_norm_g": [L, 64], "att_sink": [L, 8], "ret_decay_logit": [L, 8],
    "ret_norm_g": [L, 256], "conv_w": [L, 31, 256], "conv_b": [L, 256], "conv_norm_g": [L, 256],
    "conv_norm_b": [L, 256], "w_out": [L, D, D], "w_router": [L, D, E], "w_gate": [L, E, D, DFF],
    "w_up": [L, E, D, DFF], "w_down": [L, E, DFF, D],
}
CSHAPES = {"attC": [S, 64], "attS": [S, 64], "retC": [S + C, 64], "retS": [S + C, 64],
           "masks": [3, 128, 128], "ident": [128, 128], "dcoef": [128, 4]}

PESYNC = os.environ.get("MK_PESYNC", "") == "1"
RETSKIP = os.environ.get("MK_RETSKIP", "")
STOP = os.environ.get("MK_STOP", "")
DEBUG = os.environ.get("MK_DEBUG", "") == "1"


def build_program():
    nc = bass.Bass("TRN2", target_bir_lowering=False)
    A = {}
    A["x"] = nc.dram_tensor("x", [NS * S, D], F32, kind="ExternalInput").ap()
    A["c"] = nc.dram_tensor("c", [NS, D], F32, kind="ExternalInput").ap()
    A["ctx"] = nc.dram_tensor("ctx", [NS * C, D], F32, kind="ExternalInput").ap()
    A["c_ctx"] = nc.dram_tensor("c_ctx", [1, D], F32, kind="ExternalInput").ap()
    for n in WNAMES:
        A[n] = nc.dram_tensor(n, WSHAPES[n], F32, kind="ExternalInput").ap()
    for n, shp in CSHAPES.items():
        A[n] = nc.dram_tensor(n, shp, F32, kind="ExternalInput").ap()
    A["y"] = nc.dram_tensor("y", [NS * S, D], F32, kind="ExternalOutput").ap()
    dk = "ExternalOutput" if DEBUG else "Internal"
    A["mod_d"] = nc.dram_tensor("mod_d", [L, 4, 6 * D], F32, kind=dk).ap()
    A["u_d"] = nc.dram_tensor("u_d", [NS, S + C, UC], F32, kind=dk).ap()
    A["cv_d"] = nc.dram_tensor("cv_d", [NS, 512, S + C], F32, kind=dk).ap()
    A["mixin_d"] = nc.dram_tensor("mixin_d", [NS, S + C, 768], BF16, kind=dk).ap()
    A["convT_d"] = nc.dram_tensor("convT_d", [NS, 256, S + C], BF16, kind=dk).ap()
    A["res_d"] = nc.dram_tensor("res_d", [NROW, D], F32, kind=dk).ap()
    A["hf_d"] = nc.dram_tensor("hf_d", [NROW, D], F32, kind=dk).ap()
    A["aff_d"] = nc.dram_tensor("aff_d", [48, S], F32, kind=dk).ap()
    A["hfb_d"] = nc.dram_tensor("hfb_d", [NROW, D], BF16, kind=dk).ap()
    A["affc_d"] = nc.dram_tensor("affc_d", [48, C], F32, kind=dk).ap()

    k = KB(nc)
    P = {}
    P["ident"] = k.sb("ident", [128, 128], F32)
    P["identb"] = k.sb("identb", [128, 128], BF16)
    P["masks"] = k.sb("masks", [128, 3, 128], F32)
    P["modT"] = [k.sb("modT%d" % l, [128, 48, 4], F32) for l in range(L)]
    P["gT"] = k.sb("gT", [128, 8, 4], F32)
    P["gsA"] = [k.sb("gsA%d" % l, [128, 8, 4], F32) for l in range(L)]
    P["epsb"] = k.sb("epsb", [128, 1], F32)
    k.V(lambda e: e.memset(P["epsb"][:], EPS), [], [P["epsb"]])
    k.dma("sp", lambda e: e.dma_start(out=P["ident"][:], in_=A["ident"]), writes=[P["ident"]])
    k.dma("sp", lambda e: e.dma_start(out=P["masks"][:], in_=A["masks"].rearrange("m j i -> j m i")),
          writes=[P["masks"]])
    k.V(lambda e: e.tensor_copy(out=P["identb"][:], in_=P["ident"][:]), [P["ident"]], [P["identb"]])

    phase_mod(k, A, P)
    if STOP == "mod":
        k.finish()
        return nc
    for l in range(L):
        last = l == L - 1
        src_lat = A["x"] if l == 0 else A["res_d"][0:NLAT]
        src_ctx = A["ctx"] if l == 0 else A["res_d"][NLAT:NROW]
        dst_lat = A["y"] if last else A["res_d"][0:NLAT]
        phase_proj(k, A, P, l, src_lat, src_ctx)
        if STOP == "proj%d" % l:
            break
        phase_att(k, A, P, l, last)
        if STOP == "att%d" % l:
            break
        phase_ret(k, A, P, l, last)
        if STOP == "ret%d" % l:
            break
        phase_conv(k, A, P, l, last)
        if STOP == "conv%d" % l:
            break
        phase_out(k, A, P, l, last, src_lat, src_ctx, dst_lat)
        if STOP == "out%d" % l:
            break
        phase_moe(k, A, P, l, last, dst_lat)
        if STOP == "moe%d" % l:
            break
    k.finish()
    return nc


def phase_mod(k, A, P):
    with k.phase():
        cc = k.sb("cc", [4, D], F32)
        cs = k.sb("cs", [4, D], F32)
        csT = k.sb("csT", [128, 8, 4], BF16)
        gg = k.sb("gg", [4, D], F32)
        pt = k.ps("pt", [128, 8, 4], F32)
        pm = [k.ps("pm%d" % i, [4, 512], F32) for i in range(2)]
        ptm = k.ps("ptm", [128, 48, 4], F32)
        wm = [k.sb("wm%d" % i, [128, 8, 512], BF16) for i in range(3)]
        bm = k.sb("bm", [4, 6 * D], F32)
        modrow = k.sb("modrow", [4, 6 * D], F32)
        ident = P["ident"]
        k.V(lambda e: e.memset(cc[:], 0.0), [], [cc])
        k.dma("sp", lambda e: e.dma_start(out=cc[0:NS, :], in_=A["c"]), writes=[cc])
        k.dma("sp", lambda e: e.dma_start(out=cc[NS:NS + 1, :], in_=A["c_ctx"]), writes=[cc])
        k.A(lambda e: e.activation(out=cs[:], in_=cc[:], func=AF.Silu), [cc], [cs])
        for kc in range(8):
            k.T(lambda e, kc=kc: e.transpose(pt[:, kc, :], cs[0:4, kc * 128:(kc + 1) * 128], ident[0:4, 0:4]),
                [cs, ident], [pt])
        k.V(lambda e: e.tensor_copy(out=csT[:], in_=pt[:]), [pt], [csT])
        k.dma("sp", lambda e: e.dma_start(out=gg[0:2, :], in_=A["g_mix"]), writes=[gg])
        k.dma("sp", lambda e: e.dma_start(out=gg[2:4, :], in_=A["g_ffn"]), writes=[gg])
        for kc in range(8):
            k.T(lambda e, kc=kc: e.transpose(pt[:, kc, :], gg[0:4, kc * 128:(kc + 1) * 128], ident[0:4, 0:4]),
                [gg, ident], [pt])
        k.V(lambda e: e.tensor_copy(out=P["gT"][:], in_=pt[:]), [pt], [P["gT"]])
        for l in range(L):
            k.dma("sp", lambda e, l=l: e.dma_start(out=bm[:], in_=A["b_mod"][l, :].partition_broadcast(4)),
                  writes=[bm])
            wv = A["w_mod"][l].rearrange("(kc p) f -> p kc f", p=128)
            for j in range(12):
                w = wm[(l * 12 + j) % 3]
                k.dma("pool", lambda e, w=w, j=j, wv=wv: e.dma_start(out=w[:], in_=wv[:, :, j * 512:(j + 1) * 512]),
                      writes=[w])
                p = pm[j % 2]
                for kc in range(8):
                    k.T(lambda e, p=p, w=w, kc=kc: e.matmul(p[:], lhsT=csT[:, kc, :], rhs=w[:, kc, :],
                                                          start=(kc == 0), stop=(kc == 7)), [csT, w], [p])
                k.V(lambda e, p=p, j=j: e.tensor_tensor(out=modrow[:, j * 512:(j + 1) * 512], in0=p[:],
                                                        in1=bm[:, j * 512:(j + 1) * 512], op=ALU.add),
                    [p, bm], [modrow])
            k.dma("sp", lambda e, l=l: e.dma_start(out=A["mod_d"][l], in_=modrow[:]), reads=[modrow],
                  writes=["mod_d"])
            for j in range(48):
                k.T(lambda e, j=j: e.transpose(ptm[:, j, :], modrow[0:4, j * 128:(j + 1) * 128], ident[0:4, 0:4]),
                    [modrow, ident], [ptm])
            mT = P["modT"][l]
            k.V(lambda e, mT=mT: e.tensor_copy(out=mT[:], in_=ptm[:]), [ptm], [mT])
            gs = P["gsA"][l]
            k.V(lambda e, gs=gs, mT=mT: e.tensor_scalar(out=gs[:], in0=mT[:, 8:16, :], scalar1=1.0, scalar2=None,
                                                        op0=ALU.add), [mT], [gs])
            k.V(lambda e, gs=gs, l=l: e.tensor_tensor(out=gs[:], in0=gs[:],
                                                      in1=P["gT"][:, :, l:l + 1].to_broadcast([128, 8, 4]),
                                                      op=ALU.mult), [gs, P["gT"]], [gs])


def rms_rstd(k, xt, junk, ss, rstd, n):
    k.A(lambda e: e.activation(out=junk, in_=xt, func=AF.Square, accum_out=ss[:, 0:1]), [xt], [junk, ss])
    k.V(lambda e: e.tensor_scalar(out=ss[:, 0:1], in0=ss[:, 0:1], scalar1=1.0 / n, scalar2=EPS, op0=ALU.mult,
                                  op1=ALU.add), [ss], [ss])
    k.A(lambda e: e.sqrt(out=ss[:, 0:1], in_=ss[:, 0:1]), [ss], [ss])
    k.V(lambda e: e.reciprocal(out=rstd[:, 0:1], in_=ss[:, 0:1]), [ss], [rstd])


def phase_proj(k, A, P, l, src_lat, src_ctx):
    with k.phase():
        win = k.sb("win", [128, 8, 2304], BF16)
        wv = A["w_in"][l].rearrange("(kc p) f -> p kc f", p=128)
        for j in range(0, 2304, 384):
            k.dma("pool", lambda e, j=j: e.dma_start(out=win[:, :, j:j + 384], in_=wv[:, :, j:j + 384]),
                  writes=[("win", j)])
        winkeys = [("win", j) for j in range(0, 2304, 384)]
        NB = 8
        xts = [k.sb("xt%d" % i, [128, D], F32) for i in range(NB)]
        xn = [k.sb("xn%d" % i, [128, D], BF16) for i in range(NB)]
        junk = k.sb("junk", [128, D], F32)
        ss = [k.sb("ss%d" % i, [128, 1], F32) for i in range(NB)]
        rstd = [k.sb("rstd%d" % i, [128, 1], F32) for i in range(NB)]
        hT = [k.sb("hT%d" % i, [128, 8, 512], BF16) for i in range(2)]
        ptr = [k.ps("ptr%d" % i, [128, D], BF16) for i in range(2)]
        pu = [k.ps("pu%d" % i, [128, 512], F32) for i in range(3)]
        pc = [k.ps("pc%d" % i, [128, 512], F32) for i in range(2)]
        usb = [k.sb("usb%d" % i, [128, UC], F32) for i in range(2)]
        cvs = [k.sb("cvs%d" % i, [128, 4, 512], F32) for i in range(2)]
        identb = P["identb"]
        gs = P["gsA"][l]
        mT = P["modT"][l]
        groups = []
        for s in range(NS):
            groups.append((s, 0, 2, src_ctx[s * C:(s + 1) * C], NS))
            for g in range(4):
                groups.append((s, C + g * 512, 4, src_lat[s * S + g * 512: s * S + (g + 1) * 512], s))
        cnt = {"t": 0, "e": 0}

        def stats(gi):
            s, tok0, ntl, src, r = groups[gi]
            for t in range(ntl):
                i = (gi % 2) * 4 + t
                xt, xnn, sst, rs = xts[i], xn[i], ss[i], rstd[i]
                k.dma("sp", lambda e, xt=xt, src=src, t=t: e.dma_start(out=xt[:], in_=src[t * 128:(t + 1) * 128, :]),
                      writes=[xt])
                k.A(lambda e, xt=xt, sst=sst: e.activation(out=junk[:], in_=xt[:], func=AF.Square, accum_out=sst[:, 0:1]),
                    [xt], [junk, sst])
                k.A(lambda e, sst=sst: e.activation(out=sst[:, 0:1], in_=sst[:, 0:1], func=AF.Ln, scale=1.0 / D, bias=EPSB[:, 0:1]),
                    [sst, EPSB], [sst])
                k.A(lambda e, sst=sst, rs=rs: e.activation(out=rs[:, 0:1], in_=sst[:, 0:1], func=AF.Exp, scale=-0.5), [sst], [rs])
                k.V(lambda e, xnn=xnn, xt=xt, rs=rs: e.tensor_scalar(out=xnn[:], in0=xt[:], scalar1=rs[:, 0:1],
                                                                   scalar2=None, op0=ALU.mult), [xt, rs], [xnn])

        def trans_tile(gi, t):
            s, tok0, ntl, src, r = groups[gi]
            h = hT[gi % 2]
            if True:
                i = (gi % 2) * 4 + t
                xnn = xn[i]
                pt = ptr[cnt["t"] % 2]
                cnt["t"] += 1
                for kc in range(8):
                    k.T(lambda e, pt=pt, xnn=xnn, kc=kc: e.transpose(pt[:, kc * 128:(kc + 1) * 128],
                                                                   xnn[:, kc * 128:(kc + 1) * 128], identb[:]),
                        [xnn, identb], [pt])
                for kc in range(8):
                    if kc % 2 == 0:
                        k.A(lambda e, h=h, pt=pt, kc=kc, t=t, r=r: e.activation(
                            out=h[:, kc, t * 128:(t + 1) * 128], in_=pt[:, kc * 128:(kc + 1) * 128],
                            func=AF.Identity, scale=gs[:, kc, r:r + 1], bias=mT[:, kc, r:r + 1]),
                            [pt, gs, mT], [h])
                    else:
                        k.V(lambda e, h=h, pt=pt, kc=kc, t=t, r=r: e.tensor_scalar(
                            out=h[:, kc, t * 128:(t + 1) * 128], in0=pt[:, kc * 128:(kc + 1) * 128],
                            scalar1=gs[:, kc, r:r + 1], scalar2=mT[:, kc, r:r + 1], op0=ALU.mult, op1=ALU.add),
                            [pt, gs, mT], [h])

        def mm_tile(gi, t):
            s, tok0, ntl, src, r = groups[gi]
            h = hT[gi % 2]
            ei = cnt["e"]
            if True:
                ub = usb[t % 2]
                for ci, (c0, w) in enumerate([(0, 512), (512, 512), (1024, 512), (1536, 256)]):
                    p = pu[ei % 3]
                    for kc in range(8):
                        k.T(lambda e, p=p, h=h, kc=kc, t=t, c0=c0, w=w: e.matmul(
                            p[:, 0:w], lhsT=h[:, kc, t * 128:(t + 1) * 128], rhs=win[:, kc, c0:c0 + w],
                            start=(kc == 0), stop=(kc == 7)), [h] + winkeys, [p])
                    if ei % 2 == 0:
                        k.A(lambda e, ub=ub, p=p, c0=c0, w=w: e.copy(out=ub[:, c0:c0 + w], in_=p[:, 0:w]), [p], [ub])
                    else:
                        k.V(lambda e, ub=ub, p=p, c0=c0, w=w: e.tensor_copy(out=ub[:, c0:c0 + w], in_=p[:, 0:w]),
                            [p], [ub])
                    ei += 1
                k.dma("sp", lambda e, ub=ub, s=s, tok0=tok0, t=t: e.dma_start(
                    out=A["u_d"][s, tok0 + t * 128: tok0 + (t + 1) * 128, :], in_=ub[:]), reads=[ub], writes=["u_d"])
            cnt["e"] = ei

        def mm_conv(gi):
            s, tok0, ntl, src, r = groups[gi]
            h = hT[gi % 2]
            ntok = ntl * 128
            cv = cvs[gi % 2]
            for j in range(4):
                p = pc[j % 2]
                for kc in range(8):
                    k.T(lambda e, p=p, h=h, kc=kc, j=j, ntok=ntok: e.matmul(
                        p[:, 0:ntok], lhsT=win[:, kc, UC + j * 128: UC + (j + 1) * 128], rhs=h[:, kc, 0:ntok],
                        start=(kc == 0), stop=(kc == 7)), [h] + winkeys, [p])
                if j % 2 == 0:
                    k.A(lambda e, cv=cv, p=p, j=j, ntok=ntok: e.copy(out=cv[:, j, 0:ntok], in_=p[:, 0:ntok]), [p], [cv])
                else:
                    k.V(lambda e, cv=cv, p=p, j=j, ntok=ntok: e.tensor_copy(out=cv[:, j, 0:ntok], in_=p[:, 0:ntok]),
                        [p], [cv])
            k.dma("act", lambda e, cv=cv, s=s, tok0=tok0, ntok=ntok: e.dma_start(
                out=A["cv_d"][s, :, tok0:tok0 + ntok].rearrange("(j p) t -> p j t", p=128), in_=cv[:, :, 0:ntok]),
                reads=[cv], writes=["cv_d"])

        EPSB = P["epsb"]
        ng = len(groups)
        stats(0)
        for t in range(groups[0][2]):
            trans_tile(0, t)
        for gi in range(ng):
            if gi + 1 < ng:
                stats(gi + 1)
            n_cur = groups[gi][2]
            n_nxt = groups[gi + 1][2] if gi + 1 < ng else 0
            for t in range(max(n_cur, n_nxt)):
                if t < n_cur:
                    mm_tile(gi, t)
                if t < n_nxt:
                    trans_tile(gi + 1, t)
            mm_conv(gi)


def run_pipe(n, stages):
    ns = len(stages)
    for step in range(n + ns - 1):
        for j in range(ns - 1, -1, -1):
            i = step - j
            if 0 <= i < n:
                stages[j](i)


def phase_att(k, A, P, l, last):
    with k.phase():
        identb = P["identb"]
        EPSB = P["epsb"]
        attC = k.sb("attC", [128, 16, 64], F32)
        attS = k.sb("attS", [128, 16, 64], F32)
        k.dma("sp", lambda e: e.dma_start(out=attC[:], in_=A["attC"].rearrange("(t p) d -> p t d", p=128)), writes=[attC])
        k.dma("sp", lambda e: e.dma_start(out=attS[:], in_=A["attS"].rearrange("(t p) d -> p t d", p=128)), writes=[attS])
        gqk = k.sb("gqk", [128, 10, 64], F32)
        k.dma("sp", lambda e: e.dma_start(out=gqk[:, 0, :], in_=A["q_norm_g"][l, :].partition_broadcast(128)), writes=[gqk])
        k.dma("sp", lambda e: e.dma_start(out=gqk[:, 8, :], in_=A["k_norm_g"][l, :].partition_broadcast(128)), writes=[gqk])
        k.V(lambda e: e.tensor_scalar(out=gqk[:, 0, :], in0=gqk[:, 0, :], scalar1=0.125, scalar2=None, op0=ALU.mult),
            [gqk], [gqk])
        for h in range(1, 8):
            k.V(lambda e, h=h: e.tensor_copy(out=gqk[:, h, :], in_=gqk[:, 0, :]), [gqk], [gqk])
        k.V(lambda e: e.tensor_copy(out=gqk[:, 9, :], in_=gqk[:, 8, :]), [gqk], [gqk])
        sink = k.sb("sink", [128, 8], F32)
        k.dma("sp", lambda e: e.dma_start(out=sink[:], in_=A["att_sink"][l, :].partition_broadcast(128)), writes=[sink])
        k.A(lambda e: e.activation(out=sink[:], in_=sink[:], func=AF.Exp), [sink], [sink])
        maskb = k.sb("maskb", [128, 3, 128], BF16)
        k.V(lambda e: e.tensor_copy(out=maskb[:], in_=P["masks"][:]), [P["masks"]], [maskb])

        qT = k.sb("qT", [64, 8, NT * 128], BF16)
        kT = k.sb("kT", [64, 2, NT * 128], BF16)
        vx = k.sb("vx", [128, NT, 2, 65], BF16)
        k.V(lambda e: e.memset(vx[:], 1.0), [], [vx])
        NU, NQ = 7, 5
        uq = [k.sb("uq%d" % i, [128, 768], F32) for i in range(NU)]
        sq = [k.sb("sq%d" % i, [128, 640], F32) for i in range(2)]
        ssq = [k.sb("ssq%d" % i, [128, 10], F32) for i in range(4)]
        qn = [k.sb("qn%d" % i, [128, 640], F32) for i in range(NQ)]
        t1 = [k.sb("t1%d" % i, [128, 640], F32) for i in range(2)]
        t2 = [k.sb("t2%d" % i, [128, 640], F32) for i in range(2)]
        qb = [k.sb("qb%d" % i, [128, 640], BF16) for i in range(3)]
        pq = [k.ps("pq%d" % i, [64, 8, 128], BF16) for i in range(2)]
        pkk = k.ps("pkk", [64, 2, 128], BF16)
        psc = [k.ps("psc%d" % i, [128, 512], F32) for i in range(3)]
        po = [k.ps("po%d" % i, [128, 4, 65], F32) for i in range(2)]
        Pm = [k.sb("Pm%d" % i, [128, 5, 512], BF16) for i in range(3)]
        den = [k.sb("den%d" % i, [128, 4], F32) for i in range(2)]
        ao = [k.sb("ao%d" % i, [128, 512], BF16) for i in range(2)]
        cnt = {"sc": 0}

        for s in range(NS):
            def p0(i, s=s):
                u = uq[i % NU]
                k.dma("sp", lambda e: e.dma_start(out=u[:], in_=A["u_d"][s, i * 128:(i + 1) * 128, 0:768]),
                      reads=["u_d"], writes=[u])

            def p1(i):
                u, q2 = uq[i % NU], sq[i % 2]
                k.G(lambda e: e.tensor_tensor(out=q2[:], in0=u[:, 0:640], in1=u[:, 0:640], op=ALU.mult), [u], [q2])

            def p2(i):
                q2, sv = sq[i % 2], ssq[i % 4]
                k.V(lambda e: e.tensor_reduce(out=sv[:], in_=q2[:].rearrange("p (h d) -> p h d", d=64), axis=AX.X,
                                              op=ALU.add), [q2], [sv])

            def p3(i):
                sv = ssq[i % 4]
                k.A(lambda e: e.activation(out=sv[:], in_=sv[:], func=AF.Ln, scale=1.0 / 64, bias=EPSB[:, 0:1]), [sv, EPSB], [sv])
                k.A(lambda e: e.activation(out=sv[:], in_=sv[:], func=AF.Exp, scale=-0.5), [sv], [sv])

            def p4(i):
                u, sv, q = uq[i % NU], ssq[i % 4], qn[i % NQ]
                k.V(lambda e: e.tensor_tensor(out=q[:].rearrange("p (h d) -> p h d", d=64),
                                              in0=u[:, 0:640].rearrange("p (h d) -> p h d", d=64),
                                              in1=sv[:].unsqueeze(2).to_broadcast([128, 10, 64]), op=ALU.mult), [u, sv], [q])

            def p5(i):
                u, q = uq[i % NU], qn[i % NQ]
                k.G(lambda e: e.tensor_tensor(out=q[:], in0=q[:], in1=gqk[:].rearrange("p h d -> p (h d)"), op=ALU.mult),
                    [q, gqk], [q])
                k.G(lambda e: e.tensor_copy(out=vx[:, i, :, 0:64], in_=u[:, 640:768].rearrange("p (h d) -> p h d", d=64)),
                    [u], [vx])

            def p6(i):
                if i < 2:
                    return
                q, a1, a2 = qn[i % NQ], t1[i % 2], t2[i % 2]
                tl = i - 2
                qv = q[:].rearrange("p (h a x d) -> p h a x d", h=10, a=2, x=2, d=16)
                t1v = a1[:].rearrange("p (h a x d) -> p h a x d", h=10, a=2, x=2, d=16)
                t2v = a2[:].rearrange("p (h a x d) -> p h a x d", h=10, a=2, x=2, d=16)
                Cv = attC[:, tl, :].rearrange("p (a x d) -> p a x d", a=2, x=2, d=16)
                Sv = attS[:, tl, :].rearrange("p (a x d) -> p a x d", a=2, x=2, d=16)
                k.V(lambda e: e.tensor_tensor(
                    out=a1[:].rearrange("p (h d) -> p h d", d=64), in0=q[:].rearrange("p (h d) -> p h d", d=64),
                    in1=attC[:, tl, :].unsqueeze(1).to_broadcast([128, 10, 64]), op=ALU.mult), [q, attC], [a1])
                qv = q[:].rearrange("p (h a x d) -> p h a x d", h=10, a=2, x=2, d=16)
                t2v = a2[:].rearrange("p (h a x d) -> p h a x d", h=10, a=2, x=2, d=16)
                Sv = attS[:, tl, :].rearrange("p (a x d) -> p a x d", a=2, x=2, d=16)
                for a in range(2):
                    for x in range(2):
                        k.G(lambda e, a=a, x=x: e.tensor_tensor(
                            out=t2v[:, :, a, x, :], in0=qv[:, :, a, 1 - x, :],
                            in1=Sv[:, a, x, :].unsqueeze(1).to_broadcast([128, 10, 16]), op=ALU.mult), [q, attS], [a2])

            def p7(i):
                q, a1, a2, q_b = qn[i % NQ], t1[i % 2], t2[i % 2], qb[i % 3]
                if i >= 2:
                    k.V(lambda e: e.tensor_tensor(out=q_b[:], in0=a1[:], in1=a2[:], op=ALU.add), [a1, a2], [q_b])
                else:
                    k.V(lambda e: e.tensor_copy(out=q_b[:], in_=q[:]), [q], [q_b])

            def p8(i):
                q_b, p = qb[i % 3], pq[i % 2]
                for h in range(8):
                    k.T(lambda e, h=h: e.transpose(p[:, h, :], q_b[:, h * 64:(h + 1) * 64], identb[:]), [q_b, identb], [p])
                for h in range(2):
                    k.T(lambda e, h=h: e.transpose(pkk[:, h, :], q_b[:, (8 + h) * 64:(9 + h) * 64], identb[:]),
                        [q_b, identb], [pkk])

            def p9(i):
                p = pq[i % 2]
                k.A(lambda e: e.copy(out=qT[:, :, i * 128:(i + 1) * 128], in_=p[:]), [p], [("qT", i)])
                k.V(lambda e: e.tensor_copy(out=kT[:, :, i * 128:(i + 1) * 128], in_=pkk[:]), [pkk], [("kT", i)])

            run_pipe(NT, [p0, p1, p2, p3, p4, p5, p6, p7, p8, p9])

            qblocks = list(range(2, NT)) + ([] if last else [0, 1])
            items = [(t, kh) for t in qblocks for kh in range(2)]

            def kblocks(t):
                if t >= 2:
                    kbl = []
                    if t - 1 >= 2:
                        kbl.append((t - 1, 1))
                    kbl.append((t, None))
                    if t + 1 < NT:
                        kbl.append((t + 1, 0))
                    return kbl + [(0, None), (1, None)]
                return [(0, None), (1, None)]

            def b0(i):
                t, kh = items[i]
                Pt = Pm[i % 3]
                for bj, (kb_, mk) in enumerate(kblocks(t)):
                    ps = psc[cnt["sc"] % 3]
                    cnt["sc"] += 1
                    k.T(lambda e, ps=ps, kb_=kb_: e.matmul(
                        ps[:], lhsT=kT[:, kh, kb_ * 128:(kb_ + 1) * 128],
                        rhs=qT[:, kh * 4:(kh + 1) * 4, t * 128:(t + 1) * 128], start=True, stop=True),
                        [("kT", kb_), ("qT", t)], [ps])
                    k.A(lambda e, ps=ps, bj=bj: e.activation(out=Pt[:, bj, :], in_=ps[:], func=AF.Exp), [ps], [(Pt.name, bj)])
                    if mk is not None:
                        k.G(lambda e, bj=bj, mk=mk: e.tensor_tensor(
                            out=Pt[:, bj, :].rearrange("p (g q) -> p g q", g=4),
                            in0=Pt[:, bj, :].rearrange("p (g q) -> p g q", g=4),
                            in1=maskb[:, mk, :].unsqueeze(1).to_broadcast([128, 4, 128]), op=ALU.mult),
                            [(Pt.name, bj), maskb], [(Pt.name, bj)])

            def b1(i):
                t, kh = items[i]
                Pt = Pm[i % 3]
                pso = po[i % 2]
                kbl = kblocks(t)
                nb = len(kbl)
                for g in range(4):
                    for bj, (kb_, mk) in enumerate(kbl):
                        k.T(lambda e, g=g, bj=bj, kb_=kb_: e.matmul(
                            pso[:, g, :], lhsT=Pt[:, bj, g * 128:(g + 1) * 128], rhs=vx[:, kb_, kh, :],
                            start=(bj == 0), stop=(bj == nb - 1)), [(Pt.name, bj), vx], [pso])

            def b2(i, s=s):
                t, kh = items[i]
                pso = po[i % 2]
                dn = den[i % 2]
                a_o = ao[(i // 2) % 2]
                k.V(lambda e: e.tensor_tensor(out=dn[:], in0=pso[:, :, 64], in1=sink[:, kh * 4:(kh + 1) * 4], op=ALU.add),
                    [pso, sink], [dn])
                k.V(lambda e: e.reciprocal(out=dn[:], in_=dn[:]), [dn], [dn])
                k.V(lambda e: e.tensor_tensor(
                    out=a_o[:, kh * 256:(kh + 1) * 256].rearrange("p (g d) -> p g d", g=4),
                    in0=pso[:, :, 0:64], in1=dn[:].unsqueeze(2).to_broadcast([128, 4, 64]), op=ALU.mult), [pso, dn], [a_o])
                if kh == 1:
                    k.dma("act", lambda e: e.dma_start(out=A["mixin_d"][s, t * 128:(t + 1) * 128, 0:512], in_=a_o[:]),
                          reads=[a_o], writes=["mixin_d"])

            run_pipe(len(items), [b0, b1, b2])


def phase_ret(k, A, P, l, last):
    with k.phase():
        identb = P["identb"]
        retC = k.sb("retC", [128, NT, 64], F32)
        retS = k.sb("retS", [128, NT, 64], F32)
        k.dma("sp", lambda e: e.dma_start(out=retC[:], in_=A["retC"].rearrange("(t p) d -> p t d", p=128)), writes=[retC])
        k.dma("sp", lambda e: e.dma_start(out=retS[:], in_=A["retS"].rearrange("(t p) d -> p t d", p=128)), writes=[retS])
        lg = k.sb("lg", [128, 8], F32)
        k.dma("sp", lambda e: e.dma_start(out=lg[:], in_=A["ret_decay_logit"][l, :].partition_broadcast(128)), writes=[lg])
        k.A(lambda e: e.activation(out=lg[:], in_=lg[:], func=AF.Exp, scale=-1.0), [lg], [lg])
        k.A(lambda e: e.activation(out=lg[:], in_=lg[:], func=AF.Ln, bias=1.0), [lg], [lg])
        k.V(lambda e: e.tensor_scalar(out=lg[:], in0=lg[:], scalar1=-1.0, scalar2=None, op0=ALU.mult), [lg], [lg])
        dco = k.sb("dco", [128, 4], F32)
        k.dma("sp", lambda e: e.dma_start(out=dco[:], in_=A["dcoef"]), writes=[dco])
        DT = k.sb("DT", [128, 2, 2, 4], F32)
        for d_ in range(2):
            for qk in range(2):
                k.A(lambda e, d_=d_, qk=qk: e.activation(out=DT[:, d_, qk, :], in_=lg[:, d_ * 4:(d_ + 1) * 4], func=AF.Exp,
                                                         scale=dco[:, d_ * 2 + qk: d_ * 2 + qk + 1]), [lg, dco], [DT])
            k.V(lambda e, d_=d_: e.tensor_scalar(out=DT[:, d_, 1, :], in0=DT[:, d_, 1, :], scalar1=0.125, scalar2=None,
                                                 op0=ALU.mult), [DT], [DT])
        g128 = k.sb("g128", [128, 2, 2], F32)
        for d_ in range(2):
            for pr in range(2):
                for hh in range(2):
                    h = 2 * pr + hh
                    k.A(lambda e, d_=d_, pr=pr, hh=hh, h=h: e.activation(
                        out=g128[hh * 64:(hh + 1) * 64, d_, pr:pr + 1], in_=lg[hh * 64:(hh + 1) * 64, d_ * 4 + h: d_ * 4 + h + 1],
                        func=AF.Exp, scale=128.0), [lg], [g128])
        gn = k.sb("gn", [128, 256], F32)
        k.dma("sp", lambda e: e.dma_start(out=gn[:], in_=A["ret_norm_g"][l, :].partition_broadcast(128)), writes=[gn])
        maskb = k.sb("maskb", [128, 3, 128], BF16)
        k.V(lambda e: e.tensor_copy(out=maskb[:], in_=P["masks"][:]), [P["masks"]], [maskb])

        EPSB = P["epsb"]
        RT = k.sb("RT", [128, 2, 2, 2, NT * 128], BF16)
        KD = k.sb("KD", [128, NT, 2, 256], BF16)
        Vb = k.sb("Vb", [128, NT, 256], BF16)
        SG = k.sb("SG", [128, NT, 256], BF16)
        OA = k.sb("OA", [128, NT, 256], F32)
        ur = [k.sb("ur%d" % i, [128, 1024], F32) for i in range(3)]
        t1 = [k.sb("t1%d" % i, [128, 512], F32) for i in range(2)]
        t2 = [k.sb("t2%d" % i, [128, 512], F32) for i in range(2)]
        rot = [k.sb("rot%d" % i, [128, 512], F32) for i in range(3)]
        vr = [k.sb("vr%d" % i, [128, 2, 2, 256], BF16) for i in range(4)]
        ptr = [k.ps("ptr%d" % i, [128, 8, 128], BF16) for i in range(1)]
        pssA = [k.ps("pssA%d" % i, [128, 512], F32) for i in range(2)]
        pssB = [k.ps("pssB%d" % i, [128, 512], F32) for i in range(2)]
        pso = [k.ps("pso%d" % i, [128, 4, 64], F32) for i in range(2)]
        pkvt = k.ps("pkvt", [128, 2, 2, 128], F32)
        STm = [k.sb("STm%d" % i, [128, 4, 128], BF16) for i in range(2)]
        Sst = [k.sb("Sst%d" % i, [128, 2, 128], F32) for i in range(2)]
        Sb = [k.sb("Sb%d" % i, [128, 2, 128], BF16) for i in range(2)]
        tmp = [k.sb("tmp%d" % i, [128, 2, 128], F32) for i in range(2)]
        osum = [k.sb("osum%d" % i, [128, 256], F32) for i in range(3)]
        mean = [k.sb("mean%d" % i, [128, 4], F32) for i in range(3)]
        cen = [k.sb("cen%d" % i, [128, 256], F32) for i in range(4)]
        sq = [k.sb("sq%d" % i, [128, 256], F32) for i in range(2)]
        var = [k.sb("var%d" % i, [128, 4], F32) for i in range(3)]
        rof = [k.sb("rof%d" % i, [128, 256], F32) for i in range(3)]
        ro = [k.sb("ro%d" % i, [128, 256], BF16) for i in range(2)]

        for s in range(NS):
            def r0(i, s=s):
                u = ur[i % 3]
                k.dma("sp", lambda e: e.dma_start(out=u[:], in_=A["u_d"][s, i * 128:(i + 1) * 128, 768:1792]),
                      reads=["u_d"], writes=[u])

            def r1(i):
                u, a1, a2 = ur[i % 3], t1[i % 2], t2[i % 2]
                qk_v = u[:, 0:512].rearrange("p (h x d) -> p h x d", h=8, x=2, d=32)
                t1v = a1[:].rearrange("p (h x d) -> p h x d", h=8, x=2, d=32)
                t2v = a2[:].rearrange("p (h x d) -> p h x d", h=8, x=2, d=32)
                Cv = retC[:, i, :].rearrange("p (x d) -> p x d", x=2, d=32)
                Sv = retS[:, i, :].rearrange("p (x d) -> p x d", x=2, d=32)
                for x in range(2):
                    k.V(lambda e, x=x: e.tensor_tensor(
                        out=t1v[:, :, x, :], in0=qk_v[:, :, x, :], in1=Cv[:, x, :].unsqueeze(1).to_broadcast([128, 8, 32]),
                        op=ALU.mult), [u, retC], [a1])
                    k.G(lambda e, x=x: e.tensor_tensor(
                        out=t2v[:, :, x, :], in0=qk_v[:, :, 1 - x, :], in1=Sv[:, x, :].unsqueeze(1).to_broadcast([128, 8, 32]),
                        op=ALU.mult), [u, retS], [a2])
                k.V(lambda e: e.tensor_copy(out=Vb[:, i, :], in_=u[:, 512:768]), [u], [("Vb", i)])
                k.A(lambda e: e.activation(out=SG[:, i, :], in_=u[:, 768:1024], func=AF.Silu), [u], [("SG", i)])

            def r2(i):
                a1, a2, rt_ = t1[i % 2], t2[i % 2], rot[i % 3]
                k.V(lambda e: e.tensor_tensor(out=rt_[:], in0=a1[:], in1=a2[:], op=ALU.add), [a1, a2], [rt_])

            def r3(i):
                rt_, v_r = rot[i % 3], vr[i % 4]
                for d_ in range(2):
                    eng = k.V if d_ == 0 else k.G
                    eng(lambda e, d_=d_: e.tensor_tensor(
                        out=v_r[:, d_, :, :].rearrange("p q (h d) -> p (q h) d", d=64),
                        in0=rt_[:].rearrange("p (qh d) -> p qh d", d=64),
                        in1=DT[:, d_, :, :].rearrange("p q h -> p (q h)").unsqueeze(2).to_broadcast([128, 8, 64]),
                        op=ALU.mult), [rt_, DT], [(v_r.name, d_)])

            def r4(i):
                v_r, p = vr[i % 4], ptr[0]
                for d_ in range(2):
                    k.G(lambda e, d_=d_: e.tensor_copy(out=KD[:, i, d_, :], in_=v_r[:, d_, 1, :]), [(v_r.name, d_)], [("KD", i)])
                for d_ in range(2):
                    for qk in range(2):
                        for pr in range(2):
                            k.T(lambda e, d_=d_, qk=qk, pr=pr: e.transpose(
                                p[:, d_ * 4 + qk * 2 + pr, :], v_r[:, d_, qk, pr * 128:(pr + 1) * 128], identb[:]),
                                [(v_r.name, d_), identb], [p])

            def r5(i):
                p = ptr[0]
                k.A(lambda e: e.copy(out=RT[:, :, :, :, i * 128:(i + 1) * 128].rearrange("p a b c t -> p (a b c) t"),
                                     in_=p[:]), [p], [("RT", i)])

            run_pipe(NT, [r0, r1, r2, r3, r4, r5])

            orders = [list(range(NT)), [1, 0] + list(range(NT - 1, 1, -1))]
            for d_ in range(2):
                k.V(lambda e, d_=d_: e.memset(Sst[d_][:], 0.0), [], [Sst[d_]])
                k.V(lambda e, d_=d_: e.memset(Sb[d_][:], 0.0), [], [Sb[d_]])
            oak = [("OA", t_) for t_ in range(NT)]
            k.G(lambda e: e.memset(OA[:], 0.0), oak, oak)
            for ci in range(NT):
                tt = [orders[0][ci], orders[1][ci]]
                for d_ in range(2):
                    t = tt[d_]
                    for h in (0, 2, 1, 3):
                        pr, hh = h // 2, h % 2
                        pss = pssA[d_] if hh == 0 else pssB[d_]
                        k.T(lambda e, pss=pss, pr=pr, hh=hh, t=t, d_=d_: e.matmul(
                            pss[:, pr * 128:(pr + 1) * 128], lhsT=RT[hh * 64:(hh + 1) * 64, d_, 1, pr, t * 128:(t + 1) * 128],
                            rhs=RT[hh * 64:(hh + 1) * 64, d_, 0, pr, t * 128:(t + 1) * 128], start=True, stop=True),
                            [("RT", t)], [pss])
                for d_ in range(2):
                    mk = 0 if d_ == 0 else 2
                    stm = STm[d_]
                    sv = stm[:].rearrange("p (pr hh) j -> p pr hh j", hh=2)
                    for hh, pss in ((0, pssA[d_]), (1, pssB[d_])):
                        k.V(lambda e, hh=hh, pss=pss, mk=mk, sv=sv: e.tensor_tensor(
                            out=sv[:, :, hh, :], in0=pss[:, 0:256].rearrange("p (pr j) -> p pr j", pr=2),
                            in1=maskb[:, mk, :].unsqueeze(1).to_broadcast([128, 2, 128]), op=ALU.mult),
                            [pss, maskb], [(stm.name, hh)])
                for d_ in range(2):
                    t = tt[d_]
                    stm, po_, Sbb = STm[d_], pso[d_], Sb[d_]
                    for h in range(4):
                        pr, hh = h // 2, h % 2
                        k.T(lambda e, h=h, t=t, stm=stm, po_=po_: e.matmul(
                            po_[:, h, :], lhsT=stm[:, h, :], rhs=Vb[:, t, h * 64:(h + 1) * 64], start=True, stop=False),
                            [(stm.name, hh), ("Vb", t)], [po_])
                        k.T(lambda e, h=h, pr=pr, hh=hh, t=t, d_=d_, Sbb=Sbb, po_=po_: e.matmul(
                            po_[:, h, :], lhsT=RT[hh * 64:(hh + 1) * 64, d_, 0, pr, t * 128:(t + 1) * 128],
                            rhs=Sbb[hh * 64:(hh + 1) * 64, pr, hh * 64:(hh + 1) * 64], start=False, stop=True),
                            [("RT", t), Sbb], [po_])
                    if ci < NT - 1:
                        for pr in range(2):
                            k.T(lambda e, pr=pr, t=t, d_=d_: e.matmul(
                                pkvt[:, d_, pr, :], lhsT=KD[:, t, d_, pr * 128:(pr + 1) * 128], rhs=Vb[:, t, pr * 128:(pr + 1) * 128],
                                start=True, stop=True), [("KD", t), ("Vb", t)], [("pkv", d_)])
                for d_ in range(2):
                    t = tt[d_]
                    po_, St, Sbb, tm = pso[d_], Sst[d_], Sb[d_], tmp[d_]
                    k.V(lambda e, po_=po_, t=t: e.tensor_tensor(out=OA[:, t, :], in0=OA[:, t, :],
                                                                in1=po_[:].rearrange("p h d -> p (h d)"), op=ALU.add),
                        [po_, ("OA", t)], [("OA", t)])
                    if ci < NT - 1:
                        k.V(lambda e, St=St, tm=tm, d_=d_: e.tensor_tensor(out=tm[:], in0=pkvt[:, d_, :, :], in1=St[:], op=ALU.add),
                            [("pkv", d_), St], [tm])
                        for pr in range(2):
                            k.V(lambda e, pr=pr, St=St, d_=d_, tm=tm: e.tensor_scalar(
                                out=St[:, pr, :], in0=tm[:, pr, :], scalar1=g128[:, d_, pr:pr + 1], scalar2=None,
                                op0=ALU.mult), [tm, g128], [St])
                            k.A(lambda e, pr=pr, Sbb=Sbb, d_=d_, tm=tm: e.activation(
                                out=Sbb[:, pr, :], in_=tm[:, pr, :], func=AF.Copy, scale=g128[:, d_, pr:pr + 1]),
                                [tm, g128], [Sbb])

            tiles = list(range(2, NT)) + ([] if last else [0, 1])

            def g0(i):
                t = tiles[i]
                os_, mn = osum[i % 3], mean[i % 3]
                k.G(lambda e: e.tensor_copy(out=os_[:], in_=OA[:, t, :]), [("OA", t)], [os_])
                k.V(lambda e: e.tensor_reduce(out=mn[:], in_=os_[:].rearrange("p (h d) -> p h d", d=64), axis=AX.X,
                                              op=ALU.add), [os_], [mn])
                k.V(lambda e: e.tensor_scalar(out=mn[:], in0=mn[:], scalar1=1.0 / 64, scalar2=None, op0=ALU.mult), [mn], [mn])

            def g1(i):
                os_, mn, cn, s2 = osum[i % 3], mean[i % 3], cen[i % 4], sq[i % 2]
                k.V(lambda e: e.tensor_tensor(out=cn[:].rearrange("p (h d) -> p h d", d=64),
                                              in0=os_[:].rearrange("p (h d) -> p h d", d=64),
                                              in1=mn[:].unsqueeze(2).to_broadcast([128, 4, 64]), op=ALU.subtract),
                    [os_, mn], [cn])
                k.G(lambda e: e.tensor_tensor(out=s2[:], in0=cn[:], in1=cn[:], op=ALU.mult), [cn], [s2])

            def g2(i):
                s2, vv = sq[i % 2], var[i % 3]
                k.V(lambda e: e.tensor_reduce(out=vv[:], in_=s2[:].rearrange("p (h d) -> p h d", d=64), axis=AX.X,
                                              op=ALU.add), [s2], [vv])
                k.A(lambda e: e.activation(out=vv[:], in_=vv[:], func=AF.Ln, scale=1.0 / 64, bias=EPSB[:, 0:1]), [vv, EPSB], [vv])
                k.A(lambda e: e.activation(out=vv[:], in_=vv[:], func=AF.Exp, scale=-0.5), [vv], [vv])

            def g3(i):
                cn, vv, rf = cen[i % 4], var[i % 3], rof[i % 3]
                k.V(lambda e: e.tensor_tensor(out=rf[:].rearrange("p (h d) -> p h d", d=64),
                                              in0=cn[:].rearrange("p (h d) -> p h d", d=64),
                                              in1=vv[:].unsqueeze(2).to_broadcast([128, 4, 64]), op=ALU.mult), [cn, vv], [rf])
                k.G(lambda e: e.tensor_tensor(out=rf[:], in0=rf[:], in1=gn[:], op=ALU.mult), [rf, gn], [rf])

            def g4(i, s=s):
                t = tiles[i]
                rf, r_o = rof[i % 3], ro[i % 2]
                k.V(lambda e: e.tensor_tensor(out=r_o[:], in0=rf[:], in1=SG[:, t, :], op=ALU.mult), [rf, ("SG", t)], [r_o])
                k.dma("act", lambda e: e.dma_start(out=A["mixin_d"][s, t * 128:(t + 1) * 128, 512:768], in_=r_o[:]),
                      reads=[r_o], writes=["mixin_d"])

            run_pipe(len(tiles), [g0, g1, g2, g3, g4])


def phase_conv(k, A, P, l, last):
    with k.phase():
        ident = P["ident"]
        EPSB = P["epsb"]
        cwT = k.sb("cwT", [128, 2, 32], F32)
        vT = k.sb("vT", [128, 2, 4], F32)
        with k.phase():
            cw = k.sb("cw", [31, 256], F32)
            k.dma("sp", lambda e: e.dma_start(out=cw[:], in_=A["conv_w"][l]), writes=[cw])
            pw = k.ps("pw", [128, 2, 32], F32)
            for cc in range(2):
                k.T(lambda e, cc=cc: e.transpose(pw[:, cc, 0:31], cw[0:31, cc * 128:(cc + 1) * 128], ident[0:31, 0:31]),
                    [cw, ident], [pw])
            k.V(lambda e: e.tensor_copy(out=cwT[:, :, 0:31], in_=pw[:, :, 0:31]), [pw], [cwT])
            vec = k.sb("vec", [3, 256], F32)
            k.dma("sp", lambda e: e.dma_start(out=vec[0:1, :], in_=A["conv_b"][l:l + 1, :]), writes=[vec])
            k.dma("sp", lambda e: e.dma_start(out=vec[1:2, :], in_=A["conv_norm_g"][l:l + 1, :]), writes=[vec])
            k.dma("sp", lambda e: e.dma_start(out=vec[2:3, :], in_=A["conv_norm_b"][l:l + 1, :]), writes=[vec])
            pv = k.ps("pv", [128, 2, 4], F32)
            for cc in range(2):
                k.T(lambda e, cc=cc: e.transpose(pv[:, cc, 0:3], vec[0:3, cc * 128:(cc + 1) * 128], ident[0:3, 0:3]),
                    [vec, ident], [pv])
            k.V(lambda e: e.tensor_copy(out=vT[:, :, 0:3], in_=pv[:, :, 0:3]), [pv], [vT])
        diag = k.sb("diag", [128, 2, 31, 128], BF16)
        for cc in range(2):
            for kk in range(31):
                eng = k.V if kk % 2 == 0 else k.G
                eng(lambda e, cc=cc, kk=kk: e.tensor_scalar(out=diag[:, cc, kk, :], in0=ident[:],
                                                            scalar1=cwT[:, cc, kk:kk + 1], scalar2=None, op0=ALU.mult),
                    [ident, cwT], [diag])
        ones = k.sb("ones", [128, 128], F32R)
        onesf = k.sb("onesf", [128, 128], F32)
        k.V(lambda e: e.memset(onesf[:], 1.0 / 256), [], [onesf])
        k.V(lambda e: e.tensor_copy(out=ones[:], in_=onesf[:]), [onesf], [ones])

        LP = S + 30
        hp = [[k.sb("hp%d_%d" % (j, cc), [128, LP], BF16) for cc in range(2)] for j in range(2)]
        val = [[k.sb("val%d_%d" % (j, cc), [128, S], F32) for cc in range(2)] for j in range(2)]
        gt = [[k.sb("gt%d_%d" % (j, cc), [128, S], F32) for cc in range(2)] for j in range(2)]
        cvo = [[k.sb("cvo%d_%d" % (j, cc), [128, 512], F32R) for cc in range(2)] for j in range(5)]
        csq = [[k.sb("csq%d_%d" % (j, cc), [128, 512], F32R) for cc in range(2)] for j in range(2)]
        pcv = [[k.ps("pcv%d_%d" % (j, cc), [128, 512], F32) for cc in range(2)] for j in range(2)]
        pmean = [k.ps("pmean%d" % j, [128, 512], F32) for j in range(2)]
        pex2 = [k.ps("pex2%d" % j, [128, 512], F32) for j in range(2)]
        msb = [k.sb("msb%d" % j, [128, 512], F32) for j in range(3)]
        rsd = [k.sb("rsd%d" % j, [128, 512], F32) for j in range(3)]
        yv = [[k.sb("yv%d_%d" % (j, cc), [128, 512], F32) for cc in range(2)] for j in range(2)]
        yo = [[k.sb("yo%d_%d" % (j, cc), [128, 512], BF16) for cc in range(2)] for j in range(2)]
        segs = []
        for s in range(NS):
            segs.append((s, C, S))
            if not last:
                segs.append((s, 0, C))
        items = []
        for si, (s, tok0, Lg) in enumerate(segs):
            nb = (Lg + 511) // 512
            for b in range(nb):
                items.append((si, s, tok0, Lg, b, min(512, Lg - b * 512)))

        def pre(i):
            si, s, tok0, Lg, b, w = items[i]
            if b != 0:
                return
            j = si % 2
            for cc in range(2):
                k.dma("sp", lambda e, cc=cc: e.dma_start(
                    out=val[j][cc][:, 0:Lg], in_=A["cv_d"][s, cc * 128:(cc + 1) * 128, tok0:tok0 + Lg]),
                    reads=["cv_d"], writes=[val[j][cc]])
                k.dma("act", lambda e, cc=cc: e.dma_start(
                    out=gt[j][cc][:, 0:Lg], in_=A["cv_d"][s, 256 + cc * 128: 256 + (cc + 1) * 128, tok0:tok0 + Lg]),
                    reads=["cv_d"], writes=[gt[j][cc]])
                k.A(lambda e, cc=cc: e.activation(out=gt[j][cc][:, 0:Lg], in_=gt[j][cc][:, 0:Lg], func=AF.Sigmoid),
                    [gt[j][cc]], [gt[j][cc]])
                k.G(lambda e, cc=cc: e.memset(hp[j][cc][:], 0.0), [], [hp[j][cc]])
                k.V(lambda e, cc=cc: e.tensor_tensor(out=hp[j][cc][:, 15:15 + Lg], in0=val[j][cc][:, 0:Lg],
                                                     in1=gt[j][cc][:, 0:Lg], op=ALU.mult),
                    [val[j][cc], gt[j][cc]], [hp[j][cc]])

        def nop(i):
            return

        def c0(i):
            si, s, tok0, Lg, b, w = items[i]
            j = si % 2
            for cc in range(2):
                p = pcv[i % 2][cc]
                for kk in range(31):
                    k.T(lambda e, p=p, cc=cc, kk=kk: e.matmul(
                        p[:, 0:w], lhsT=diag[:, cc, kk, :], rhs=hp[j][cc][:, b * 512 + kk: b * 512 + kk + w],
                        start=(kk == 0), stop=(kk == 30)), [diag, hp[j][cc]], [p])

        def c1(i):
            si, s, tok0, Lg, b, w = items[i]
            for cc in range(2):
                p, co, cq = pcv[i % 2][cc], cvo[i % 5][cc], csq[i % 2][cc]
                k.A(lambda e, p=p, co=co, cc=cc: e.activation(out=co[:, 0:w], in_=p[:, 0:w], func=AF.Identity,
                                                             bias=vT[:, cc, 0:1], scale=1.0), [p, vT], [co])
                k.V(lambda e, co=co, cq=cq: e.tensor_tensor(out=cq[:, 0:w], in0=co[:, 0:w].bitcast(F32),
                                                            in1=co[:, 0:w].bitcast(F32), op=ALU.mult), [co], [cq])

        def c2(i):
            si, s, tok0, Lg, b, w = items[i]
            pm_, px_ = pmean[i % 2], pex2[i % 2]
            for cc in range(2):
                k.T(lambda e, cc=cc: e.matmul(pm_[:, 0:w], lhsT=ones[:], rhs=cvo[i % 5][cc][:, 0:w],
                                              start=(cc == 0), stop=(cc == 1)), [ones, cvo[i % 5][cc]], [pm_])
            for cc in range(2):
                k.T(lambda e, cc=cc: e.matmul(px_[:, 0:w], lhsT=ones[:], rhs=csq[i % 2][cc][:, 0:w],
                                              start=(cc == 0), stop=(cc == 1)), [ones, csq[i % 2][cc]], [px_])

        def c3(i):
            si, s, tok0, Lg, b, w = items[i]
            pm_, px_, ms, rs = pmean[i % 2], pex2[i % 2], msb[i % 3], rsd[i % 3]
            k.A(lambda e: e.copy(out=ms[:, 0:w], in_=pm_[:, 0:w]), [pm_], [ms])
            k.V(lambda e: e.tensor_tensor(out=rs[:, 0:w], in0=ms[:, 0:w], in1=ms[:, 0:w], op=ALU.mult), [ms], [rs])
            k.V(lambda e: e.tensor_tensor(out=rs[:, 0:w], in0=px_[:, 0:w], in1=rs[:, 0:w], op=ALU.subtract), [px_, rs], [rs])

        def c4(i):
            si, s, tok0, Lg, b, w = items[i]
            rs = rsd[i % 3]
            k.A(lambda e: e.activation(out=rs[:, 0:w], in_=rs[:, 0:w], func=AF.Ln, bias=EPSB[:, 0:1], scale=1.0), [rs, EPSB], [rs])
            k.A(lambda e: e.activation(out=rs[:, 0:w], in_=rs[:, 0:w], func=AF.Exp, scale=-0.5), [rs], [rs])

        def c5(i):
            si, s, tok0, Lg, b, w = items[i]
            ms, rs = msb[i % 3], rsd[i % 3]
            for cc in range(2):
                co, y = cvo[i % 5][cc], yv[i % 2][cc]
                k.G(lambda e, co=co, y=y: e.tensor_tensor(out=y[:, 0:w], in0=co[:, 0:w].bitcast(F32), in1=ms[:, 0:w],
                                                          op=ALU.subtract), [co, ms], [y])
                k.V(lambda e, y=y: e.tensor_tensor(out=y[:, 0:w], in0=y[:, 0:w], in1=rs[:, 0:w], op=ALU.mult), [y, rs], [y])

        def c6(i):
            si, s, tok0, Lg, b, w = items[i]
            for cc in range(2):
                y, o_ = yv[i % 2][cc], yo[i % 2][cc]
                k.A(lambda e, y=y, o_=o_, cc=cc: e.activation(out=o_[:, 0:w], in_=y[:, 0:w], func=AF.Silu,
                                                             scale=vT[:, cc, 1:2], bias=vT[:, cc, 2:3]), [y, vT], [o_])
                k.dma("act", lambda e, cc=cc, o_=o_: e.dma_start(
                    out=A["convT_d"][s, cc * 128:(cc + 1) * 128, tok0 + b * 512: tok0 + b * 512 + w],
                    in_=o_[:, 0:w]), reads=[o_], writes=["convT_d"])

        run_pipe(len(items), [pre, nop, c0, c1, c2, c3, c4, c5, c6])


def phase_out(k, A, P, l, last, src_lat, src_ctx, dst_lat):
    with k.phase():
        ident = P["ident"]
        identb = P["identb"]
        EPSB = P["epsb"]
        wout = k.sb("wout", [128, 8, D], BF16)
        wv = A["w_out"][l].rearrange("(kc p) f -> p kc f", p=128)
        for j in range(0, D, 512):
            k.dma("pool", lambda e, j=j: e.dma_start(out=wout[:, :, j:j + 512], in_=wv[:, :, j:j + 512]), writes=[("wout", j)])
        woutk = [("wout", 0), ("wout", 512)]
        wr = k.sb("wr", [128, 8, E], F32)
        k.dma("sp", lambda e: e.dma_start(out=wr[:], in_=A["w_router"][l].rearrange("(kc p) e -> p kc e", p=128)),
              writes=[wr])
        nr = NS + (0 if last else 1)
        gta = [k.sb("gta%d" % r, [128, D], F32) for r in range(nr)]
        gsf = [k.sb("gsf%d" % r, [128, D], F32) for r in range(nr)]
        shf = [k.sb("shf%d" % r, [128, D], F32) for r in range(nr)]
        gf = k.sb("gf", [128, D], F32)
        k.dma("sp", lambda e: e.dma_start(out=gf[:], in_=A["g_ffn"][l, :].partition_broadcast(128)), writes=[gf])
        for r in range(nr):
            k.dma("sp", lambda e, r=r: e.dma_start(out=gta[r][:], in_=A["mod_d"][l, r, 2 * D:3 * D].partition_broadcast(128)),
                  reads=["mod_d"], writes=[gta[r]])
            k.dma("sp", lambda e, r=r: e.dma_start(out=shf[r][:], in_=A["mod_d"][l, r, 3 * D:4 * D].partition_broadcast(128)),
                  reads=["mod_d"], writes=[shf[r]])
            k.dma("sp", lambda e, r=r: e.dma_start(out=gsf[r][:], in_=A["mod_d"][l, r, 4 * D:5 * D].partition_broadcast(128)),
                  reads=["mod_d"], writes=[gsf[r]])
            k.V(lambda e, r=r: e.scalar_tensor_tensor(out=gsf[r][:], in0=gsf[r][:], scalar=1.0, in1=gf[:], op0=ALU.add,
                                                      op1=ALU.mult), [gsf[r], gf], [gsf[r]])
        NB = 6
        mi = [k.sb("mi%d" % i, [128, 768], BF16) for i in range(3)]
        mixT = [k.sb("mixT%d" % i, [128, 8, 128], BF16) for i in range(4)]
        xt = [k.sb("xt%d" % i, [128, D], F32) for i in range(5)]
        xm = [k.sb("xm%d" % i, [128, D], F32) for i in range(5)]
        hf = [k.sb("hf%d" % i, [128, D], F32) for i in range(4)]
        hfb = [k.sb("hfb%d" % i, [128, D], BF16) for i in range(2)]
        junk = k.sb("junk", [128, D], BF16)
        ss = [k.sb("ss%d" % i, [128, 1], F32) for i in range(NB)]
        rstd = [k.sb("rstd%d" % i, [128, 1], F32) for i in range(NB)]
        hfT = [k.sb("hfT%d" % i, [128, 8, 128], F32) for i in range(2)]
        ptm = k.ps("ptm", [128, 6, 128], BF16)
        py = [k.ps("py%d" % i, [128, 512], F32) for i in range(2)]
        pth = k.ps("pth", [128, 8, 128], F32)
        plg = k.ps("plg", [128, 16], F32)
        pat = k.ps("pat", [48, 128], F32)
        lmax = [k.sb("lmax%d" % i, [128, 1], F32) for i in range(2)]
        lsum = [k.sb("lsum%d" % i, [128, 1], F32) for i in range(2)]
        afftm = [k.sb("afftm%d" % i, [128, 48], F32) for i in range(3)]
        affT = k.sb("affT", [48, S + C], F32)
        for a_ in afftm:
            k.V(lambda e, a_=a_: e.memset(a_[:], 0.0), [], [a_])
        tiles = list(range(2, NT)) + ([] if last else [0, 1])
        items = [(t, s) for t in tiles for s in range(NS)]

        def info(i):
            t, s = items[i]
            isctx = t < 2
            r = NS if isctx else s
            if isctx:
                src = src_ctx[s * C + t * 128: s * C + (t + 1) * 128, :]
                dst = A["res_d"][NLAT + s * C + t * 128: NLAT + s * C + (t + 1) * 128, :]
                hrow = NLAT + s * C + t * 128
            else:
                src = src_lat[s * S + (t - 2) * 128: s * S + (t - 1) * 128, :]
                dst = dst_lat[s * S + (t - 2) * 128: s * S + (t - 1) * 128, :]
                hrow = s * S + (t - 2) * 128
            return t, s, r, src, dst, hrow

        NX, NM, NH = 5, 4, 4

        def stL(i):
            t, s, r, src, dst, hrow = info(i)
            m, mt, x_t = mi[i % 3], mixT[i % NM], xt[i % NX]
            k.dma("sp", lambda e: e.dma_start(out=m[:], in_=A["mixin_d"][s, t * 128:(t + 1) * 128, :]),
                  reads=["mixin_d"], writes=[m])
            k.dma("sp", lambda e: e.dma_start(
                out=mt[:, 6:8, :], in_=A["convT_d"][s, :, t * 128:(t + 1) * 128].rearrange("(c p) t -> p c t", p=128)),
                reads=["convT_d"], writes=[mt])
            k.dma("act", lambda e: e.dma_start(out=x_t[:], in_=src), writes=[x_t])

        def stA1(i):
            m, mt = mi[i % 3], mixT[i % NM]
            for j in range(6):
                k.T(lambda e, j=j: e.transpose(ptm[:, j, :], m[:, j * 128:(j + 1) * 128], identb[:]), [m, identb], [ptm])
            k.A(lambda e: e.copy(out=mt[:, 0:6, :], in_=ptm[:]), [ptm], [mt])

        def stA2(i):
            t, s, r, src, dst, hrow = info(i)
            mt, x_m = mixT[i % NM], xm[i % NX]
            for hh in range(2):
                p = py[hh]
                for kc in range(8):
                    k.T(lambda e, p=p, kc=kc, hh=hh: e.matmul(p[:], lhsT=mt[:, kc, :], rhs=wout[:, kc, hh * 512:(hh + 1) * 512],
                                                              start=(kc == 0), stop=(kc == 7)), [mt] + woutk, [p])
                k.V(lambda e, p=p, hh=hh: e.tensor_tensor(out=x_m[:, hh * 512:(hh + 1) * 512], in0=p[:],
                                                          in1=gta[r][:, hh * 512:(hh + 1) * 512], op=ALU.mult),
                    [p, gta[r]], [x_m])

        def stA3(i):
            t, s, r, src, dst, hrow = info(i)
            x_t, x_m = xt[i % NX], xm[i % NX]
            k.G(lambda e: e.tensor_tensor(out=x_m[:], in0=x_m[:], in1=x_t[:], op=ALU.add), [x_m, x_t], [x_m])
            k.dma("sp", lambda e: e.dma_start(out=dst, in_=x_m[:]), reads=[x_m], writes=["res"])

        def stB1(i):
            x_m, sst, rs = xm[i % NX], ss[i % NX], rstd[i % NX]
            k.A(lambda e: e.activation(out=junk[:], in_=x_m[:], func=AF.Square, accum_out=sst[:, 0:1]), [x_m], [junk, sst])
            k.A(lambda e: e.activation(out=sst[:, 0:1], in_=sst[:, 0:1], func=AF.Ln, scale=1.0 / D, bias=EPSB[:, 0:1]),
                [sst, EPSB], [sst])
            k.A(lambda e: e.activation(out=rs[:, 0:1], in_=sst[:, 0:1], func=AF.Exp, scale=-0.5), [sst], [rs])

        def stB2(i):
            t, s, r, src, dst, hrow = info(i)
            x_m, h_f, rs = xm[i % NX], hf[i % NH], rstd[i % NX]
            k.V(lambda e: e.scalar_tensor_tensor(out=h_f[:], in0=x_m[:], scalar=rs[:, 0:1], in1=gsf[r][:], op0=ALU.mult,
                                                 op1=ALU.mult), [x_m, rs, gsf[r]], [h_f])

        def stB3(i):
            t, s, r, src, dst, hrow = info(i)
            h_f = hf[i % NH]
            k.G(lambda e: e.tensor_tensor(out=h_f[:], in0=h_f[:], in1=shf[r][:], op=ALU.add), [h_f, shf[r]], [h_f])

        def stB4(i):
            t, s, r, src, dst, hrow = info(i)
            h_f, h_b = hf[i % NH], hfb[i % 2]
            for kc in range(8):
                k.T(lambda e, kc=kc: e.transpose(pth[:, kc, :], h_f[:, kc * 128:(kc + 1) * 128], ident[:]), [h_f, ident], [pth])
            k.A(lambda e: e.copy(out=h_b[:], in_=h_f[:]), [h_f], [h_b])
            k.dma("act", lambda e: e.dma_start(out=A["hfb_d"][hrow:hrow + 128, :], in_=h_b[:]), reads=[h_b], writes=["hfb_d"])

        def stC1(i):
            h_T = hfT[i % 2]
            k.A(lambda e: e.copy(out=h_T[:], in_=pth[:]), [pth], [h_T])

        def stC2(i):
            h_T = hfT[i % 2]
            for kc in range(8):
                k.T(lambda e, kc=kc: e.matmul(plg[:], lhsT=h_T[:, kc, :], rhs=wr[:, kc, :], start=(kc == 0), stop=(kc == 7)),
                    [h_T, wr], [plg])
            lm = lmax[i % 2]
            k.V(lambda e: e.tensor_reduce(out=lm[:], in_=plg[:], axis=AX.X, op=ALU.max, negate=True), [plg], [lm])

        def stC3(i):
            t, s, r, src, dst, hrow = info(i)
            af = afftm[(i // NS) % 3]
            lm, ls = lmax[i % 2], lsum[i % 2]
            k.A(lambda e: e.activation(out=af[:, s * 32:s * 32 + 16], in_=plg[:], func=AF.Exp, bias=lm[:, 0:1],
                                       scale=1.0, accum_out=ls[:, 0:1]), [plg, lm], [af, ls])

        def stC4(i):
            t, s, r, src, dst, hrow = info(i)
            af = afftm[(i // NS) % 3]
            ls = lsum[i % 2]
            k.V(lambda e: e.reciprocal(out=ls[:], in_=ls[:]), [ls], [ls])
            k.V(lambda e: e.tensor_scalar(out=af[:, s * 32:s * 32 + 16], in0=af[:, s * 32:s * 32 + 16],
                                          scalar1=ls[:, 0:1], scalar2=None, op0=ALU.mult), [af, ls], [af])
            if s == NS - 1:
                k.T(lambda e: e.transpose(pat[:], af[:], ident[:]), [af, ident], [pat])

        def stC5(i):
            t, s, r, src, dst, hrow = info(i)
            if s == NS - 1:
                k.A(lambda e: e.copy(out=affT[:, t * 128:(t + 1) * 128], in_=pat[:]), [pat], [affT])

        stages = [stL, stA1, stA2, stA3, stB1, stB2, stB3, stB4, stC1, stC2, stC3, stC4, stC5]
        n = len(items)
        ns = len(stages)
        for step in range(n + ns - 1):
            for j in range(ns - 1, -1, -1):
                i = step - j
                if 0 <= i < n:
                    stages[j](i)
        k.dma("sp", lambda e: e.dma_start(out=A["aff_d"], in_=affT[:, C:C + S]), reads=[affT], writes=["aff_d"])
        if not last:
            k.dma("sp", lambda e: e.dma_start(out=A["affc_d"], in_=affT[:, 0:C]), reads=[affT], writes=["affc_d"])


def phase_moe(k, A, P, l, last, dst_lat):
    with k.phase():
        ident = P["ident"]
        nctx = 0 if last else NS
        NSL = NS * CAPL + nctx * CAPC
        if last:
            cgroups = [(0, 512)]
        else:
            cgroups = [(0, 288), (288, 288)]
        idxT = k.sb("idxT", [128, 2, 48], I32)
        gT = k.sb("gTm", [128, 2, 48], F32)
        if not last:
            idcT = k.sb("idcT", [32, 48], I32)
            gcT = k.sb("gcT", [32, 48], F32)
            idc64 = k.sb("idc64", [64, E], I32)
            gc64 = k.sb("gc64", [64, E], F32)
        with k.phase():
            wa = k.sb("wa", [48, S], F32)
            wb = k.sb("wb", [48, S], F32)
            vals = k.sb("vals", [48, CAPL], F32)
            idx = k.sb("idx", [48, CAPL], U32)
            idxf = k.sb("idxf", [48, CAPL], F32)
            offs = k.sb("offs", [48, 1], F32)
            k.dma("sp", lambda e: e.dma_start(out=wa[:], in_=A["aff_d"]), reads=["aff_d"], writes=[wa])
            cur, oth = wa, wb
            for r in range(CAPL // 8):
                k.V(lambda e, cur=cur, r=r: e.max(out=vals[:, r * 8:(r + 1) * 8], in_=cur[:]), [cur], [vals])
                k.V(lambda e, cur=cur, r=r: e.max_index(out=idx[:, r * 8:(r + 1) * 8], in_max=vals[:, r * 8:(r + 1) * 8],
                                                        in_values=cur[:]), [cur, vals], [idx])
                if r < CAPL // 8 - 1:
                    k.V(lambda e, cur=cur, oth=oth, r=r: e.match_replace(out=oth[:], in_to_replace=vals[:, r * 8:(r + 1) * 8],
                                                                         in_values=cur[:], imm_value=-1.0), [cur, vals], [oth])
                    cur, oth = oth, cur
            k.V(lambda e: e.memset(offs[0:32, :], 0.0), [], [offs])
            k.V(lambda e: e.memset(offs[32:48, :], float(S)), [], [offs])
            k.V(lambda e: e.tensor_copy(out=idxf[:], in_=idx[:]), [idx], [idxf])
            k.V(lambda e: e.tensor_scalar(out=idxf[:], in0=idxf[:], scalar1=offs[:, 0:1], scalar2=None, op0=ALU.add),
                [idxf, offs], [idxf])
            pti = k.ps("pti", [128, 2, 48], F32)
            ptg = k.ps("ptg", [128, 2, 48], F32)
            for j in range(2):
                k.T(lambda e, j=j: e.transpose(pti[:, j, :], idxf[:, j * 128:(j + 1) * 128], ident[0:48, 0:48]), [idxf, ident], [pti])
                k.T(lambda e, j=j: e.transpose(ptg[:, j, :], vals[:, j * 128:(j + 1) * 128], ident[0:48, 0:48]), [vals, ident], [ptg])
            k.V(lambda e: e.tensor_copy(out=idxT[:], in_=pti[:]), [pti], [idxT])
            k.V(lambda e: e.tensor_copy(out=gT[:], in_=ptg[:]), [ptg], [gT])
            if not last:
                wc = k.sb("wc", [48, C], F32)
                wd_ = k.sb("wd_", [48, C], F32)
                valc = k.sb("valc", [48, CAPC], F32)
                idc = k.sb("idc", [48, CAPC], U32)
                idcf = k.sb("idcf", [48, CAPC], F32)
                offc = k.sb("offc", [48, 1], F32)
                k.dma("sp", lambda e: e.dma_start(out=wc[:], in_=A["affc_d"]), reads=["affc_d"], writes=[wc])
                cur, oth = wc, wd_
                for r in range(CAPC // 8):
                    k.V(lambda e, cur=cur, r=r: e.max(out=valc[:, r * 8:(r + 1) * 8], in_=cur[:]), [cur], [valc])
                    k.V(lambda e, cur=cur, r=r: e.max_index(out=idc[:, r * 8:(r + 1) * 8], in_max=valc[:, r * 8:(r + 1) * 8],
                                                            in_values=cur[:]), [cur, valc], [idc])
                    if r < CAPC // 8 - 1:
                        k.V(lambda e, cur=cur, oth=oth, r=r: e.match_replace(out=oth[:], in_to_replace=valc[:, r * 8:(r + 1) * 8],
                                                                             in_values=cur[:], imm_value=-1.0), [cur, valc], [oth])
                        cur, oth = oth, cur
                k.V(lambda e: e.memset(offc[0:32, :], float(NLAT)), [], [offc])
                k.V(lambda e: e.memset(offc[32:48, :], float(NLAT + C)), [], [offc])
                k.V(lambda e: e.tensor_copy(out=idcf[:], in_=idc[:]), [idc], [idcf])
                k.V(lambda e: e.tensor_scalar(out=idcf[:], in0=idcf[:], scalar1=offc[:, 0:1], scalar2=None, op0=ALU.add),
                    [idcf, offc], [idcf])
                ptic = k.ps("ptic", [32, 2, 48], F32)
                k.T(lambda e: e.transpose(ptic[:, 0, :], idcf[:, 0:32], ident[0:48, 0:48]), [idcf, ident], [ptic])
                k.T(lambda e: e.transpose(ptic[:, 1, :], valc[:, 0:32], ident[0:48, 0:48]), [valc, ident], [ptic])
                k.V(lambda e: e.tensor_copy(out=idcT[:], in_=ptic[:, 0, :]), [ptic], [idcT])
                k.V(lambda e: e.tensor_copy(out=gcT[:], in_=ptic[:, 1, :]), [ptic], [gcT])
        if not last:
            for s_ in range(NS):
                k.dma("sp", lambda e, s_=s_: e.dma_start(out=idc64[s_ * 32:(s_ + 1) * 32, :], in_=idcT[:, s_ * 32:s_ * 32 + E]),
                      reads=[idcT], writes=[idc64])
                k.dma("sp", lambda e, s_=s_: e.dma_start(out=gc64[s_ * 32:(s_ + 1) * 32, :], in_=gcT[:, s_ * 32:s_ * 32 + E]),
                      reads=[gcT], writes=[gc64])
        identb = P["identb"]
        nr = NS + (0 if last else 1)
        gtf = [k.sb("gtf%d" % r, [128, D], F32) for r in range(nr)]
        for r in range(nr):
            k.dma("sp", lambda e, r=r: e.dma_start(out=gtf[r][:], in_=A["mod_d"][l, r, 5 * D:6 * D].partition_broadcast(128)),
                  reads=["mod_d"], writes=[gtf[r]])
        xe = [k.sb("xe%d" % i, [128, D], BF16) for i in range(3)]
        xeT = [k.sb("xeT%d" % i, [128, 8, NSL], BF16) for i in range(2)]
        hT = k.sb("hT", [128, NFC, NSL], BF16)
        sg = [k.sb("sg%d" % i, [128, NSL], F32) for i in range(2)]
        NW = 3
        NPRE = NW - 1
        wg = [k.sb("wg%d" % i, [128, 8, 512], BF16) for i in range(NW)]
        wu = [k.sb("wu%d" % i, [128, 8, 512], BF16) for i in range(NW)]
        wdn = k.sb("wdn", [128, NFC, D], BF16)
        ysb = [k.sb("ysb%d" % i, [128, D], F32) for i in range(2)]
        ncg = len(cgroups)
        pg = [k.ps("pg%d" % i, [128, 512], F32) for i in range(ncg)]
        pu = [k.ps("pu%d" % i, [128, 512], F32) for i in range(ncg)]
        pxt = k.ps("pxt", [128, 8, 128], BF16)
        pyd = [k.ps("pyd%d" % i, [128, 512], F32) for i in range(8 - 2 * ncg - 1)]
        target = dst_lat
        pieces = [(i * 512, min(512, DFF - i * 512)) for i in range((DFF + 511) // 512)]
        NP_ = len(pieces)
        cnt = {"y": 0}

        def tiles_of(ex):
            tl = []
            for s in range(NS):
                for j in range(2):
                    tl.append((128, s * 256 + j * 128, idxT[:, j, s * 32 + ex: s * 32 + ex + 1],
                               gT[:, j, s * 32 + ex: s * 32 + ex + 1], s, "lat"))
            if not last:
                tl.append((NS * CAPC, NS * CAPL, idc64[:, ex:ex + 1], gc64[:, ex:ex + 1], NS, "ctx"))
            return tl

        idx_reads = [idxT] + ([] if last else [idc64])

        def gather_T(ex):
            xT = xeT[ex % 2]
            for ti, (rows, c0, iap, gap, r, kind) in enumerate(tiles_of(ex)):
                x_e = xe[ti % 3]
                k.dma("pool", lambda e, x_e=x_e, rows=rows, iap=iap: e.indirect_dma_start(
                    out=x_e[0:rows, :], out_offset=None, in_=A["hfb_d"],
                    in_offset=bass.IndirectOffsetOnAxis(ap=iap[0:rows, :], axis=0)),
                    reads=["hfb_d"] + idx_reads, writes=[x_e])
                for kc in range(8):
                    k.T(lambda e, x_e=x_e, rows=rows, kc=kc: e.transpose(pxt[:, kc, 0:rows], x_e[0:rows, kc * 128:(kc + 1) * 128],
                                                                       identb[0:rows, 0:rows]), [x_e, identb], [pxt])
                if ti % 2 == 0:
                    k.A(lambda e, xT=xT, rows=rows, c0=c0: e.copy(out=xT[:, :, c0:c0 + rows], in_=pxt[:, :, 0:rows]), [pxt], [xT])
                else:
                    k.V(lambda e, xT=xT, rows=rows, c0=c0: e.tensor_copy(out=xT[:, :, c0:c0 + rows], in_=pxt[:, :, 0:rows]),
                        [pxt], [xT])

        def load_gu(ex, pi):
            c0, w = pieces[pi]
            gi_ = (ex * NP_ + pi) % NW
            wgv = A["w_gate"][l, ex].rearrange("(kc p) f -> p kc f", p=128)
            wuv = A["w_up"][l, ex].rearrange("(kc p) f -> p kc f", p=128)
            k.dma("pool", lambda e: e.dma_start(out=wg[gi_][:, :, 0:w], in_=wgv[:, :, c0:c0 + w]), writes=[wg[gi_]])
            k.dma("pool", lambda e: e.dma_start(out=wu[gi_][:, :, 0:w], in_=wuv[:, :, c0:c0 + w]), writes=[wu[gi_]])

        def load_dn(ex, pi):
            c0, w = pieces[pi]
            f0, nf = c0 // 128, w // 128
            wdv = A["w_down"][l, ex].rearrange("(fc p) d -> p fc d", p=128)
            for f in range(f0, f0 + nf, 2):
                k.dma("pool", lambda e, f=f: e.dma_start(out=wdn[:, f:f + 2, :], in_=wdv[:, f:f + 2, :]),
                      writes=[("wdn", f // 2)])

        def gu(ex, pi):
            c0, w = pieces[pi]
            gi_ = (ex * NP_ + pi) % NW
            xT = xeT[ex % 2]
            for f2 in range(w // 128):
                fc = c0 // 128 + f2
                for (W, ps_list) in ((wg[gi_], pg), (wu[gi_], pu)):
                    for gj, (g0, gw) in enumerate(cgroups):
                        p = ps_list[gj]
                        for kc in range(8):
                            k.T(lambda e, p=p, W=W, kc=kc, f2=f2, g0=g0, gw=gw: e.matmul(
                                p[:, 0:gw], lhsT=W[:, kc, f2 * 128:(f2 + 1) * 128], rhs=xT[:, kc, g0:g0 + gw],
                                start=(kc == 0), stop=(kc == 7)), [W, xT], [p])
                s_g = sg[fc % 2]
                for gj, (g0, gw) in enumerate(cgroups):
                    k.A(lambda e, s_g=s_g, gj=gj, g0=g0, gw=gw: e.activation(out=s_g[:, g0:g0 + gw], in_=pg[gj][:, 0:gw],
                                                                            func=AF.Silu), [pg[gj]], [s_g])
                    k.V(lambda e, s_g=s_g, gj=gj, g0=g0, gw=gw, fc=fc: e.tensor_tensor(
                        out=hT[:, fc, g0:g0 + gw], in0=s_g[:, g0:g0 + gw], in1=pu[gj][:, 0:gw], op=ALU.mult),
                        [s_g, pu[gj]], [("hT", fc)])

        def down(ex):
            for ti, (rows, c0, iap, gap, r, kind) in enumerate(tiles_of(ex)):
                y_s = ysb[ti % 2]
                for hh in range(2):
                    p = pyd[cnt["y"] % len(pyd)]
                    cnt["y"] += 1
                    for fc in range(NFC):
                        k.T(lambda e, p=p, rows=rows, c0=c0, fc=fc, hh=hh: e.matmul(
                            p[0:rows, :], lhsT=hT[:, fc, c0:c0 + rows], rhs=wdn[:, fc, hh * 512:(hh + 1) * 512],
                            start=(fc == 0), stop=(fc == NFC - 1)), [("hT", fc), ("wdn", fc // 2)], [p])
                    k.V(lambda e, p=p, rows=rows, y_s=y_s, hh=hh, gap=gap, r=r: e.scalar_tensor_tensor(
                        out=y_s[0:rows, hh * 512:(hh + 1) * 512], in0=p[0:rows, :], scalar=gap[0:rows, :],
                        in1=gtf[r][0:rows, hh * 512:(hh + 1) * 512], op0=ALU.mult, op1=ALU.mult),
                        [p, gT, gtf[r]] + ([] if last else [gc64]), [y_s])
                tgt = target if kind == "lat" else A["res_d"]
                k.dma("pool", lambda e, y_s=y_s, rows=rows, iap=iap, tgt=tgt: e.indirect_dma_start(
                    out=tgt, out_offset=bass.IndirectOffsetOnAxis(ap=iap[0:rows, :], axis=0), in_=y_s[0:rows, :],
                    in_offset=None, compute_op=ALU.add), reads=[y_s] + idx_reads, writes=["res"])

        gather_T(0)
        for pi in range(NPRE):
            load_gu(0, pi)
        for ex in range(E):
            for pi in range(NP_):
                load_dn(ex, pi)
                gu(ex, pi)
                if pi + NPRE < NP_:
                    load_gu(ex, pi + NPRE)
            if ex + 1 < E:
                gather_T(ex + 1)
                for pi in range(NPRE):
                    load_gu(ex + 1, pi)
            down(ex)


_NC_CACHE = {}


def kernel(**inputs):
    consts = host_consts()
    if "nc" not in _NC_CACHE:
        _NC_CACHE["nc"] = build_program()
    nc = _NC_CACHE["nc"]
    x = np.ascontiguousarray(inputs["x"], dtype=np.float32)
    c = np.ascontiguousarray(inputs["c"], dtype=np.float32)
    ctx = np.ascontiguousarray(inputs["ctx"], dtype=np.float32)
    shared = {"c_ctx": np.ascontiguousarray(inputs["c_ctx"], dtype=np.float32).reshape(1, D)}
    for n in WNAMES:
        shared[n] = np.ascontiguousarray(inputs[n], dtype=np.float32).reshape(WSHAPES[n])
    for n, v in consts.items():
        shared[n] = v
    in_maps = []
    for ci in range(NCORES):
        m = dict(shared)
        m["x"] = x[ci * NS:(ci + 1) * NS].reshape(NS * S, D)
        m["c"] = c[ci * NS:(ci + 1) * NS]
        m["ctx"] = ctx[ci * NS:(ci + 1) * NS].reshape(NS * C, D)
        in_maps.append(m)
    res = run_bass_kernel_spmd(nc, in_maps, core_ids=list(range(NCORES)))
    out = np.concatenate([r["y"].reshape(NS, S, D) for r in res.results], axis=0)
    return out.astype(np.float32)
```

```python
import os
import numpy as np
import concourse.bass as bass
import concourse.mybir as mybir
from concourse.bass_utils import run_bass_kernel_spmd
from contextlib import ExitStack, contextmanager

F32 = mybir.dt.float32
F32R = mybir.dt.float32r
BF16 = mybir.dt.bfloat16
U32 = mybir.dt.uint32
I32 = mybir.dt.int32
AF = mybir.ActivationFunctionType
ALU = mybir.AluOpType
AX = mybir.AxisListType

NCORES = 8
NS = 2
D = 1024
S = 2048
C = 256
NT = (S + C) // 128
L = 2
E = 16
DFF = 2816
NFC = DFF // 128
CAPL = 256
CAPC = 32
EPS = 1e-6
UC = 1792
NLAT = NS * S
NROW = NS * (S + C)


class KB:
    ENGS = ("pe", "dve", "act", "pool", "sp")

    def __init__(self, nc, ndma=24):
        self.nc = nc
        self.st = ExitStack()
        self.q = {e: [] for e in self.ENGS}
        self.cnt = {e: 0 for e in self.ENGS}
        self.sem = {e: self.st.enter_context(nc.semaphore("prog_" + e)) for e in self.ENGS}
        self.waited = {(c, p): 0 for c in self.ENGS for p in self.ENGS}
        self.waited_dma = {}
        self.lastw = {}
        self.readers = {}
        self.dma_sems = []
        self.ndma = {"sp": 0, "act": 0, "pool": 0}
        self.pool_of = {"sp": (0, 10), "act": (10, 8), "pool": (18, 14)}
        for i in range(32):
            s = self.st.enter_context(nc.semaphore("dma%d" % i))
            self.dma_sems.append([s, 0, None])
        self.cur = self.st

    def sb(self, name, shape, dt):
        self.uid = getattr(self, "uid", 0) + 1
        return self.cur.enter_context(self.nc.sbuf_tensor("%s_s%d" % (name, self.uid), shape, dt))

    def ps(self, name, shape, dt):
        self.uid = getattr(self, "uid", 0) + 1
        return self.cur.enter_context(self.nc.psum_tensor("%s_p%d" % (name, self.uid), shape, dt))

    @contextmanager
    def phase(self):
        prev = self.cur
        with ExitStack() as es:
            self.cur = es
            yield
            self.barrier()
        self.cur = prev

    def barrier(self):
        toks = [(p, self.cnt[p]) for p in self.ENGS if self.cnt[p] > 0]
        dtoks = [d[2] for d in self.dma_sems if d[2] is not None]
        for e in self.ENGS:
            for t in toks:
                if t[0] != e:
                    self._wait(e, t)
            for t in dtoks:
                self._wait(e, t)
        self.lastw = {}
        self.readers = {}

    def _key(self, x):
        if isinstance(x, (str, tuple)):
            return x
        return x.tensor.name if hasattr(x, "tensor") else x.name

    def _wait(self, eng, tok):
        if tok is None:
            return
        if tok[0] == "dma":
            _, i, val = tok
            if self.waited_dma.get((eng, i), 0) >= val:
                return
            self.waited_dma[(eng, i)] = val
            sem = self.dma_sems[i][0]
            self.q[eng].append(lambda e, sem=sem, val=val: e.wait_ge(sem, val))
            return
        p, c = tok
        if eng == "pe" and p == "pe" and not (PESYNC or getattr(self, "pesync", False)):
            return
        if self.waited[(eng, p)] >= c:
            return
        self.waited[(eng, p)] = c
        s = self.sem[p]
        self.q[eng].append(lambda e, s=s, c=c: e.wait_ge(s, c))

    def _deps(self, eng, rk, wk):
        for k in rk:
            self._wait(eng, self.lastw.get(k))
        for k in wk:
            self._wait(eng, self.lastw.get(k))
            for t in self.readers.get(k, ()):
                self._wait(eng, t)

    def _record(self, tok, rk, wk):
        for k in wk:
            self.lastw[k] = tok
            self.readers[k] = []
        for k in rk:
            self.readers.setdefault(k, []).append(tok)

    def op(self, eng, fn, reads=(), writes=()):
        rk = [self._key(r) for r in reads]
        wk = [self._key(w) for w in writes]
        self._deps(eng, rk, wk)
        self.cnt[eng] += 1
        tok = (eng, self.cnt[eng])
        s = self.sem[eng]
        self.q[eng].append(lambda e, fn=fn, s=s: fn(e).then_inc(s, 1))
        self._record(tok, rk, wk)
        return tok

    def V(self, fn, r=(), w=()):
        return self.op("dve", fn, r, w)

    def A(self, fn, r=(), w=()):
        return self.op("act", fn, r, w)

    def G(self, fn, r=(), w=()):
        return self.op("pool", fn, r, w)

    def T(self, fn, r=(), w=()):
        return self.op("pe", fn, r, w)

    def dma(self, eng, fn, reads=(), writes=()):
        rk = [self._key(r) for r in reads]
        wk = [self._key(w) for w in writes]
        self._deps(eng, rk, wk)
        base, n = self.pool_of[eng]
        i = base + self.ndma[eng] % n
        self.ndma[eng] += 1
        ent = self.dma_sems[i]
        if ent[2] is not None:
            self._wait(eng, ent[2])
        ent[1] += 16
        tok = ("dma", i, ent[1])
        ent[2] = tok
        sem = ent[0]
        self.q[eng].append(lambda e, fn=fn, sem=sem: fn(e).then_inc(sem, 16))
        self._record(tok, rk, wk)
        return tok

    def finish(self):
        nc = self.nc
        self.barrier()
        q = self.q
        with nc.Block() as block:
            @block.tensor
            def _(e):
                for f in q["pe"]:
                    f(e)

            @block.vector
            def _(e):
                for f in q["dve"]:
                    f(e)

            @block.scalar
            def _(e):
                for f in q["act"]:
                    f(e)

            @block.gpsimd
            def _(e):
                for f in q["pool"]:
                    f(e)

            @block.sync
            def _(e):
                for f in q["sp"]:
                    f(e)
        self.st.close()


def host_consts():
    f32 = np.float32
    half = 16
    inv = (10000.0 ** (-np.arange(half, dtype=f32) / half)).astype(f32)
    t = np.arange(S)
    rows = (t // 64).astype(f32)
    cols = (t % 64).astype(f32)
    ar = (rows[:, None] * inv[None, :]).astype(f32)
    ac = (cols[:, None] * inv[None, :]).astype(f32)
    attC = np.concatenate([np.cos(ar), np.cos(ar), np.cos(ac), np.cos(ac)], axis=1).astype(f32)
    attS = np.concatenate([-np.sin(ar), np.sin(ar), -np.sin(ac), np.sin(ac)], axis=1).astype(f32)
    half = 32
    inv = (10000.0 ** (-np.arange(half, dtype=f32) / half)).astype(f32)
    pos = np.arange(S + C).astype(f32)
    a = (pos[:, None] * inv[None, :]).astype(f32)
    retC = np.concatenate([np.cos(a), np.cos(a)], axis=1).astype(f32)
    retS = np.concatenate([-np.sin(a), np.sin(a)], axis=1).astype(f32)
    j = np.arange(128)[:, None]
    i = np.arange(128)[None, :]
    masks = np.stack([(j <= i), (j >= i), (j > i)], axis=0).astype(f32)
    ident = np.eye(128, dtype=f32)
    p = np.arange(128, dtype=f32)
    dcoef = np.stack([p + 1, -(p + 1), -p, p], axis=1).astype(f32)
    return {"attC": attC, "attS": attS, "retC": retC, "retS": retS, "masks": masks,
            "ident": ident, "dcoef": dcoef}


WNAMES = ["w_mod", "b_mod", "g_mix", "g_ffn", "w_in", "q_norm_g", "k_norm_g", "att_sink",
          "ret_decay_logit", "ret_norm_g", "conv_w", "conv_b", "conv_norm_g", "conv_norm_b",
          "w_out", "w_router", "w_gate", "w_up", "w_down"]
WSHAPES = {
    "w_mod": [L, D, 6 * D], "b_mod": [L, 6 * D], "g_mix": [L, D], "g_ffn": [L, D], "w_in": [L, D, 2304],
    "q_norm_g": [L, 64], "k_norm_g": [L, 64], "att_sink": [L, 8], "ret_decay_logit": [L, 8],
    "ret_norm_g": [L, 256], "conv_w": [L, 31, 256], "conv_b": [L, 256], "conv_norm_g": [L, 256],
    "conv_norm_b": [L, 256], "w_out": [L, D, D], "w_router": [L, D, E], "w_gate": [L, E, D, DFF],
    "w_up": [L, E, D, DFF], "w_down": [L, E, DFF, D],
}
CSHAPES = {"attC": [S, 64], "attS": [S, 64], "retC": [S + C, 64], "retS": [S + C, 64],
           "masks": [3, 128, 128], "ident": [128, 128], "dcoef": [128, 4]}

PESYNC = os.environ.get("MK_PESYNC", "") == "1"
RETSKIP = os.environ.get("MK_RETSKIP", "")
STOP = os.environ.get("MK_STOP", "")
DEBUG = os.environ.get("MK_DEBUG", "") == "1"


def build_program():
    nc = bass.Bass("TRN2", target_bir_lowering=False)
    A = {}
    A["x"] = nc.dram_tensor("x", [NS * S, D], F32, kind="ExternalInput").ap()
    A["c"] = nc.dram_tensor("c", [NS, D], F32, kind="ExternalInput").ap()
    A["ctx"] = nc.dram_tensor("ctx", [NS * C, D], F32, kind="ExternalInput").ap()
    A["c_ctx"] = nc.dram_tensor("c_ctx", [1, D], F32, kind="ExternalInput").ap()
    for n in WNAMES:
        A[n] = nc.dram_tensor(n, WSHAPES[n], F32, kind="ExternalInput").ap()
    for n, shp in CSHAPES.items():
        A[n] = nc.dram_tensor(n, shp, F32, kind="ExternalInput").ap()
    A["y"] = nc.dram_tensor("y", [NS * S, D], F32, kind="ExternalOutput").ap()
    dk = "ExternalOutput" if DEBUG else "Internal"
    A["mod_d"] = nc.dram_tensor("mod_d", [L, 4, 6 * D], F32, kind=dk).ap()
    A["u_d"] = nc.dram_tensor("u_d", [NS, S + C, UC], F32, kind=dk).ap()
    A["cv_d"] = nc.dram_tensor("cv_d", [NS, 512, S + C], F32, kind=dk).ap()
    A["mixin_d"] = nc.dram_tensor("mixin_d", [NS, S + C, 768], BF16, kind=dk).ap()
    A["convT_d"] = nc.dram_tensor("convT_d", [NS, 256, S + C], BF16, kind=dk).ap()
    A["res_d"] = nc.dram_tensor("res_d", [NROW, D], F32, kind=dk).ap()
    A["hf_d"] = nc.dram_tensor("hf_d", [NROW, D], F32, kind=dk).ap()
    A["aff_d"] = nc.dram_tensor("aff_d", [48, S], F32, kind=dk).ap()
    A["hfb_d"] = nc.dram_tensor("hfb_d", [NROW, D], BF16, kind=dk).ap()
    A["affc_d"] = nc.dram_tensor("affc_d", [48, C], F32, kind=dk).ap()

    k = KB(nc)
    P = {}
    P["ident"] = k.sb("ident", [128, 128], F32)
    P["identb"] = k.sb("identb", [128, 128], BF16)
    P["masks"] = k.sb("masks", [128, 3, 128], F32)
    P["modT"] = [k.sb("modT%d" % l, [128, 48, 4], F32) for l in range(L)]
    P["gT"] = k.sb("gT", [128, 8, 4], F32)
    P["gsA"] = [k.sb("gsA%d" % l, [128, 8, 4], F32) for l in range(L)]
    P["epsb"] = k.sb("epsb", [128, 1], F32)
    k.V(lambda e: e.memset(P["epsb"][:], EPS), [], [P["epsb"]])
    k.dma("sp", lambda e: e.dma_start(out=P["ident"][:], in_=A["ident"]), writes=[P["ident"]])
    k.dma("sp", lambda e: e.dma_start(out=P["masks"][:], in_=A["masks"].rearrange("m j i -> j m i")),
          writes=[P["masks"]])
    k.V(lambda e: e.tensor_copy(out=P["identb"][:], in_=P["ident"][:]), [P["ident"]], [P["identb"]])

    phase_mod(k, A, P)
    if STOP == "mod":
        k.finish()
        return nc
    for l in range(L):
        last = l == L - 1
        src_lat = A["x"] if l == 0 else A["res_d"][0:NLAT]
        src_ctx = A["ctx"] if l == 0 else A["res_d"][NLAT:NROW]
        dst_lat = A["y"] if last else A["res_d"][0:NLAT]
        phase_proj(k, A, P, l, src_lat, src_ctx)
        if STOP == "proj%d" % l:
            break
        phase_att(k, A, P, l, last)
        if STOP == "att%d" % l:
            break
        phase_ret(k, A, P, l, last)
        if STOP == "ret%d" % l:
            break
        phase_conv(k, A, P, l, last)
        if STOP == "conv%d" % l:
            break
        phase_out(k, A, P, l, last, src_lat, src_ctx, dst_lat)
        if STOP == "out%d" % l:
            break
        phase_moe(k, A, P, l, last, dst_lat)
        if STOP == "moe%d" % l:
            break
    k.finish()
    return nc


def phase_mod(k, A, P):
    with k.phase():
        cc = k.sb("cc", [4, D], F32)
        cs = k.sb("cs", [4, D], F32)
        csT = k.sb("csT", [128, 8, 4], BF16)
        gg = k.sb("gg", [4, D], F32)
        pt = k.ps("pt", [128, 8, 4], F32)
        pm = [k.ps("pm%d" % i, [4, 512], F32) for i in range(2)]
        ptm = k.ps("ptm", [128, 48, 4], F32)
        wm = [k.sb("wm%d" % i, [128, 8, 512], BF16) for i in range(3)]
        bm = k.sb("bm", [4, 6 * D], F32)
        modrow = k.sb("modrow", [4, 6 * D], F32)
        ident = P["ident"]
        k.V(lambda e: e.memset(cc[:], 0.0), [], [cc])
        k.dma("sp", lambda e: e.dma_start(out=cc[0:NS, :], in_=A["c"]), writes=[cc])
        k.dma("sp", lambda e: e.dma_start(out=cc[NS:NS + 1, :], in_=A["c_ctx"]), writes=[cc])
        k.A(lambda e: e.activation(out=cs[:], in_=cc[:], func=AF.Silu), [cc], [cs])
        for kc in range(8):
            k.T(lambda e, kc=kc: e.transpose(pt[:, kc, :], cs[0:4, kc * 128:(kc + 1) * 128], ident[0:4, 0:4]),
                [cs, ident], [pt])
        k.V(lambda e: e.tensor_copy(out=csT[:], in_=pt[:]), [pt], [csT])
        k.dma("sp", lambda e: e.dma_start(out=gg[0:2, :], in_=A["g_mix"]), writes=[gg])
        k.dma("sp", lambda e: e.dma_start(out=gg[2:4, :], in_=A["g_ffn"]), writes=[gg])
        for kc in range(8):
            k.T(lambda e, kc=kc: e.transpose(pt[:, kc, :], gg[0:4, kc * 128:(kc + 1) * 128], ident[0:4, 0:4]),
                [gg, ident], [pt])
        k.V(lambda e: e.tensor_copy(out=P["gT"][:], in_=pt[:]), [pt], [P["gT"]])
        for l in range(L):
            k.dma("sp", lambda e, l=l: e.dma_start(out=bm[:], in_=A["b_mod"][l, :].partition_broadcast(4)),
                  writes=[bm])
            wv = A["w_mod"][l].rearrange("(kc p) f -> p kc f", p=128)
            for j in range(12):
                w = wm[(l * 12 + j) % 3]
                k.dma("pool", lambda e, w=w, j=j, wv=wv: e.dma_start(out=w[:], in_=wv[:, :, j * 512:(j + 1) * 512]),
                      writes=[w])
                p = pm[j % 2]
                for kc in range(8):
                    k.T(lambda e, p=p, w=w, kc=kc: e.matmul(p[:], lhsT=csT[:, kc, :], rhs=w[:, kc, :],
                                                          start=(kc == 0), stop=(kc == 7)), [csT, w], [p])
                k.V(lambda e, p=p, j=j: e.tensor_tensor(out=modrow[:, j * 512:(j + 1) * 512], in0=p[:],
                                                        in1=bm[:, j * 512:(j + 1) * 512], op=ALU.add),
                    [p, bm], [modrow])
            k.dma("sp", lambda e, l=l: e.dma_start(out=A["mod_d"][l], in_=modrow[:]), reads=[modrow],
                  writes=["mod_d"])
            for j in range(48):
                k.T(lambda e, j=j: e.transpose(ptm[:, j, :], modrow[0:4, j * 128:(j + 1) * 128], ident[0:4, 0:4]),
                    [modrow, ident], [ptm])
            mT = P["modT"][l]
            k.V(lambda e, mT=mT: e.tensor_copy(out=mT[:], in_=ptm[:]), [ptm], [mT])
            gs = P["gsA"][l]
            k.V(lambda e, gs=gs, mT=mT: e.tensor_scalar(out=gs[:], in0=mT[:, 8:16, :], scalar1=1.0, scalar2=None,
                                                        op0=ALU.add), [mT], [gs])
            k.V(lambda e, gs=gs, l=l: e.tensor_tensor(out=gs[:], in0=gs[:],
                                                      in1=P["gT"][:, :, l:l + 1].to_broadcast([128, 8, 4]),
                                                      op=ALU.mult), [gs, P["gT"]], [gs])


def rms_rstd(k, xt, junk, ss, rstd, n):
    k.A(lambda e: e.activation(out=junk, in_=xt, func=AF.Square, accum_out=ss[:, 0:1]), [xt], [junk, ss])
    k.V(lambda e: e.tensor_scalar(out=ss[:, 0:1], in0=ss[:, 0:1], scalar1=1.0 / n, scalar2=EPS, op0=ALU.mult,
                                  op1=ALU.add), [ss], [ss])
    k.A(lambda e: e.sqrt(out=ss[:, 0:1], in_=ss[:, 0:1]), [ss], [ss])
    k.V(lambda e: e.reciprocal(out=rstd[:, 0:1], in_=ss[:, 0:1]), [ss], [rstd])


def phase_proj(k, A, P, l, src_lat, src_ctx):
    with k.phase():
        win = k.sb("win", [128, 8, 2304], BF16)
        wv = A["w_in"][l].rearrange("(kc p) f -> p kc f", p=128)
        for j in range(0, 2304, 384):
            k.dma("pool", lambda e, j=j: e.dma_start(out=win[:, :, j:j + 384], in_=wv[:, :, j:j + 384]),
                  writes=[("win", j)])
        winkeys = [("win", j) for j in range(0, 2304, 384)]
        NB = 8
        xts = [k.sb("xt%d" % i, [128, D], F32) for i in range(NB)]
        xn = [k.sb("xn%d" % i, [128, D], BF16) for i in range(NB)]
        junk = k.sb("junk", [128, D], F32)
        ss = [k.sb("ss%d" % i, [128, 1], F32) for i in range(NB)]
        rstd = [k.sb("rstd%d" % i, [128, 1], F32) for i in range(NB)]
        hT = [k.sb("hT%d" % i, [128, 8, 512], BF16) for i in range(2)]
        ptr = [k.ps("ptr%d" % i, [128, D], BF16) for i in range(2)]
        pu = [k.ps("pu%d" % i, [128, 512], F32) for i in range(3)]
        pc = [k.ps("pc%d" % i, [128, 512], F32) for i in range(2)]
        usb = [k.sb("usb%d" % i, [128, UC], F32) for i in range(3)]
        cvs = [k.sb("cvs%d" % i, [128, 4, 512], F32) for i in range(2)]
        identb = P["identb"]
        gs = P["gsA"][l]
        mT = P["modT"][l]
        groups = []
        for s in range(NS):
            groups.append((s, 0, 2, src_ctx[s * C:(s + 1) * C], NS))
            for g in range(4):
                groups.append((s, C + g * 512, 4, src_lat[s * S + g * 512: s * S + (g + 1) * 512], s))
        cnt = {"t": 0, "e": 0}

        def stats(gi):
            s, tok0, ntl, src, r = groups[gi]
            for t in range(ntl):
                i = (gi % 2) * 4 + t
                xt, xnn, sst, rs = xts[i], xn[i], ss[i], rstd[i]
                k.dma("sp", lambda e, xt=xt, src=src, t=t: e.dma_start(out=xt[:], in_=src[t * 128:(t + 1) * 128, :]),
                      writes=[xt])
                k.A(lambda e, xt=xt, sst=sst: e.activation(out=junk[:], in_=xt[:], func=AF.Square, accum_out=sst[:, 0:1]),
                    [xt], [junk, sst])
                k.A(lambda e, sst=sst: e.activation(out=sst[:, 0:1], in_=sst[:, 0:1], func=AF.Ln, scale=1.0 / D, bias=EPSB[:, 0:1]),
                    [sst, EPSB], [sst])
                k.A(lambda e, sst=sst, rs=rs: e.activation(out=rs[:, 0:1], in_=sst[:, 0:1], func=AF.Exp, scale=-0.5), [sst], [rs])
                k.V(lambda e, xnn=xnn, xt=xt, rs=rs: e.tensor_scalar(out=xnn[:], in0=xt[:], scalar1=rs[:, 0:1],
                                                                   scalar2=None, op0=ALU.mult), [xt, rs], [xnn])

        def trans_tile(gi, t):
            s, tok0, ntl, src, r = groups[gi]
            h = hT[gi % 2]
            if True:
                i = (gi % 2) * 4 + t
                xnn = xn[i]
                pt = ptr[cnt["t"] % 2]
                cnt["t"] += 1
                for kc in range(8):
                    k.T(lambda e, pt=pt, xnn=xnn, kc=kc: e.transpose(pt[:, kc * 128:(kc + 1) * 128],
                                                                   xnn[:, kc * 128:(kc + 1) * 128], identb[:]),
                        [xnn, identb], [pt])
                for kc in range(8):
                    if kc % 2 == 0:
                        k.A(lambda e, h=h, pt=pt, kc=kc, t=t, r=r: e.activation(
                            out=h[:, kc, t * 128:(t + 1) * 128], in_=pt[:, kc * 128:(kc + 1) * 128],
                            func=AF.Identity, scale=gs[:, kc, r:r + 1], bias=mT[:, kc, r:r + 1]),
                            [pt, gs, mT], [h])
                    else:
                        k.V(lambda e, h=h, pt=pt, kc=kc, t=t, r=r: e.tensor_scalar(
                            out=h[:, kc, t * 128:(t + 1) * 128], in0=pt[:, kc * 128:(kc + 1) * 128],
                            scalar1=gs[:, kc, r:r + 1], scalar2=mT[:, kc, r:r + 1], op0=ALU.mult, op1=ALU.add),
                            [pt, gs, mT], [h])

        def mm_tile(gi, t):
            s, tok0, ntl, src, r = groups[gi]
            h = hT[gi % 2]
            ei = cnt["e"]
            if True:
                ub = usb[(gi * 4 + t) % 3]
                for ci, (c0, w) in enumerate([(0, 512), (512, 512), (1024, 512), (1536, 256)]):
                    p = pu[ei % 3]
                    for kc in range(8):
                        k.T(lambda e, p=p, h=h, kc=kc, t=t, c0=c0, w=w: e.matmul(
                            p[:, 0:w], lhsT=h[:, kc, t * 128:(t + 1) * 128], rhs=win[:, kc, c0:c0 + w],
                            start=(kc == 0), stop=(kc == 7)), [h] + winkeys, [p])
                    if ei % 2 == 0:
                        k.A(lambda e, ub=ub, p=p, c0=c0, w=w: e.copy(out=ub[:, c0:c0 + w], in_=p[:, 0:w]), [p], [ub])
                    else:
                        k.V(lambda e, ub=ub, p=p, c0=c0, w=w: e.tensor_copy(out=ub[:, c0:c0 + w], in_=p[:, 0:w]),
                            [p], [ub])
                    ei += 1
                k.dma("sp", lambda e, ub=ub, s=s, tok0=tok0, t=t: e.dma_start(
                    out=A["u_d"][s, tok0 + t * 128: tok0 + (t + 1) * 128, :], in_=ub[:]), reads=[ub], writes=["u_d"])
            cnt["e"] = ei

        def mm_conv(gi):
            s, tok0, ntl, src, r = groups[gi]
            h = hT[gi % 2]
            ntok = ntl * 128
            cv = cvs[gi % 2]
            for j in range(4):
                p = pc[j % 2]
                for kc in range(8):
                    k.T(lambda e, p=p, h=h, kc=kc, j=j, ntok=ntok: e.matmul(
                        p[:, 0:ntok], lhsT=win[:, kc, UC + j * 128: UC + (j + 1) * 128], rhs=h[:, kc, 0:ntok],
                        start=(kc == 0), stop=(kc == 7)), [h] + winkeys, [p])
                if j % 2 == 0:
                    k.A(lambda e, cv=cv, p=p, j=j, ntok=ntok: e.copy(out=cv[:, j, 0:ntok], in_=p[:, 0:ntok]), [p], [cv])
                else:
                    k.V(lambda e, cv=cv, p=p, j=j, ntok=ntok: e.tensor_copy(out=cv[:, j, 0:ntok], in_=p[:, 0:ntok]),
                        [p], [cv])
            k.dma("act", lambda e, cv=cv, s=s, tok0=tok0, ntok=ntok: e.dma_start(
                out=A["cv_d"][s, :, tok0:tok0 + ntok].rearrange("(j p) t -> p j t", p=128), in_=cv[:, :, 0:ntok]),
                reads=[cv], writes=["cv_d"])

        EPSB = P["epsb"]
        ng = len(groups)
        stats(0)
        for t in range(groups[0][2]):
            trans_tile(0, t)
        for gi in range(ng):
            if gi + 1 < ng:
                stats(gi + 1)
            n_cur = groups[gi][2]
            n_nxt = groups[gi + 1][2] if gi + 1 < ng else 0
            for t in range(max(n_cur, n_nxt)):
                if t < n_cur:
                    mm_tile(gi, t)
                if t < n_nxt:
                    trans_tile(gi + 1, t)
            mm_conv(gi)


def run_pipe(n, stages):
    ns = len(stages)
    for step in range(n + ns - 1):
        for j in range(ns - 1, -1, -1):
            i = step - j
            if 0 <= i < n:
                stages[j](i)


def phase_att(k, A, P, l, last):
    with k.phase():
        identb = P["identb"]
        EPSB = P["epsb"]
        attC = k.sb("attC", [128, 16, 64], F32)
        attS = k.sb("attS", [128, 16, 64], F32)
        k.dma("sp", lambda e: e.dma_start(out=attC[:], in_=A["attC"].rearrange("(t p) d -> p t d", p=128)), writes=[attC])
        k.dma("sp", lambda e: e.dma_start(out=attS[:], in_=A["attS"].rearrange("(t p) d -> p t d", p=128)), writes=[attS])
        gqk = k.sb("gqk", [128, 10, 64], F32)
        k.dma("sp", lambda e: e.dma_start(out=gqk[:, 0, :], in_=A["q_norm_g"][l, :].partition_broadcast(128)), writes=[gqk])
        k.dma("sp", lambda e: e.dma_start(out=gqk[:, 8, :], in_=A["k_norm_g"][l, :].partition_broadcast(128)), writes=[gqk])
        k.V(lambda e: e.tensor_scalar(out=gqk[:, 0, :], in0=gqk[:, 0, :], scalar1=0.125, scalar2=None, op0=ALU.mult),
            [gqk], [gqk])
        for h in range(1, 8):
            k.V(lambda e, h=h: e.tensor_copy(out=gqk[:, h, :], in_=gqk[:, 0, :]), [gqk], [gqk])
        k.V(lambda e: e.tensor_copy(out=gqk[:, 9, :], in_=gqk[:, 8, :]), [gqk], [gqk])
        sink = k.sb("sink", [128, 8], F32)
        k.dma("sp", lambda e: e.dma_start(out=sink[:], in_=A["att_sink"][l, :].partition_broadcast(128)), writes=[sink])
        k.A(lambda e: e.activation(out=sink[:], in_=sink[:], func=AF.Exp), [sink], [sink])
        maskb = k.sb("maskb", [128, 3, 128], BF16)
        k.V(lambda e: e.tensor_copy(out=maskb[:], in_=P["masks"][:]), [P["masks"]], [maskb])

        qT = k.sb("qT", [64, 8, NT * 128], BF16)
        kT = k.sb("kT", [64, 2, NT * 128], BF16)
        vx = k.sb("vx", [128, NT, 2, 65], BF16)
        k.V(lambda e: e.memset(vx[:], 1.0), [], [vx])
        NU, NQ = 7, 5
        uq = [k.sb("uq%d" % i, [128, 768], F32) for i in range(NU)]
        sq = [k.sb("sq%d" % i, [128, 640], F32) for i in range(2)]
        ssq = [k.sb("ssq%d" % i, [128, 10], F32) for i in range(4)]
        qn = [k.sb("qn%d" % i, [128, 640], F32) for i in range(NQ)]
        t1 = [k.sb("t1%d" % i, [128, 640], F32) for i in range(2)]
        t2 = [k.sb("t2%d" % i, [128, 640], F32) for i in range(2)]
        qb = [k.sb("qb%d" % i, [128, 640], BF16) for i in range(3)]
        pq = [k.ps("pq%d" % i, [64, 8, 128], BF16) for i in range(2)]
        pkk = k.ps("pkk", [64, 2, 128], BF16)
        psc = [k.ps("psc%d" % i, [128, 512], F32) for i in range(3)]
        po = [k.ps("po%d" % i, [128, 4, 65], F32) for i in range(2)]
        Pm = [k.sb("Pm%d" % i, [128, 5, 512], BF16) for i in range(3)]
        den = [k.sb("den%d" % i, [128, 4], F32) for i in range(2)]
        ao = [k.sb("ao%d" % i, [128, 512], BF16) for i in range(2)]
        cnt = {"sc": 0}

        for s in range(NS):
            def p0(i, s=s):
                u = uq[i % NU]
                k.dma("sp", lambda e: e.dma_start(out=u[:], in_=A["u_d"][s, i * 128:(i + 1) * 128, 0:768]),
                      reads=["u_d"], writes=[u])

            def p1(i):
                u, q2 = uq[i % NU], sq[i % 2]
                k.G(lambda e: e.tensor_tensor(out=q2[:], in0=u[:, 0:640], in1=u[:, 0:640], op=ALU.mult), [u], [q2])

            def p2(i):
                q2, sv = sq[i % 2], ssq[i % 4]
                k.V(lambda e: e.tensor_reduce(out=sv[:], in_=q2[:].rearrange("p (h d) -> p h d", d=64), axis=AX.X,
                                              op=ALU.add), [q2], [sv])

            def p3(i):
                sv = ssq[i % 4]
                k.A(lambda e: e.activation(out=sv[:], in_=sv[:], func=AF.Ln, scale=1.0 / 64, bias=EPSB[:, 0:1]), [sv, EPSB], [sv])
                k.A(lambda e: e.activation(out=sv[:], in_=sv[:], func=AF.Exp, scale=-0.5), [sv], [sv])

            def p4(i):
                u, sv, q = uq[i % NU], ssq[i % 4], qn[i % NQ]
                k.V(lambda e: e.tensor_tensor(out=q[:].rearrange("p (h d) -> p h d", d=64),
                                              in0=u[:, 0:640].rearrange("p (h d) -> p h d", d=64),
                                              in1=sv[:].unsqueeze(2).to_broadcast([128, 10, 64]), op=ALU.mult), [u, sv], [q])

            def p5(i):
                u, q = uq[i % NU], qn[i % NQ]
                k.G(lambda e: e.tensor_tensor(out=q[:], in0=q[:], in1=gqk[:].rearrange("p h d -> p (h d)"), op=ALU.mult),
                    [q, gqk], [q])
                k.G(lambda e: e.tensor_copy(out=vx[:, i, :, 0:64], in_=u[:, 640:768].rearrange("p (h d) -> p h d", d=64)),
                    [u], [vx])

            def p6(i):
                if i < 2:
                    return
                q, a1, a2 = qn[i % NQ], t1[i % 2], t2[i % 2]
                tl = i - 2
                qv = q[:].rearrange("p (h a x d) -> p h a x d", h=10, a=2, x=2, d=16)
                t1v = a1[:].rearrange("p (h a x d) -> p h a x d", h=10, a=2, x=2, d=16)
                t2v = a2[:].rearrange("p (h a x d) -> p h a x d", h=10, a=2, x=2, d=16)
                Cv = attC[:, tl, :].rearrange("p (a x d) -> p a x d", a=2, x=2, d=16)
                Sv = attS[:, tl, :].rearrange("p (a x d) -> p a x d", a=2, x=2, d=16)
                k.V(lambda e: e.tensor_tensor(
                    out=a1[:].rearrange("p (h d) -> p h d", d=64), in0=q[:].rearrange("p (h d) -> p h d", d=64),
                    in1=attC[:, tl, :].unsqueeze(1).to_broadcast([128, 10, 64]), op=ALU.mult), [q, attC], [a1])
                qv = q[:].rearrange("p (h a x d) -> p h a x d", h=10, a=2, x=2, d=16)
                t2v = a2[:].rearrange("p (h a x d) -> p h a x d", h=10, a=2, x=2, d=16)
                Sv = attS[:, tl, :].rearrange("p (a x d) -> p a x d", a=2, x=2, d=16)
                for a in range(2):
                    for x in range(2):
                        k.G(lambda e, a=a, x=x: e.tensor_tensor(
                            out=t2v[:, :, a, x, :], in0=qv[:, :, a, 1 - x, :],
                            in1=Sv[:, a, x, :].unsqueeze(1).to_broadcast([128, 10, 16]), op=ALU.mult), [q, attS], [a2])

            def p7(i):
                q, a1, a2, q_b = qn[i % NQ], t1[i % 2], t2[i % 2], qb[i % 3]
                if i >= 2:
                    k.V(lambda e: e.tensor_tensor(out=q_b[:], in0=a1[:], in1=a2[:], op=ALU.add), [a1, a2], [q_b])
                else:
                    k.V(lambda e: e.tensor_copy(out=q_b[:], in_=q[:]), [q], [q_b])

            def p8(i):
                q_b, p = qb[i % 3], pq[i % 2]
                for h in range(8):
                    k.T(lambda e, h=h: e.transpose(p[:, h, :], q_b[:, h * 64:(h + 1) * 64], identb[:]), [q_b, identb], [p])
                for h in range(2):
                    k.T(lambda e, h=h: e.transpose(pkk[:, h, :], q_b[:, (8 + h) * 64:(9 + h) * 64], identb[:]),
                        [q_b, identb], [pkk])

            def p9(i):
                p = pq[i % 2]
                k.A(lambda e: e.copy(out=qT[:, :, i * 128:(i + 1) * 128], in_=p[:]), [p], [("qT", i)])
                k.V(lambda e: e.tensor_copy(out=kT[:, :, i * 128:(i + 1) * 128], in_=pkk[:]), [pkk], [("kT", i)])

            run_pipe(NT, [p0, p1, p2, p3, p4, p5, p6, p7, p8, p9])

            qblocks = list(range(2, NT)) + ([] if last else [0, 1])
            items = [(t, kh) for t in qblocks for kh in range(2)]

            def kblocks(t):
                if t >= 2:
                    kbl = []
                    if t - 1 >= 2:
                        kbl.append((t - 1, 1))
                    kbl.append((t, None))
                    if t + 1 < NT:
                        kbl.append((t + 1, 0))
                    return kbl + [(0, None), (1, None)]
                return [(0, None), (1, None)]

            def b0(i):
                t, kh = items[i]
                Pt = Pm[i % 3]
                for bj, (kb_, mk) in enumerate(kblocks(t)):
                    ps = psc[cnt["sc"] % 3]
                    cnt["sc"] += 1
                    k.T(lambda e, ps=ps, kb_=kb_: e.matmul(
                        ps[:], lhsT=kT[:, kh, kb_ * 128:(kb_ + 1) * 128],
                        rhs=qT[:, kh * 4:(kh + 1) * 4, t * 128:(t + 1) * 128], start=True, stop=True),
                        [("kT", kb_), ("qT", t)], [ps])
                    k.A(lambda e, ps=ps, bj=bj: e.activation(out=Pt[:, bj, :], in_=ps[:], func=AF.Exp), [ps], [(Pt.name, bj)])
                    if mk is not None:
                        k.G(lambda e, bj=bj, mk=mk: e.tensor_tensor(
                            out=Pt[:, bj, :].rearrange("p (g q) -> p g q", g=4),
                            in0=Pt[:, bj, :].rearrange("p (g q) -> p g q", g=4),
                            in1=maskb[:, mk, :].unsqueeze(1).to_broadcast([128, 4, 128]), op=ALU.mult),
                            [(Pt.name, bj), maskb], [(Pt.name, bj)])

            def b1(i):
                t, kh = items[i]
                Pt = Pm[i % 3]
                pso = po[i % 2]
                kbl = kblocks(t)
                nb = len(kbl)
                for g in range(4):
                    for bj, (kb_, mk) in enumerate(kbl):
                        k.T(lambda e, g=g, bj=bj, kb_=kb_: e.matmul(
                            pso[:, g, :], lhsT=Pt[:, bj, g * 128:(g + 1) * 128], rhs=vx[:, kb_, kh, :],
                            start=(bj == 0), stop=(bj == nb - 1)), [(Pt.name, bj), vx], [pso])

            def b2(i, s=s):
                t, kh = items[i]
                pso = po[i % 2]
                dn = den[i % 2]
                a_o = ao[(i // 2) % 2]
                k.V(lambda e: e.tensor_tensor(out=dn[:], in0=pso[:, :, 64], in1=sink[:, kh * 4:(kh + 1) * 4], op=ALU.add),
                    [pso, sink], [dn])
                k.V(lambda e: e.reciprocal(out=dn[:], in_=dn[:]), [dn], [dn])
                k.V(lambda e: e.tensor_tensor(
                    out=a_o[:, kh * 256:(kh + 1) * 256].rearrange("p (g d) -> p g d", g=4),
                    in0=pso[:, :, 0:64], in1=dn[:].unsqueeze(2).to_broadcast([128, 4, 64]), op=ALU.mult), [pso, dn], [a_o])
                if kh == 1:
                    k.dma("act", lambda e: e.dma_start(out=A["mixin_d"][s, t * 128:(t + 1) * 128, 0:512], in_=a_o[:]),
                          reads=[a_o], writes=["mixin_d"])

            run_pipe(len(items), [b0, b1, b2])


def phase_ret(k, A, P, l, last):
    with k.phase():
        identb = P["identb"]
        retC = k.sb("retC", [128, NT, 64], F32)
        retS = k.sb("retS", [128, NT, 64], F32)
        k.dma("sp", lambda e: e.dma_start(out=retC[:], in_=A["retC"].rearrange("(t p) d -> p t d", p=128)), writes=[retC])
        k.dma("sp", lambda e: e.dma_start(out=retS[:], in_=A["retS"].rearrange("(t p) d -> p t d", p=128)), writes=[retS])
        lg = k.sb("lg", [128, 8], F32)
        k.dma("sp", lambda e: e.dma_start(out=lg[:], in_=A["ret_decay_logit"][l, :].partition_broadcast(128)), writes=[lg])
        k.A(lambda e: e.activation(out=lg[:], in_=lg[:], func=AF.Exp, scale=-1.0), [lg], [lg])
        k.A(lambda e: e.activation(out=lg[:], in_=lg[:], func=AF.Ln, bias=1.0), [lg], [lg])
        k.V(lambda e: e.tensor_scalar(out=lg[:], in0=lg[:], scalar1=-1.0, scalar2=None, op0=ALU.mult), [lg], [lg])
        dco = k.sb("dco", [128, 4], F32)
        k.dma("sp", lambda e: e.dma_start(out=dco[:], in_=A["dcoef"]), writes=[dco])
        DT = k.sb("DT", [128, 2, 2, 4], F32)
        for d_ in range(2):
            for qk in range(2):
                k.A(lambda e, d_=d_, qk=qk: e.activation(out=DT[:, d_, qk, :], in_=lg[:, d_ * 4:(d_ + 1) * 4], func=AF.Exp,
                                                         scale=dco[:, d_ * 2 + qk: d_ * 2 + qk + 1]), [lg, dco], [DT])
            k.V(lambda e, d_=d_: e.tensor_scalar(out=DT[:, d_, 1, :], in0=DT[:, d_, 1, :], scalar1=0.125, scalar2=None,
                                                 op0=ALU.mult), [DT], [DT])
        g128 = k.sb("g128", [128, 2, 2], F32)
        for d_ in range(2):
            for pr in range(2):
                for hh in range(2):
                    h = 2 * pr + hh
                    k.A(lambda e, d_=d_, pr=pr, hh=hh, h=h: e.activation(
                        out=g128[hh * 64:(hh + 1) * 64, d_, pr:pr + 1], in_=lg[hh * 64:(hh + 1) * 64, d_ * 4 + h: d_ * 4 + h + 1],
                        func=AF.Exp, scale=128.0), [lg], [g128])
        gn = k.sb("gn", [128, 256], F32)
        k.dma("sp", lambda e: e.dma_start(out=gn[:], in_=A["ret_norm_g"][l, :].partition_broadcast(128)), writes=[gn])
        maskb = k.sb("maskb", [128, 3, 128], BF16)
        k.V(lambda e: e.tensor_copy(out=maskb[:], in_=P["masks"][:]), [P["masks"]], [maskb])

        EPSB = P["epsb"]
        RT = k.sb("RT", [128, 2, 2, 2, NT * 128], BF16)
        KD = k.sb("KD", [128, NT, 2, 256], BF16)
        Vb = k.sb("Vb", [128, NT, 256], BF16)
        SG = k.sb("SG", [128, NT, 256], BF16)
        OA = k.sb("OA", [128, NT, 256], F32)
        ur = [k.sb("ur%d" % i, [128, 1024], F32) for i in range(3)]
        t1 = [k.sb("t1%d" % i, [128, 512], F32) for i in range(2)]
        t2 = [k.sb("t2%d" % i, [128, 512], F32) for i in range(2)]
        rot = [k.sb("rot%d" % i, [128, 512], F32) for i in range(3)]
        vr = [k.sb("vr%d" % i, [128, 2, 2, 256], BF16) for i in range(4)]
        ptr = [k.ps("ptr%d" % i, [128, 8, 128], BF16) for i in range(1)]
        pssA = [k.ps("pssA%d" % i, [128, 512], F32) for i in range(2)]
        pssB = [k.ps("pssB%d" % i, [128, 512], F32) for i in range(2)]
        pso = [k.ps("pso%d" % i, [128, 4, 64], F32) for i in range(2)]
        pkvt = k.ps("pkvt", [128, 2, 2, 128], F32)
        STm = [k.sb("STm%d" % i, [128, 4, 128], BF16) for i in range(2)]
        Sst = [k.sb("Sst%d" % i, [128, 2, 128], F32) for i in range(2)]
        Sb = [k.sb("Sb%d" % i, [128, 2, 128], BF16) for i in range(2)]
        tmp = [k.sb("tmp%d" % i, [128, 2, 128], F32) for i in range(2)]
        osum = [k.sb("osum%d" % i, [128, 256], F32) for i in range(3)]
        mean = [k.sb("mean%d" % i, [128, 4], F32) for i in range(3)]
        cen = [k.sb("cen%d" % i, [128, 256], F32) for i in range(4)]
        sq = [k.sb("sq%d" % i, [128, 256], F32) for i in range(2)]
        var = [k.sb("var%d" % i, [128, 4], F32) for i in range(3)]
        rof = [k.sb("rof%d" % i, [128, 256], F32) for i in range(3)]
        ro = [k.sb("ro%d" % i, [128, 256], BF16) for i in range(2)]

        for s in range(NS):
            def r0(i, s=s):
                u = ur[i % 3]
                k.dma("sp", lambda e: e.dma_start(out=u[:], in_=A["u_d"][s, i * 128:(i + 1) * 128, 768:1792]),
                      reads=["u_d"], writes=[u])

            def r1(i):
                u, a1, a2 = ur[i % 3], t1[i % 2], t2[i % 2]
                qk_v = u[:, 0:512].rearrange("p (h x d) -> p h x d", h=8, x=2, d=32)
                t1v = a1[:].rearrange("p (h x d) -> p h x d", h=8, x=2, d=32)
                t2v = a2[:].rearrange("p (h x d) -> p h x d", h=8, x=2, d=32)
                Cv = retC[:, i, :].rearrange("p (x d) -> p x d", x=2, d=32)
                Sv = retS[:, i, :].rearrange("p (x d) -> p x d", x=2, d=32)
                for x in range(2):
                    k.V(lambda e, x=x: e.tensor_tensor(
                        out=t1v[:, :, x, :], in0=qk_v[:, :, x, :], in1=Cv[:, x, :].unsqueeze(1).to_broadcast([128, 8, 32]),
                        op=ALU.mult), [u, retC], [a1])
                    k.G(lambda e, x=x: e.tensor_tensor(
                        out=t2v[:, :, x, :], in0=qk_v[:, :, 1 - x, :], in1=Sv[:, x, :].unsqueeze(1).to_broadcast([128, 8, 32]),
                        op=ALU.mult), [u, retS], [a2])
                k.V(lambda e: e.tensor_copy(out=Vb[:, i, :], in_=u[:, 512:768]), [u], [("Vb", i)])
                k.A(lambda e: e.activation(out=SG[:, i, :], in_=u[:, 768:1024], func=AF.Silu), [u], [("SG", i)])

            def r2(i):
                a1, a2, rt_ = t1[i % 2], t2[i % 2], rot[i % 3]
                k.V(lambda e: e.tensor_tensor(out=rt_[:], in0=a1[:], in1=a2[:], op=ALU.add), [a1, a2], [rt_])

            def r3(i):
                rt_, v_r = rot[i % 3], vr[i % 4]
                for d_ in range(2):
                    eng = k.V if d_ == 0 else k.G
                    eng(lambda e, d_=d_: e.tensor_tensor(
                        out=v_r[:, d_, :, :].rearrange("p q (h d) -> p (q h) d", d=64),
                        in0=rt_[:].rearrange("p (qh d) -> p qh d", d=64),
                        in1=DT[:, d_, :, :].rearrange("p q h -> p (q h)").unsqueeze(2).to_broadcast([128, 8, 64]),
                        op=ALU.mult), [rt_, DT], [(v_r.name, d_)])

            def r4(i):
                v_r, p = vr[i % 4], ptr[0]
                for d_ in range(2):
                    k.G(lambda e, d_=d_: e.tensor_copy(out=KD[:, i, d_, :], in_=v_r[:, d_, 1, :]), [(v_r.name, d_)], [("KD", i)])
                for d_ in range(2):
                    for qk in range(2):
                        for pr in range(2):
                            k.T(lambda e, d_=d_, qk=qk, pr=pr: e.transpose(
                                p[:, d_ * 4 + qk * 2 + pr, :], v_r[:, d_, qk, pr * 128:(pr + 1) * 128], identb[:]),
                                [(v_r.name, d_), identb], [p])

            def r5(i):
                p = ptr[0]
                k.A(lambda e: e.copy(out=RT[:, :, :, :, i * 128:(i + 1) * 128].rearrange("p a b c t -> p (a b c) t"),
                                     in_=p[:]), [p], [("RT", i)])

            run_pipe(NT, [r0, r1, r2, r3, r4, r5])

            orders = [list(range(NT)), [1, 0] + list(range(NT - 1, 1, -1))]
            for d_ in range(2):
                k.V(lambda e, d_=d_: e.memset(Sst[d_][:], 0.0), [], [Sst[d_]])
                k.V(lambda e, d_=d_: e.memset(Sb[d_][:], 0.0), [], [Sb[d_]])
            oak = [("OA", t_) for t_ in range(NT)]
            k.G(lambda e: e.memset(OA[:], 0.0), oak, oak)
            for ci in range(NT):
                tt = [orders[0][ci], orders[1][ci]]
                for d_ in range(2):
                    t = tt[d_]
                    for h in (0, 2, 1, 3):
                        pr, hh = h // 2, h % 2
                        pss = pssA[d_] if hh == 0 else pssB[d_]
                        k.T(lambda e, pss=pss, pr=pr, hh=hh, t=t, d_=d_: e.matmul(
                            pss[:, pr * 128:(pr + 1) * 128], lhsT=RT[hh * 64:(hh + 1) * 64, d_, 1, pr, t * 128:(t + 1) * 128],
                            rhs=RT[hh * 64:(hh + 1) * 64, d_, 0, pr, t * 128:(t + 1) * 128], start=True, stop=True),
                            [("RT", t)], [pss])
                for d_ in range(2):
                    mk = 0 if d_ == 0 else 2
                    stm = STm[d_]
                    sv = stm[:].rearrange("p (pr hh) j -> p pr hh j", hh=2)
                    for hh, pss in ((0, pssA[d_]), (1, pssB[d_])):
                        k.V(lambda e, hh=hh, pss=pss, mk=mk, sv=sv: e.tensor_tensor(
                            out=sv[:, :, hh, :], in0=pss[:, 0:256].rearrange("p (pr j) -> p pr j", pr=2),
                            in1=maskb[:, mk, :].unsqueeze(1).to_broadcast([128, 2, 128]), op=ALU.mult),
                            [pss, maskb], [(stm.name, hh)])
                for d_ in range(2):
                    t = tt[d_]
                    stm, po_, Sbb = STm[d_], pso[d_], Sb[d_]
                    for h in range(4):
                        pr, hh = h // 2, h % 2
                        k.T(lambda e, h=h, t=t, stm=stm, po_=po_: e.matmul(
                            po_[:, h, :], lhsT=stm[:, h, :], rhs=Vb[:, t, h * 64:(h + 1) * 64], start=True, stop=False),
                            [(stm.name, hh), ("Vb", t)], [po_])
                        k.T(lambda e, h=h, pr=pr, hh=hh, t=t, d_=d_, Sbb=Sbb, po_=po_: e.matmul(
                            po_[:, h, :], lhsT=RT[hh * 64:(hh + 1) * 64, d_, 0, pr, t * 128:(t + 1) * 128],
                            rhs=Sbb[hh * 64:(hh + 1) * 64, pr, hh * 64:(hh + 1) * 64], start=False, stop=True),
                            [("RT", t), Sbb], [po_])
                    if ci < NT - 1:
                        for pr in range(2):
                            k.T(lambda e, pr=pr, t=t, d_=d_: e.matmul(
                                pkvt[:, d_, pr, :], lhsT=KD[:, t, d_, pr * 128:(pr + 1) * 128], rhs=Vb[:, t, pr * 128:(pr + 1) * 128],
                                start=True, stop=True), [("KD", t), ("Vb", t)], [("pkv", d_)])
                for d_ in range(2):
                    t = tt[d_]
                    po_, St, Sbb, tm = pso[d_], Sst[d_], Sb[d_], tmp[d_]
                    k.V(lambda e, po_=po_, t=t: e.tensor_tensor(out=OA[:, t, :], in0=OA[:, t, :],
                                                                in1=po_[:].rearrange("p h d -> p (h d)"), op=ALU.add),
                        [po_, ("OA", t)], [("OA", t)])
                    if ci < NT - 1:
                        k.V(lambda e, St=St, tm=tm, d_=d_: e.tensor_tensor(out=tm[:], in0=pkvt[:, d_, :, :], in1=St[:], op=ALU.add),
                            [("pkv", d_), St], [tm])
                        for pr in range(2):
                            k.V(lambda e, pr=pr, St=St, d_=d_, tm=tm: e.tensor_scalar(
                                out=St[:, pr, :], in0=tm[:, pr, :], scalar1=g128[:, d_, pr:pr + 1], scalar2=None,
                                op0=ALU.mult), [tm, g128], [St])
                            k.A(lambda e, pr=pr, Sbb=Sbb, d_=d_, tm=tm: e.activation(
                                out=Sbb[:, pr, :], in_=tm[:, pr, :], func=AF.Copy, scale=g128[:, d_, pr:pr + 1]),
                                [tm, g128], [Sbb])

            tiles = list(range(2, NT)) + ([] if last else [0, 1])

            def g0(i):
                t = tiles[i]
                os_, mn = osum[i % 3], mean[i % 3]
                k.G(lambda e: e.tensor_copy(out=os_[:], in_=OA[:, t, :]), [("OA", t)], [os_])
                k.V(lambda e: e.tensor_reduce(out=mn[:], in_=os_[:].rearrange("p (h d) -> p h d", d=64), axis=AX.X,
                                              op=ALU.add), [os_], [mn])
                k.V(lambda e: e.tensor_scalar(out=mn[:], in0=mn[:], scalar1=1.0 / 64, scalar2=None, op0=ALU.mult), [mn], [mn])

            def g1(i):
                os_, mn, cn, s2 = osum[i % 3], mean[i % 3], cen[i % 4], sq[i % 2]
                k.V(lambda e: e.tensor_tensor(out=cn[:].rearrange("p (h d) -> p h d", d=64),
                                              in0=os_[:].rearrange("p (h d) -> p h d", d=64),
                                              in1=mn[:].unsqueeze(2).to_broadcast([128, 4, 64]), op=ALU.subtract),
                    [os_, mn], [cn])
                k.G(lambda e: e.tensor_tensor(out=s2[:], in0=cn[:], in1=cn[:], op=ALU.mult), [cn], [s2])

            def g2(i):
                s2, vv = sq[i % 2], var[i % 3]
                k.V(lambda e: e.tensor_reduce(out=vv[:], in_=s2[:].rearrange("p (h d) -> p h d", d=64), axis=AX.X,
                                              op=ALU.add), [s2], [vv])
                k.A(lambda e: e.activation(out=vv[:], in_=vv[:], func=AF.Ln, scale=1.0 / 64, bias=EPSB[:, 0:1]), [vv, EPSB], [vv])
                k.A(lambda e: e.activation(out=vv[:], in_=vv[:], func=AF.Exp, scale=-0.5), [vv], [vv])

            def g3(i):
                cn, vv, rf = cen[i % 4], var[i % 3], rof[i % 3]
                k.V(lambda e: e.tensor_tensor(out=rf[:].rearrange("p (h d) -> p h d", d=64),
                                              in0=cn[:].rearrange("p (h d) -> p h d", d=64),
                                              in1=vv[:].unsqueeze(2).to_broadcast([128, 4, 64]), op=ALU.mult), [cn, vv], [rf])
                k.G(lambda e: e.tensor_tensor(out=rf[:], in0=rf[:], in1=gn[:], op=ALU.mult), [rf, gn], [rf])

            def g4(i, s=s):
                t = tiles[i]
                rf, r_o = rof[i % 3], ro[i % 2]
                k.V(lambda e: e.tensor_tensor(out=r_o[:], in0=rf[:], in1=SG[:, t, :], op=ALU.mult), [rf, ("SG", t)], [r_o])
                k.dma("act", lambda e: e.dma_start(out=A["mixin_d"][s, t * 128:(t + 1) * 128, 512:768], in_=r_o[:]),
                      reads=[r_o], writes=["mixin_d"])

            run_pipe(len(tiles), [g0, g1, g2, g3, g4])


def phase_conv(k, A, P, l, last):
    with k.phase():
        ident = P["ident"]
        EPSB = P["epsb"]
        cwT = k.sb("cwT", [128, 2, 32], F32)
        vT = k.sb("vT", [128, 2, 4], F32)
        with k.phase():
            cw = k.sb("cw", [31, 256], F32)
            k.dma("sp", lambda e: e.dma_start(out=cw[:], in_=A["conv_w"][l]), writes=[cw])
            pw = k.ps("pw", [128, 2, 32], F32)
            for cc in range(2):
                k.T(lambda e, cc=cc: e.transpose(pw[:, cc, 0:31], cw[0:31, cc * 128:(cc + 1) * 128], ident[0:31, 0:31]),
                    [cw, ident], [pw])
            k.V(lambda e: e.tensor_copy(out=cwT[:, :, 0:31], in_=pw[:, :, 0:31]), [pw], [cwT])
            vec = k.sb("vec", [3, 256], F32)
            k.dma("sp", lambda e: e.dma_start(out=vec[0:1, :], in_=A["conv_b"][l:l + 1, :]), writes=[vec])
            k.dma("sp", lambda e: e.dma_start(out=vec[1:2, :], in_=A["conv_norm_g"][l:l + 1, :]), writes=[vec])
            k.dma("sp", lambda e: e.dma_start(out=vec[2:3, :], in_=A["conv_norm_b"][l:l + 1, :]), writes=[vec])
            pv = k.ps("pv", [128, 2, 4], F32)
            for cc in range(2):
                k.T(lambda e, cc=cc: e.transpose(pv[:, cc, 0:3], vec[0:3, cc * 128:(cc + 1) * 128], ident[0:3, 0:3]),
                    [vec, ident], [pv])
            k.V(lambda e: e.tensor_copy(out=vT[:, :, 0:3], in_=pv[:, :, 0:3]), [pv], [vT])
        diag = k.sb("diag", [128, 2, 31, 128], BF16)
        for cc in range(2):
            for kk in range(31):
                eng = k.V if kk % 2 == 0 else k.G
                eng(lambda e, cc=cc, kk=kk: e.tensor_scalar(out=diag[:, cc, kk, :], in0=ident[:],
                                                            scalar1=cwT[:, cc, kk:kk + 1], scalar2=None, op0=ALU.mult),
                    [ident, cwT], [diag])
        ones = k.sb("ones", [128, 128], F32R)
        onesf = k.sb("onesf", [128, 128], F32)
        k.V(lambda e: e.memset(onesf[:], 1.0 / 256), [], [onesf])
        k.V(lambda e: e.tensor_copy(out=ones[:], in_=onesf[:]), [onesf], [ones])

        LP = S + 30
        hp = [[k.sb("hp%d_%d" % (j, cc), [128, LP], BF16) for cc in range(2)] for j in range(2)]
        val = [[k.sb("val%d_%d" % (j, cc), [128, S], F32) for cc in range(2)] for j in range(2)]
        gt = [[k.sb("gt%d_%d" % (j, cc), [128, S], F32) for cc in range(2)] for j in range(2)]
        cvo = [[k.sb("cvo%d_%d" % (j, cc), [128, 512], F32R) for cc in range(2)] for j in range(5)]
        csq = [[k.sb("csq%d_%d" % (j, cc), [128, 512], F32R) for cc in range(2)] for j in range(2)]
        pcv = [[k.ps("pcv%d_%d" % (j, cc), [128, 512], F32) for cc in range(2)] for j in range(2)]
        pmean = [k.ps("pmean%d" % j, [128, 512], F32) for j in range(2)]
        pex2 = [k.ps("pex2%d" % j, [128, 512], F32) for j in range(2)]
        msb = [k.sb("msb%d" % j, [128, 512], F32) for j in range(3)]
        rsd = [k.sb("rsd%d" % j, [128, 512], F32) for j in range(3)]
        yv = [[k.sb("yv%d_%d" % (j, cc), [128, 512], F32) for cc in range(2)] for j in range(2)]
        yo = [[k.sb("yo%d_%d" % (j, cc), [128, 512], BF16) for cc in range(2)] for j in range(2)]
        segs = []
        for s in range(NS):
            segs.append((s, C, S))
            if not last:
                segs.append((s, 0, C))
        items = []
        for si, (s, tok0, Lg) in enumerate(segs):
            nb = (Lg + 511) // 512
            for b in range(nb):
                items.append((si, s, tok0, Lg, b, min(512, Lg - b * 512)))

        def pre(i):
            si, s, tok0, Lg, b, w = items[i]
            if b != 0:
                return
            j = si % 2
            for cc in range(2):
                k.dma("sp", lambda e, cc=cc: e.dma_start(
                    out=val[j][cc][:, 0:Lg], in_=A["cv_d"][s, cc * 128:(cc + 1) * 128, tok0:tok0 + Lg]),
                    reads=["cv_d"], writes=[val[j][cc]])
                k.dma("act", lambda e, cc=cc: e.dma_start(
                    out=gt[j][cc][:, 0:Lg], in_=A["cv_d"][s, 256 + cc * 128: 256 + (cc + 1) * 128, tok0:tok0 + Lg]),
                    reads=["cv_d"], writes=[gt[j][cc]])
                k.A(lambda e, cc=cc: e.activation(out=gt[j][cc][:, 0:Lg], in_=gt[j][cc][:, 0:Lg], func=AF.Sigmoid),
                    [gt[j][cc]], [gt[j][cc]])
                k.G(lambda e, cc=cc: e.memset(hp[j][cc][:], 0.0), [], [hp[j][cc]])
                k.V(lambda e, cc=cc: e.tensor_tensor(out=hp[j][cc][:, 15:15 + Lg], in0=val[j][cc][:, 0:Lg],
                                                     in1=gt[j][cc][:, 0:Lg], op=ALU.mult),
                    [val[j][cc], gt[j][cc]], [hp[j][cc]])

        def nop(i):
            return

        def c0(i):
            si, s, tok0, Lg, b, w = items[i]
            j = si % 2
            for cc in range(2):
                p = pcv[i % 2][cc]
                for kk in range(31):
                    k.T(lambda e, p=p, cc=cc, kk=kk: e.matmul(
                        p[:, 0:w], lhsT=diag[:, cc, kk, :], rhs=hp[j][cc][:, b * 512 + kk: b * 512 + kk + w],
                        start=(kk == 0), stop=(kk == 30)), [diag, hp[j][cc]], [p])

        def c1(i):
            si, s, tok0, Lg, b, w = items[i]
            for cc in range(2):
                p, co, cq = pcv[i % 2][cc], cvo[i % 5][cc], csq[i % 2][cc]
                k.A(lambda e, p=p, co=co, cc=cc: e.activation(out=co[:, 0:w], in_=p[:, 0:w], func=AF.Identity,
                                                             bias=vT[:, cc, 0:1], scale=1.0), [p, vT], [co])
                k.V(lambda e, co=co, cq=cq: e.tensor_tensor(out=cq[:, 0:w], in0=co[:, 0:w].bitcast(F32),
                                                            in1=co[:, 0:w].bitcast(F32), op=ALU.mult), [co], [cq])

        def c2(i):
            si, s, tok0, Lg, b, w = items[i]
            pm_, px_ = pmean[i % 2], pex2[i % 2]
            for cc in range(2):
                k.T(lambda e, cc=cc: e.matmul(pm_[:, 0:w], lhsT=ones[:], rhs=cvo[i % 5][cc][:, 0:w],
                                              start=(cc == 0), stop=(cc == 1)), [ones, cvo[i % 5][cc]], [pm_])
            for cc in range(2):
                k.T(lambda e, cc=cc: e.matmul(px_[:, 0:w], lhsT=ones[:], rhs=csq[i % 2][cc][:, 0:w],
                                              start=(cc == 0), stop=(cc == 1)), [ones, csq[i % 2][cc]], [px_])

        def c3(i):
            si, s, tok0, Lg, b, w = items[i]
            pm_, px_, ms, rs = pmean[i % 2], pex2[i % 2], msb[i % 3], rsd[i % 3]
            k.A(lambda e: e.copy(out=ms[:, 0:w], in_=pm_[:, 0:w]), [pm_], [ms])
            k.V(lambda e: e.tensor_tensor(out=rs[:, 0:w], in0=ms[:, 0:w], in1=ms[:, 0:w], op=ALU.mult), [ms], [rs])
            k.V(lambda e: e.tensor_tensor(out=rs[:, 0:w], in0=px_[:, 0:w], in1=rs[:, 0:w], op=ALU.subtract), [px_, rs], [rs])

        def c4(i):
            si, s, tok0, Lg, b, w = items[i]
            rs = rsd[i % 3]
            k.A(lambda e: e.activation(out=rs[:, 0:w], in_=rs[:, 0:w], func=AF.Ln, bias=EPSB[:, 0:1], scale=1.0), [rs, EPSB], [rs])
            k.A(lambda e: e.activation(out=rs[:, 0:w], in_=rs[:, 0:w], func=AF.Exp, scale=-0.5), [rs], [rs])

        def c5(i):
            si, s, tok0, Lg, b, w = items[i]
            ms, rs = msb[i % 3], rsd[i % 3]
            for cc in range(2):
                co, y = cvo[i % 5][cc], yv[i % 2][cc]
                k.G(lambda e, co=co, y=y: e.tensor_tensor(out=y[:, 0:w], in0=co[:, 0:w].bitcast(F32), in1=ms[:, 0:w],
                                                          op=ALU.subtract), [co, ms], [y])
                k.V(lambda e, y=y: e.tensor_tensor(out=y[:, 0:w], in0=y[:, 0:w], in1=rs[:, 0:w], op=ALU.mult), [y, rs], [y])

        def c6(i):
            si, s, tok0, Lg, b, w = items[i]
            for cc in range(2):
                y, o_ = yv[i % 2][cc], yo[i % 2][cc]
                k.A(lambda e, y=y, o_=o_, cc=cc: e.activation(out=o_[:, 0:w], in_=y[:, 0:w], func=AF.Silu,
                                                             scale=vT[:, cc, 1:2], bias=vT[:, cc, 2:3]), [y, vT], [o_])
                k.dma("act", lambda e, cc=cc, o_=o_: e.dma_start(
                    out=A["convT_d"][s, cc * 128:(cc + 1) * 128, tok0 + b * 512: tok0 + b * 512 + w],
                    in_=o_[:, 0:w]), reads=[o_], writes=["convT_d"])

        run_pipe(len(items), [pre, nop, c0, c1, c2, c3, c4, c5, c6])


def phase_out(k, A, P, l, last, src_lat, src_ctx, dst_lat):
    with k.phase():
        ident = P["ident"]
        identb = P["identb"]
        EPSB = P["epsb"]
        wout = k.sb("wout", [128, 8, D], BF16)
        wv = A["w_out"][l].rearrange("(kc p) f -> p kc f", p=128)
        for j in range(0, D, 512):
            k.dma("pool", lambda e, j=j: e.dma_start(out=wout[:, :, j:j + 512], in_=wv[:, :, j:j + 512]), writes=[("wout", j)])
        woutk = [("wout", 0), ("wout", 512)]
        wr = k.sb("wr", [128, 8, E], F32)
        k.dma("sp", lambda e: e.dma_start(out=wr[:], in_=A["w_router"][l].rearrange("(kc p) e -> p kc e", p=128)),
              writes=[wr])
        nr = NS + (0 if last else 1)
        gta = [k.sb("gta%d" % r, [128, D], F32) for r in range(nr)]
        gsf = [k.sb("gsf%d" % r, [128, D], F32) for r in range(nr)]
        shf = [k.sb("shf%d" % r, [128, D], F32) for r in range(nr)]
        gf = k.sb("gf", [128, D], F32)
        k.dma("sp", lambda e: e.dma_start(out=gf[:], in_=A["g_ffn"][l, :].partition_broadcast(128)), writes=[gf])
        for r in range(nr):
            k.dma("sp", lambda e, r=r: e.dma_start(out=gta[r][:], in_=A["mod_d"][l, r, 2 * D:3 * D].partition_broadcast(128)),
                  reads=["mod_d"], writes=[gta[r]])
            k.dma("sp", lambda e, r=r: e.dma_start(out=shf[r][:], in_=A["mod_d"][l, r, 3 * D:4 * D].partition_broadcast(128)),
                  reads=["mod_d"], writes=[shf[r]])
            k.dma("sp", lambda e, r=r: e.dma_start(out=gsf[r][:], in_=A["mod_d"][l, r, 4 * D:5 * D].partition_broadcast(128)),
                  reads=["mod_d"], writes=[gsf[r]])
            k.V(lambda e, r=r: e.scalar_tensor_tensor(out=gsf[r][:], in0=gsf[r][:], scalar=1.0, in1=gf[:], op0=ALU.add,
                                                      op1=ALU.mult), [gsf[r], gf], [gsf[r]])
        NB = 6
        mi = [k.sb("mi%d" % i, [128, 768], BF16) for i in range(3)]
        mixT = [k.sb("mixT%d" % i, [128, 8, 128], BF16) for i in range(4)]
        xt = [k.sb("xt%d" % i, [128, D], F32) for i in range(5)]
        xm = [k.sb("xm%d" % i, [128, D], F32) for i in range(5)]
        hf = [k.sb("hf%d" % i, [128, D], F32) for i in range(4)]
        hfb = [k.sb("hfb%d" % i, [128, D], BF16) for i in range(2)]
        junk = k.sb("junk", [128, D], BF16)
        ss = [k.sb("ss%d" % i, [128, 1], F32) for i in range(NB)]
        rstd = [k.sb("rstd%d" % i, [128, 1], F32) for i in range(NB)]
        hfT = [k.sb("hfT%d" % i, [128, 8, 128], F32) for i in range(2)]
        ptm = k.ps("ptm", [128, 6, 128], BF16)
        py = [k.ps("py%d" % i, [128, 512], F32) for i in range(2)]
        pth = k.ps("pth", [128, 8, 128], F32)
        plg = k.ps("plg", [128, 16], F32)
        pat = k.ps("pat", [48, 128], F32)
        lmax = [k.sb("lmax%d" % i, [128, 1], F32) for i in range(2)]
        lsum = [k.sb("lsum%d" % i, [128, 1], F32) for i in range(2)]
        afftm = [k.sb("afftm%d" % i, [128, 48], F32) for i in range(3)]
        affT = k.sb("affT", [48, S + C], F32)
        for a_ in afftm:
            k.V(lambda e, a_=a_: e.memset(a_[:], 0.0), [], [a_])
        tiles = list(range(2, NT)) + ([] if last else [0, 1])
        items = [(t, s) for t in tiles for s in range(NS)]

        def info(i):
            t, s = items[i]
            isctx = t < 2
            r = NS if isctx else s
            if isctx:
                src = src_ctx[s * C + t * 128: s * C + (t + 1) * 128, :]
                dst = A["res_d"][NLAT + s * C + t * 128: NLAT + s * C + (t + 1) * 128, :]
                hrow = NLAT + s * C + t * 128
            else:
                src = src_lat[s * S + (t - 2) * 128: s * S + (t - 1) * 128, :]
                dst = dst_lat[s * S + (t - 2) * 128: s * S + (t - 1) * 128, :]
                hrow = s * S + (t - 2) * 128
            return t, s, r, src, dst, hrow

        NX, NM, NH = 5, 4, 4

        def stL(i):
            t, s, r, src, dst, hrow = info(i)
            m, mt, x_t = mi[i % 3], mixT[i % NM], xt[i % NX]
            k.dma("sp", lambda e: e.dma_start(out=m[:], in_=A["mixin_d"][s, t * 128:(t + 1) * 128, :]),
                  reads=["mixin_d"], writes=[m])
            k.dma("sp", lambda e: e.dma_start(
                out=mt[:, 6:8, :], in_=A["convT_d"][s, :, t * 128:(t + 1) * 128].rearrange("(c p) t -> p c t", p=128)),
                reads=["convT_d"], writes=[mt])
            k.dma("act", lambda e: e.dma_start(out=x_t[:], in_=src), writes=[x_t])

        def stA1(i):
            m, mt = mi[i % 3], mixT[i % NM]
            for j in range(6):
                k.T(lambda e, j=j: e.transpose(ptm[:, j, :], m[:, j * 128:(j + 1) * 128], identb[:]), [m, identb], [ptm])
            k.A(lambda e: e.copy(out=mt[:, 0:6, :], in_=ptm[:]), [ptm], [mt])

        def stA2(i):
            t, s, r, src, dst, hrow = info(i)
            mt, x_m = mixT[i % NM], xm[i % NX]
            for hh in range(2):
                p = py[hh]
                for kc in range(8):
                    k.T(lambda e, p=p, kc=kc, hh=hh: e.matmul(p[:], lhsT=mt[:, kc, :], rhs=wout[:, kc, hh * 512:(hh + 1) * 512],
                                                              start=(kc == 0), stop=(kc == 7)), [mt] + woutk, [p])
                k.V(lambda e, p=p, hh=hh: e.tensor_tensor(out=x_m[:, hh * 512:(hh + 1) * 512], in0=p[:],
                                                          in1=gta[r][:, hh * 512:(hh + 1) * 512], op=ALU.mult),
                    [p, gta[r]], [x_m])

        def stA3(i):
            t, s, r, src, dst, hrow = info(i)
            x_t, x_m = xt[i % NX], xm[i % NX]
            k.G(lambda e: e.tensor_tensor(out=x_m[:], in0=x_m[:], in1=x_t[:], op=ALU.add), [x_m, x_t], [x_m])
            k.dma("sp", lambda e: e.dma_start(out=dst, in_=x_m[:]), reads=[x_m], writes=["res"])

        def stB1(i):
            x_m, sst, rs = xm[i % NX], ss[i % NX], rstd[i % NX]
            k.A(lambda e: e.activation(out=junk[:], in_=x_m[:], func=AF.Square, accum_out=sst[:, 0:1]), [x_m], [junk, sst])
            k.A(lambda e: e.activation(out=sst[:, 0:1], in_=sst[:, 0:1], func=AF.Ln, scale=1.0 / D, bias=EPSB[:, 0:1]),
                [sst, EPSB], [sst])
            k.A(lambda e: e.activation(out=rs[:, 0:1], in_=sst[:, 0:1], func=AF.Exp, scale=-0.5), [sst], [rs])

        def stB2(i):
            t, s, r, src, dst, hrow = info(i)
            x_m, h_f, rs = xm[i % NX], hf[i % NH], rstd[i % NX]
            k.V(lambda e: e.scalar_tensor_tensor(out=h_f[:], in0=x_m[:], scalar=rs[:, 0:1], in1=gsf[r][:], op0=ALU.mult,
                                                 op1=ALU.mult), [x_m, rs, gsf[r]], [h_f])

        def stB3(i):
            t, s, r, src, dst, hrow = info(i)
            h_f = hf[i % NH]
            k.G(lambda e: e.tensor_tensor(out=h_f[:], in0=h_f[:], in1=shf[r][:], op=ALU.add), [h_f, shf[r]], [h_f])

        def stB4(i):
            t, s, r, src, dst, hrow = info(i)
            h_f, h_b = hf[i % NH], hfb[i % 2]
            for kc in range(8):
                k.T(lambda e, kc=kc: e.transpose(pth[:, kc, :], h_f[:, kc * 128:(kc + 1) * 128], ident[:]), [h_f, ident], [pth])
            k.A(lambda e: e.copy(out=h_b[:], in_=h_f[:]), [h_f], [h_b])
            k.dma("act", lambda e: e.dma_start(out=A["hfb_d"][hrow:hrow + 128, :], in_=h_b[:]), reads=[h_b], writes=["hfb_d"])

        def stC1(i):
            h_T = hfT[i % 2]
            k.A(lambda e: e.copy(out=h_T[:], in_=pth[:]), [pth], [h_T])

        def stC2(i):
            h_T = hfT[i % 2]
            for kc in range(8):
                k.T(lambda e, kc=kc: e.matmul(plg[:], lhsT=h_T[:, kc, :], rhs=wr[:, kc, :], start=(kc == 0), stop=(kc == 7)),
                    [h_T, wr], [plg])
            lm = lmax[i % 2]
            k.V(lambda e: e.tensor_reduce(out=lm[:], in_=plg[:], axis=AX.X, op=ALU.max, negate=True), [plg], [lm])

        def stC3(i):
            t, s, r, src, dst, hrow = info(i)
            af = afftm[(i // NS) % 3]
            lm, ls = lmax[i % 2], lsum[i % 2]
            k.A(lambda e: e.activation(out=af[:, s * 32:s * 32 + 16], in_=plg[:], func=AF.Exp, bias=lm[:, 0:1],
                                       scale=1.0, accum_out=ls[:, 0:1]), [plg, lm], [af, ls])

        def stC4(i):
            t, s, r, src, dst, hrow = info(i)
            af = afftm[(i // NS) % 3]
            ls = lsum[i % 2]
            k.V(lambda e: e.reciprocal(out=ls[:], in_=ls[:]), [ls], [ls])
            k.V(lambda e: e.tensor_scalar(out=af[:, s * 32:s * 32 + 16], in0=af[:, s * 32:s * 32 + 16],
                                          scalar1=ls[:, 0:1], scalar2=None, op0=ALU.mult), [af, ls], [af])
            if s == NS - 1:
                k.T(lambda e: e.transpose(pat[:], af[:], ident[:]), [af, ident], [pat])

        def stC5(i):
            t, s, r, src, dst, hrow = info(i)
            if s == NS - 1:
                k.A(lambda e: e.copy(out=affT[:, t * 128:(t + 1) * 128], in_=pat[:]), [pat], [affT])

        stages = [stL, stA1, stA2, stA3, stB1, stB2, stB3, stB4, stC1, stC2, stC3, stC4, stC5]
        n = len(items)
        ns = len(stages)
        for step in range(n + ns - 1):
            for j in range(ns - 1, -1, -1):
                i = step - j
                if 0 <= i < n:
                    stages[j](i)
        k.dma("sp", lambda e: e.dma_start(out=A["aff_d"], in_=affT[:, C:C + S]), reads=[affT], writes=["aff_d"])
        if not last:
            k.dma("sp", lambda e: e.dma_start(out=A["affc_d"], in_=affT[:, 0:C]), reads=[affT], writes=["affc_d"])


def phase_moe(k, A, P, l, last, dst_lat):
    with k.phase():
        ident = P["ident"]
        nctx = 0 if last else NS
        NSL = NS * CAPL + nctx * CAPC
        if last:
            cgroups = [(0, 512)]
        else:
            cgroups = [(0, 288), (288, 288)]
        idxT = k.sb("idxT", [128, 2, 48], I32)
        gT = k.sb("gTm", [128, 2, 48], F32)
        if not last:
            idcT = k.sb("idcT", [32, 48], I32)
            gcT = k.sb("gcT", [32, 48], F32)
            idc64 = k.sb("idc64", [64, E], I32)
            gc64 = k.sb("gc64", [64, E], F32)
        with k.phase():
            wa = k.sb("wa", [48, S], F32)
            wb = k.sb("wb", [48, S], F32)
            vals = k.sb("vals", [48, CAPL], F32)
            idx = k.sb("idx", [48, CAPL], U32)
            idxf = k.sb("idxf", [48, CAPL], F32)
            offs = k.sb("offs", [48, 1], F32)
            k.dma("sp", lambda e: e.dma_start(out=wa[:], in_=A["aff_d"]), reads=["aff_d"], writes=[wa])
            cur, oth = wa, wb
            for r in range(CAPL // 8):
                k.V(lambda e, cur=cur, r=r: e.max(out=vals[:, r * 8:(r + 1) * 8], in_=cur[:]), [cur], [vals])
                k.V(lambda e, cur=cur, r=r: e.max_index(out=idx[:, r * 8:(r + 1) * 8], in_max=vals[:, r * 8:(r + 1) * 8],
                                                        in_values=cur[:]), [cur, vals], [idx])
                if r < CAPL // 8 - 1:
                    k.V(lambda e, cur=cur, oth=oth, r=r: e.match_replace(out=oth[:], in_to_replace=vals[:, r * 8:(r + 1) * 8],
                                                                         in_values=cur[:], imm_value=-1.0), [cur, vals], [oth])
                    cur, oth = oth, cur
            k.V(lambda e: e.memset(offs[0:32, :], 0.0), [], [offs])
            k.V(lambda e: e.memset(offs[32:48, :], float(S)), [], [offs])
            k.V(lambda e: e.tensor_copy(out=idxf[:], in_=idx[:]), [idx], [idxf])
            k.V(lambda e: e.tensor_scalar(out=idxf[:], in0=idxf[:], scalar1=offs[:, 0:1], scalar2=None, op0=ALU.add),
                [idxf, offs], [idxf])
            pti = k.ps("pti", [128, 2, 48], F32)
            ptg = k.ps("ptg", [128, 2, 48], F32)
            for j in range(2):
                k.T(lambda e, j=j: e.transpose(pti[:, j, :], idxf[:, j * 128:(j + 1) * 128], ident[0:48, 0:48]), [idxf, ident], [pti])
                k.T(lambda e, j=j: e.transpose(ptg[:, j, :], vals[:, j * 128:(j + 1) * 128], ident[0:48, 0:48]), [vals, ident], [ptg])
            k.V(lambda e: e.tensor_copy(out=idxT[:], in_=pti[:]), [pti], [idxT])
            k.V(lambda e: e.tensor_copy(out=gT[:], in_=ptg[:]), [ptg], [gT])
            if not last:
                wc = k.sb("wc", [48, C], F32)
                wd_ = k.sb("wd_", [48, C], F32)
                valc = k.sb("valc", [48, CAPC], F32)
                idc = k.sb("idc", [48, CAPC], U32)
                idcf = k.sb("idcf", [48, CAPC], F32)
                offc = k.sb("offc", [48, 1], F32)
                k.dma("sp", lambda e: e.dma_start(out=wc[:], in_=A["affc_d"]), reads=["affc_d"], writes=[wc])
                cur, oth = wc, wd_
                for r in range(CAPC // 8):
                    k.V(lambda e, cur=cur, r=r: e.max(out=valc[:, r * 8:(r + 1) * 8], in_=cur[:]), [cur], [valc])
                    k.V(lambda e, cur=cur, r=r: e.max_index(out=idc[:, r * 8:(r + 1) * 8], in_max=valc[:, r * 8:(r + 1) * 8],
                                                            in_values=cur[:]), [cur, valc], [idc])
                    if r < CAPC // 8 - 1:
                        k.V(lambda e, cur=cur, oth=oth, r=r: e.match_replace(out=oth[:], in_to_replace=valc[:, r * 8:(r + 1) * 8],
                                                                             in_values=cur[:], imm_value=-1.0), [cur, valc], [oth])
                        cur, oth = oth, cur
                k.V(lambda e: e.memset(offc[0:32, :], float(NLAT)), [], [offc])
                k.V(lambda e: e.memset(offc[32:48, :], float(NLAT + C)), [], [offc])
                k.V(lambda e: e.tensor_copy(out=idcf[:], in_=idc[:]), [idc], [idcf])
                k.V(lambda e: e.tensor_scalar(out=idcf[:], in0=idcf[:], scalar1=offc[:, 0:1], scalar2=None, op0=ALU.add),
                    [idcf, offc], [idcf])
                ptic = k.ps("ptic", [32, 2, 48], F32)
                k.T(lambda e: e.transpose(ptic[:, 0, :], idcf[:, 0:32], ident[0:48, 0:48]), [idcf, ident], [ptic])
                k.T(lambda e: e.transpose(ptic[:, 1, :], valc[:, 0:32], ident[0:48, 0:48]), [valc, ident], [ptic])
                k.V(lambda e: e.tensor_copy(out=idcT[:], in_=ptic[:, 0, :]), [ptic], [idcT])
                k.V(lambda e: e.tensor_copy(out=gcT[:], in_=ptic[:, 1, :]), [ptic], [gcT])
        if not last:
            for s_ in range(NS):
                k.dma("sp", lambda e, s_=s_: e.dma_start(out=idc64[s_ * 32:(s_ + 1) * 32, :], in_=idcT[:, s_ * 32:s_ * 32 + E]),
                      reads=[idcT], writes=[idc64])
                k.dma("sp", lambda e, s_=s_: e.dma_start(out=gc64[s_ * 32:(s_ + 1) * 32, :], in_=gcT[:, s_ * 32:s_ * 32 + E]),
                      reads=[gcT], writes=[gc64])
        identb = P["identb"]
        nr = NS + (0 if last else 1)
        gtf = [k.sb("gtf%d" % r, [128, D], F32) for r in range(nr)]
        for r in range(nr):
            k.dma("sp", lambda e, r=r: e.dma_start(out=gtf[r][:], in_=A["mod_d"][l, r, 5 * D:6 * D].partition_broadcast(128)),
                  reads=["mod_d"], writes=[gtf[r]])
        xe = [k.sb("xe%d" % i, [128, D], BF16) for i in range(3)]
        xeT = [k.sb("xeT%d" % i, [128, 8, NSL], BF16) for i in range(2)]
        hT = k.sb("hT", [128, NFC, NSL], BF16)
        sg = [k.sb("sg%d" % i, [128, NSL], F32) for i in range(2)]
        NW = 3
        NPRE = NW - 1
        wg = [k.sb("wg%d" % i, [128, 8, 512], BF16) for i in range(NW)]
        wu = [k.sb("wu%d" % i, [128, 8, 512], BF16) for i in range(NW)]
        wdn = k.sb("wdn", [128, NFC, D], BF16)
        ysb = [k.sb("ysb%d" % i, [128, D], F32) for i in range(2)]
        ncg = len(cgroups)
        pg = [k.ps("pg%d" % i, [128, 512], F32) for i in range(ncg)]
        pu = [k.ps("pu%d" % i, [128, 512], F32) for i in range(ncg)]
        pxt = k.ps("pxt", [128, 8, 128], BF16)
        pyd = [k.ps("pyd%d" % i, [128, 512], F32) for i in range(8 - 2 * ncg - 1)]
        target = dst_lat
        pieces = [(i * 512, min(512, DFF - i * 512)) for i in range((DFF + 511) // 512)]
        NP_ = len(pieces)
        cnt = {"y": 0}

        def tiles_of(ex):
            tl = []
            for s in range(NS):
                for j in range(2):
                    tl.append((128, s * 256 + j * 128, idxT[:, j, s * 32 + ex: s * 32 + ex + 1],
                               gT[:, j, s * 32 + ex: s * 32 + ex + 1], s, "lat"))
            if not last:
                tl.append((NS * CAPC, NS * CAPL, idc64[:, ex:ex + 1], gc64[:, ex:ex + 1], NS, "ctx"))
            return tl

        idx_reads = [idxT] + ([] if last else [idc64])

        def gather_T(ex):
            xT = xeT[ex % 2]
            for ti, (rows, c0, iap, gap, r, kind) in enumerate(tiles_of(ex)):
                x_e = xe[ti % 3]
                k.dma("pool", lambda e, x_e=x_e, rows=rows, iap=iap: e.indirect_dma_start(
                    out=x_e[0:rows, :], out_offset=None, in_=A["hfb_d"],
                    in_offset=bass.IndirectOffsetOnAxis(ap=iap[0:rows, :], axis=0)),
                    reads=["hfb_d"] + idx_reads, writes=[x_e])
                for kc in range(8):
                    k.T(lambda e, x_e=x_e, rows=rows, kc=kc: e.transpose(pxt[:, kc, 0:rows], x_e[0:rows, kc * 128:(kc + 1) * 128],
                                                                       identb[0:rows, 0:rows]), [x_e, identb], [pxt])
                if ti % 2 == 0:
                    k.A(lambda e, xT=xT, rows=rows, c0=c0: e.copy(out=xT[:, :, c0:c0 + rows], in_=pxt[:, :, 0:rows]), [pxt], [xT])
                else:
                    k.V(lambda e, xT=xT, rows=rows, c0=c0: e.tensor_copy(out=xT[:, :, c0:c0 + rows], in_=pxt[:, :, 0:rows]),
                        [pxt], [xT])

        def load_gu(ex, pi):
            c0, w = pieces[pi]
            gi_ = (ex * NP_ + pi) % NW
            wgv = A["w_gate"][l, ex].rearrange("(kc p) f -> p kc f", p=128)
            wuv = A["w_up"][l, ex].rearrange("(kc p) f -> p kc f", p=128)
            k.dma("pool", lambda e: e.dma_start(out=wg[gi_][:, :, 0:w], in_=wgv[:, :, c0:c0 + w]), writes=[wg[gi_]])
            k.dma("pool", lambda e: e.dma_start(out=wu[gi_][:, :, 0:w], in_=wuv[:, :, c0:c0 + w]), writes=[wu[gi_]])

        def load_dn(ex, pi):
            c0, w = pieces[pi]
            f0, nf = c0 // 128, w // 128
            wdv = A["w_down"][l, ex].rearrange("(fc p) d -> p fc d", p=128)
            for f in range(f0, f0 + nf, 2):
                k.dma("pool", lambda e, f=f: e.dma_start(out=wdn[:, f:f + 2, :], in_=wdv[:, f:f + 2, :]),
                      writes=[("wdn", f // 2)])

        def gu(ex, pi):
            c0, w = pieces[pi]
            gi_ = (ex * NP_ + pi) % NW
            xT = xeT[ex % 2]
            for f2 in range(w // 128):
                fc = c0 // 128 + f2
                for (W, ps_list) in ((wg[gi_], pg), (wu[gi_], pu)):
                    for gj, (g0, gw) in enumerate(cgroups):
                        p = ps_list[gj]
                        for kc in range(8):
                            k.T(lambda e, p=p, W=W, kc=kc, f2=f2, g0=g0, gw=gw: e.matmul(
                                p[:, 0:gw], lhsT=W[:, kc, f2 * 128:(f2 + 1) * 128], rhs=xT[:, kc, g0:g0 + gw],
                                start=(kc == 0), stop=(kc == 7)), [W, xT], [p])
                s_g = sg[fc % 2]
                for gj, (g0, gw) in enumerate(cgroups):
                    k.A(lambda e, s_g=s_g, gj=gj, g0=g0, gw=gw: e.activation(out=s_g[:, g0:g0 + gw], in_=pg[gj][:, 0:gw],
                                                                            func=AF.Silu), [pg[gj]], [s_g])
                    k.V(lambda e, s_g=s_g, gj=gj, g0=g0, gw=gw, fc=fc: e.tensor_tensor(
                        out=hT[:, fc, g0:g0 + gw], in0=s_g[:, g0:g0 + gw], in1=pu[gj][:, 0:gw], op=ALU.mult),
                        [s_g, pu[gj]], [("hT", fc)])

        def down(ex):
            for ti, (rows, c0, iap, gap, r, kind) in enumerate(tiles_of(ex)):
                y_s = ysb[ti % 2]
                for hh in range(2):
                    p = pyd[cnt["y"] % len(pyd)]
                    cnt["y"] += 1
                    for fc in range(NFC):
                        k.T(lambda e, p=p, rows=rows, c0=c0, fc=fc, hh=hh: e.matmul(
                            p[0:rows, :], lhsT=hT[:, fc, c0:c0 + rows], rhs=wdn[:, fc, hh * 512:(hh + 1) * 512],
                            start=(fc == 0), stop=(fc == NFC - 1)), [("hT", fc), ("wdn", fc // 2)], [p])
                    k.V(lambda e, p=p, rows=rows, y_s=y_s, hh=hh, gap=gap, r=r: e.scalar_tensor_tensor(
                        out=y_s[0:rows, hh * 512:(hh + 1) * 512], in0=p[0:rows, :], scalar=gap[0:rows, :],
                        in1=gtf[r][0:rows, hh * 512:(hh + 1) * 512], op0=ALU.mult, op1=ALU.mult),
                        [p, gT, gtf[r]] + ([] if last else [gc64]), [y_s])
                tgt = target if kind == "lat" else A["res_d"]
                k.dma("pool", lambda e, y_s=y_s, rows=rows, iap=iap, tgt=tgt: e.indirect_dma_start(
                    out=tgt, out_offset=bass.IndirectOffsetOnAxis(ap=iap[0:rows, :], axis=0), in_=y_s[0:rows, :],
                    in_offset=None, compute_op=ALU.add), reads=[y_s] + idx_reads, writes=["res"])

        gather_T(0)
        for pi in range(NPRE):
            load_gu(0, pi)
        for ex in range(E):
            for pi in range(NP_):
                load_dn(ex, pi)
                gu(ex, pi)
                if pi + NPRE < NP_:
                    load_gu(ex, pi + NPRE)
            if ex + 1 < E:
                gather_T(ex + 1)
                for pi in range(NPRE):
                    load_gu(ex + 1, pi)
            down(ex)


_NC_CACHE = {}


def kernel(**inputs):
    consts = host_consts()
    if "nc" not in _NC_CACHE:
        _NC_CACHE["nc"] = build_program()
    nc = _NC_CACHE["nc"]
    x = np.ascontiguousarray(inputs["x"], dtype=np.float32)
    c = np.ascontiguousarray(inputs["c"], dtype=np.float32)
    ctx = np.ascontiguousarray(inputs["ctx"], dtype=np.float32)
    shared = {"c_ctx": np.ascontiguousarray(inputs["c_ctx"], dtype=np.float32).reshape(1, D)}
    for n in WNAMES:
        shared[n] = np.ascontiguousarray(inputs[n], dtype=np.float32).reshape(WSHAPES[n])
    for n, v in consts.items():
        shared[n] = v
    in_maps = []
    for ci in range(NCORES):
        m = dict(shared)
        m["x"] = x[ci * NS:(ci + 1) * NS].reshape(NS * S, D)
        m["c"] = c[ci * NS:(ci + 1) * NS]
        m["ctx"] = ctx[ci * NS:(ci + 1) * NS].reshape(NS * C, D)
        in_maps.append(m)
    res = run_bass_kernel_spmd(nc, in_maps, core_ids=list(range(NCORES)))
    out = np.concatenate([r["y"].reshape(NS, S, D) for r in res.results], axis=0)
    return out.astype(np.float32)
```
